# Optimizing a Trainium2 kernel written in Bass

```python
import math
import jax, jax.numpy as jnp
from jax import lax
import numpy as np

D_MODEL = 1024
BATCH = 8
SEQ = 4096
DEPTH = 2

CHUNK = 64
QBLOCK = 128
A_HEADS = 8
A_QK_DIM = 64
A_V_DIM = 64
A_KV_RANK = 256
IDX_HEADS = 4
IDX_DIM = 64
TOPK_MAX = 256
REL_BUCKETS = 32
REL_MAX_DIST = 1024
B_WIDTH = 512
B_GROUPS = 8
SHORT_CONV = 3
SSM_HEADS = 16
SSM_HEAD_DIM = 64
SSM_INNER = SSM_HEADS * SSM_HEAD_DIM
SSM_GROUPS = 2
SSM_HEADS_PER_GROUP = SSM_HEADS // SSM_GROUPS
SSM_STATE = 128
SSM_CONV = 4
SSM_XBC = SSM_INNER + 2 * SSM_GROUPS * SSM_STATE
MIX_WIDTH = A_HEADS * A_V_DIM + B_WIDTH + SSM_INNER
D_FF = -(-8 * D_MODEL // (3 * 256)) * 256
PLE_DIM = 256
NORM_EPS = 1e-6
IN_SPLITS = (A_HEADS * A_QK_DIM, A_KV_RANK, IDX_HEADS * IDX_DIM, IDX_DIM, IDX_HEADS,
             B_WIDTH, B_WIDTH, B_WIDTH,
             SSM_INNER, SSM_XBC, SSM_HEADS)
IN_WIDTH = sum(IN_SPLITS)

kernel_name = "hybrid_dsa_shortconv_ssd_trunk"


def rmsnorm(x, g):
    xf = x.astype(jnp.float32)
    y = xf * lax.rsqrt(jnp.mean(xf * xf, axis=-1, keepdims=True) + NORM_EPS)
    return (y * g.astype(jnp.float32)).astype(x.dtype)


def layernorm(x, g, b):
    xf = x.astype(jnp.float32)
    mu = jnp.mean(xf, axis=-1, keepdims=True)
    var = jnp.mean(jnp.square(xf - mu), axis=-1, keepdims=True)
    y = (xf - mu) * lax.rsqrt(var + NORM_EPS)
    return (y * g.astype(jnp.float32) + b.astype(jnp.float32)).astype(x.dtype)


def causal_depthwise_conv(u, w):
    k_w = w.shape[0]
    s = u.shape[1]
    up = jnp.pad(u, ((0, 0), (k_w - 1, 0), (0, 0)))
    y = up[:, 0:s] * w[0]
    for j in range(1, k_w):
        y = y + up[:, j:j + s] * w[j]
    return y


def t5_bucket(rel):
    half = REL_BUCKETS // 2
    max_exact = half // 2
    ret = jnp.where(rel > 0, half, 0)
    n = jnp.abs(rel)
    nf = jnp.maximum(n, 1).astype(jnp.float32)
    large = max_exact + (jnp.log(nf / max_exact) / math.log(REL_MAX_DIST / max_exact)
                         * (half - max_exact)).astype(jnp.int32)
    large = jnp.minimum(large, half - 1)
    return ret + jnp.where(n < max_exact, n, large)


def sparse_indexed_attention(q, c_kv, iq, ik, iw, w_uk, w_uv, rel_bias):
    bsz, s = q.shape[0], q.shape[1]
    k_top = min(TOPK_MAX, s // 4)
    n_blocks = s // QBLOCK
    q_lat = jnp.einsum("bshd,rhd->bshr", q, w_uk) * (A_QK_DIM ** -0.5)
    iq = iq * (IDX_DIM ** -0.5)
    iw = iw * (IDX_HEADS ** -0.5)
    key_chunk = jnp.arange(s, dtype=jnp.int32) // CHUNK

    def to_blocks(a):
        return jnp.moveaxis(a.reshape((bsz, n_blocks, QBLOCK) + a.shape[2:]), 1, 0)

    def block(args):
        ql, iqb, iwb, t0 = args
        qpos = t0 + jnp.arange(QBLOCK, dtype=jnp.int32)
        qchunk = qpos // CHUNK
        raw = jnp.einsum("bthd,bsd->bths", iqb, ik).astype(jnp.float32)
        idx_score = jnp.einsum("bths,bth->bts", jax.nn.relu(raw), iwb.astype(jnp.float32))
        admissible = key_chunk[None, :] <= qchunk[:, None]
        idx_score = jnp.where(admissible[None], idx_score, -jnp.inf)
        _, sel = lax.top_k(idx_score, k_top)
        valid = (sel // CHUNK) <= qchunk[None, :, None]
        kv_sel = jax.vmap(lambda c, i: c[i])(c_kv, sel)
        logits = jnp.einsum("bthr,btkr->bthk", ql, kv_sel).astype(jnp.float32)
        bias = rel_bias[t5_bucket(sel - qpos[None, :, None])].astype(jnp.float32)
        logits = logits + jnp.transpose(bias, (0, 1, 3, 2))
        logits = jnp.where(valid[:, :, None, :], logits, -jnp.inf)
        probs = jax.nn.softmax(logits, axis=-1).astype(kv_sel.dtype)
        return jnp.einsum("bthk,btkr->bthr", probs, kv_sel)

    t0s = jnp.arange(n_blocks, dtype=jnp.int32) * QBLOCK
    out = lax.map(block, (to_blocks(q_lat), to_blocks(iq), to_blocks(iw), t0s))
    out = jnp.moveaxis(out, 0, 1).reshape(bsz, s, A_HEADS, A_KV_RANK)
    out = jnp.einsum("bshr,rhd->bshd", out, w_uv)
    return out.reshape(bsz, s, A_HEADS * A_V_DIM)


def gated_short_conv(h, b_gate, c_gate, w_conv):
    return b_gate * causal_depthwise_conv(c_gate * h, w_conv)


def segsum(x):
    t = x.shape[-1]
    xr = jnp.broadcast_to(x[..., :, None], x.shape + (t,))
    xr = jnp.where(jnp.tril(jnp.ones((t, t), dtype=bool), -1), xr, 0.0)
    cs = jnp.cumsum(xr, axis=-2)
    return jnp.where(jnp.tril(jnp.ones((t, t), dtype=bool)), cs, -jnp.inf)


def ssd_chunked(xh, a, bm, cm):
    b, s, g, r, p = xh.shape
    n = bm.shape[-1]
    nc = s // CHUNK
    xh = xh.reshape(b, nc, CHUNK, g, r, p)
    bm = bm.reshape(b, nc, CHUNK, g, n)
    cm = cm.reshape(b, nc, CHUNK, g, n)
    a = jnp.transpose(a.reshape(b, nc, CHUNK, g, r), (0, 3, 4, 1, 2))
    a_cs = jnp.cumsum(a, axis=-1)
    l_mat = jnp.exp(segsum(a))
    cb = jnp.einsum("bclgn,bcsgn->bcgls", cm, bm)
    y_diag = jnp.einsum("bcgls,bgrcls,bcsgrp->bclgrp", cb, l_mat, xh)
    decay_states = jnp.exp(a_cs[..., -1:] - a_cs)
    states = jnp.einsum("bclgn,bgrcl,bclgrp->bcgrpn", bm, decay_states, xh)
    chunk_decay = jnp.exp(a_cs[..., -1])

    def step(carry, inp):
        st, dec = inp
        return carry * dec[..., None, None] + st, carry

    init = jnp.zeros((b, g, r, p, n), dtype=states.dtype)
    _, prev = lax.scan(step, init, (jnp.moveaxis(states, 1, 0), jnp.moveaxis(chunk_decay, -1, 0)))
    prev = jnp.moveaxis(prev, 0, 1)
    y_off = jnp.einsum("bclgn,bcgrpn,bgrcl->bclgrp", cm, prev, jnp.exp(a_cs))
    return (y_diag + y_off).reshape(b, s, g, r, p)


def mamba2_mixer(z, xbc, dt_raw, conv_w, conv_b, dt_bias, a_log, d_skip, norm_w):
    bsz, s = z.shape[0], z.shape[1]
    xbc = jax.nn.silu(causal_depthwise_conv(xbc, conv_w) + conv_b)
    xs = xbc[..., :SSM_INNER].reshape(bsz, s, SSM_GROUPS, SSM_HEADS_PER_GROUP, SSM_HEAD_DIM)
    bm = xbc[..., SSM_INNER:SSM_INNER + SSM_GROUPS * SSM_STATE].reshape(bsz, s, SSM_GROUPS, SSM_STATE)
    cm = xbc[..., SSM_INNER + SSM_GROUPS * SSM_STATE:].reshape(bsz, s, SSM_GROUPS, SSM_STATE)
    dt = jax.nn.softplus(dt_raw.astype(jnp.float32) + dt_bias.astype(jnp.float32))
    dt = dt.reshape(bsz, s, SSM_GROUPS, SSM_HEADS_PER_GROUP)
    a = -jnp.exp(a_log.astype(jnp.float32)).reshape(SSM_GROUPS, SSM_HEADS_PER_GROUP)
    xf = xs.astype(jnp.float32)
    y = ssd_chunked(xf * dt[..., None], dt * a, bm.astype(jnp.float32), cm.astype(jnp.float32))
    y = y + d_skip.astype(jnp.float32).reshape(SSM_GROUPS, SSM_HEADS_PER_GROUP)[..., None] * xf
    gw = SSM_HEADS_PER_GROUP * SSM_HEAD_DIM
    y = y.reshape(bsz, s, SSM_GROUPS, gw) * jax.nn.silu(z.astype(jnp.float32)).reshape(bsz, s, SSM_GROUPS, gw)
    y = rmsnorm(y, norm_w.reshape(SSM_GROUPS, gw))
    return y.reshape(bsz, s, SSM_INNER).astype(z.dtype)


def split_offsets():
    return [int(v) for v in np.cumsum(np.array(IN_SPLITS))[:-1]]


def setup_inputs(seed: int = 0) -> dict:
    key = jax.random.key(seed)
    ks = jax.random.split(key, 26)
    f32 = jnp.float32

    def nrm(k, shape, scale):
        return jax.random.normal(k, shape, f32) * scale

    def gain(k, shape):
        return 1.0 + 0.05 * jax.random.normal(k, shape, f32)

    u_dt = jax.random.uniform(ks[13], (DEPTH, SSM_HEADS), f32)
    dt0 = jnp.exp(u_dt * (math.log(0.1) - math.log(0.001)) + math.log(0.001))
    return {
        "x": nrm(ks[0], (BATCH, SEQ, D_MODEL), 1.0),
        "p": nrm(ks[1], (DEPTH, BATCH, SEQ, PLE_DIM), 1.0),
        "pre_mix_norm": gain(ks[2], (DEPTH, D_MODEL)),
        "post_mix_norm": gain(ks[3], (DEPTH, D_MODEL)),
        "pre_ffn_norm": gain(ks[4], (DEPTH, D_MODEL)),
        "post_ffn_norm": gain(ks[5], (DEPTH, D_MODEL)),
        "w_in": nrm(ks[6], (DEPTH, D_MODEL, IN_WIDTH), D_MODEL ** -0.5),
        "kv_norm": gain(ks[7], (DEPTH, A_KV_RANK)),
        "idx_k_norm_g": gain(ks[8], (DEPTH, IDX_DIM)),
        "idx_k_norm_b": nrm(ks[9], (DEPTH, IDX_DIM), 0.02),
        "w_uk": nrm(ks[10], (DEPTH, A_KV_RANK, A_HEADS, A_QK_DIM), A_KV_RANK ** -0.5),
        "w_uv": nrm(ks[11], (DEPTH, A_KV_RANK, A_HEADS, A_V_DIM), A_KV_RANK ** -0.5),
        "rel_bias": nrm(ks[12], (REL_BUCKETS, A_HEADS), 0.5),
        "short_conv_w": nrm(ks[14], (DEPTH, SHORT_CONV, B_WIDTH), SHORT_CONV ** -0.5),
        "ssm_conv_w": nrm(ks[15], (DEPTH, SSM_CONV, SSM_XBC), SSM_CONV ** -0.5),
        "ssm_conv_b": nrm(ks[16], (DEPTH, SSM_XBC), 0.02),
        "ssm_dt_bias": dt0 + jnp.log(-jnp.expm1(-dt0)),
        "ssm_a_log": jnp.log(jax.random.uniform(ks[17], (DEPTH, SSM_HEADS), f32, 1.0, 16.0)),
        "ssm_d": 1.0 + 0.1 * jax.random.normal(ks[18], (DEPTH, SSM_HEADS), f32),
        "ssm_norm": gain(ks[19], (DEPTH, SSM_INNER)),
        "w_out": nrm(ks[20], (DEPTH, MIX_WIDTH, D_MODEL), MIX_WIDTH ** -0.5),
        "w_ffn_gate": nrm(ks[21], (DEPTH, D_MODEL, D_FF), D_MODEL ** -0.5),
        "w_ffn_up": nrm(ks[22], (DEPTH, D_MODEL, D_FF), D_MODEL ** -0.5),
        "w_ffn_down": nrm(ks[23], (DEPTH, D_FF, D_MODEL), D_FF ** -0.5),
        "w_ple_proj": nrm(ks[24], (DEPTH, PLE_DIM, D_MODEL), PLE_DIM ** -0.5),
        "w_ple_gate": nrm(ks[25], (DEPTH, D_MODEL, D_MODEL), D_MODEL ** -0.5),
    }


def reference(x, p, pre_mix_norm, post_mix_norm, pre_ffn_norm, post_ffn_norm, w_in,
              kv_norm, idx_k_norm_g, idx_k_norm_b, w_uk, w_uv, rel_bias, short_conv_w,
              ssm_conv_w, ssm_conv_b, ssm_dt_bias, ssm_a_log, ssm_d, ssm_norm, w_out,
              w_ffn_gate, w_ffn_up, w_ffn_down, w_ple_proj, w_ple_gate):
    bsz, s = x.shape[0], x.shape[1]
    offsets = split_offsets()
    for i in range(DEPTH):
        h = rmsnorm(x, pre_mix_norm[i])
        proj = h @ w_in[i]
        q, ckv, iq, ik, iw, b_gate, c_gate, hb, z, xbc, dt_raw = jnp.split(proj, offsets, axis=-1)
        a_out = sparse_indexed_attention(
            q.reshape(bsz, s, A_HEADS, A_QK_DIM),
            rmsnorm(ckv, kv_norm[i]),
            iq.reshape(bsz, s, IDX_HEADS, IDX_DIM),
            layernorm(ik, idx_k_norm_g[i], idx_k_norm_b[i]),
            iw, w_uk[i], w_uv[i], rel_bias)
        b_out = gated_short_conv(hb, b_gate, c_gate, short_conv_w[i])
        c_out = mamba2_mixer(z, xbc, dt_raw, ssm_conv_w[i], ssm_conv_b[i], ssm_dt_bias[i],
                             ssm_a_log[i], ssm_d[i], ssm_norm[i])
        mix = jnp.concatenate([a_out, b_out, c_out], axis=-1) @ w_out[i]
        x = x + rmsnorm(mix, post_mix_norm[i])
        h = rmsnorm(x, pre_ffn_norm[i])
        f = (jax.nn.silu(h @ w_ffn_gate[i]) * (h @ w_ffn_up[i])) @ w_ffn_down[i]
        x = x + rmsnorm(f, post_ffn_norm[i])
        x = x + jax.nn.sigmoid(x @ w_ple_gate[i]) * (p[i] @ w_ple_proj[i])
    return x
```

```python
import math
from contextlib import ExitStack
import numpy as np
import concourse.bass as bass
import concourse.mybir as mybir
from concourse.bass_utils import run_bass_kernel_spmd

F32 = mybir.dt.float32
BF16 = mybir.dt.bfloat16
AF = mybir.ActivationFunctionType
ALU = mybir.AluOpType
AX = mybir.AxisListType

D = 1024
DEPTH = 2
NH = 8
R = 256
IN_W = 5204
DFF = 2816
MIXW = 2048
EPS = 1e-6
NEG = -30000.0
O_Q, O_CKV, O_IQ, O_IK, O_IW, O_BG, O_CG, O_HB, O_Z, O_XBC, O_DT = 0, 512, 768, 1024, 1088, 1092, 1604, 2116, 2628, 3652, 5188
GW = 1582
GC = 1070
SEM_LIMIT = 30000


class Sem:
    def __init__(self, h):
        self.h = h
        self.val = 0


class Res:
    __slots__ = ("name", "w", "r", "dsem")

    def __init__(self, name):
        self.name = name
        self.w = {}
        self.r = {}
        self.dsem = None


class EngW:
    def __init__(self, name, eng, is_pe=False):
        self.name = name
        self.eng = eng
        self.is_pe = is_pe
        self.sem = None
        self.seen = {}


class KB:
    def __init__(self, nc):
        self.nc = nc
        self.nsem = 0
        self.pe = EngW("pe", nc.tensor, True)
        self.act = EngW("act", nc.scalar)
        self.dve = EngW("dve", nc.vector)
        self.pool = EngW("pool", nc.gpsimd)
        self.sp = EngW("sp", nc.sync)
        for e in (self.pe, self.act, self.dve, self.pool):
            e.sem = self.new_sem(e.name)
        self.stack = ExitStack()
        self.n_ops = 0
        self.free_dsems = []
        self.phase_dsems = [[]]

    def new_sem(self, name="s"):
        self.nsem += 1
        return Sem(self.nc.alloc_semaphore(name=f"{name}_{self.nsem}"))

    def res(self, name):
        return Res(name)

    def sb(self, name, shape, dt):
        self.n_sb = getattr(self, "n_sb", 0) + 1
        return self.stack.enter_context(self.nc.sbuf_tensor(f"{name}_{self.n_sb}", list(shape), dt))

    def _wait(self, e, reads, writes):
        raw = {}
        oth = {}
        for r in reads:
            for s, v in r.w.items():
                if raw.get(s, 0) < v:
                    raw[s] = v
        for w in writes:
            for s, v in w.w.items():
                if oth.get(s, 0) < v:
                    oth[s] = v
            for s, v in w.r.items():
                if oth.get(s, 0) < v:
                    oth[s] = v
        for s, v in oth.items():
            if s is e.sem:
                continue
            if raw.get(s, 0) < v:
                raw[s] = v
        for s, v in raw.items():
            if s is e.sem and e.is_pe:
                continue
            if e.seen.get(s, 0) >= v:
                continue
            e.eng.wait_ge(s.h, v)
            e.seen[s] = v

    def op(self, e, fn, reads=(), writes=(), inc=True):
        self._wait(e, reads, writes)
        ins = fn()
        self.n_ops += 1
        if inc:
            if e.sem.val >= SEM_LIMIT and not getattr(e, "pending", False):
                e.sem = self.new_sem(e.name)
            e.sem.val += 1
            ins.then_inc(e.sem.h, 1)
            tv = e.sem.val
            e.pending = False
        else:
            tv = e.sem.val + 1
            e.pending = True
        for w in writes:
            w.w[e.sem] = tv
        for r in reads:
            r.r[e.sem] = tv
        return ins

    def dma(self, out, in_, reads=(), writes=(), q=None, own=None):
        q = q or self.sp
        self._wait(q, reads, writes)
        ins = q.eng.dma_start(out=out, in_=in_)
        self.n_ops += 1
        if own.dsem is None or own.dsem.val >= SEM_LIMIT:
            if getattr(self, "free_dsems", None):
                own.dsem = self.free_dsems.pop()
                if own.dsem.val >= SEM_LIMIT:
                    own.dsem = self.new_sem("d" + own.name)
            else:
                own.dsem = self.new_sem("d" + own.name)
            self.phase_dsems[-1].append(own.dsem)
        s = own.dsem
        s.val += 16
        ins.then_inc(s.h, 16)
        for w in writes:
            w.w[s] = s.val
        for r in reads:
            r.r[s] = s.val
        return ins

    def drain(self, e, ress):
        self._wait(e, ress, ())


class Slots:
    def __init__(self, kb, name, shape, dt, n):
        self.t = [kb.sb(f"{name}{i}", shape, dt) for i in range(n)]
        self.r = [kb.res(f"{name}{i}") for i in range(n)]
        self.i = 0
        self.n = n

    def next(self):
        k = self.i % self.n
        self.i += 1
        return self.t[k], self.r[k]


def t5_bucket_np(rel):
    half, max_exact = 16, 8
    ret = np.where(rel > 0, half, 0)
    n = np.abs(rel)
    nf = np.maximum(n, 1).astype(np.float32)
    large = max_exact + (np.log(nf / max_exact) / math.log(1024 / max_exact) * (half - max_exact)).astype(np.int32)
    large = np.minimum(large, half - 1)
    return ret + np.where(n < max_exact, n, large)


def build_program(S, dbg=False, phases=None):
    NT = S // 128
    NB = S // 512
    nc = bass.Bass("TRN2", target_bir_lowering=False)
    kb = KB(nc)
    pe, act, dve, pool, sp = kb.pe, kb.act, kb.dve, kb.pool, kb.sp
    T, V, A, G = nc.tensor, nc.vector, nc.scalar, nc.gpsimd

    def din(name, shape, dt=F32):
        return nc.dram_tensor(name, list(shape), dt, kind="ExternalInput").ap()

    def dscr(name, shape, dt):
        return nc.dram_tensor(name, list(shape), dt, kind="ExternalOutput" if dbg else "Internal").ap()

    x_in = din("x", [S, D])
    p_in = din("p", [DEPTH, S, 256])
    w_in = din("w_in", [DEPTH, D, IN_W])
    w_out = din("w_out", [DEPTH, MIXW, D])
    w_g = din("w_ffn_gate", [DEPTH, D, DFF])
    w_u = din("w_ffn_up", [DEPTH, D, DFF])
    w_d = din("w_ffn_down", [DEPTH, DFF, D])
    w_pp = din("w_ple_proj", [DEPTH, 256, D])
    w_pg = din("w_ple_gate", [DEPTH, D, D])
    wukT = din("wukT", [DEPTH, 128, 4, R])
    wuv = din("wuv", [DEPTH, 128, 2, NH * 64])
    gtab = din("gtab", [128, NH, GW])
    vecD = din("vecD", [DEPTH, 5, D])
    kvn = din("kv_norm", [DEPTH, R])
    ikg = din("idx_k_norm_g", [DEPTH, 64])
    ikb = din("idx_k_norm_b", [DEPTH, 64])
    scw = din("scw", [DEPTH, 128, 4, 3])
    sscw = din("sscw", [DEPTH, 128, 12, 4])
    sscb = din("sscb", [DEPTH, 128, 12])
    v16 = din("v16", [DEPTH, 3, 16])
    y_out = nc.dram_tensor("y", [S, D], F32, kind="ExternalOutput").ap()

    xs_d = dscr("xs_d", [S, D], F32)
    xm_d = dscr("xm_d", [S, D], F32)
    qlat_d = dscr("qlat_d", [NH, R, S], BF16)
    iq_d = dscr("iq_d", [256, S], BF16)
    ik_d = dscr("ik_d", [64, S], BF16)
    iw_d = dscr("iw_d", [S, 4], F32)
    ckv_d = dscr("ckv_d", [S, R], BF16)
    ckvT_d = dscr("ckvT_d", [R, S], BF16)
    mixT_d = dscr("mixT_d", [MIXW, S], BF16)
    zs_d = dscr("zs_d", [S, D], F32)
    xsc_d = dscr("xsc_d", [S, D], BF16)
    bm_d = dscr("bm_d", [S, 256], BF16)
    bmT_d = dscr("bmT_d", [256, S], BF16)
    cmT_d = dscr("cmT_d", [256, S], BF16)
    dt_d = dscr("dt_d", [S, 32], F32)
    nmT_d = dscr("nmT_d", [S, S], BF16)
    gu_d = dscr("gu_d", [DFF, S], BF16)
    R_ = {n: kb.res(n) for n in ["xs", "qlat", "iq", "ik", "iw", "ckv", "ckvT", "mixT", "zs", "xsc", "bm", "bmT",
                                 "cmT", "dt", "nmT", "gu", "y", "xm"]}

    ps = [kb.stack.enter_context(nc.psum_tensor(f"ps{i}", [128, 512], F32)) for i in range(8)]
    psr = [kb.res(f"ps{i}") for i in range(8)]

    ident_f = kb.sb("ident_f", [128, 128], F32)
    ident = kb.sb("ident", [128, 128], BF16)
    ones_f = kb.sb("ones_f", [128, 128], F32)
    utri_f = kb.sb("utri_f", [128, 128], F32)
    tri_f = kb.sb("tri_f", [128, 128], F32)
    cres = kb.res("consts")
    kb.op(pool, lambda: G.memset(ident_f[:], 0.0), writes=[cres])
    kb.op(pool, lambda: G.affine_select(out=ident_f[:], in_=ident_f[:], pattern=[[-1, 128]], compare_op=ALU.not_equal,
                                        fill=1.0, base=0, channel_multiplier=1), reads=[cres], writes=[cres])
    kb.op(pool, lambda: G.tensor_copy(out=ident[:], in_=ident_f[:]), reads=[cres], writes=[cres])
    kb.op(pool, lambda: G.memset(ones_f[:], 1.0), writes=[cres])
    kb.op(pool, lambda: G.affine_select(out=utri_f[:], in_=ones_f[:], pattern=[[1, 128]], compare_op=ALU.is_ge,
                                        fill=0.0, base=0, channel_multiplier=-1), reads=[cres], writes=[cres])
    kb.op(pool, lambda: G.tensor_copy(out=tri_f[:], in_=utri_f[:]), reads=[cres], writes=[cres])

    eps_t = kb.sb("eps_t", [128, 1], F32)
    negb_t = kb.sb("negb_t", [128, 1], F32)
    kb.op(pool, lambda: G.memset(negb_t[:], NEG), writes=[cres])
    kb.op(pool, lambda: G.memset(eps_t[:], EPS), writes=[cres])
    HT = {}

    def bcast_load(dst, src_vec, n, rs):
        kb.dma(out=dst, in_=src_vec.partition_broadcast(128), writes=[rs], own=rs)

    def rms_rstd(dst, ssq, n, eng=None):
        V.tensor_scalar(out=dst, in0=ssq, scalar1=1.0 / n, scalar2=EPS, op0=ALU.mult, op1=ALU.add)

    all_sems = []
    _orig_new_sem = kb.new_sem

    def _new_sem(name="s"):
        s = _orig_new_sem(name)
        all_sems.append(s)
        return s
    kb.new_sem = _new_sem
    for e in (pe, act, dve, pool):
        all_sems.append(e.sem)

    def barrier():
        for e in (pe, act, dve, pool, sp):
            for s in all_sems:
                if s is e.sem or s.val == 0:
                    continue
                if e.seen.get(s, 0) >= s.val:
                    continue
                e.eng.wait_ge(s.h, s.val)
                e.seen[s] = s.val

    class Phase:
        def __enter__(self):
            self.es = ExitStack()
            self.es.__enter__()
            self.old = kb.stack
            kb.stack = self.es
            kb.phase_dsems.append([])
            return self

        def __exit__(self, *a):
            barrier()
            kb.free_dsems.extend(kb.phase_dsems.pop())
            kb.stack = self.old
            self.es.__exit__(None, None, None)
            return False

    def mm(out, lhsT, rhs, start, stop, reads, writes, inc=True):
        return kb.op(pe, lambda: T.matmul(out, lhsT=lhsT, rhs=rhs, start=start, stop=stop), reads=reads, writes=writes, inc=inc)

    def tp(out, in_, reads, writes, inc=True, idn=None):
        idn = ident if idn is None else idn
        k = in_.shape[0]
        return kb.op(pe, lambda: T.transpose(out, in_, idn[0:k, 0:k]), reads=list(reads) + [cres], writes=writes, inc=inc)

    def vop(fn, reads, writes):
        return kb.op(dve, fn, reads=reads, writes=writes)

    def aop(fn, reads, writes):
        return kb.op(act, fn, reads=reads, writes=writes)

    def gop(fn, reads, writes):
        return kb.op(pool, fn, reads=reads, writes=writes)

    def rstd(st, st_r, src, dst, n):
        aop(lambda: A.activation(out=st[:, 15:16], in_=st[:, src:src + 1], func=AF.Ln, scale=1.0 / n, bias=eps_t[:, 0:1]), [st_r, cres], [st_r])
        aop(lambda: A.activation(out=st[:, dst:dst + 1], in_=st[:, 15:16], func=AF.Exp, scale=-0.5), [st_r], [st_r])

    def norm_to_hT(xt, xr, g_bc, g_r, tt, tmp):
        junk, junk_r, stl, hb, hb_r = tmp
        st, st_r = stl.next()
        aop(lambda: A.activation(out=junk[:], in_=xt, func=AF.Square, accum_out=st[:, 0:1]), [xr], [junk_r, st_r])
        rstd(st, st_r, 0, 2, D)
        vop(lambda: V.scalar_tensor_tensor(out=hb[:], in0=xt, scalar=st[:, 2:3], in1=g_bc[:], op0=ALU.mult, op1=ALU.mult),
            [xr, st_r, g_r], [hb_r])
        for half in range(2):
            pb = 6 + half
            pv = ps[pb][:].bitcast(BF16)
            for j in range(4):
                dc = half * 4 + j
                tp(pv[:, j * 128:(j + 1) * 128], hb[:, dc * 128:(dc + 1) * 128], [hb_r], [psr[pb]], inc=(j == 3))
            src = pv[:, 0:512].rearrange("p (j t) -> p j t", j=4)
            dst = HT['t'][:, half * 4:(half + 1) * 4, tt * 128:(tt + 1) * 128]
            if half == 0:
                aop(lambda src=src, dst=dst: A.copy(out=dst, in_=src), [psr[pb]], [HT['r']])
            else:
                vop(lambda src=src, dst=dst: V.tensor_copy(out=dst, in_=src), [psr[pb]], [HT['r']])

    class HTScope(Phase):
        def __enter__(self):
            Phase.__enter__(self)
            HT['t'] = kb.sb("hT", [128, 8, S], BF16)
            HT['r'] = kb.res("hT")
            return self

    def phase0(l, src=None):
        src = x_in if src is None else src
        with Phase():
            g_bc = kb.sb("p0_g", [128, D], F32)
            g_r = kb.res("p0_g")
            bcast_load(g_bc[:], vecD[l, 0, :], D, g_r)
            xsl = Slots(kb, "p0_x", [128, D], F32, 2)
            junk = kb.sb("p0_junk", [128, D], F32)
            stl = Slots(kb, "p0_st", [128, 16], F32, 2)
            hb = kb.sb("p0_hb", [128, D], BF16)
            tmp = (junk, kb.res("p0_junk"), stl, hb, kb.res("p0_hb"))
            for tt in range(NT):
                xt, xr = xsl.next()
                kb.dma(out=xt[:], in_=src[tt * 128:(tt + 1) * 128, :], reads=[R_['xs']], writes=[xr], own=xr)
                norm_to_hT(xt[:], xr, g_bc, g_r, tt, tmp)

    class WStream:
        def __init__(self, name, KC, maxc, nbuf=2, nstg=2):
            self.KC = KC
            self.stg = Slots(kb, name + "_stg", [128, KC, 256], F32, nstg)
            self.wb = Slots(kb, name + "_wb", [128, KC, maxc], BF16, nbuf)

        def load(self, wap, c0, ncols, r0=0, kcn=None):
            kcn = kcn or self.KC
            wt, wr = self.wb.next()
            for p0 in range(0, ncols, 256):
                pn = min(256, ncols - p0)
                st, sr = self.stg.next()
                src = wap[r0:r0 + kcn * 128, :].rearrange("(kc p) c -> p kc c", p=128)[:, :, c0 + p0:c0 + p0 + pn]
                kb.dma(out=st[:, 0:kcn, 0:pn], in_=src, writes=[sr], own=sr)
                gop(lambda: G.tensor_copy(out=wt[:, 0:kcn, p0:p0 + pn], in_=st[:, 0:kcn, 0:pn]), [sr], [wr])
            return wt, wr

    class RR:
        def __init__(self, items):
            self.items = items
            self.i = 0

        def next(self):
            k = self.items[self.i % len(self.items)]
            self.i += 1
            return k

    def phase1(l):
        with Phase():
            hT, hT_r = HT['t'], HT['r']
            ws = WStream("w1", 8, 512, nbuf=3)
            c_r = kb.res("p1c")
            wuk_f = kb.sb("wuk_f", [128, 4, R], F32)
            wuk_b = kb.sb("wuk_b", [128, 4, R], BF16)
            kb.dma(out=wuk_f[:], in_=wukT[l], writes=[c_r], own=c_r)
            gop(lambda: G.tensor_copy(out=wuk_b[:], in_=wuk_f[:]), [c_r], [c_r])
            scw_t = kb.sb("scw_t", [128, 4, 3], F32)
            kb.dma(out=scw_t[:], in_=scw[l], writes=[c_r], own=c_r)
            sscw_t = kb.sb("sscw_t", [128, 12, 4], F32)
            kb.dma(out=sscw_t[:], in_=sscw[l], writes=[c_r], own=c_r)
            sscb_t = kb.sb("sscb_t", [128, 12], F32)
            kb.dma(out=sscb_t[:], in_=sscb[l], writes=[c_r], own=c_r)
            kvn_bc = kb.sb("kvn_bc", [128, R], F32)
            bcast_load(kvn_bc[:], kvn[l, :], R, c_r)
            ikg_bc = kb.sb("ikg_bc", [128, 64], F32)
            bcast_load(ikg_bc[:], ikg[l, :], 64, c_r)
            ikb_bc = kb.sb("ikb_bc", [128, 64], F32)
            bcast_load(ikb_bc[:], ikb[l, :], 64, c_r)
            v16_bc = kb.sb("v16_bc", [128, 3, 16], F32)
            kb.dma(out=v16_bc[:], in_=v16[l].partition_broadcast(128), writes=[c_r], own=c_r)
            A_bc = kb.sb("A_bc", [128, 16], F32)
            aop(lambda: A.activation(out=A_bc[:], in_=v16_bc[:, 1, :], func=AF.Exp), [c_r], [c_r])
            vop(lambda: V.tensor_scalar(out=A_bc[:], in0=A_bc[:], scalar1=-1.0, scalar2=None, op0=ALU.mult), [c_r], [c_r])

            bankA = RR([0, 1])
            bankB = RR([2, 3])
            o512 = Slots(kb, "p1_o512", [128, 512], BF16, 3)
            o512b = Slots(kb, "p1_o512b", [128, 512], BF16, 3)

            def fm_block(wt, wr, c, tb, ncols=128):
                b = bankA.next()
                for kc in range(8):
                    mm(ps[b][0:ncols, :], wt[:, kc, c * 128:c * 128 + ncols], hT[:, kc, tb * 512:(tb + 1) * 512],
                       kc == 0, kc == 7, [wr, hT_r], [psr[b]], inc=(kc == 7))
                return b

            wt, wr = ws.load(w_in[l], O_Q, 512)
            for c in range(4):
                for tb in range(NB):
                    b = fm_block(wt, wr, c, tb)
                    qs, qr = o512.next()
                    aop(lambda b=b, qs=qs: A.mul(out=qs[:], in_=ps[b][:], mul=0.125), [psr[b]], [qr])
                    for hh in range(2):
                        for rc in range(2):
                            b2 = bankB.next()
                            mm(ps[b2][:], wuk_b[hh * 64:(hh + 1) * 64, c, rc * 128:(rc + 1) * 128],
                               qs[hh * 64:(hh + 1) * 64, :], True, True, [c_r, qr], [psr[b2]])
                            ql, qlr = o512b.next()
                            vop(lambda b2=b2, ql=ql: V.tensor_copy(out=ql[:], in_=ps[b2][:]), [psr[b2]], [qlr])
                            kb.dma(out=qlat_d[2 * c + hh, rc * 128:(rc + 1) * 128, tb * 512:(tb + 1) * 512], in_=ql[:],
                                   reads=[qlr], writes=[R_["qlat"]], own=qlr)
            wt, wr = ws.load(w_in[l], O_IQ, 256)
            for c in range(2):
                for tb in range(NB):
                    b = fm_block(wt, wr, c, tb)
                    qs, qr = o512.next()
                    aop(lambda b=b, qs=qs: A.mul(out=qs[:], in_=ps[b][:], mul=0.125), [psr[b]], [qr])
                    kb.dma(out=iq_d[c * 128:(c + 1) * 128, tb * 512:(tb + 1) * 512], in_=qs[:], reads=[qr],
                           writes=[R_["iq"]], own=qr)
            cg = kb.sb("p1_cg", [128, S], F32)
            cg_r = kb.res("p1_cg")
            U = kb.sb("p1_U", [128, S + 4], F32)
            U_r = kb.res("p1_U")
            vop(lambda: V.memset(U[:, 0:4], 0.0), [], [U_r])
            wbg, wbg_r = ws.load(w_in[l], O_BG, 512)
            wcg, wcg_r = ws.load(w_in[l], O_CG, 512)
            whb, whb_r = ws.load(w_in[l], O_HB, 512)
            for c in range(4):
                for tb in range(NB):
                    b = fm_block(wcg, wcg_r, c, tb)
                    aop(lambda b=b, tb=tb: A.copy(out=cg[:, tb * 512:(tb + 1) * 512], in_=ps[b][:]), [psr[b]], [cg_r])
                for tb in range(NB):
                    b = fm_block(whb, whb_r, c, tb)
                    vop(lambda b=b, tb=tb: V.tensor_tensor(out=U[:, 4 + tb * 512:4 + (tb + 1) * 512], in0=ps[b][:],
                                                           in1=cg[:, tb * 512:(tb + 1) * 512], op=ALU.mult),
                        [psr[b], cg_r], [U_r])
                vop(lambda c=c: V.tensor_scalar(out=cg[:, :], in0=U[:, 2:S + 2], scalar1=scw_t[:, c, 0:1], scalar2=None,
                                                op0=ALU.mult), [U_r, c_r], [cg_r])
                for j in (1, 2):
                    vop(lambda c=c, j=j: V.scalar_tensor_tensor(out=cg[:, :], in0=U[:, 2 + j:S + 2 + j], scalar=scw_t[:, c, j:j + 1],
                                                                in1=cg[:, :], op0=ALU.mult, op1=ALU.add), [U_r, c_r, cg_r], [cg_r])
                for tb in range(NB):
                    b = fm_block(wbg, wbg_r, c, tb)
                    ob, obr = o512.next()
                    vop(lambda b=b, tb=tb, ob=ob: V.tensor_tensor(out=ob[:], in0=ps[b][:], in1=cg[:, tb * 512:(tb + 1) * 512],
                                                                  op=ALU.mult), [psr[b], cg_r], [obr])
                    kb.dma(out=mixT_d[512 + c * 128:512 + (c + 1) * 128, tb * 512:(tb + 1) * 512], in_=ob[:], reads=[obr],
                           writes=[R_["mixT"]], own=obr)
            sx = kb.sb("p1_sx", [128, S], BF16)
            sx_r = kb.res("p1_sx")
            U2 = kb.sb("p1_U2", [128, S + 4], F32)
            U2_r = kb.res("p1_U2")
            vop(lambda: V.memset(U2[:, 0:4], 0.0), [], [U2_r])
            Ua = [(U, U_r), (U2, U2_r)]
            tstg = Slots(kb, "p1_tstg", [128, 4, 128], BF16, 2)
            for blk in range(3):
                wt, wr = ws.load(w_in[l], O_XBC + blk * 512, 512)
                for c in range(4):
                    cc = blk * 4 + c
                    U, U_r = Ua[cc % 2]
                    for tb in range(NB):
                        b = fm_block(wt, wr, c, tb)
                        aop(lambda b=b, tb=tb: A.copy(out=U[:, 4 + tb * 512:4 + (tb + 1) * 512], in_=ps[b][:]), [psr[b]], [U_r])
                    vop(lambda cc=cc: V.tensor_scalar(out=cg[:, :], in0=U[:, 1:S + 1], scalar1=sscw_t[:, cc, 0:1],
                                                      scalar2=sscb_t[:, cc:cc + 1], op0=ALU.mult, op1=ALU.add), [U_r, c_r], [cg_r])
                    for j in (1, 2, 3):
                        vop(lambda cc=cc, j=j: V.scalar_tensor_tensor(out=cg[:, :], in0=U[:, 1 + j:S + 1 + j],
                                                                      scalar=sscw_t[:, cc, j:j + 1], in1=cg[:, :],
                                                                      op0=ALU.mult, op1=ALU.add), [U_r, c_r, cg_r], [cg_r])
                    aop(lambda: A.activation(out=sx[:, :], in_=cg[:, :], func=AF.Silu), [cg_r], [sx_r])
                    if cc >= 8:
                        g = (cc - 8) % 2
                        dst = bmT_d if cc < 10 else cmT_d
                        kb.dma(out=dst[g * 128:(g + 1) * 128, :], in_=sx[:, :], reads=[sx_r],
                               writes=[R_["bmT" if cc < 10 else "cmT"]], own=sx_r)
                    if cc < 10:
                        for t4 in range(NT // 4):
                            pb = bankB.next()
                            pv = ps[pb][:].bitcast(BF16)
                            for j in range(4):
                                tt = t4 * 4 + j
                                tp(pv[:, j * 128:(j + 1) * 128], sx[:, tt * 128:(tt + 1) * 128], [sx_r], [psr[pb]], inc=(j == 3))
                            tsg, tsr = tstg.next()
                            vop(lambda pv=pv, tsg=tsg: V.tensor_copy(out=tsg[:], in_=pv[:, 0:512].rearrange("p (j c) -> p j c", j=4)),
                                [psr[pb]], [tsr])
                            if cc < 8:
                                dst = xsc_d.rearrange("(tt p) c -> p tt c", p=128)[:, t4 * 4:(t4 + 1) * 4, cc * 128:(cc + 1) * 128]
                                rn = "xsc"
                            else:
                                dst = bm_d.rearrange("(tt p) c -> p tt c", p=128)[:, t4 * 4:(t4 + 1) * 4, (cc - 8) * 128:(cc - 7) * 128]
                                rn = "bm"
                            kb.dma(out=dst, in_=tsg[:], reads=[tsr], writes=[R_[rn]], own=tsr)
            zsl = Slots(kb, "p1_z", [128, 512], F32, 3)
            for zb in range(2):
                wt, wr = ws.load(w_in[l], O_Z + zb * 512, 512)
                for tt in range(NT):
                    b = bankA.next()
                    for kc in range(8):
                        mm(ps[b][:], hT[:, kc, tt * 128:(tt + 1) * 128], wt[:, kc, 0:512], kc == 0, kc == 7, [wr, hT_r], [psr[b]],
                           inc=(kc == 7))
                    zt, zr = zsl.next()
                    aop(lambda b=b, zt=zt: A.activation(out=zt[:], in_=ps[b][:], func=AF.Silu), [psr[b]], [zr])
                    kb.dma(out=zs_d[tt * 128:(tt + 1) * 128, zb * 512:(zb + 1) * 512], in_=zt[:], reads=[zr], writes=[R_["zs"]], own=zr)
            wt, wr = ws.wb.next()
            wv = w_in[l].rearrange("(kc p) c -> p kc c", p=128)
            st_, sr_ = ws.stg.next()
            kb.dma(out=st_[:, :, 0:256], in_=wv[:, :, O_CKV:O_CKV + 256], writes=[sr_], own=sr_)
            gop(lambda: G.tensor_copy(out=wt[:, :, 0:256], in_=st_[:, :, 0:256]), [sr_], [wr])
            st_, sr_ = ws.stg.next()
            kb.dma(out=st_[:, :, 0:68], in_=wv[:, :, O_IK:O_IK + 68], writes=[sr_], own=sr_)
            kb.dma(out=st_[:, :, 68:84], in_=wv[:, :, O_DT:O_DT + 16], writes=[sr_], own=sr_)
            gop(lambda: G.tensor_copy(out=wt[:, :, 256:340], in_=st_[:, :, 0:84]), [sr_], [wr])
            stl = Slots(kb, "p1_st", [128, 16], F32, 2)
            junk = kb.sb("p1_junk", [128, 256], F32)
            junk_r = kb.res("p1_junk")
            cnl = Slots(kb, "p1_cn", [128, 256], BF16, 2)
            ctl = Slots(kb, "p1_ct", [128, 2, 128], BF16, 2)
            ikl = Slots(kb, "p1_ik", [128, 64], F32, 2)
            iknl = Slots(kb, "p1_ikn", [128, 64], BF16, 2)
            iktl = Slots(kb, "p1_ikt", [64, 128], BF16, 2)
            iwl = Slots(kb, "p1_iw", [128, 4], F32, 2)
            dtl = Slots(kb, "p1_dt", [128, 32], F32, 2)
            for tt in range(NT):
                b = bankA.next()
                for kc in range(8):
                    mm(ps[b][:, 0:340], hT[:, kc, tt * 128:(tt + 1) * 128], wt[:, kc, 0:340], kc == 0, kc == 7, [wr, hT_r], [psr[b]],
                       inc=(kc == 7))
                P = ps[b]
                st, str_ = stl.next()
                aop(lambda P=P, st=st: A.activation(out=junk[:, 0:256], in_=P[:, 0:256], func=AF.Square, accum_out=st[:, 0:1]),
                    [psr[b]], [junk_r, str_])
                rstd(st, str_, 0, 2, R)
                cn, cnr = cnl.next()
                vop(lambda P=P, st=st, cn=cn: V.scalar_tensor_tensor(out=cn[:], in0=P[:, 0:256], scalar=st[:, 2:3], in1=kvn_bc[:],
                                                                    op0=ALU.mult, op1=ALU.mult), [psr[b], str_, c_r], [cnr])
                kb.dma(out=ckv_d[tt * 128:(tt + 1) * 128, :], in_=cn[:], reads=[cnr], writes=[R_["ckv"]], own=cnr)
                pb = bankB.next()
                pv = ps[pb][:].bitcast(BF16)
                for rc in range(2):
                    tp(pv[:, rc * 128:(rc + 1) * 128], cn[:, rc * 128:(rc + 1) * 128], [cnr], [psr[pb]], inc=(rc == 1))
                ct, ctr = ctl.next()
                aop(lambda pv=pv, ct=ct: A.copy(out=ct[:], in_=pv[:, 0:256].rearrange("p (j c) -> p j c", j=2)), [psr[pb]], [ctr])
                kb.dma(out=ckvT_d.rearrange("(rc p) s -> p rc s", p=128)[:, :, tt * 128:(tt + 1) * 128], in_=ct[:], reads=[ctr],
                       writes=[R_["ckvT"]], own=ctr)
                vop(lambda P=P, st=st: V.tensor_reduce(out=st[:, 4:5], in_=P[:, 256:320], axis=AX.X, op=ALU.add), [psr[b]], [str_])
                aop(lambda P=P, st=st: A.activation(out=junk[:, 0:64], in_=P[:, 256:320], func=AF.Square, accum_out=st[:, 5:6]),
                    [psr[b]], [junk_r, str_])
                vop(lambda st=st: V.tensor_scalar(out=st[:, 6:7], in0=st[:, 4:5], scalar1=1.0 / 64, scalar2=None, op0=ALU.mult), [str_], [str_])
                vop(lambda st=st: V.tensor_tensor(out=st[:, 7:8], in0=st[:, 6:7], in1=st[:, 6:7], op=ALU.mult), [str_], [str_])
                vop(lambda st=st: V.scalar_tensor_tensor(out=st[:, 8:9], in0=st[:, 5:6], scalar=1.0 / 64, in1=st[:, 7:8],
                                                         op0=ALU.mult, op1=ALU.subtract), [str_], [str_])
                rstd(st, str_, 8, 9, 1)
                ik, ikr = ikl.next()
                vop(lambda P=P, st=st, ik=ik: V.tensor_scalar(out=ik[:], in0=P[:, 256:320], scalar1=st[:, 6:7], scalar2=st[:, 9:10],
                                                              op0=ALU.subtract, op1=ALU.mult), [psr[b], str_], [ikr])
                vop(lambda ik=ik: V.tensor_tensor(out=ik[:], in0=ik[:], in1=ikg_bc[:], op=ALU.mult), [ikr, c_r], [ikr])
                ikn, iknr = iknl.next()
                vop(lambda ik=ik, ikn=ikn: V.tensor_tensor(out=ikn[:], in0=ik[:], in1=ikb_bc[:], op=ALU.add), [ikr, c_r], [iknr])
                pb = bankB.next()
                pv = ps[pb][:].bitcast(BF16)
                tp(pv[0:64, 0:128], ikn[:, 0:64], [iknr], [psr[pb]])
                ikt, iktr = iktl.next()
                aop(lambda pv=pv, ikt=ikt: A.copy(out=ikt[:], in_=pv[0:64, 0:128]), [psr[pb]], [iktr])
                kb.dma(out=ik_d[:, tt * 128:(tt + 1) * 128], in_=ikt[:], reads=[iktr], writes=[R_["ik"]], own=iktr)
                iwt, iwr = iwl.next()
                aop(lambda P=P, iwt=iwt: A.mul(out=iwt[:], in_=P[:, 320:324], mul=0.5), [psr[b]], [iwr])
                kb.dma(out=iw_d[tt * 128:(tt + 1) * 128, :], in_=iwt[:], reads=[iwr], writes=[R_["iw"]], own=iwr)
                dtt, dtr = dtl.next()
                vop(lambda P=P, dtt=dtt: V.tensor_tensor(out=dtt[:, 16:32], in0=P[:, 324:340], in1=v16_bc[:, 0, :], op=ALU.add),
                    [psr[b], c_r], [dtr])
                aop(lambda dtt=dtt: A.activation(out=dtt[:, 16:32], in_=dtt[:, 16:32], func=AF.Exp), [dtr], [dtr])
                aop(lambda dtt=dtt: A.activation(out=dtt[:, 0:16], in_=dtt[:, 16:32], func=AF.Ln, bias=1.0, scale=1.0), [dtr], [dtr])
                vop(lambda dtt=dtt: V.tensor_tensor(out=dtt[:, 16:32], in0=dtt[:, 0:16], in1=A_bc[:], op=ALU.mult), [dtr, c_r], [dtr])
                kb.dma(out=dt_d[tt * 128:(tt + 1) * 128, :], in_=dtt[:], reads=[dtr], writes=[R_["dt"]], own=dtr)

    NIT = 16

    def gen2(l):
        c_r = kb.res("p2c")
        iqT = kb.sb("p2_iqT", [128, 2, S], BF16)
        ikT = kb.sb("p2_ikT", [128, S], BF16)
        iwa = kb.sb("p2_iw", [128, NT, 4], F32)
        kb.dma(out=iqT[:], in_=iq_d.rearrange("(c p) s -> p c s", p=128), reads=[R_["iq"]], writes=[c_r], own=c_r)
        kb.dma(out=ikT[0:64, :], in_=ik_d, reads=[R_["ik"]], writes=[c_r], own=c_r)
        kb.dma(out=ikT[64:128, :], in_=ik_d, reads=[R_["ik"]], writes=[c_r], own=c_r)
        for t0_ in range(0, NT, 8):
            t1_ = min(NT, t0_ + 8)
            kb.dma(out=iwa[:, t0_:t1_, :], in_=iw_d.rearrange("(t p) h -> p t h", p=128)[:, t0_:t1_, :], reads=[R_["iw"]], writes=[c_r], own=c_r)
        pw2 = kb.sb("p2_pw2", [128, NIT], F32)
        for k in range(NIT):
            gop(lambda: G.memset(pw2[:, k:k + 1], 2.0 ** (-(k + 1))), [], [c_r])
        SCl = Slots(kb, "p2_SC", [128, S], F32, 2)
        mkl = Slots(kb, "p2_mk", [128, S], BF16, 3)
        Rl = Slots(kb, "p2_R", [128, 512], F32, 4)
        bl = Slots(kb, "p2_b", [128, 8 + 2 * NIT], F32, 4)
        nml = Slots(kb, "p2_nm", [128, 4, 128], BF16, 3)
        bankA = RR([0, 1])
        bankT = RR([2])
        yield

        def scores(qt, out):
            N = (qt + 1) * 128
            SC, SC_r = SCl.next()
            for sb in range((N + 511) // 512):
                cols = min(512, N - sb * 512)
                cs = slice(sb * 512, sb * 512 + cols)
                for h in range(4):
                    c, hh = divmod(h, 2)
                    pr_ = slice(hh * 64, (hh + 1) * 64)
                    b = bankA.next()
                    mm(ps[b][:, 0:cols], iqT[pr_, c, qt * 128:(qt + 1) * 128], ikT[pr_, cs], True, True, [c_r], [psr[b]])
                    Rt, Rr = Rl.next()
                    aop(lambda: A.activation(out=Rt[:, 0:cols], in_=ps[b][:, 0:cols], func=AF.Relu), [psr[b]], [Rr])
                    if h == 0:
                        vop(lambda: V.tensor_scalar(out=SC[:, cs], in0=Rt[:, 0:cols], scalar1=iwa[:, qt, 0:1], scalar2=None, op0=ALU.mult),
                            [Rr, c_r], [SC_r])
                    else:
                        vop(lambda: V.scalar_tensor_tensor(out=SC[:, cs], in0=Rt[:, 0:cols], scalar=iwa[:, qt, h:h + 1], in1=SC[:, cs],
                                                           op0=ALU.mult, op1=ALU.add), [Rr, c_r, SC_r], [SC_r])
                yield
            bt, b_r = bl.next()
            mk, mk_r = mkl.next()
            vop(lambda: V.tensor_reduce(out=bt[:, 0:1], in_=SC[:, 0:N], axis=AX.X, op=ALU.max), [SC_r], [b_r])
            vop(lambda: V.tensor_reduce(out=bt[:, 1:2], in_=SC[:, 0:N], axis=AX.X, op=ALU.min), [SC_r], [b_r])
            vop(lambda: V.memset(SC[0:64, N - 64:N], -1.0e30), [], [SC_r])
            vop(lambda: V.tensor_scalar(out=bt[:, 2:3], in0=bt[:, 1:2], scalar1=-0.01, scalar2=None, op0=ALU.add), [b_r], [b_r])
            vop(lambda: V.scalar_tensor_tensor(out=bt[:, 3:4], in0=bt[:, 0:1], scalar=0.02, in1=bt[:, 1:2], op0=ALU.add, op1=ALU.subtract),
                [b_r], [b_r])
            vop(lambda: V.tensor_scalar(out=bt[:, 8:8 + NIT], in0=pw2[:], scalar1=bt[:, 3:4], scalar2=None, op0=ALU.mult), [b_r, c_r], [b_r])
            vop(lambda: V.memset(bt[:, 8 + NIT:8 + 2 * NIT], 0.0), [], [b_r])
            out.update(dict(qt=qt, N=N, SC=SC, SC_r=SC_r, bt=bt, b_r=b_r, mk=mk, mk_r=mk_r))
            yield

        def first_mid(st, on_act):
            bt, b_r = st["bt"], st["b_r"]
            if on_act:
                vop(lambda: V.scalar_tensor_tensor(out=bt[:, 4:5], in0=bt[:, 2:3], scalar=-1.0, in1=bt[:, 8:9], op0=ALU.mult, op1=ALU.subtract),
                    [b_r], [b_r])
            else:
                vop(lambda: V.tensor_tensor(out=bt[:, 4:5], in0=bt[:, 2:3], in1=bt[:, 8:9], op=ALU.add), [b_r], [b_r])

        def iteration(st, k, on_act):
            N, SC, SC_r, bt, b_r = st["N"], st["SC"], st["SC_r"], st["bt"], st["b_r"]
            jt, jr = st["mk"], st["mk_r"]
            ck = 8 + NIT + k
            if on_act:
                aop(lambda: A.activation(out=jt[:, 0:N], in_=SC[:, 0:N], func=AF.Sign, bias=bt[:, 4:5], scale=1.0,
                                         accum_out=bt[:, ck:ck + 1]), [SC_r, b_r], [jr, b_r])
                thr = 511.0 - N
            else:
                vop(lambda: V.tensor_scalar(out=jt[:, 0:N], in0=SC[:, 0:N], scalar1=bt[:, 4:5], scalar2=0.0, op0=ALU.is_ge, op1=ALU.add,
                                            accum_out=bt[:, ck:ck + 1]), [SC_r, b_r], [jr, b_r])
                thr = 255.5
            vop(lambda: V.scalar_tensor_tensor(out=bt[:, 5:6], in0=bt[:, ck:ck + 1], scalar=thr, in1=bt[:, 8 + k:9 + k],
                                               op0=ALU.is_ge, op1=ALU.mult), [b_r], [b_r])
            vop(lambda: V.tensor_tensor(out=bt[:, 2:3], in0=bt[:, 2:3], in1=bt[:, 5:6], op=ALU.add), [b_r], [b_r])
            if k + 1 < NIT:
                if on_act:
                    vop(lambda: V.scalar_tensor_tensor(out=bt[:, 4:5], in0=bt[:, 2:3], scalar=-1.0, in1=bt[:, 9 + k:10 + k],
                                                       op0=ALU.mult, op1=ALU.subtract), [b_r], [b_r])
                else:
                    vop(lambda: V.tensor_tensor(out=bt[:, 4:5], in0=bt[:, 2:3], in1=bt[:, 9 + k:10 + k], op=ALU.add), [b_r], [b_r])

        def finalize(st):
            qt, N, SC, SC_r, bt, b_r, mk, mk_r = st["qt"], st["N"], st["SC"], st["SC_r"], st["bt"], st["b_r"], st["mk"], st["mk_r"]
            vop(lambda: V.tensor_scalar(out=mk[:, 0:N], in0=SC[:, 0:N], scalar1=bt[:, 2:3], scalar2=None, op0=ALU.is_ge), [SC_r, b_r], [mk_r])
            for k0 in range(0, qt + 1, 4):
                n = min(4, qt + 1 - k0)
                pb = bankT.next()
                pv = ps[pb][:].bitcast(BF16)
                for j in range(n):
                    tp(pv[:, j * 128:(j + 1) * 128], mk[:, (k0 + j) * 128:(k0 + j + 1) * 128], [mk_r], [psr[pb]], inc=(j == n - 1))
                nm, nm_r = nml.next()
                aop(lambda: A.copy(out=nm[:, 0:n, :], in_=pv[:, 0:n * 128].rearrange("p (j t) -> p j t", j=n)), [psr[pb]], [nm_r])
                kb.dma(out=nmT_d.rearrange("(kt p) t -> p kt t", p=128)[:, k0:k0 + n, qt * 128:(qt + 1) * 128], in_=nm[:, 0:n, :],
                       reads=[nm_r], writes=[R_["nmT"]], own=nm_r)
                yield

        for q0 in range(0, NT, 2):
            sa, sb_ = {}, {}
            yield from scores(q0, sa)
            yield from scores(q0 + 1, sb_)
            first_mid(sa, True)
            first_mid(sb_, True)
            for k in range(NIT):
                iteration(sa, k, True)
                iteration(sb_, k, True)
                yield
            yield from finalize(sa)
            yield from finalize(sb_)

    def run_interleaved(gens, weights=None):
        gens = list(gens)
        weights = list(weights or [1] * len(gens))
        acc = [0.0] * len(gens)
        alive = [True] * len(gens)
        while any(alive):
            for i, g in enumerate(gens):
                if not alive[i]:
                    continue
                acc[i] += weights[i]
                while acc[i] >= 1.0 and alive[i]:
                    acc[i] -= 1.0
                    try:
                        next(g)
                    except StopIteration:
                        alive[i] = False

    def phase2(l):
        with Phase():
            run_interleaved([gen2(l)])

    def phase24(l):
        with Phase():
            run_interleaved([gen2(l), gen4(l)], [1.0, 5.5])

    def phase3(l):
        with Phase():
            c_r = kb.res("p3c")
            ckvT = kb.sb("p3_ckvT", [128, 2, S], BF16)
            kb.dma(out=ckvT[:], in_=ckvT_d.rearrange("(rc p) s -> p rc s", p=128), reads=[R_["ckvT"]], writes=[c_r], own=c_r)
            ckva = kb.sb("p3_ckva", [128, NT, 258], BF16)
            gop(lambda: G.memset(ckva[:, :, 256:258], 1.0), [], [c_r])
            for t0_ in range(0, NT, 8):
                t1_ = min(NT, t0_ + 8)
                kb.dma(out=ckva[:, t0_:t1_, 0:256], in_=ckv_d.rearrange("(kt p) r -> p kt r", p=128)[:, t0_:t1_, :], reads=[R_["ckv"]], writes=[c_r], own=c_r)
            gt = kb.sb("p3_gt", [128, NH, GW], BF16)
            b15 = kb.sb("p3_b15", [128, NH], F32)
            gstg = Slots(kb, "p3_gstg", [128, GW], F32, 2)
            for h in range(NH):
                st, sr = gstg.next()
                kb.dma(out=st[:], in_=gtab[:, h, :], writes=[sr], own=sr)
                gop(lambda st=st, h=h: G.tensor_copy(out=gt[:, h, :], in_=st[:]), [sr], [c_r])
                gop(lambda st=st, h=h: G.tensor_copy(out=b15[:, h:h + 1], in_=st[:, GW - 1:GW]), [sr], [c_r])
            wuv_f = kb.sb("p3_wuvf", [128, 2, NH * 64], F32)
            wuv_b = kb.sb("p3_wuvb", [128, 2, NH * 64], BF16)
            kb.dma(out=wuv_f[:], in_=wuv[l], writes=[c_r], own=c_r)
            gop(lambda: G.tensor_copy(out=wuv_b[:], in_=wuv_f[:]), [c_r], [c_r])
            nml = Slots(kb, "p3_nm", [128, NT, 512], BF16, 2)
            ql = Slots(kb, "p3_q", [128, 2, 512], BF16, 3)
            El = Slots(kb, "p3_E", [128, 512], BF16, 3)
            PTl = Slots(kb, "p3_PT", [128, 512], BF16, 3)
            rsl = Slots(kb, "p3_rs", [128, 4], F32, 2)
            ol = Slots(kb, "p3_o", [128, 256], BF16, 2)
            oTl = Slots(kb, "p3_oT", [128, 2, 128], BF16, 2)
            aTl = Slots(kb, "p3_aT", [64, 512], BF16, 2)
            bankS = RR([0, 1, 2])
            nmv = nmT_d.rearrange("(kt p) t -> p kt t", p=128)
            nm_tiles = {}
            q_tiles = {}

            def load_nm(tb):
                if tb >= NB or tb in nm_tiles:
                    return
                nm, nm_r = nml.next()
                for t0_ in range(0, 4 * tb, 8):
                    t1_ = min(4 * tb, t0_ + 8)
                    kb.dma(out=nm[:, t0_:t1_, :], in_=nmv[:, t0_:t1_, tb * 512:(tb + 1) * 512], reads=[R_["nmT"]], writes=[nm_r], own=nm_r)
                for i in range(4):
                    kb.dma(out=nm[:, 4 * tb + i, i * 128:512], in_=nmv[:, 4 * tb + i, tb * 512 + i * 128:(tb + 1) * 512], reads=[R_["nmT"]],
                           writes=[nm_r], own=nm_r)
                nm_tiles[tb] = (nm, nm_r)

            def load_q(idx):
                if idx >= NB * NH or idx in q_tiles:
                    return
                tb, h = divmod(idx, NH)
                q, q_r = ql.next()
                kb.dma(out=q[:], in_=qlat_d[h].rearrange("(rc p) s -> p rc s", p=128)[:, :, tb * 512:(tb + 1) * 512], reads=[R_["qlat"]],
                       writes=[q_r], own=q_r)
                q_tiles[idx] = (q, q_r)

            groups = [(tb, h, kt) for tb in range(NB) for h in range(NH) for kt in range(4 * tb + 4)]
            sbank = {}

            def emit_qk(g):
                tb, h, kt = g
                if kt == 0:
                    load_nm(tb)
                    load_q(tb * NH + h)
                    load_q(tb * NH + h + 1)
                q, q_r = q_tiles[tb * NH + h]
                nm, nm_r = nm_tiles[tb]
                cl = max(0, kt - 4 * tb) * 128
                b = bankS.next()
                sbank[g] = b
                ks = slice(kt * 128, (kt + 1) * 128)
                c0 = tb * 512 - kt * 128 + 384
                const = c0 >= GC
                mm(ps[b][:, cl:512], ckvT[:, 0, ks], q[:, 0, cl:512], True, False, [c_r, q_r], [psr[b]], inc=False)
                mm(ps[b][:, cl:512], ckvT[:, 1, ks], q[:, 1, cl:512], False, const, [c_r, q_r], [psr[b]], inc=const)
                if not const:
                    mm(ps[b][:, cl:512], ident[:], gt[:, h, c0 + cl:c0 + 512], False, True, [cres, c_r], [psr[b]])

            def emit_rest(g):
                tb, h, kt = g
                if h == 0 and kt == 0:
                    load_nm(tb + 1)
                nm, nm_r = nm_tiles[tb]
                i = max(0, kt - 4 * tb)
                cl = i * 128
                b = sbank.pop(g)
                const = (tb * 512 - kt * 128 + 384) >= GC
                E, E_r = El.next()
                if const:
                    aop(lambda: A.activation(out=E[:, cl:512], in_=ps[b][:, cl:512], func=AF.Exp, bias=b15[:, h:h + 1], scale=1.0),
                        [psr[b], c_r], [E_r])
                else:
                    aop(lambda: A.activation(out=E[:, cl:512], in_=ps[b][:, cl:512], func=AF.Exp), [psr[b]], [E_r])
                PT, PT_r = PTl.next()
                vop(lambda: V.tensor_tensor(out=PT[:, cl:512], in0=E[:, cl:512], in1=nm[:, kt, cl:512], op=ALU.mult), [E_r, nm_r], [PT_r])
                for j in range(i, 4):
                    mm(ps[4 + j][:, 0:257], PT[:, j * 128:(j + 1) * 128], ckva[:, kt, 0:257], kt == 0, kt == 4 * tb + j, [PT_r, c_r],
                       [psr[4 + j]], inc=(j == 3 or kt == 4 * tb + j))

            def emit_post(tb, h):
                aT, aT_r = aTl.next()
                for j in range(4):
                    rs, rs_r = rsl.next()
                    vop(lambda: V.reciprocal(out=rs[:, 0:1], in_=ps[4 + j][:, 256:257]), [psr[4 + j]], [rs_r])
                    o, o_r = ol.next()
                    aop(lambda: A.activation(out=o[:], in_=ps[4 + j][:, 0:256], func=AF.Identity, scale=rs[:, 0:1]),
                        [psr[4 + j], rs_r], [o_r])
                    pv = ps[3][:].bitcast(BF16)
                    for rc in range(2):
                        tp(pv[:, rc * 128:(rc + 1) * 128], o[:, rc * 128:(rc + 1) * 128], [o_r], [psr[3]], inc=(rc == 1))
                    oT, oT_r = oTl.next()
                    vop(lambda: V.tensor_copy(out=oT[:], in_=pv[:, 0:256].rearrange("p (j t) -> p j t", j=2)), [psr[3]], [oT_r])
                    for rc in range(2):
                        mm(ps[3][0:64, 256:384], wuv_b[:, rc, h * 64:(h + 1) * 64], oT[:, rc, :], rc == 0, rc == 1, [c_r, oT_r], [psr[3]],
                           inc=(rc == 1))
                    vop(lambda: V.tensor_copy(out=aT[:, j * 128:(j + 1) * 128], in_=ps[3][0:64, 256:384]), [psr[3]], [aT_r])
                kb.dma(out=mixT_d[h * 64:(h + 1) * 64, tb * 512:(tb + 1) * 512], in_=aT[:], reads=[aT_r], writes=[R_["mixT"]], own=aT_r)
                q_tiles.pop(tb * NH + h, None)
                if h == NH - 1:
                    nm_tiles.pop(tb, None)

            emit_qk(groups[0])
            for idx, g in enumerate(groups):
                if idx + 1 < len(groups):
                    emit_qk(groups[idx + 1])
                emit_rest(g)
                tb, h, kt = g
                if kt == 4 * tb + 3:
                    emit_post(tb, h)

    def gen4(l):
        if True:
            c_r = kb.res("p4c")
            negtri4 = kb.sb("negtri4", [128, 4, 128], F32)
            gop(lambda: G.memset(negtri4[:], 0.0), [], [c_r])
            for j in range(4):
                gop(lambda j=j: G.affine_select(out=negtri4[:, j, :], in_=negtri4[:, j, :], pattern=[[1, 128]],
                                                compare_op=ALU.is_ge, fill=NEG, base=0, channel_multiplier=-1), [c_r], [c_r])
            v16_bc = kb.sb("p4_v16", [128, 3, 16], F32)
            kb.dma(out=v16_bc[:], in_=v16[l].partition_broadcast(128), writes=[c_r], own=c_r)
            gn = kb.sb("p4_gn", [128, D], F32)
            bcast_load(gn[:], vecD[l, 4, :], D, c_r)
            ST = kb.sb("p4_ST", [128, 2, 512], F32)
            ST_r = kb.res("p4_ST")
            STb = kb.sb("p4_STb", [128, 2, 512], BF16)
            STb_r = kb.res("p4_STb")
            vop(lambda: V.memset(ST[:], 0.0), [], [ST_r])
            vop(lambda: V.memset(STb[:], 0.0), [], [STb_r])
            xsl = Slots(kb, "p4_xs", [128, 16, 64], BF16, 2)
            bml = Slots(kb, "p4_bm", [128, 256], BF16, 2)
            bmTl = Slots(kb, "p4_bmT", [128, 2, 128], BF16, 2)
            cmTl = Slots(kb, "p4_cmT", [128, 2, 128], BF16, 2)
            dtl = Slots(kb, "p4_dt", [128, 32], F32, 2)
            zl = Slots(kb, "p4_z", [128, D], F32, 2)
            sml = Slots(kb, "p4_sm", [128, 6, 16], F32, 2)
            xdtl = Slots(kb, "p4_xdt", [128, 16, 64], BF16, 2)
            xdwl = Slots(kb, "p4_xdw", [128, 16, 64], BF16, 2)
            CBm = kb.sb("p4_CBm", [128, 2, 128], F32)
            CBm_r = kb.res("p4_CBm")
            rhsD = kb.sb("p4_rhsD", [128, 16, 128], F32)
            rhsD_r = kb.res("p4_rhsD")
            L = kb.sb("p4_L", [128, 16, 128], F32)
            L_r = kb.res("p4_L")
            M = kb.sb("p4_M", [128, 16, 128], BF16)
            M_r = kb.res("p4_M")
            Y = kb.sb("p4_Y", [128, 16, 64], F32)
            Y_r = kb.res("p4_Y")
            tmpY = kb.sb("p4_tmpY", [128, 16, 64], F32)
            tmpY_r = kb.res("p4_tmpY")
            Yn = kb.sb("p4_Yn", [128, D], BF16)
            Yn_r = kb.res("p4_Yn")
            stl = Slots(kb, "p4_st", [128, 16], F32, 2)
            cTl = Slots(kb, "p4_cT", [128, 8, 128], BF16, 2)
            bankD = RR([4])
            stA = {}

            def stageA(ct):
                rows = slice(ct * 128, (ct + 1) * 128)
                yb = (5, 6)
                xdt, xdt_r = xdtl.next()
                xdw, xdw_r = xdwl.next()
                xs, xs_r = xsl.next()
                kb.dma(out=xs[:].rearrange("p h d -> p (h d)"), in_=xsc_d[rows, :], reads=[R_["xsc"]], writes=[xs_r], own=xs_r)
                bm, bm_r = bml.next()
                kb.dma(out=bm[:], in_=bm_d[rows, :], reads=[R_["bm"]], writes=[bm_r], own=bm_r)
                bmT, bmT_r = bmTl.next()
                kb.dma(out=bmT[:], in_=bmT_d.rearrange("(g n) s -> n g s", g=2)[:, :, rows], reads=[R_["bmT"]], writes=[bmT_r], own=bmT_r)
                cmT, cmT_r = cmTl.next()
                kb.dma(out=cmT[:], in_=cmT_d.rearrange("(g n) s -> n g s", g=2)[:, :, rows], reads=[R_["cmT"]], writes=[cmT_r], own=cmT_r)
                dtt, dt_r = dtl.next()
                kb.dma(out=dtt[:], in_=dt_d[rows, :], reads=[R_["dt"]], writes=[dt_r], own=dt_r)
                zt, z_r = zl.next()
                kb.dma(out=zt[:], in_=zs_d[rows, :], reads=[R_["zs"]], writes=[z_r], own=z_r)
                sm, sm_r = sml.next()
                mm(ps[3][:, 0:16], utri_f[:], dtt[:, 16:32], True, True, [cres, dt_r], [psr[3]])
                yield
                mm(ps[3][:, 16:32], ones_f[:], dtt[:, 16:32], True, True, [cres, dt_r], [psr[3]])
                yield
                aop(lambda sm=sm: A.copy(out=sm[:, 0, :], in_=ps[3][:, 0:16]), [psr[3]], [sm_r])
                yield
                aop(lambda sm=sm: A.mul(out=sm[:, 1, :], in_=ps[3][:, 0:16], mul=-1.0), [psr[3]], [sm_r])
                yield
                aop(lambda sm=sm: A.activation(out=sm[:, 2, :], in_=ps[3][:, 0:16], func=AF.Exp), [psr[3]], [sm_r])
                yield
                aop(lambda sm=sm: A.activation(out=sm[:, 4, :], in_=ps[3][:, 16:32], func=AF.Exp), [psr[3]], [sm_r])
                yield
                vop(lambda sm=sm: V.tensor_tensor(out=sm[:, 5, :], in0=ps[3][:, 16:32], in1=sm[:, 0, :], op=ALU.subtract), [psr[3], sm_r], [sm_r])
                yield
                aop(lambda sm=sm: A.activation(out=sm[:, 3, :], in_=sm[:, 5, :], func=AF.Exp), [sm_r], [sm_r])
                yield
                vop(lambda xs=xs, dtt=dtt: V.tensor_tensor(out=xdt[:], in0=xs[:], in1=dtt[:, 0:16].unsqueeze(2).to_broadcast([128, 16, 64]),
                                                           op=ALU.mult), [xs_r, dt_r], [xdt_r])
                yield
                gop(lambda sm=sm: G.tensor_tensor(out=xdw[:], in0=xdt[:], in1=sm[:, 3, :].unsqueeze(2).to_broadcast([128, 16, 64]),
                                                  op=ALU.mult), [xdt_r, sm_r], [xdw_r])
                yield
                for g in range(2):
                    mm(ps[3][:, 64 + g * 128:64 + (g + 1) * 128], bmT[:, g, :], cmT[:, g, :], True, True, [bmT_r, cmT_r], [psr[3]], inc=(g == 1))
                    yield
                vop(lambda: V.tensor_tensor(out=CBm[:], in0=ps[3][:, 64:320].rearrange("p (g l) -> p g l", g=2),
                                            in1=tri_f[:].unsqueeze(1).to_broadcast([128, 2, 128]), op=ALU.mult), [psr[3], cres], [CBm_r])
                yield
                gop(lambda sm=sm: G.tensor_tensor(out=rhsD[:], in0=ident_f[:].unsqueeze(1).to_broadcast([128, 16, 128]),
                                                  in1=sm[:, 0, :].unsqueeze(2).to_broadcast([128, 16, 128]), op=ALU.mult),
                    [cres, sm_r], [rhsD_r])
                yield
                for q in range(4):
                    b = bankD.next()
                    mm(ps[b][:], ones_f[:], rhsD[:, 4 * q:4 * q + 4, :].rearrange("p h l -> p (h l)"), True, False, [cres, rhsD_r], [psr[b]],
                       inc=False)
                    yield
                    mm(ps[b][:], ident_f[:], negtri4[:].rearrange("p h l -> p (h l)"), False, True, [cres, c_r], [psr[b]])
                    yield
                    for hq in range(4):
                        h = 4 * q + hq
                        aop(lambda b=b, hq=hq, h=h, sm=sm: A.activation(out=L[:, h, :], in_=ps[b][:, hq * 128:(hq + 1) * 128], func=AF.Exp,
                                                                       bias=sm[:, 1, h:h + 1], scale=1.0), [psr[b], sm_r], [L_r])
                        yield
                for g in range(2):
                    gop(lambda g=g: G.tensor_tensor(out=M[:, g * 8:(g + 1) * 8, :], in0=L[:, g * 8:(g + 1) * 8, :],
                                                    in1=CBm[:, g, :].unsqueeze(1).to_broadcast([128, 8, 128]), op=ALU.mult),
                        [L_r, CBm_r], [M_r])
                    yield
                for h in range(16):
                    b = yb[h // 8]
                    hh = h % 8
                    mm(ps[b][:, hh * 64:(hh + 1) * 64], M[:, h, :], xdt[:, h, :], True, True, [M_r, xdt_r], [psr[b]], inc=(hh == 7))
                    yield
                stA[ct] = dict(rows=rows, yb=yb, xs=xs, xs_r=xs_r, bm=bm, bm_r=bm_r, cmT=cmT, cmT_r=cmT_r, zt=zt, z_r=z_r, sm=sm, sm_r=sm_r,
                               xdw=xdw, xdw_r=xdw_r)

            def stageB(ct):
                d = stA.pop(ct)
                rows, yb, xs, xs_r, bm, bm_r, cmT, cmT_r = d["rows"], d["yb"], d["xs"], d["xs_r"], d["bm"], d["bm_r"], d["cmT"], d["cmT_r"]
                zt, z_r, sm, sm_r, xdw, xdw_r = d["zt"], d["z_r"], d["sm"], d["sm_r"], d["xdw"], d["xdw_r"]
                yield
                for g in range(2):
                    mm(ps[7][:], cmT[:, g, :], STb[:, g, :], True, True, [cmT_r, STb_r], [psr[7]])
                    yield
                    vop(lambda g=g, sm=sm: V.tensor_tensor(out=Y[:, g * 8:(g + 1) * 8, :], in0=ps[7][:].rearrange("p (h d) -> p h d", h=8),
                                                           in1=sm[:, 2, g * 8:(g + 1) * 8].unsqueeze(2).to_broadcast([128, 8, 64]),
                                                           op=ALU.mult), [psr[7], sm_r], [Y_r])
                    yield
                    vop(lambda g=g: V.tensor_tensor(out=Y[:, g * 8:(g + 1) * 8, :], in0=Y[:, g * 8:(g + 1) * 8, :],
                                                    in1=ps[yb[g]][:].rearrange("p (h d) -> p h d", h=8), op=ALU.add),
                        [Y_r, psr[yb[g]]], [Y_r])
                    yield
                gop(lambda xs=xs: G.tensor_tensor(out=tmpY[:], in0=xs[:], in1=v16_bc[:, 2, :].unsqueeze(2).to_broadcast([128, 16, 64]),
                                                  op=ALU.mult), [xs_r, c_r], [tmpY_r])
                yield
                gop(lambda: G.tensor_tensor(out=Y[:], in0=Y[:], in1=tmpY[:], op=ALU.add), [Y_r, tmpY_r], [Y_r])
                yield
                for g in range(2):
                    mm(ps[7][:], bm[:, g * 128:(g + 1) * 128], xdw[:, g * 8:(g + 1) * 8, :].rearrange("p h d -> p (h d)"), True, True,
                       [bm_r, xdw_r], [psr[7]])
                    yield
                    vop(lambda g=g, sm=sm: V.tensor_tensor(out=ST[:, g, :].rearrange("p (h d) -> p h d", h=8),
                                                           in0=ST[:, g, :].rearrange("p (h d) -> p h d", h=8),
                                                           in1=sm[:, 4, g * 8:(g + 1) * 8].unsqueeze(2).to_broadcast([128, 8, 64]),
                                                           op=ALU.mult), [ST_r, sm_r, STb_r], [ST_r])
                    yield
                    vop(lambda g=g: V.tensor_tensor(out=ST[:, g, :], in0=ST[:, g, :], in1=ps[7][:], op=ALU.add), [ST_r, psr[7]], [ST_r])
                    yield
                aop(lambda: A.copy(out=STb[:], in_=ST[:]), [ST_r], [STb_r])
                yield
                Yf = Y[:].rearrange("p h d -> p (h d)")
                gop(lambda zt=zt: G.tensor_tensor(out=Yf, in0=Yf, in1=zt[:], op=ALU.mult), [Y_r, z_r], [Y_r])
                yield
                st, st_r = stl.next()
                for g in range(2):
                    aop(lambda g=g, st=st: A.activation(out=tmpY[:].rearrange("p h d -> p (h d)")[:, g * 512:(g + 1) * 512],
                                                        in_=Yf[:, g * 512:(g + 1) * 512], func=AF.Square, accum_out=st[:, g:g + 1]),
                        [Y_r], [tmpY_r, st_r])
                    yield
                    rstd(st, st_r, g, 4 + g, 512)
                    vop(lambda g=g, st=st: V.scalar_tensor_tensor(out=Yn[:, g * 512:(g + 1) * 512], in0=Yf[:, g * 512:(g + 1) * 512],
                                                                  scalar=st[:, 4 + g:5 + g], in1=gn[:, g * 512:(g + 1) * 512],
                                                                  op0=ALU.mult, op1=ALU.mult), [Y_r, st_r, c_r], [Yn_r])
                    yield
                cT, cT_r = cTl.next()
                pv = ps[7][:].bitcast(BF16)
                for c in range(8):
                    tp(pv[:, c * 128:(c + 1) * 128], Yn[:, c * 128:(c + 1) * 128], [Yn_r], [psr[7]], inc=(c == 7))
                    yield
                aop(lambda: A.copy(out=cT[:], in_=pv[:, 0:1024].rearrange("p (j t) -> p j t", j=8)), [psr[7]], [cT_r])
                yield
                kb.dma(out=mixT_d.rearrange("(kc p) s -> p kc s", p=128)[:, 8:16, rows], in_=cT[:], reads=[cT_r], writes=[R_["mixT"]], own=cT_r)

            yield
            for ct in range(NT):
                yield from stageA(ct)
                yield from stageB(ct)

    def phase4(l):
        with Phase():
            run_interleaved([gen4(l)])

    class WLoad:
        def __init__(self, name, kc=8, nc_=256, n=3):
            self.kc, self.nc_ = kc, nc_
            self.stg = Slots(kb, name + "_stg", [128, kc, nc_], F32, n)

        def load(self, dst, dst_r, wap, r0, kcn, c0, ncols):
            assert kcn <= self.kc and ncols <= self.nc_
            st, sr = self.stg.next()
            src = wap[r0:r0 + kcn * 128, :].rearrange("(kc p) c -> p kc c", p=128)[:, :, c0:c0 + ncols]
            kb.dma(out=st[:, 0:kcn, 0:ncols], in_=src, writes=[sr], own=sr)
            self.i = getattr(self, "i", 0) + 1
            e = ("dve", "act", "pool", "dve", "act")[self.i % 5]
            if e == "pool":
                gop(lambda: G.tensor_copy(out=dst, in_=st[:, 0:kcn, 0:ncols]), [sr], [dst_r])
            elif e == "dve":
                vop(lambda: V.tensor_copy(out=dst, in_=st[:, 0:kcn, 0:ncols]), [sr], [dst_r])
            else:
                aop(lambda: A.copy(out=dst, in_=st[:, 0:kcn, 0:ncols]), [sr], [dst_r])

        def load_full(self, dst_t, dst_r, wap, KC, C):
            for k0 in range(0, KC, self.kc):
                kn = min(self.kc, KC - k0)
                for c0 in range(0, C, self.nc_):
                    cn = min(self.nc_, C - c0)
                    self.load(dst_t[:, k0:k0 + kn, c0:c0 + cn], dst_r, wap, k0 * 128, kn, c0, cn)

    def post_norm_residual(P0, P1, r0, r1, xt, xr, g_bc, g_r, stl, junk, junk_r, tmp, tmp_r):
        st, st_r = stl.next()
        aop(lambda: A.activation(out=junk[:, 0:512], in_=P0[:], func=AF.Square, accum_out=st[:, 0:1]), [r0], [junk_r, st_r])
        aop(lambda: A.activation(out=junk[:, 512:1024], in_=P1[:], func=AF.Square, accum_out=st[:, 1:2]), [r1], [junk_r, st_r])
        vop(lambda: V.tensor_tensor(out=st[:, 3:4], in0=st[:, 0:1], in1=st[:, 1:2], op=ALU.add), [st_r], [st_r])
        rstd(st, st_r, 3, 2, D)
        for half, (P, r) in enumerate(((P0, r0), (P1, r1))):
            sl = slice(half * 512, (half + 1) * 512)
            vop(lambda P=P, sl=sl: V.scalar_tensor_tensor(out=tmp[:, sl], in0=P[:], scalar=st[:, 2:3], in1=g_bc[:, sl],
                                                         op0=ALU.mult, op1=ALU.mult), [r, st_r, g_r], [tmp_r])
            gop(lambda sl=sl: G.tensor_tensor(out=xt[:, sl], in0=tmp[:, sl], in1=xt[:, sl], op=ALU.add), [tmp_r, xr], [xr])

    def phase5(l):
        with Phase():
            src = x_in if l == 0 else xs_d
            wl = WLoad("p5w")
            wo = kb.sb("p5_wo", [128, 16, D], BF16)
            wo_r = kb.res("p5_wo")
            wl.load_full(wo, wo_r, w_out[l], 16, D)
            g1 = kb.sb("p5_g1", [128, D], F32)
            g2 = kb.sb("p5_g2", [128, D], F32)
            g_r = kb.res("p5_g")
            bcast_load(g1[:], vecD[l, 1, :], D, g_r)
            bcast_load(g2[:], vecD[l, 2, :], D, g_r)
            xsl = Slots(kb, "p5_x", [128, D], F32, 2)
            mxl = Slots(kb, "p5_mx", [128, 16, 128], BF16, 2)
            junk = kb.sb("p5_junk", [128, D], F32)
            junk_r = kb.res("p5_junk")
            tmp = kb.sb("p5_tmp", [128, D], F32)
            tmp_r = kb.res("p5_tmp")
            stl = Slots(kb, "p5_st", [128, 16], F32, 2)
            hb = kb.sb("p5_hb", [128, D], BF16)
            ntmp = (junk, junk_r, stl, hb, kb.res("p5_hb"))
            banks = RR([0, 1, 2, 3])
            for tt in range(NT):
                xt, xr = xsl.next()
                kb.dma(out=xt[:], in_=src[tt * 128:(tt + 1) * 128, :], reads=[R_["xs"]], writes=[xr], own=xr)
                mx, mr = mxl.next()
                kb.dma(out=mx[:], in_=mixT_d.rearrange("(kc p) s -> p kc s", p=128)[:, :, tt * 128:(tt + 1) * 128],
                       reads=[R_["mixT"]], writes=[mr], own=mr)
                b0, b1 = banks.next(), banks.next()
                for half, b in enumerate((b0, b1)):
                    for kc in range(16):
                        mm(ps[b][:], mx[:, kc, :], wo[:, kc, half * 512:(half + 1) * 512], kc == 0, kc == 15, [mr, wo_r], [psr[b]],
                           inc=(kc == 15))
                post_norm_residual(ps[b0], ps[b1], psr[b0], psr[b1], xt, xr, g1, g_r, stl, junk, junk_r, tmp, tmp_r)
                kb.dma(out=xm_d[tt * 128:(tt + 1) * 128, :], in_=xt[:], reads=[xr], writes=[R_["xm"]], own=xr)
                norm_to_hT(xt[:], xr, g2, g_r, tt, ntmp)

    def phase6a(l):
        with Phase():
            hT, hT_r = HT['t'], HT['r']
            wl = WLoad("p6w", 8, 512, 2)
            wgl = Slots(kb, "p6_wg", [128, 8, 512], BF16, 2)
            wul = Slots(kb, "p6_wu", [128, 8, 512], BF16, 2)
            sgl = Slots(kb, "p6_sg", [128, 512], F32, 2)
            gul = Slots(kb, "p6_gu", [128, 512], BF16, 3)
            bankA = RR([0, 1])
            bankB = RR([2, 3])
            for c0 in range(0, DFF, 512):
                cn = min(512, DFF - c0)
                wg_t, wg_r = wgl.next()
                wu_t, wu_r = wul.next()
                wl.load(wg_t[:, :, 0:cn], wg_r, w_g[l], 0, 8, c0, cn)
                wl.load(wu_t[:, :, 0:cn], wu_r, w_u[l], 0, 8, c0, cn)
                for c in range(cn // 128):
                    for tb in range(NB):
                        bg_, bu_ = bankA.next(), bankB.next()
                        for kc in range(8):
                            mm(ps[bg_][:], wg_t[:, kc, c * 128:(c + 1) * 128], hT[:, kc, tb * 512:(tb + 1) * 512], kc == 0, kc == 7,
                               [wg_r, hT_r], [psr[bg_]], inc=(kc == 7))
                        for kc in range(8):
                            mm(ps[bu_][:], wu_t[:, kc, c * 128:(c + 1) * 128], hT[:, kc, tb * 512:(tb + 1) * 512], kc == 0, kc == 7,
                               [wu_r, hT_r], [psr[bu_]], inc=(kc == 7))
                        sg, sgr = sgl.next()
                        aop(lambda sg=sg, bg_=bg_: A.activation(out=sg[:], in_=ps[bg_][:], func=AF.Silu), [psr[bg_]], [sgr])
                        gu, gur = gul.next()
                        vop(lambda sg=sg, gu=gu, bu_=bu_: V.tensor_tensor(out=gu[:], in0=ps[bu_][:], in1=sg[:], op=ALU.mult),
                            [psr[bu_], sgr], [gur])
                        f0 = c0 + c * 128
                        kb.dma(out=gu_d[f0:f0 + 128, tb * 512:(tb + 1) * 512], in_=gu[:], reads=[gur], writes=[R_["gu"]], own=gur)

    def phase6b(l):
        last = (l == DEPTH - 1)
        with Phase():
            wl = WLoad("p6bw")
            KF = DFF // 128
            wd = kb.sb("p6b_wd", [128, KF, D], BF16)
            wd_r = kb.res("p6b_wd")
            wl.load_full(wd, wd_r, w_d[l], KF, D)
            wpg = kb.sb("p6b_wpg", [128, 8, D], BF16)
            wpg_r = kb.res("p6b_wpg")
            wl.load_full(wpg, wpg_r, w_pg[l], 8, D)
            wpp = kb.sb("p6b_wpp", [128, 2, D], BF16)
            wpp_r = kb.res("p6b_wpp")
            wl.load_full(wpp, wpp_r, w_pp[l], 2, D)
            g1 = kb.sb("p6b_g1", [128, D], F32)
            g_r = kb.res("p6b_g")
            bcast_load(g1[:], vecD[l, 3, :], D, g_r)
            xsl = Slots(kb, "p6b_x", [128, D], F32, 2)
            gtl = Slots(kb, "p6b_gt", [128, KF, 128], BF16, 2)
            pl = Slots(kb, "p6b_p", [128, 256], F32, 2)
            junk = kb.sb("p6b_junk", [128, D], F32)
            junk_r = kb.res("p6b_junk")
            tmp = kb.sb("p6b_tmp", [128, D], F32)
            tmp_r = kb.res("p6b_tmp")
            stl = Slots(kb, "p6b_st", [128, 16], F32, 2)
            xb = kb.sb("p6b_xb", [128, D], BF16)
            xb_r = kb.res("p6b_xb")
            xT = kb.sb("p6b_xT", [128, 8, 128], BF16)
            xT_r = kb.res("p6b_xT")
            pb16 = kb.sb("p6b_pb", [128, 256], BF16)
            pb_r = kb.res("p6b_pb")
            pT = kb.sb("p6b_pT", [128, 2, 128], BF16)
            pT_r = kb.res("p6b_pT")
            sg = kb.sb("p6b_sg", [128, D], F32)
            sg_r = kb.res("p6b_sg")
            dst_d = y_out if last else xs_d
            dst_r = R_["y"] if last else R_["xs"]
            S1 = {}

            def stage1(tt):
                xt, xr = xsl.next()
                kb.dma(out=xt[:], in_=xm_d[tt * 128:(tt + 1) * 128, :], reads=[R_["xm"]], writes=[xr], own=xr)
                gt, gr = gtl.next()
                kb.dma(out=gt[:], in_=gu_d.rearrange("(kc p) s -> p kc s", p=128)[:, :, tt * 128:(tt + 1) * 128],
                       reads=[R_["gu"]], writes=[gr], own=gr)
                pt, pr = pl.next()
                kb.dma(out=pt[:], in_=p_in[l, tt * 128:(tt + 1) * 128, :], writes=[pr], own=pr)
                db = (0, 1) if tt % 2 == 0 else (2, 3)
                for half, b in enumerate(db):
                    for kc in range(KF):
                        mm(ps[b][:], gt[:, kc, :], wd[:, kc, half * 512:(half + 1) * 512], kc == 0, kc == KF - 1, [gr, wd_r], [psr[b]],
                           inc=(kc == KF - 1))
                S1[tt] = (xt, xr, pt, pr, db)

            def stage2(tt):
                xt, xr, pt, pr, db = S1.pop(tt)
                post_norm_residual(ps[db[0]], ps[db[1]], psr[db[0]], psr[db[1]], xt, xr, g1, g_r, stl, junk, junk_r, tmp, tmp_r)
                aop(lambda: A.copy(out=xb[:], in_=xt[:]), [xr], [xb_r])
                pv = ps[7][:].bitcast(BF16)
                for dc in range(8):
                    tp(pv[:, dc * 128:(dc + 1) * 128], xb[:, dc * 128:(dc + 1) * 128], [xb_r], [psr[7]], inc=(dc == 7))
                vop(lambda: V.tensor_copy(out=xT[:], in_=pv[:, 0:1024].rearrange("p (j t) -> p j t", j=8)), [psr[7]], [xT_r])
                for half, b in enumerate((4, 5)):
                    for kc in range(8):
                        mm(ps[b][:], xT[:, kc, :], wpg[:, kc, half * 512:(half + 1) * 512], kc == 0, kc == 7, [xT_r, wpg_r], [psr[b]],
                           inc=(kc == 7))
                    aop(lambda: A.activation(out=sg[:, half * 512:(half + 1) * 512], in_=ps[b][:], func=AF.Sigmoid), [psr[b]], [sg_r])
                vop(lambda: V.tensor_copy(out=pb16[:], in_=pt[:]), [pr], [pb_r])
                pv6 = ps[6][:].bitcast(BF16)
                for j in range(2):
                    tp(pv6[:, j * 128:(j + 1) * 128], pb16[:, j * 128:(j + 1) * 128], [pb_r], [psr[6]], inc=(j == 1))
                vop(lambda: V.tensor_copy(out=pT[:], in_=pv6[:, 0:256].rearrange("p (j t) -> p j t", j=2)), [psr[6]], [pT_r])
                for half in range(2):
                    for kc in range(2):
                        mm(ps[6][:], pT[:, kc, :], wpp[:, kc, half * 512:(half + 1) * 512], kc == 0, kc == 1, [pT_r, wpp_r], [psr[6]],
                           inc=(kc == 1))
                    sl = slice(half * 512, (half + 1) * 512)
                    vop(lambda: V.tensor_tensor(out=tmp[:, sl], in0=ps[6][:], in1=sg[:, sl], op=ALU.mult), [psr[6], sg_r], [tmp_r])
                    gop(lambda: G.tensor_tensor(out=xt[:, sl], in0=tmp[:, sl], in1=xt[:, sl], op=ALU.add), [tmp_r, xr], [xr])
                kb.dma(out=dst_d[tt * 128:(tt + 1) * 128, :], in_=xt[:], reads=[xr], writes=[dst_r], own=xr)

            stage1(0)
            for tt in range(NT):
                if tt + 1 < NT:
                    stage1(tt + 1)
                stage2(tt)

    def front(l):
        with HTScope():
            phase0(l, None if l == 0 else xs_d)
            phase1(l)

    def mid(l):
        phase24(l)
        phase3(l)

    def back(l):
        with HTScope():
            phase5(l)
            phase6a(l)
        phase6b(l)

    def run_all():
        for l in range(DEPTH):
            front(l)
            mid(l)
            back(l)

    phases = phases or ["all"]
    kb.fn = dict(front=front, mid=mid, back=back, phase2=phase2, phase3=phase3, phase4=phase4, phase24=phase24)
    if phases == ["all"]:
        run_all()
    else:
        for ph in phases:
            name, l = ph
            kb.fn[name](l)
    barrier()
    return nc, kb


def prep_inputs(inp, S):
    f = lambda a: np.ascontiguousarray(np.asarray(a, dtype=np.float32))
    w_uk = f(inp["w_uk"])
    wukT = np.ascontiguousarray(w_uk.reshape(DEPTH, R, 4, 2, 64).transpose(0, 3, 4, 2, 1).reshape(DEPTH, 128, 4, R))
    w_uv = f(inp["w_uv"])
    wuv = np.ascontiguousarray(w_uv.reshape(DEPTH, 2, 128, NH * 64).transpose(0, 2, 1, 3))
    rel = f(inp["rel_bias"])
    i = np.arange(128)[:, None]
    c = np.arange(GW)[None, :]
    bucket = t5_bucket_np((i - c + 384).astype(np.int32))
    gtab = np.ascontiguousarray(rel[bucket].transpose(0, 2, 1))
    vecD = np.ascontiguousarray(np.stack([f(inp["pre_mix_norm"]), f(inp["post_mix_norm"]), f(inp["pre_ffn_norm"]),
                                          f(inp["post_ffn_norm"]), f(inp["ssm_norm"])], axis=1))
    scw = np.ascontiguousarray(f(inp["short_conv_w"]).reshape(DEPTH, 3, 4, 128).transpose(0, 3, 2, 1))
    sscw = np.ascontiguousarray(f(inp["ssm_conv_w"]).reshape(DEPTH, 4, 12, 128).transpose(0, 3, 2, 1))
    sscb = np.ascontiguousarray(f(inp["ssm_conv_b"]).reshape(DEPTH, 12, 128).transpose(0, 2, 1))
    v16 = np.ascontiguousarray(np.stack([f(inp["ssm_dt_bias"]), f(inp["ssm_a_log"]), f(inp["ssm_d"])], axis=1))
    shared = dict(w_in=f(inp["w_in"]), w_out=f(inp["w_out"]), w_ffn_gate=f(inp["w_ffn_gate"]), w_ffn_up=f(inp["w_ffn_up"]),
                  w_ffn_down=f(inp["w_ffn_down"]), w_ple_proj=f(inp["w_ple_proj"]), w_ple_gate=f(inp["w_ple_gate"]),
                  wukT=wukT, wuv=wuv, gtab=gtab, vecD=vecD, kv_norm=f(inp["kv_norm"]), idx_k_norm_g=f(inp["idx_k_norm_g"]),
                  idx_k_norm_b=f(inp["idx_k_norm_b"]), scw=scw, sscw=sscw, sscb=sscb, v16=v16)
    x = f(inp["x"])
    p = f(inp["p"])
    B = x.shape[0]
    maps = []
    for b in range(B):
        m = dict(shared)
        m["x"] = np.ascontiguousarray(x[b, :S])
        m["p"] = np.ascontiguousarray(p[:, b, :S])
        maps.append(m)
    return maps


_CACHE = {}


def kernel(**inputs):
    S = 4096
    maps = prep_inputs(inputs, S)
    if "nc" not in _CACHE:
        _CACHE["nc"] = build_program(S)[0]
    res = run_bass_kernel_spmd(_CACHE["nc"], maps, core_ids=list(range(8)))
    return np.stack([np.asarray(r["y"], dtype=np.float32) for r in res.results], axis=0)
```

```python
import math
from contextlib import ExitStack
import numpy as np
import concourse.bass as bass
import concourse.mybir as mybir
from concourse.bass_utils import run_bass_kernel_spmd

F32 = mybir.dt.float32
BF16 = mybir.dt.bfloat16
AF = mybir.ActivationFunctionType
ALU = mybir.AluOpType
AX = mybir.AxisListType

D = 1024
DEPTH = 2
NH = 8
R = 256
IN_W = 5204
DFF = 2816
MIXW = 2048
EPS = 1e-6
NEG = -30000.0
O_Q, O_CKV, O_IQ, O_IK, O_IW, O_BG, O_CG, O_HB, O_Z, O_XBC, O_DT = 0, 512, 768, 1024, 1088, 1092, 1604, 2116, 2628, 3652, 5188
GW = 1582
GC = 1070
SEM_LIMIT = 30000


class Sem:
    def __init__(self, h):
        self.h = h
        self.val = 0


class Res:
    __slots__ = ("name", "w", "r", "dsem")

    def __init__(self, name):
        self.name = name
        self.w = {}
        self.r = {}
        self.dsem = None


class EngW:
    def __init__(self, name, eng, is_pe=False):
        self.name = name
        self.eng = eng
        self.is_pe = is_pe
        self.sem = None
        self.seen = {}


class KB:
    def __init__(self, nc):
        self.nc = nc
        self.nsem = 0
        self.pe = EngW("pe", nc.tensor, True)
        self.act = EngW("act", nc.scalar)
        self.dve = EngW("dve", nc.vector)
        self.pool = EngW("pool", nc.gpsimd)
        self.sp = EngW("sp", nc.sync)
        for e in (self.pe, self.act, self.dve, self.pool):
            e.sem = self.new_sem(e.name)
        self.stack = ExitStack()
        self.n_ops = 0
        self.free_dsems = []
        self.phase_dsems = [[]]

    def new_sem(self, name="s"):
        self.nsem += 1
        return Sem(self.nc.alloc_semaphore(name=f"{name}_{self.nsem}"))

    def res(self, name):
        return Res(name)

    def sb(self, name, shape, dt):
        self.n_sb = getattr(self, "n_sb", 0) + 1
        return self.stack.enter_context(self.nc.sbuf_tensor(f"{name}_{self.n_sb}", list(shape), dt))

    def _wait(self, e, reads, writes):
        raw = {}
        oth = {}
        for r in reads:
            for s, v in r.w.items():
                if raw.get(s, 0) < v:
                    raw[s] = v
        for w in writes:
            for s, v in w.w.items():
                if oth.get(s, 0) < v:
                    oth[s] = v
            for s, v in w.r.items():
                if oth.get(s, 0) < v:
                    oth[s] = v
        for s, v in oth.items():
            if s is e.sem:
                continue
            if raw.get(s, 0) < v:
                raw[s] = v
        for s, v in raw.items():
            if s is e.sem and e.is_pe:
                continue
            if e.seen.get(s, 0) >= v:
                continue
            e.eng.wait_ge(s.h, v)
            e.seen[s] = v

    def op(self, e, fn, reads=(), writes=(), inc=True):
        self._wait(e, reads, writes)
        ins = fn()
        self.n_ops += 1
        if inc:
            if e.sem.val >= SEM_LIMIT and not getattr(e, "pending", False):
                e.sem = self.new_sem(e.name)
            e.sem.val += 1
            ins.then_inc(e.sem.h, 1)
            tv = e.sem.val
            e.pending = False
        else:
            tv = e.sem.val + 1
            e.pending = True
        for w in writes:
            w.w[e.sem] = tv
        for r in reads:
            r.r[e.sem] = tv
        return ins

    def dma(self, out, in_, reads=(), writes=(), q=None, own=None):
        q = q or self.sp
        self._wait(q, reads, writes)
        ins = q.eng.dma_start(out=out, in_=in_)
        self.n_ops += 1
        if own.dsem is None or own.dsem.val >= SEM_LIMIT:
            if getattr(self, "free_dsems", None):
                own.dsem = self.free_dsems.pop()
                if own.dsem.val >= SEM_LIMIT:
                    own.dsem = self.new_sem("d" + own.name)
            else:
                own.dsem = self.new_sem("d" + own.name)
            self.phase_dsems[-1].append(own.dsem)
        s = own.dsem
        s.val += 16
        ins.then_inc(s.h, 16)
        for w in writes:
            w.w[s] = s.val
        for r in reads:
            r.r[s] = s.val
        return ins

    def drain(self, e, ress):
        self._wait(e, ress, ())


class Slots:
    def __init__(self, kb, name, shape, dt, n):
        self.t = [kb.sb(f"{name}{i}", shape, dt) for i in range(n)]
        self.r = [kb.res(f"{name}{i}") for i in range(n)]
        self.i = 0
        self.n = n

    def next(self):
        k = self.i % self.n
        self.i += 1
        return self.t[k], self.r[k]


def t5_bucket_np(rel):
    half, max_exact = 16, 8
    ret = np.where(rel > 0, half, 0)
    n = np.abs(rel)
    nf = np.maximum(n, 1).astype(np.float32)
    large = max_exact + (np.log(nf / max_exact) / math.log(1024 / max_exact) * (half - max_exact)).astype(np.int32)
    large = np.minimum(large, half - 1)
    return ret + np.where(n < max_exact, n, large)


def build_program(S, dbg=False, phases=None):
    NT = S // 128
    NB = S // 512
    nc = bass.Bass("TRN2", target_bir_lowering=False)
    kb = KB(nc)
    pe, act, dve, pool, sp = kb.pe, kb.act, kb.dve, kb.pool, kb.sp
    T, V, A, G = nc.tensor, nc.vector, nc.scalar, nc.gpsimd

    def din(name, shape, dt=F32):
        return nc.dram_tensor(name, list(shape), dt, kind="ExternalInput").ap()

    def dscr(name, shape, dt):
        return nc.dram_tensor(name, list(shape), dt, kind="ExternalOutput" if dbg else "Internal").ap()

    x_in = din("x", [S, D])
    p_in = din("p", [DEPTH, S, 256])
    w_in = din("w_in", [DEPTH, D, IN_W])
    w_out = din("w_out", [DEPTH, MIXW, D])
    w_g = din("w_ffn_gate", [DEPTH, D, DFF])
    w_u = din("w_ffn_up", [DEPTH, D, DFF])
    w_d = din("w_ffn_down", [DEPTH, DFF, D])
    w_pp = din("w_ple_proj", [DEPTH, 256, D])
    w_pg = din("w_ple_gate", [DEPTH, D, D])
    wukT = din("wukT", [DEPTH, 128, 4, R])
    wuv = din("wuv", [DEPTH, 128, 2, NH * 64])
    gtab = din("gtab", [128, NH, GW])
    vecD = din("vecD", [DEPTH, 5, D])
    kvn = din("kv_norm", [DEPTH, R])
    ikg = din("idx_k_norm_g", [DEPTH, 64])
    ikb = din("idx_k_norm_b", [DEPTH, 64])
    scw = din("scw", [DEPTH, 128, 4, 3])
    sscw = din("sscw", [DEPTH, 128, 12, 4])
    sscb = din("sscb", [DEPTH, 128, 12])
    v16 = din("v16", [DEPTH, 3, 16])
    y_out = nc.dram_tensor("y", [S, D], F32, kind="ExternalOutput").ap()

    xs_d = dscr("xs_d", [S, D], F32)
    xm_d = dscr("xm_d", [S, D], F32)
    qlat_d = dscr("qlat_d", [NH, R, S], BF16)
    iq_d = dscr("iq_d", [256, S], BF16)
    ik_d = dscr("ik_d", [64, S], BF16)
    iw_d = dscr("iw_d", [S, 4], F32)
    ckv_d = dscr("ckv_d", [S, R], BF16)
    ckvT_d = dscr("ckvT_d", [R, S], BF16)
    mixT_d = dscr("mixT_d", [MIXW, S], BF16)
    zs_d = dscr("zs_d", [S, D], F32)
    xsc_d = dscr("xsc_d", [S, D], BF16)
    bm_d = dscr("bm_d", [S, 256], BF16)
    bmT_d = dscr("bmT_d", [256, S], BF16)
    cmT_d = dscr("cmT_d", [256, S], BF16)
    dt_d = dscr("dt_d", [S, 32], F32)
    nmT_d = dscr("nmT_d", [S, S], BF16)
    gu_d = dscr("gu_d", [DFF, S], BF16)
    R_ = {n: kb.res(n) for n in ["xs", "qlat", "iq", "ik", "iw", "ckv", "ckvT", "mixT", "zs", "xsc", "bm", "bmT",
                                 "cmT", "dt", "nmT", "gu", "y", "xm"]}

    ps = [kb.stack.enter_context(nc.psum_tensor(f"ps{i}", [128, 512], F32)) for i in range(8)]
    psr = [kb.res(f"ps{i}") for i in range(8)]

    ident_f = kb.sb("ident_f", [128, 128], F32)
    ident = kb.sb("ident", [128, 128], BF16)
    ones_f = kb.sb("ones_f", [128, 128], F32)
    utri_f = kb.sb("utri_f", [128, 128], F32)
    tri_f = kb.sb("tri_f", [128, 128], F32)
    cres = kb.res("consts")
    kb.op(pool, lambda: G.memset(ident_f[:], 0.0), writes=[cres])
    kb.op(pool, lambda: G.affine_select(out=ident_f[:], in_=ident_f[:], pattern=[[-1, 128]], compare_op=ALU.not_equal,
                                        fill=1.0, base=0, channel_multiplier=1), reads=[cres], writes=[cres])
    kb.op(pool, lambda: G.tensor_copy(out=ident[:], in_=ident_f[:]), reads=[cres], writes=[cres])
    kb.op(pool, lambda: G.memset(ones_f[:], 1.0), writes=[cres])
    kb.op(pool, lambda: G.affine_select(out=utri_f[:], in_=ones_f[:], pattern=[[1, 128]], compare_op=ALU.is_ge,
                                        fill=0.0, base=0, channel_multiplier=-1), reads=[cres], writes=[cres])
    kb.op(pool, lambda: G.tensor_copy(out=tri_f[:], in_=utri_f[:]), reads=[cres], writes=[cres])

    eps_t = kb.sb("eps_t", [128, 1], F32)
    negb_t = kb.sb("negb_t", [128, 1], F32)
    kb.op(pool, lambda: G.memset(negb_t[:], NEG), writes=[cres])
    kb.op(pool, lambda: G.memset(eps_t[:], EPS), writes=[cres])
    HT = {}

    def bcast_load(dst, src_vec, n, rs):
        kb.dma(out=dst, in_=src_vec.partition_broadcast(128), writes=[rs], own=rs)

    def rms_rstd(dst, ssq, n, eng=None):
        V.tensor_scalar(out=dst, in0=ssq, scalar1=1.0 / n, scalar2=EPS, op0=ALU.mult, op1=ALU.add)

    all_sems = []
    _orig_new_sem = kb.new_sem

    def _new_sem(name="s"):
        s = _orig_new_sem(name)
        all_sems.append(s)
        return s
    kb.new_sem = _new_sem
    for e in (pe, act, dve, pool):
        all_sems.append(e.sem)

    def barrier():
        for e in (pe, act, dve, pool, sp):
            for s in all_sems:
                if s is e.sem or s.val == 0:
                    continue
                if e.seen.get(s, 0) >= s.val:
                    continue
                e.eng.wait_ge(s.h, s.val)
                e.seen[s] = s.val

    class Phase:
        def __enter__(self):
            self.es = ExitStack()
            self.es.__enter__()
            self.old = kb.stack
            kb.stack = self.es
            kb.phase_dsems.append([])
            return self

        def __exit__(self, *a):
            barrier()
            kb.free_dsems.extend(kb.phase_dsems.pop())
            kb.stack = self.old
            self.es.__exit__(None, None, None)
            return False

    def mm(out, lhsT, rhs, start, stop, reads, writes, inc=True):
        return kb.op(pe, lambda: T.matmul(out, lhsT=lhsT, rhs=rhs, start=start, stop=stop), reads=reads, writes=writes, inc=inc)

    def tp(out, in_, reads, writes, inc=True, idn=None):
        idn = ident if idn is None else idn
        k = in_.shape[0]
        return kb.op(pe, lambda: T.transpose(out, in_, idn[0:k, 0:k]), reads=list(reads) + [cres], writes=writes, inc=inc)

    def vop(fn, reads, writes):
        return kb.op(dve, fn, reads=reads, writes=writes)

    def aop(fn, reads, writes):
        return kb.op(act, fn, reads=reads, writes=writes)

    def gop(fn, reads, writes):
        return kb.op(pool, fn, reads=reads, writes=writes)

    def rstd(st, st_r, src, dst, n):
        aop(lambda: A.activation(out=st[:, 15:16], in_=st[:, src:src + 1], func=AF.Ln, scale=1.0 / n, bias=eps_t[:, 0:1]), [st_r, cres], [st_r])
        aop(lambda: A.activation(out=st[:, dst:dst + 1], in_=st[:, 15:16], func=AF.Exp, scale=-0.5), [st_r], [st_r])

    def norm_to_hT(xt, xr, g_bc, g_r, tt, tmp):
        junk, junk_r, stl, hb, hb_r = tmp
        st, st_r = stl.next()
        aop(lambda: A.activation(out=junk[:], in_=xt, func=AF.Square, accum_out=st[:, 0:1]), [xr], [junk_r, st_r])
        rstd(st, st_r, 0, 2, D)
        vop(lambda: V.scalar_tensor_tensor(out=hb[:], in0=xt, scalar=st[:, 2:3], in1=g_bc[:], op0=ALU.mult, op1=ALU.mult),
            [xr, st_r, g_r], [hb_r])
        for half in range(2):
            pb = 6 + half
            pv = ps[pb][:].bitcast(BF16)
            for j in range(4):
                dc = half * 4 + j
                tp(pv[:, j * 128:(j + 1) * 128], hb[:, dc * 128:(dc + 1) * 128], [hb_r], [psr[pb]], inc=(j == 3))
            src = pv[:, 0:512].rearrange("p (j t) -> p j t", j=4)
            dst = HT['t'][:, half * 4:(half + 1) * 4, tt * 128:(tt + 1) * 128]
            if half == 0:
                aop(lambda src=src, dst=dst: A.copy(out=dst, in_=src), [psr[pb]], [HT['r']])
            else:
                vop(lambda src=src, dst=dst: V.tensor_copy(out=dst, in_=src), [psr[pb]], [HT['r']])

    class HTScope(Phase):
        def __enter__(self):
            Phase.__enter__(self)
            HT['t'] = kb.sb("hT", [128, 8, S], BF16)
            HT['r'] = kb.res("hT")
            return self

    def phase0(l, src=None):
        src = x_in if src is None else src
        with Phase():
            g_bc = kb.sb("p0_g", [128, D], F32)
            g_r = kb.res("p0_g")
            bcast_load(g_bc[:], vecD[l, 0, :], D, g_r)
            xsl = Slots(kb, "p0_x", [128, D], F32, 2)
            junk = kb.sb("p0_junk", [128, D], F32)
            stl = Slots(kb, "p0_st", [128, 16], F32, 2)
            hb = kb.sb("p0_hb", [128, D], BF16)
            tmp = (junk, kb.res("p0_junk"), stl, hb, kb.res("p0_hb"))
            for tt in range(NT):
                xt, xr = xsl.next()
                kb.dma(out=xt[:], in_=src[tt * 128:(tt + 1) * 128, :], reads=[R_['xs']], writes=[xr], own=xr)
                norm_to_hT(xt[:], xr, g_bc, g_r, tt, tmp)

    class WStream:
        def __init__(self, name, KC, maxc, nbuf=2, nstg=2):
            self.KC = KC
            self.stg = Slots(kb, name + "_stg", [128, KC, 256], F32, nstg)
            self.wb = Slots(kb, name + "_wb", [128, KC, maxc], BF16, nbuf)

        def load(self, wap, c0, ncols, r0=0, kcn=None):
            kcn = kcn or self.KC
            wt, wr = self.wb.next()
            for p0 in range(0, ncols, 256):
                pn = min(256, ncols - p0)
                st, sr = self.stg.next()
                src = wap[r0:r0 + kcn * 128, :].rearrange("(kc p) c -> p kc c", p=128)[:, :, c0 + p0:c0 + p0 + pn]
                kb.dma(out=st[:, 0:kcn, 0:pn], in_=src, writes=[sr], own=sr)
                gop(lambda: G.tensor_copy(out=wt[:, 0:kcn, p0:p0 + pn], in_=st[:, 0:kcn, 0:pn]), [sr], [wr])
            return wt, wr

    class RR:
        def __init__(self, items):
            self.items = items
            self.i = 0

        def next(self):
            k = self.items[self.i % len(self.items)]
            self.i += 1
            return k

    def phase1(l):
        with Phase():
            hT, hT_r = HT['t'], HT['r']
            ws = WStream("w1", 8, 512, nbuf=3)
            c_r = kb.res("p1c")
            wuk_f = kb.sb("wuk_f", [128, 4, R], F32)
            wuk_b = kb.sb("wuk_b", [128, 4, R], BF16)
            kb.dma(out=wuk_f[:], in_=wukT[l], writes=[c_r], own=c_r)
            gop(lambda: G.tensor_copy(out=wuk_b[:], in_=wuk_f[:]), [c_r], [c_r])
            scw_t = kb.sb("scw_t", [128, 4, 3], F32)
            kb.dma(out=scw_t[:], in_=scw[l], writes=[c_r], own=c_r)
            sscw_t = kb.sb("sscw_t", [128, 12, 4], F32)
            kb.dma(out=sscw_t[:], in_=sscw[l], writes=[c_r], own=c_r)
            sscb_t = kb.sb("sscb_t", [128, 12], F32)
            kb.dma(out=sscb_t[:], in_=sscb[l], writes=[c_r], own=c_r)
            kvn_bc = kb.sb("kvn_bc", [128, R], F32)
            bcast_load(kvn_bc[:], kvn[l, :], R, c_r)
            ikg_bc = kb.sb("ikg_bc", [128, 64], F32)
            bcast_load(ikg_bc[:], ikg[l, :], 64, c_r)
            ikb_bc = kb.sb("ikb_bc", [128, 64], F32)
            bcast_load(ikb_bc[:], ikb[l, :], 64, c_r)
            v16_bc = kb.sb("v16_bc", [128, 3, 16], F32)
            kb.dma(out=v16_bc[:], in_=v16[l].partition_broadcast(128), writes=[c_r], own=c_r)
            A_bc = kb.sb("A_bc", [128, 16], F32)
            aop(lambda: A.activation(out=A_bc[:], in_=v16_bc[:, 1, :], func=AF.Exp), [c_r], [c_r])
            vop(lambda: V.tensor_scalar(out=A_bc[:], in0=A_bc[:], scalar1=-1.0, scalar2=None, op0=ALU.mult), [c_r], [c_r])

            bankA = RR([0, 1])
            bankB = RR([2, 3])
            o512 = Slots(kb, "p1_o512", [128, 512], BF16, 3)
            o512b = Slots(kb, "p1_o512b", [128, 512], BF16, 3)

            def fm_block(wt, wr, c, tb, ncols=128):
                b = bankA.next()
                for kc in range(8):
                    mm(ps[b][0:ncols, :], wt[:, kc, c * 128:c * 128 + ncols], hT[:, kc, tb * 512:(tb + 1) * 512],
                       kc == 0, kc == 7, [wr, hT_r], [psr[b]], inc=(kc == 7))
                return b

            wt, wr = ws.load(w_in[l], O_Q, 512)
            for c in range(4):
                for tb in range(NB):
                    b = fm_block(wt, wr, c, tb)
                    qs, qr = o512.next()
                    aop(lambda b=b, qs=qs: A.mul(out=qs[:], in_=ps[b][:], mul=0.125), [psr[b]], [qr])
                    for hh in range(2):
                        for rc in range(2):
                            b2 = bankB.next()
                            mm(ps[b2][:], wuk_b[hh * 64:(hh + 1) * 64, c, rc * 128:(rc + 1) * 128],
                               qs[hh * 64:(hh + 1) * 64, :], True, True, [c_r, qr], [psr[b2]])
                            ql, qlr = o512b.next()
                            vop(lambda b2=b2, ql=ql: V.tensor_copy(out=ql[:], in_=ps[b2][:]), [psr[b2]], [qlr])
                            kb.dma(out=qlat_d[2 * c + hh, rc * 128:(rc + 1) * 128, tb * 512:(tb + 1) * 512], in_=ql[:],
                                   reads=[qlr], writes=[R_["qlat"]], own=qlr)
            wt, wr = ws.load(w_in[l], O_IQ, 256)
            for c in range(2):
                for tb in range(NB):
                    b = fm_block(wt, wr, c, tb)
                    qs, qr = o512.next()
                    aop(lambda b=b, qs=qs: A.mul(out=qs[:], in_=ps[b][:], mul=0.125), [psr[b]], [qr])
                    kb.dma(out=iq_d[c * 128:(c + 1) * 128, tb * 512:(tb + 1) * 512], in_=qs[:], reads=[qr],
                           writes=[R_["iq"]], own=qr)
            cg = kb.sb("p1_cg", [128, S], F32)
            cg_r = kb.res("p1_cg")
            U = kb.sb("p1_U", [128, S + 4], F32)
            U_r = kb.res("p1_U")
            vop(lambda: V.memset(U[:, 0:4], 0.0), [], [U_r])
            wbg, wbg_r = ws.load(w_in[l], O_BG, 512)
            wcg, wcg_r = ws.load(w_in[l], O_CG, 512)
            whb, whb_r = ws.load(w_in[l], O_HB, 512)
            for c in range(4):
                for tb in range(NB):
                    b = fm_block(wcg, wcg_r, c, tb)
                    aop(lambda b=b, tb=tb: A.copy(out=cg[:, tb * 512:(tb + 1) * 512], in_=ps[b][:]), [psr[b]], [cg_r])
                for tb in range(NB):
                    b = fm_block(whb, whb_r, c, tb)
                    vop(lambda b=b, tb=tb: V.tensor_tensor(out=U[:, 4 + tb * 512:4 + (tb + 1) * 512], in0=ps[b][:],
                                                           in1=cg[:, tb * 512:(tb + 1) * 512], op=ALU.mult),
                        [psr[b], cg_r], [U_r])
                vop(lambda c=c: V.tensor_scalar(out=cg[:, :], in0=U[:, 2:S + 2], scalar1=scw_t[:, c, 0:1], scalar2=None,
                                                op0=ALU.mult), [U_r, c_r], [cg_r])
                for j in (1, 2):
                    vop(lambda c=c, j=j: V.scalar_tensor_tensor(out=cg[:, :], in0=U[:, 2 + j:S + 2 + j], scalar=scw_t[:, c, j:j + 1],
                                                                in1=cg[:, :], op0=ALU.mult, op1=ALU.add), [U_r, c_r, cg_r], [cg_r])
                for tb in range(NB):
                    b = fm_block(wbg, wbg_r, c, tb)
                    ob, obr = o512.next()
                    vop(lambda b=b, tb=tb, ob=ob: V.tensor_tensor(out=ob[:], in0=ps[b][:], in1=cg[:, tb * 512:(tb + 1) * 512],
                                                                  op=ALU.mult), [psr[b], cg_r], [obr])
                    kb.dma(out=mixT_d[512 + c * 128:512 + (c + 1) * 128, tb * 512:(tb + 1) * 512], in_=ob[:], reads=[obr],
                           writes=[R_["mixT"]], own=obr)
            sx = kb.sb("p1_sx", [128, S], BF16)
            sx_r = kb.res("p1_sx")
            U2 = kb.sb("p1_U2", [128, S + 4], F32)
            U2_r = kb.res("p1_U2")
            vop(lambda: V.memset(U2[:, 0:4], 0.0), [], [U2_r])
            Ua = [(U, U_r), (U2, U2_r)]
            tstg = Slots(kb, "p1_tstg", [128, 4, 128], BF16, 2)
            for blk in range(3):
                wt, wr = ws.load(w_in[l], O_XBC + blk * 512, 512)
                for c in range(4):
                    cc = blk * 4 + c
                    U, U_r = Ua[cc % 2]
                    for tb in range(NB):
                        b = fm_block(wt, wr, c, tb)
                        aop(lambda b=b, tb=tb: A.copy(out=U[:, 4 + tb * 512:4 + (tb + 1) * 512], in_=ps[b][:]), [psr[b]], [U_r])
                    vop(lambda cc=cc: V.tensor_scalar(out=cg[:, :], in0=U[:, 1:S + 1], scalar1=sscw_t[:, cc, 0:1],
                                                      scalar2=sscb_t[:, cc:cc + 1], op0=ALU.mult, op1=ALU.add), [U_r, c_r], [cg_r])
                    for j in (1, 2, 3):
                        vop(lambda cc=cc, j=j: V.scalar_tensor_tensor(out=cg[:, :], in0=U[:, 1 + j:S + 1 + j],
                                                                      scalar=sscw_t[:, cc, j:j + 1], in1=cg[:, :],
                                                                      op0=ALU.mult, op1=ALU.add), [U_r, c_r, cg_r], [cg_r])
                    aop(lambda: A.activation(out=sx[:, :], in_=cg[:, :], func=AF.Silu), [cg_r], [sx_r])
                    if cc >= 8:
                        g = (cc - 8) % 2
                        dst = bmT_d if cc < 10 else cmT_d
                        kb.dma(out=dst[g * 128:(g + 1) * 128, :], in_=sx[:, :], reads=[sx_r],
                               writes=[R_["bmT" if cc < 10 else "cmT"]], own=sx_r)
                    if cc < 10:
                        for t4 in range(NT // 4):
                            pb = bankB.next()
                            pv = ps[pb][:].bitcast(BF16)
                            for j in range(4):
                                tt = t4 * 4 + j
                                tp(pv[:, j * 128:(j + 1) * 128], sx[:, tt * 128:(tt + 1) * 128], [sx_r], [psr[pb]], inc=(j == 3))
                            tsg, tsr = tstg.next()
                            vop(lambda pv=pv, tsg=tsg: V.tensor_copy(out=tsg[:], in_=pv[:, 0:512].rearrange("p (j c) -> p j c", j=4)),
                                [psr[pb]], [tsr])
                            if cc < 8:
                                dst = xsc_d.rearrange("(tt p) c -> p tt c", p=128)[:, t4 * 4:(t4 + 1) * 4, cc * 128:(cc + 1) * 128]
                                rn = "xsc"
                            else:
                                dst = bm_d.rearrange("(tt p) c -> p tt c", p=128)[:, t4 * 4:(t4 + 1) * 4, (cc - 8) * 128:(cc - 7) * 128]
                                rn = "bm"
                            kb.dma(out=dst, in_=tsg[:], reads=[tsr], writes=[R_[rn]], own=tsr)
            zsl = Slots(kb, "p1_z", [128, 512], F32, 3)
            for zb in range(2):
                wt, wr = ws.load(w_in[l], O_Z + zb * 512, 512)
                for tt in range(NT):
                    b = bankA.next()
                    for kc in range(8):
                        mm(ps[b][:], hT[:, kc, tt * 128:(tt + 1) * 128], wt[:, kc, 0:512], kc == 0, kc == 7, [wr, hT_r], [psr[b]],
                           inc=(kc == 7))
                    zt, zr = zsl.next()
                    aop(lambda b=b, zt=zt: A.activation(out=zt[:], in_=ps[b][:], func=AF.Silu), [psr[b]], [zr])
                    kb.dma(out=zs_d[tt * 128:(tt + 1) * 128, zb * 512:(zb + 1) * 512], in_=zt[:], reads=[zr], writes=[R_["zs"]], own=zr)
            wt, wr = ws.wb.next()
            wv = w_in[l].rearrange("(kc p) c -> p kc c", p=128)
            st_, sr_ = ws.stg.next()
            kb.dma(out=st_[:, :, 0:256], in_=wv[:, :, O_CKV:O_CKV + 256], writes=[sr_], own=sr_)
            gop(lambda: G.tensor_copy(out=wt[:, :, 0:256], in_=st_[:, :, 0:256]), [sr_], [wr])
            st_, sr_ = ws.stg.next()
            kb.dma(out=st_[:, :, 0:68], in_=wv[:, :, O_IK:O_IK + 68], writes=[sr_], own=sr_)
            kb.dma(out=st_[:, :, 68:84], in_=wv[:, :, O_DT:O_DT + 16], writes=[sr_], own=sr_)
            gop(lambda: G.tensor_copy(out=wt[:, :, 256:340], in_=st_[:, :, 0:84]), [sr_], [wr])
            stl = Slots(kb, "p1_st", [128, 16], F32, 2)
            junk = kb.sb("p1_junk", [128, 256], F32)
            junk_r = kb.res("p1_junk")
            cnl = Slots(kb, "p1_cn", [128, 256], BF16, 2)
            ctl = Slots(kb, "p1_ct", [128, 2, 128], BF16, 2)
            ikl = Slots(kb, "p1_ik", [128, 64], F32, 2)
            iknl = Slots(kb, "p1_ikn", [128, 64], BF16, 2)
            iktl = Slots(kb, "p1_ikt", [64, 128], BF16, 2)
            iwl = Slots(kb, "p1_iw", [128, 4], F32, 2)
            dtl = Slots(kb, "p1_dt", [128, 32], F32, 2)
            for tt in range(NT):
                b = bankA.next()
                for kc in range(8):
                    mm(ps[b][:, 0:340], hT[:, kc, tt * 128:(tt + 1) * 128], wt[:, kc, 0:340], kc == 0, kc == 7, [wr, hT_r], [psr[b]],
                       inc=(kc == 7))
                P = ps[b]
                st, str_ = stl.next()
                aop(lambda P=P, st=st: A.activation(out=junk[:, 0:256], in_=P[:, 0:256], func=AF.Square, accum_out=st[:, 0:1]),
                    [psr[b]], [junk_r, str_])
                rstd(st, str_, 0, 2, R)
                cn, cnr = cnl.next()
                vop(lambda P=P, st=st, cn=cn: V.scalar_tensor_tensor(out=cn[:], in0=P[:, 0:256], scalar=st[:, 2:3], in1=kvn_bc[:],
                                                                    op0=ALU.mult, op1=ALU.mult), [psr[b], str_, c_r], [cnr])
                kb.dma(out=ckv_d[tt * 128:(tt + 1) * 128, :], in_=cn[:], reads=[cnr], writes=[R_["ckv"]], own=cnr)
                pb = bankB.next()
                pv = ps[pb][:].bitcast(BF16)
                for rc in range(2):
                    tp(pv[:, rc * 128:(rc + 1) * 128], cn[:, rc * 128:(rc + 1) * 128], [cnr], [psr[pb]], inc=(rc == 1))
                ct, ctr = ctl.next()
                aop(lambda pv=pv, ct=ct: A.copy(out=ct[:], in_=pv[:, 0:256].rearrange("p (j c) -> p j c", j=2)), [psr[pb]], [ctr])
                kb.dma(out=ckvT_d.rearrange("(rc p) s -> p rc s", p=128)[:, :, tt * 128:(tt + 1) * 128], in_=ct[:], reads=[ctr],
                       writes=[R_["ckvT"]], own=ctr)
                vop(lambda P=P, st=st: V.tensor_reduce(out=st[:, 4:5], in_=P[:, 256:320], axis=AX.X, op=ALU.add), [psr[b]], [str_])
                aop(lambda P=P, st=st: A.activation(out=junk[:, 0:64], in_=P[:, 256:320], func=AF.Square, accum_out=st[:, 5:6]),
                    [psr[b]], [junk_r, str_])
                vop(lambda st=st: V.tensor_scalar(out=st[:, 6:7], in0=st[:, 4:5], scalar1=1.0 / 64, scalar2=None, op0=ALU.mult), [str_], [str_])
                vop(lambda st=st: V.tensor_tensor(out=st[:, 7:8], in0=st[:, 6:7], in1=st[:, 6:7], op=ALU.mult), [str_], [str_])
                vop(lambda st=st: V.scalar_tensor_tensor(out=st[:, 8:9], in0=st[:, 5:6], scalar=1.0 / 64, in1=st[:, 7:8],
                                                         op0=ALU.mult, op1=ALU.subtract), [str_], [str_])
                rstd(st, str_, 8, 9, 1)
                ik, ikr = ikl.next()
                vop(lambda P=P, st=st, ik=ik: V.tensor_scalar(out=ik[:], in0=P[:, 256:320], scalar1=st[:, 6:7], scalar2=st[:, 9:10],
                                                              op0=ALU.subtract, op1=ALU.mult), [psr[b], str_], [ikr])
                vop(lambda ik=ik: V.tensor_tensor(out=ik[:], in0=ik[:], in1=ikg_bc[:], op=ALU.mult), [ikr, c_r], [ikr])
                ikn, iknr = iknl.next()
                vop(lambda ik=ik, ikn=ikn: V.tensor_tensor(out=ikn[:], in0=ik[:], in1=ikb_bc[:], op=ALU.add), [ikr, c_r], [iknr])
                pb = bankB.next()
                pv = ps[pb][:].bitcast(BF16)
                tp(pv[0:64, 0:128], ikn[:, 0:64], [iknr], [psr[pb]])
                ikt, iktr = iktl.next()
                aop(lambda pv=pv, ikt=ikt: A.copy(out=ikt[:], in_=pv[0:64, 0:128]), [psr[pb]], [iktr])
                kb.dma(out=ik_d[:, tt * 128:(tt + 1) * 128], in_=ikt[:], reads=[iktr], writes=[R_["ik"]], own=iktr)
                iwt, iwr = iwl.next()
                aop(lambda P=P, iwt=iwt: A.mul(out=iwt[:], in_=P[:, 320:324], mul=0.5), [psr[b]], [iwr])
                kb.dma(out=iw_d[tt * 128:(tt + 1) * 128, :], in_=iwt[:], reads=[iwr], writes=[R_["iw"]], own=iwr)
                dtt, dtr = dtl.next()
                vop(lambda P=P, dtt=dtt: V.tensor_tensor(out=dtt[:, 16:32], in0=P[:, 324:340], in1=v16_bc[:, 0, :], op=ALU.add),
                    [psr[b], c_r], [dtr])
                aop(lambda dtt=dtt: A.activation(out=dtt[:, 16:32], in_=dtt[:, 16:32], func=AF.Exp), [dtr], [dtr])
                aop(lambda dtt=dtt: A.activation(out=dtt[:, 0:16], in_=dtt[:, 16:32], func=AF.Ln, bias=1.0, scale=1.0), [dtr], [dtr])
                vop(lambda dtt=dtt: V.tensor_tensor(out=dtt[:, 16:32], in0=dtt[:, 0:16], in1=A_bc[:], op=ALU.mult), [dtr, c_r], [dtr])
                kb.dma(out=dt_d[tt * 128:(tt + 1) * 128, :], in_=dtt[:], reads=[dtr], writes=[R_["dt"]], own=dtr)

    NIT = 16

    def gen2(l):
        c_r = kb.res("p2c")
        iqT = kb.sb("p2_iqT", [128, 2, S], BF16)
        ikT = kb.sb("p2_ikT", [128, S], BF16)
        iwa = kb.sb("p2_iw", [128, NT, 4], F32)
        kb.dma(out=iqT[:], in_=iq_d.rearrange("(c p) s -> p c s", p=128), reads=[R_["iq"]], writes=[c_r], own=c_r)
        kb.dma(out=ikT[0:64, :], in_=ik_d, reads=[R_["ik"]], writes=[c_r], own=c_r)
        kb.dma(out=ikT[64:128, :], in_=ik_d, reads=[R_["ik"]], writes=[c_r], own=c_r)
        for t0_ in range(0, NT, 8):
            t1_ = min(NT, t0_ + 8)
            kb.dma(out=iwa[:, t0_:t1_, :], in_=iw_d.rearrange("(t p) h -> p t h", p=128)[:, t0_:t1_, :], reads=[R_["iw"]], writes=[c_r], own=c_r)
        pw2 = kb.sb("p2_pw2", [128, NIT], F32)
        for k in range(NIT):
            gop(lambda: G.memset(pw2[:, k:k + 1], 2.0 ** (-(k + 1))), [], [c_r])
        SCl = Slots(kb, "p2_SC", [128, S], F32, 2)
        mkl = Slots(kb, "p2_mk", [128, S], BF16, 3)
        Rl = Slots(kb, "p2_R", [128, 512], F32, 4)
        bl = Slots(kb, "p2_b", [128, 8 + 2 * NIT], F32, 4)
        nml = Slots(kb, "p2_nm", [128, 4, 128], BF16, 3)
        bankA = RR([0, 1])
        bankT = RR([2])
        yield

        def scores(qt, out):
            N = (qt + 1) * 128
            SC, SC_r = SCl.next()
            for sb in range((N + 511) // 512):
                cols = min(512, N - sb * 512)
                cs = slice(sb * 512, sb * 512 + cols)
                for h in range(4):
                    c, hh = divmod(h, 2)
                    pr_ = slice(hh * 64, (hh + 1) * 64)
                    b = bankA.next()
                    mm(ps[b][:, 0:cols], iqT[pr_, c, qt * 128:(qt + 1) * 128], ikT[pr_, cs], True, True, [c_r], [psr[b]])
                    Rt, Rr = Rl.next()
                    aop(lambda: A.activation(out=Rt[:, 0:cols], in_=ps[b][:, 0:cols], func=AF.Relu), [psr[b]], [Rr])
                    if h == 0:
                        vop(lambda: V.tensor_scalar(out=SC[:, cs], in0=Rt[:, 0:cols], scalar1=iwa[:, qt, 0:1], scalar2=None, op0=ALU.mult),
                            [Rr, c_r], [SC_r])
                    else:
                        vop(lambda: V.scalar_tensor_tensor(out=SC[:, cs], in0=Rt[:, 0:cols], scalar=iwa[:, qt, h:h + 1], in1=SC[:, cs],
                                                           op0=ALU.mult, op1=ALU.add), [Rr, c_r, SC_r], [SC_r])
                yield
            bt, b_r = bl.next()
            mk, mk_r = mkl.next()
            vop(lambda: V.tensor_reduce(out=bt[:, 0:1], in_=SC[:, 0:N], axis=AX.X, op=ALU.max), [SC_r], [b_r])
            vop(lambda: V.tensor_reduce(out=bt[:, 1:2], in_=SC[:, 0:N], axis=AX.X, op=ALU.min), [SC_r], [b_r])
            vop(lambda: V.memset(SC[0:64, N - 64:N], -1.0e30), [], [SC_r])
            vop(lambda: V.tensor_scalar(out=bt[:, 2:3], in0=bt[:, 1:2], scalar1=-0.01, scalar2=None, op0=ALU.add), [b_r], [b_r])
            vop(lambda: V.scalar_tensor_tensor(out=bt[:, 3:4], in0=bt[:, 0:1], scalar=0.02, in1=bt[:, 1:2], op0=ALU.add, op1=ALU.subtract),
                [b_r], [b_r])
            vop(lambda: V.tensor_scalar(out=bt[:, 8:8 + NIT], in0=pw2[:], scalar1=bt[:, 3:4], scalar2=None, op0=ALU.mult), [b_r, c_r], [b_r])
            vop(lambda: V.memset(bt[:, 8 + NIT:8 + 2 * NIT], 0.0), [], [b_r])
            out.update(dict(qt=qt, N=N, SC=SC, SC_r=SC_r, bt=bt, b_r=b_r, mk=mk, mk_r=mk_r))
            yield

        def first_mid(st, on_act):
            bt, b_r = st["bt"], st["b_r"]
            if on_act:
                vop(lambda: V.scalar_tensor_tensor(out=bt[:, 4:5], in0=bt[:, 2:3], scalar=-1.0, in1=bt[:, 8:9], op0=ALU.mult, op1=ALU.subtract),
                    [b_r], [b_r])
            else:
                vop(lambda: V.tensor_tensor(out=bt[:, 4:5], in0=bt[:, 2:3], in1=bt[:, 8:9], op=ALU.add), [b_r], [b_r])

        def iteration(st, k, on_act):
            N, SC, SC_r, bt, b_r = st["N"], st["SC"], st["SC_r"], st["bt"], st["b_r"]
            jt, jr = st["mk"], st["mk_r"]
            ck = 8 + NIT + k
            if on_act:
                aop(lambda: A.activation(out=jt[:, 0:N], in_=SC[:, 0:N], func=AF.Sign, bias=bt[:, 4:5], scale=1.0,
                                         accum_out=bt[:, ck:ck + 1]), [SC_r, b_r], [jr, b_r])
                thr = 511.0 - N
            else:
                vop(lambda: V.tensor_scalar(out=jt[:, 0:N], in0=SC[:, 0:N], scalar1=bt[:, 4:5], scalar2=0.0, op0=ALU.is_ge, op1=ALU.add,
                                            accum_out=bt[:, ck:ck + 1]), [SC_r, b_r], [jr, b_r])
                thr = 255.5
            vop(lambda: V.scalar_tensor_tensor(out=bt[:, 5:6], in0=bt[:, ck:ck + 1], scalar=thr, in1=bt[:, 8 + k:9 + k],
                                               op0=ALU.is_ge, op1=ALU.mult), [b_r], [b_r])
            vop(lambda: V.tensor_tensor(out=bt[:, 2:3], in0=bt[:, 2:3], in1=bt[:, 5:6], op=ALU.add), [b_r], [b_r])
            if k + 1 < NIT:
                if on_act:
                    vop(lambda: V.scalar_tensor_tensor(out=bt[:, 4:5], in0=bt[:, 2:3], scalar=-1.0, in1=bt[:, 9 + k:10 + k],
                                                       op0=ALU.mult, op1=ALU.subtract), [b_r], [b_r])
                else:
                    vop(lambda: V.tensor_tensor(out=bt[:, 4:5], in0=bt[:, 2:3], in1=bt[:, 9 + k:10 + k], op=ALU.add), [b_r], [b_r])

        def finalize(st):
            qt, N, SC, SC_r, bt, b_r, mk, mk_r = st["qt"], st["N"], st["SC"], st["SC_r"], st["bt"], st["b_r"], st["mk"], st["mk_r"]
            vop(lambda: V.tensor_scalar(out=mk[:, 0:N], in0=SC[:, 0:N], scalar1=bt[:, 2:3], scalar2=None, op0=ALU.is_ge), [SC_r, b_r], [mk_r])
            for k0 in range(0, qt + 1, 4):
                n = min(4, qt + 1 - k0)
                pb = bankT.next()
                pv = ps[pb][:].bitcast(BF16)
                for j in range(n):
                    tp(pv[:, j * 128:(j + 1) * 128], mk[:, (k0 + j) * 128:(k0 + j + 1) * 128], [mk_r], [psr[pb]], inc=(j == n - 1))
                nm, nm_r = nml.next()
                aop(lambda: A.copy(out=nm[:, 0:n, :], in_=pv[:, 0:n * 128].rearrange("p (j t) -> p j t", j=n)), [psr[pb]], [nm_r])
                kb.dma(out=nmT_d.rearrange("(kt p) t -> p kt t", p=128)[:, k0:k0 + n, qt * 128:(qt + 1) * 128], in_=nm[:, 0:n, :],
                       reads=[nm_r], writes=[R_["nmT"]], own=nm_r)
                yield

        for q0 in range(0, NT, 2):
            sa, sb_ = {}, {}
            yield from scores(q0, sa)
            yield from scores(q0 + 1, sb_)
            first_mid(sa, False)
            first_mid(sb_, True)
            for k in range(NIT):
                iteration(sa, k, False)
                iteration(sb_, k, True)
                yield
            yield from finalize(sa)
            yield from finalize(sb_)

    def run_interleaved(gens, weights=None):
        gens = list(gens)
        weights = list(weights or [1] * len(gens))
        acc = [0.0] * len(gens)
        alive = [True] * len(gens)
        while any(alive):
            for i, g in enumerate(gens):
                if not alive[i]:
                    continue
                acc[i] += weights[i]
                while acc[i] >= 1.0 and alive[i]:
                    acc[i] -= 1.0
                    try:
                        next(g)
                    except StopIteration:
                        alive[i] = False

    def phase2(l):
        with Phase():
            run_interleaved([gen2(l)])

    def phase24(l):
        with Phase():
            run_interleaved([gen2(l), gen4(l)], [1.0, 5.5])

    def phase3(l):
        with Phase():
            c_r = kb.res("p3c")
            ckvT = kb.sb("p3_ckvT", [128, 2, S], BF16)
            kb.dma(out=ckvT[:], in_=ckvT_d.rearrange("(rc p) s -> p rc s", p=128), reads=[R_["ckvT"]], writes=[c_r], own=c_r)
            ckva = kb.sb("p3_ckva", [128, NT, 258], BF16)
            gop(lambda: G.memset(ckva[:, :, 256:258], 1.0), [], [c_r])
            for t0_ in range(0, NT, 8):
                t1_ = min(NT, t0_ + 8)
                kb.dma(out=ckva[:, t0_:t1_, 0:256], in_=ckv_d.rearrange("(kt p) r -> p kt r", p=128)[:, t0_:t1_, :], reads=[R_["ckv"]], writes=[c_r], own=c_r)
            gt = kb.sb("p3_gt", [128, NH, GW], BF16)
            b15 = kb.sb("p3_b15", [128, NH], F32)
            gstg = Slots(kb, "p3_gstg", [128, GW], F32, 2)
            for h in range(NH):
                st, sr = gstg.next()
                kb.dma(out=st[:], in_=gtab[:, h, :], writes=[sr], own=sr)
                gop(lambda st=st, h=h: G.tensor_copy(out=gt[:, h, :], in_=st[:]), [sr], [c_r])
                gop(lambda st=st, h=h: G.tensor_copy(out=b15[:, h:h + 1], in_=st[:, GW - 1:GW]), [sr], [c_r])
            wuv_f = kb.sb("p3_wuvf", [128, 2, NH * 64], F32)
            wuv_b = kb.sb("p3_wuvb", [128, 2, NH * 64], BF16)
            kb.dma(out=wuv_f[:], in_=wuv[l], writes=[c_r], own=c_r)
            gop(lambda: G.tensor_copy(out=wuv_b[:], in_=wuv_f[:]), [c_r], [c_r])
            nml = Slots(kb, "p3_nm", [128, NT, 512], BF16, 2)
            ql = Slots(kb, "p3_q", [128, 2, 512], BF16, 3)
            El = Slots(kb, "p3_E", [128, 512], BF16, 3)
            PTl = Slots(kb, "p3_PT", [128, 512], BF16, 3)
            rsl = Slots(kb, "p3_rs", [128, 4], F32, 2)
            ol = Slots(kb, "p3_o", [128, 256], BF16, 8)
            oTl = Slots(kb, "p3_oT", [128, 2, 128], BF16, 2)
            aTl = Slots(kb, "p3_aT", [64, 512], BF16, 3)
            bankS = RR([0, 1, 2])
            nmv = nmT_d.rearrange("(kt p) t -> p kt t", p=128)
            nm_tiles = {}
            q_tiles = {}

            def load_nm(tb):
                if tb >= NB or tb in nm_tiles:
                    return
                nm, nm_r = nml.next()
                for t0_ in range(0, 4 * tb, 8):
                    t1_ = min(4 * tb, t0_ + 8)
                    kb.dma(out=nm[:, t0_:t1_, :], in_=nmv[:, t0_:t1_, tb * 512:(tb + 1) * 512], reads=[R_["nmT"]], writes=[nm_r], own=nm_r)
                for i in range(4):
                    kb.dma(out=nm[:, 4 * tb + i, i * 128:512], in_=nmv[:, 4 * tb + i, tb * 512 + i * 128:(tb + 1) * 512], reads=[R_["nmT"]],
                           writes=[nm_r], own=nm_r)
                nm_tiles[tb] = (nm, nm_r)

            def load_q(idx):
                if idx >= NB * NH or idx in q_tiles:
                    return
                tb, h = divmod(idx, NH)
                q, q_r = ql.next()
                kb.dma(out=q[:], in_=qlat_d[h].rearrange("(rc p) s -> p rc s", p=128)[:, :, tb * 512:(tb + 1) * 512], reads=[R_["qlat"]],
                       writes=[q_r], own=q_r)
                q_tiles[idx] = (q, q_r)

            groups = [(tb, h, kt) for tb in range(NB) for h in range(NH) for kt in range(4 * tb + 4)]
            sbank = {}

            def emit_qk(g):
                tb, h, kt = g
                if kt == 0:
                    load_nm(tb)
                    load_q(tb * NH + h)
                    load_q(tb * NH + h + 1)
                q, q_r = q_tiles[tb * NH + h]
                nm, nm_r = nm_tiles[tb]
                cl = max(0, kt - 4 * tb) * 128
                b = bankS.next()
                sbank[g] = b
                ks = slice(kt * 128, (kt + 1) * 128)
                c0 = tb * 512 - kt * 128 + 384
                const = c0 >= GC
                mm(ps[b][:, cl:512], ckvT[:, 0, ks], q[:, 0, cl:512], True, False, [c_r, q_r], [psr[b]], inc=False)
                mm(ps[b][:, cl:512], ckvT[:, 1, ks], q[:, 1, cl:512], False, const, [c_r, q_r], [psr[b]], inc=const)
                if not const:
                    mm(ps[b][:, cl:512], ident[:], gt[:, h, c0 + cl:c0 + 512], False, True, [cres, c_r], [psr[b]])

            def emit_rest(g):
                tb, h, kt = g
                if h == 0 and kt == 0:
                    load_nm(tb + 1)
                nm, nm_r = nm_tiles[tb]
                i = max(0, kt - 4 * tb)
                cl = i * 128
                b = sbank.pop(g)
                const = (tb * 512 - kt * 128 + 384) >= GC
                E, E_r = El.next()
                if const:
                    aop(lambda: A.activation(out=E[:, cl:512], in_=ps[b][:, cl:512], func=AF.Exp, bias=b15[:, h:h + 1], scale=1.0),
                        [psr[b], c_r], [E_r])
                else:
                    aop(lambda: A.activation(out=E[:, cl:512], in_=ps[b][:, cl:512], func=AF.Exp), [psr[b]], [E_r])
                PT, PT_r = PTl.next()
                vop(lambda: V.tensor_tensor(out=PT[:, cl:512], in0=E[:, cl:512], in1=nm[:, kt, cl:512], op=ALU.mult), [E_r, nm_r], [PT_r])
                for j in range(i, 4):
                    mm(ps[4 + j][:, 0:257], PT[:, j * 128:(j + 1) * 128], ckva[:, kt, 0:257], kt == 0, kt == 4 * tb + j, [PT_r, c_r],
                       [psr[4 + j]], inc=(j == 3 or kt == 4 * tb + j))

            def emit_post_head(tb, h):
                tiles = []
                for j in range(4):
                    rs, rs_r = rsl.next()
                    vop(lambda: V.reciprocal(out=rs[:, 0:1], in_=ps[4 + j][:, 256:257]), [psr[4 + j]], [rs_r])
                    o, o_r = ol.next()
                    aop(lambda: A.activation(out=o[:], in_=ps[4 + j][:, 0:256], func=AF.Identity, scale=rs[:, 0:1]),
                        [psr[4 + j], rs_r], [o_r])
                    tiles.append((o, o_r))
                q_tiles.pop(tb * NH + h, None)
                if h == NH - 1:
                    nm_tiles.pop(tb, None)
                return tiles

            def post_gen(tb, h, tiles):
                aT, aT_r = aTl.next()
                for j, (o, o_r) in enumerate(tiles):
                    pv = ps[3][:].bitcast(BF16)
                    for rc in range(2):
                        tp(pv[:, rc * 128:(rc + 1) * 128], o[:, rc * 128:(rc + 1) * 128], [o_r], [psr[3]], inc=(rc == 1))
                    yield
                    oT, oT_r = oTl.next()
                    vop(lambda: V.tensor_copy(out=oT[:], in_=pv[:, 0:256].rearrange("p (j t) -> p j t", j=2)), [psr[3]], [oT_r])
                    yield
                    for rc in range(2):
                        mm(ps[3][0:64, 256:384], wuv_b[:, rc, h * 64:(h + 1) * 64], oT[:, rc, :], rc == 0, rc == 1, [c_r, oT_r], [psr[3]],
                           inc=(rc == 1))
                    yield
                    vop(lambda: V.tensor_copy(out=aT[:, j * 128:(j + 1) * 128], in_=ps[3][0:64, 256:384]), [psr[3]], [aT_r])
                    yield
                kb.dma(out=mixT_d[h * 64:(h + 1) * 64, tb * 512:(tb + 1) * 512], in_=aT[:], reads=[aT_r], writes=[R_["mixT"]], own=aT_r)

            def drain(gen_):
                if gen_ is not None:
                    for _ in gen_:
                        pass

            pending = None
            emit_qk(groups[0])
            for idx, g in enumerate(groups):
                if idx + 1 < len(groups):
                    emit_qk(groups[idx + 1])
                emit_rest(g)
                tb, h, kt = g
                if pending is not None:
                    nsteps = -(-17 // (4 * tb + 4))
                    for _ in range(nsteps):
                        try:
                            next(pending)
                        except StopIteration:
                            pending = None
                            break
                if kt == 4 * tb + 3:
                    drain(pending)
                    tiles = emit_post_head(tb, h)
                    pending = post_gen(tb, h, tiles)
            drain(pending)

    def gen4(l):
        if True:
            c_r = kb.res("p4c")
            negtri4 = kb.sb("negtri4", [128, 4, 128], F32)
            gop(lambda: G.memset(negtri4[:], 0.0), [], [c_r])
            for j in range(4):
                gop(lambda j=j: G.affine_select(out=negtri4[:, j, :], in_=negtri4[:, j, :], pattern=[[1, 128]],
                                                compare_op=ALU.is_ge, fill=NEG, base=0, channel_multiplier=-1), [c_r], [c_r])
            v16_bc = kb.sb("p4_v16", [128, 3, 16], F32)
            kb.dma(out=v16_bc[:], in_=v16[l].partition_broadcast(128), writes=[c_r], own=c_r)
            gn = kb.sb("p4_gn", [128, D], F32)
            bcast_load(gn[:], vecD[l, 4, :], D, c_r)
            ST = kb.sb("p4_ST", [128, 2, 512], F32)
            ST_r = kb.res("p4_ST")
            STb = kb.sb("p4_STb", [128, 2, 512], BF16)
            STb_r = kb.res("p4_STb")
            vop(lambda: V.memset(ST[:], 0.0), [], [ST_r])
            vop(lambda: V.memset(STb[:], 0.0), [], [STb_r])
            xsl = Slots(kb, "p4_xs", [128, 16, 64], BF16, 2)
            bml = Slots(kb, "p4_bm", [128, 256], BF16, 2)
            bmTl = Slots(kb, "p4_bmT", [128, 2, 128], BF16, 2)
            cmTl = Slots(kb, "p4_cmT", [128, 2, 128], BF16, 2)
            dtl = Slots(kb, "p4_dt", [128, 32], F32, 2)
            zl = Slots(kb, "p4_z", [128, D], F32, 2)
            sml = Slots(kb, "p4_sm", [128, 6, 16], F32, 2)
            xdtl = Slots(kb, "p4_xdt", [128, 16, 64], BF16, 2)
            xdwl = Slots(kb, "p4_xdw", [128, 16, 64], BF16, 2)
            CBm = kb.sb("p4_CBm", [128, 2, 128], F32)
            CBm_r = kb.res("p4_CBm")
            rhsD = kb.sb("p4_rhsD", [128, 16, 128], F32)
            rhsD_r = kb.res("p4_rhsD")
            L = kb.sb("p4_L", [128, 16, 128], F32)
            L_r = kb.res("p4_L")
            M = kb.sb("p4_M", [128, 16, 128], BF16)
            M_r = kb.res("p4_M")
            Y = kb.sb("p4_Y", [128, 16, 64], F32)
            Y_r = kb.res("p4_Y")
            tmpY = kb.sb("p4_tmpY", [128, 16, 64], F32)
            tmpY_r = kb.res("p4_tmpY")
            Yn = kb.sb("p4_Yn", [128, D], BF16)
            Yn_r = kb.res("p4_Yn")
            stl = Slots(kb, "p4_st", [128, 16], F32, 2)
            cTl = Slots(kb, "p4_cT", [128, 8, 128], BF16, 2)
            bankD = RR([4])
            stA = {}

            def stageA(ct):
                rows = slice(ct * 128, (ct + 1) * 128)
                yb = (5, 6)
                xdt, xdt_r = xdtl.next()
                xdw, xdw_r = xdwl.next()
                xs, xs_r = xsl.next()
                kb.dma(out=xs[:].rearrange("p h d -> p (h d)"), in_=xsc_d[rows, :], reads=[R_["xsc"]], writes=[xs_r], own=xs_r)
                bm, bm_r = bml.next()
                kb.dma(out=bm[:], in_=bm_d[rows, :], reads=[R_["bm"]], writes=[bm_r], own=bm_r)
                bmT, bmT_r = bmTl.next()
                kb.dma(out=bmT[:], in_=bmT_d.rearrange("(g n) s -> n g s", g=2)[:, :, rows], reads=[R_["bmT"]], writes=[bmT_r], own=bmT_r)
                cmT, cmT_r = cmTl.next()
                kb.dma(out=cmT[:], in_=cmT_d.rearrange("(g n) s -> n g s", g=2)[:, :, rows], reads=[R_["cmT"]], writes=[cmT_r], own=cmT_r)
                dtt, dt_r = dtl.next()
                kb.dma(out=dtt[:], in_=dt_d[rows, :], reads=[R_["dt"]], writes=[dt_r], own=dt_r)
                zt, z_r = zl.next()
                kb.dma(out=zt[:], in_=zs_d[rows, :], reads=[R_["zs"]], writes=[z_r], own=z_r)
                sm, sm_r = sml.next()
                mm(ps[3][:, 0:16], utri_f[:], dtt[:, 16:32], True, True, [cres, dt_r], [psr[3]])
                yield
                mm(ps[3][:, 16:32], ones_f[:], dtt[:, 16:32], True, True, [cres, dt_r], [psr[3]])
                yield
                aop(lambda sm=sm: A.copy(out=sm[:, 0, :], in_=ps[3][:, 0:16]), [psr[3]], [sm_r])
                yield
                aop(lambda sm=sm: A.mul(out=sm[:, 1, :], in_=ps[3][:, 0:16], mul=-1.0), [psr[3]], [sm_r])
                yield
                aop(lambda sm=sm: A.activation(out=sm[:, 2, :], in_=ps[3][:, 0:16], func=AF.Exp), [psr[3]], [sm_r])
                yield
                aop(lambda sm=sm: A.activation(out=sm[:, 4, :], in_=ps[3][:, 16:32], func=AF.Exp), [psr[3]], [sm_r])
                yield
                vop(lambda sm=sm: V.tensor_tensor(out=sm[:, 5, :], in0=ps[3][:, 16:32], in1=sm[:, 0, :], op=ALU.subtract), [psr[3], sm_r], [sm_r])
                yield
                aop(lambda sm=sm: A.activation(out=sm[:, 3, :], in_=sm[:, 5, :], func=AF.Exp), [sm_r], [sm_r])
                yield
                vop(lambda xs=xs, dtt=dtt: V.tensor_tensor(out=xdt[:], in0=xs[:], in1=dtt[:, 0:16].unsqueeze(2).to_broadcast([128, 16, 64]),
                                                           op=ALU.mult), [xs_r, dt_r], [xdt_r])
                yield
                gop(lambda sm=sm: G.tensor_tensor(out=xdw[:], in0=xdt[:], in1=sm[:, 3, :].unsqueeze(2).to_broadcast([128, 16, 64]),
                                                  op=ALU.mult), [xdt_r, sm_r], [xdw_r])
                yield
                for g in range(2):
                    mm(ps[3][:, 64 + g * 128:64 + (g + 1) * 128], bmT[:, g, :], cmT[:, g, :], True, True, [bmT_r, cmT_r], [psr[3]], inc=(g == 1))
                    yield
                vop(lambda: V.tensor_tensor(out=CBm[:], in0=ps[3][:, 64:320].rearrange("p (g l) -> p g l", g=2),
                                            in1=tri_f[:].unsqueeze(1).to_broadcast([128, 2, 128]), op=ALU.mult), [psr[3], cres], [CBm_r])
                yield
                gop(lambda sm=sm: G.tensor_tensor(out=rhsD[:], in0=ident_f[:].unsqueeze(1).to_broadcast([128, 16, 128]),
                                                  in1=sm[:, 0, :].unsqueeze(2).to_broadcast([128, 16, 128]), op=ALU.mult),
                    [cres, sm_r], [rhsD_r])
                yield
                for q in range(4):
                    b = bankD.next()
                    mm(ps[b][:], ones_f[:], rhsD[:, 4 * q:4 * q + 4, :].rearrange("p h l -> p (h l)"), True, False, [cres, rhsD_r], [psr[b]],
                       inc=False)
                    yield
                    mm(ps[b][:], ident_f[:], negtri4[:].rearrange("p h l -> p (h l)"), False, True, [cres, c_r], [psr[b]])
                    yield
                    for hq in range(4):
                        h = 4 * q + hq
                        aop(lambda b=b, hq=hq, h=h, sm=sm: A.activation(out=L[:, h, :], in_=ps[b][:, hq * 128:(hq + 1) * 128], func=AF.Exp,
                                                                       bias=sm[:, 1, h:h + 1], scale=1.0), [psr[b], sm_r], [L_r])
                        yield
                for g in range(2):
                    gop(lambda g=g: G.tensor_tensor(out=M[:, g * 8:(g + 1) * 8, :], in0=L[:, g * 8:(g + 1) * 8, :],
                                                    in1=CBm[:, g, :].unsqueeze(1).to_broadcast([128, 8, 128]), op=ALU.mult),
                        [L_r, CBm_r], [M_r])
                    yield
                for h in range(16):
                    b = yb[h // 8]
                    hh = h % 8
                    mm(ps[b][:, hh * 64:(hh + 1) * 64], M[:, h, :], xdt[:, h, :], True, True, [M_r, xdt_r], [psr[b]], inc=(hh == 7))
                    yield
                stA[ct] = dict(rows=rows, yb=yb, xs=xs, xs_r=xs_r, bm=bm, bm_r=bm_r, cmT=cmT, cmT_r=cmT_r, zt=zt, z_r=z_r, sm=sm, sm_r=sm_r,
                               xdw=xdw, xdw_r=xdw_r)

            def stageB(ct):
                d = stA.pop(ct)
                rows, yb, xs, xs_r, bm, bm_r, cmT, cmT_r = d["rows"], d["yb"], d["xs"], d["xs_r"], d["bm"], d["bm_r"], d["cmT"], d["cmT_r"]
                zt, z_r, sm, sm_r, xdw, xdw_r = d["zt"], d["z_r"], d["sm"], d["sm_r"], d["xdw"], d["xdw_r"]
                yield
                for g in range(2):
                    mm(ps[7][:], cmT[:, g, :], STb[:, g, :], True, True, [cmT_r, STb_r], [psr[7]])
                    yield
                    vop(lambda g=g, sm=sm: V.tensor_tensor(out=Y[:, g * 8:(g + 1) * 8, :], in0=ps[7][:].rearrange("p (h d) -> p h d", h=8),
                                                           in1=sm[:, 2, g * 8:(g + 1) * 8].unsqueeze(2).to_broadcast([128, 8, 64]),
                                                           op=ALU.mult), [psr[7], sm_r], [Y_r])
                    yield
                    vop(lambda g=g: V.tensor_tensor(out=Y[:, g * 8:(g + 1) * 8, :], in0=Y[:, g * 8:(g + 1) * 8, :],
                                                    in1=ps[yb[g]][:].rearrange("p (h d) -> p h d", h=8), op=ALU.add),
                        [Y_r, psr[yb[g]]], [Y_r])
                    yield
                gop(lambda xs=xs: G.tensor_tensor(out=tmpY[:], in0=xs[:], in1=v16_bc[:, 2, :].unsqueeze(2).to_broadcast([128, 16, 64]),
                                                  op=ALU.mult), [xs_r, c_r], [tmpY_r])
                yield
                gop(lambda: G.tensor_tensor(out=Y[:], in0=Y[:], in1=tmpY[:], op=ALU.add), [Y_r, tmpY_r], [Y_r])
                yield
                for g in range(2):
                    mm(ps[7][:], bm[:, g * 128:(g + 1) * 128], xdw[:, g * 8:(g + 1) * 8, :].rearrange("p h d -> p (h d)"), True, True,
                       [bm_r, xdw_r], [psr[7]])
                    yield
                    vop(lambda g=g, sm=sm: V.tensor_tensor(out=ST[:, g, :].rearrange("p (h d) -> p h d", h=8),
                                                           in0=ST[:, g, :].rearrange("p (h d) -> p h d", h=8),
                                                           in1=sm[:, 4, g * 8:(g + 1) * 8].unsqueeze(2).to_broadcast([128, 8, 64]),
                                                           op=ALU.mult), [ST_r, sm_r, STb_r], [ST_r])
                    yield
                    vop(lambda g=g: V.tensor_tensor(out=ST[:, g, :], in0=ST[:, g, :], in1=ps[7][:], op=ALU.add), [ST_r, psr[7]], [ST_r])
                    yield
                aop(lambda: A.copy(out=STb[:], in_=ST[:]), [ST_r], [STb_r])
                yield
                Yf = Y[:].rearrange("p h d -> p (h d)")
                gop(lambda zt=zt: G.tensor_tensor(out=Yf, in0=Yf, in1=zt[:], op=ALU.mult), [Y_r, z_r], [Y_r])
                yield
                st, st_r = stl.next()
                for g in range(2):
                    aop(lambda g=g, st=st: A.activation(out=tmpY[:].rearrange("p h d -> p (h d)")[:, g * 512:(g + 1) * 512],
                                                        in_=Yf[:, g * 512:(g + 1) * 512], func=AF.Square, accum_out=st[:, g:g + 1]),
                        [Y_r], [tmpY_r, st_r])
                    yield
                    rstd(st, st_r, g, 4 + g, 512)
                    vop(lambda g=g, st=st: V.scalar_tensor_tensor(out=Yn[:, g * 512:(g + 1) * 512], in0=Yf[:, g * 512:(g + 1) * 512],
                                                                  scalar=st[:, 4 + g:5 + g], in1=gn[:, g * 512:(g + 1) * 512],
                                                                  op0=ALU.mult, op1=ALU.mult), [Y_r, st_r, c_r], [Yn_r])
                    yield
                cT, cT_r = cTl.next()
                pv = ps[7][:].bitcast(BF16)
                for c in range(8):
                    tp(pv[:, c * 128:(c + 1) * 128], Yn[:, c * 128:(c + 1) * 128], [Yn_r], [psr[7]], inc=(c == 7))
                    yield
                aop(lambda: A.copy(out=cT[:], in_=pv[:, 0:1024].rearrange("p (j t) -> p j t", j=8)), [psr[7]], [cT_r])
                yield
                kb.dma(out=mixT_d.rearrange("(kc p) s -> p kc s", p=128)[:, 8:16, rows], in_=cT[:], reads=[cT_r], writes=[R_["mixT"]], own=cT_r)

            yield
            for ct in range(NT):
                yield from stageA(ct)
                yield from stageB(ct)

    def phase4(l):
        with Phase():
            run_interleaved([gen4(l)])

    class WLoad:
        def __init__(self, name, kc=8, nc_=256, n=3):
            self.kc, self.nc_ = kc, nc_
            self.stg = Slots(kb, name + "_stg", [128, kc, nc_], F32, n)

        def load(self, dst, dst_r, wap, r0, kcn, c0, ncols):
            assert kcn <= self.kc and ncols <= self.nc_
            st, sr = self.stg.next()
            src = wap[r0:r0 + kcn * 128, :].rearrange("(kc p) c -> p kc c", p=128)[:, :, c0:c0 + ncols]
            kb.dma(out=st[:, 0:kcn, 0:ncols], in_=src, writes=[sr], own=sr)
            self.i = getattr(self, "i", 0) + 1
            e = ("dve", "act", "pool", "dve", "act")[self.i % 5]
            if e == "pool":
                gop(lambda: G.tensor_copy(out=dst, in_=st[:, 0:kcn, 0:ncols]), [sr], [dst_r])
            elif e == "dve":
                vop(lambda: V.tensor_copy(out=dst, in_=st[:, 0:kcn, 0:ncols]), [sr], [dst_r])
            else:
                aop(lambda: A.copy(out=dst, in_=st[:, 0:kcn, 0:ncols]), [sr], [dst_r])

        def load_full(self, dst_t, dst_r, wap, KC, C):
            for k0 in range(0, KC, self.kc):
                kn = min(self.kc, KC - k0)
                for c0 in range(0, C, self.nc_):
                    cn = min(self.nc_, C - c0)
                    self.load(dst_t[:, k0:k0 + kn, c0:c0 + cn], dst_r, wap, k0 * 128, kn, c0, cn)

    def post_norm_residual(P0, P1, r0, r1, xt, xr, g_bc, g_r, stl, junk, junk_r, tmp, tmp_r):
        st, st_r = stl.next()
        aop(lambda: A.activation(out=junk[:, 0:512], in_=P0[:], func=AF.Square, accum_out=st[:, 0:1]), [r0], [junk_r, st_r])
        aop(lambda: A.activation(out=junk[:, 512:1024], in_=P1[:], func=AF.Square, accum_out=st[:, 1:2]), [r1], [junk_r, st_r])
        vop(lambda: V.tensor_tensor(out=st[:, 3:4], in0=st[:, 0:1], in1=st[:, 1:2], op=ALU.add), [st_r], [st_r])
        rstd(st, st_r, 3, 2, D)
        for half, (P, r) in enumerate(((P0, r0), (P1, r1))):
            sl = slice(half * 512, (half + 1) * 512)
            vop(lambda P=P, sl=sl: V.scalar_tensor_tensor(out=tmp[:, sl], in0=P[:], scalar=st[:, 2:3], in1=g_bc[:, sl],
                                                         op0=ALU.mult, op1=ALU.mult), [r, st_r, g_r], [tmp_r])
            gop(lambda sl=sl: G.tensor_tensor(out=xt[:, sl], in0=tmp[:, sl], in1=xt[:, sl], op=ALU.add), [tmp_r, xr], [xr])

    def phase5(l):
        with Phase():
            src = x_in if l == 0 else xs_d
            wl = WLoad("p5w")
            wo = kb.sb("p5_wo", [128, 16, D], BF16)
            wo_r = kb.res("p5_wo")
            wl.load_full(wo, wo_r, w_out[l], 16, D)
            g1 = kb.sb("p5_g1", [128, D], F32)
            g2 = kb.sb("p5_g2", [128, D], F32)
            g_r = kb.res("p5_g")
            bcast_load(g1[:], vecD[l, 1, :], D, g_r)
            bcast_load(g2[:], vecD[l, 2, :], D, g_r)
            xsl = Slots(kb, "p5_x", [128, D], F32, 2)
            mxl = Slots(kb, "p5_mx", [128, 16, 128], BF16, 2)
            junk = kb.sb("p5_junk", [128, D], F32)
            junk_r = kb.res("p5_junk")
            tmp = kb.sb("p5_tmp", [128, D], F32)
            tmp_r = kb.res("p5_tmp")
            stl = Slots(kb, "p5_st", [128, 16], F32, 2)
            hb = kb.sb("p5_hb", [128, D], BF16)
            ntmp = (junk, junk_r, stl, hb, kb.res("p5_hb"))
            banks = RR([0, 1, 2, 3])
            for tt in range(NT):
                xt, xr = xsl.next()
                kb.dma(out=xt[:], in_=src[tt * 128:(tt + 1) * 128, :], reads=[R_["xs"]], writes=[xr], own=xr)
                mx, mr = mxl.next()
                kb.dma(out=mx[:], in_=mixT_d.rearrange("(kc p) s -> p kc s", p=128)[:, :, tt * 128:(tt + 1) * 128],
                       reads=[R_["mixT"]], writes=[mr], own=mr)
                b0, b1 = banks.next(), banks.next()
                for half, b in enumerate((b0, b1)):
                    for kc in range(16):
                        mm(ps[b][:], mx[:, kc, :], wo[:, kc, half * 512:(half + 1) * 512], kc == 0, kc == 15, [mr, wo_r], [psr[b]],
                           inc=(kc == 15))
                post_norm_residual(ps[b0], ps[b1], psr[b0], psr[b1], xt, xr, g1, g_r, stl, junk, junk_r, tmp, tmp_r)
                kb.dma(out=xm_d[tt * 128:(tt + 1) * 128, :], in_=xt[:], reads=[xr], writes=[R_["xm"]], own=xr)
                norm_to_hT(xt[:], xr, g2, g_r, tt, ntmp)

    def phase6a(l):
        with Phase():
            hT, hT_r = HT['t'], HT['r']
            wl = WLoad("p6w", 8, 512, 2)
            wgl = Slots(kb, "p6_wg", [128, 8, 512], BF16, 2)
            wul = Slots(kb, "p6_wu", [128, 8, 512], BF16, 2)
            sgl = Slots(kb, "p6_sg", [128, 512], F32, 2)
            gul = Slots(kb, "p6_gu", [128, 512], BF16, 3)
            bankA = RR([0, 1])
            bankB = RR([2, 3])
            for c0 in range(0, DFF, 512):
                cn = min(512, DFF - c0)
                wg_t, wg_r = wgl.next()
                wu_t, wu_r = wul.next()
                wl.load(wg_t[:, :, 0:cn], wg_r, w_g[l], 0, 8, c0, cn)
                wl.load(wu_t[:, :, 0:cn], wu_r, w_u[l], 0, 8, c0, cn)
                for c in range(cn // 128):
                    for tb in range(NB):
                        bg_, bu_ = bankA.next(), bankB.next()
                        for kc in range(8):
                            mm(ps[bg_][:], wg_t[:, kc, c * 128:(c + 1) * 128], hT[:, kc, tb * 512:(tb + 1) * 512], kc == 0, kc == 7,
                               [wg_r, hT_r], [psr[bg_]], inc=(kc == 7))
                        for kc in range(8):
                            mm(ps[bu_][:], wu_t[:, kc, c * 128:(c + 1) * 128], hT[:, kc, tb * 512:(tb + 1) * 512], kc == 0, kc == 7,
                               [wu_r, hT_r], [psr[bu_]], inc=(kc == 7))
                        sg, sgr = sgl.next()
                        aop(lambda sg=sg, bg_=bg_: A.activation(out=sg[:], in_=ps[bg_][:], func=AF.Silu), [psr[bg_]], [sgr])
                        gu, gur = gul.next()
                        vop(lambda sg=sg, gu=gu, bu_=bu_: V.tensor_tensor(out=gu[:], in0=ps[bu_][:], in1=sg[:], op=ALU.mult),
                            [psr[bu_], sgr], [gur])
                        f0 = c0 + c * 128
                        kb.dma(out=gu_d[f0:f0 + 128, tb * 512:(tb + 1) * 512], in_=gu[:], reads=[gur], writes=[R_["gu"]], own=gur)

    def phase6b(l):
        last = (l == DEPTH - 1)
        with Phase():
            wl = WLoad("p6bw")
            KF = DFF // 128
            wd = kb.sb("p6b_wd", [128, KF, D], BF16)
            wd_r = kb.res("p6b_wd")
            wl.load_full(wd, wd_r, w_d[l], KF, D)
            wpg = kb.sb("p6b_wpg", [128, 8, D], BF16)
            wpg_r = kb.res("p6b_wpg")
            wl.load_full(wpg, wpg_r, w_pg[l], 8, D)
            wpp = kb.sb("p6b_wpp", [128, 2, D], BF16)
            wpp_r = kb.res("p6b_wpp")
            wl.load_full(wpp, wpp_r, w_pp[l], 2, D)
            g1 = kb.sb("p6b_g1", [128, D], F32)
            g_r = kb.res("p6b_g")
            bcast_load(g1[:], vecD[l, 3, :], D, g_r)
            xsl = Slots(kb, "p6b_x", [128, D], F32, 2)
            gtl = Slots(kb, "p6b_gt", [128, KF, 128], BF16, 2)
            pl = Slots(kb, "p6b_p", [128, 256], F32, 2)
            junk = kb.sb("p6b_junk", [128, D], F32)
            junk_r = kb.res("p6b_junk")
            tmp = kb.sb("p6b_tmp", [128, D], F32)
            tmp_r = kb.res("p6b_tmp")
            stl = Slots(kb, "p6b_st", [128, 16], F32, 2)
            xb = kb.sb("p6b_xb", [128, D], BF16)
            xb_r = kb.res("p6b_xb")
            xT = kb.sb("p6b_xT", [128, 8, 128], BF16)
            xT_r = kb.res("p6b_xT")
            pb16 = kb.sb("p6b_pb", [128, 256], BF16)
            pb_r = kb.res("p6b_pb")
            pT = kb.sb("p6b_pT", [128, 2, 128], BF16)
            pT_r = kb.res("p6b_pT")
            sg = kb.sb("p6b_sg", [128, D], F32)
            sg_r = kb.res("p6b_sg")
            dst_d = y_out if last else xs_d
            dst_r = R_["y"] if last else R_["xs"]
            S1 = {}

            def stage1(tt):
                xt, xr = xsl.next()
                kb.dma(out=xt[:], in_=xm_d[tt * 128:(tt + 1) * 128, :], reads=[R_["xm"]], writes=[xr], own=xr)
                gt, gr = gtl.next()
                kb.dma(out=gt[:], in_=gu_d.rearrange("(kc p) s -> p kc s", p=128)[:, :, tt * 128:(tt + 1) * 128],
                       reads=[R_["gu"]], writes=[gr], own=gr)
                pt, pr = pl.next()
                kb.dma(out=pt[:], in_=p_in[l, tt * 128:(tt + 1) * 128, :], writes=[pr], own=pr)
                db = (0, 1) if tt % 2 == 0 else (2, 3)
                for half, b in enumerate(db):
                    for kc in range(KF):
                        mm(ps[b][:], gt[:, kc, :], wd[:, kc, half * 512:(half + 1) * 512], kc == 0, kc == KF - 1, [gr, wd_r], [psr[b]],
                           inc=(kc == KF - 1))
                S1[tt] = (xt, xr, pt, pr, db)

            def stage2(tt):
                xt, xr, pt, pr, db = S1.pop(tt)
                post_norm_residual(ps[db[0]], ps[db[1]], psr[db[0]], psr[db[1]], xt, xr, g1, g_r, stl, junk, junk_r, tmp, tmp_r)
                aop(lambda: A.copy(out=xb[:], in_=xt[:]), [xr], [xb_r])
                pv = ps[7][:].bitcast(BF16)
                for dc in range(8):
                    tp(pv[:, dc * 128:(dc + 1) * 128], xb[:, dc * 128:(dc + 1) * 128], [xb_r], [psr[7]], inc=(dc == 7))
                vop(lambda: V.tensor_copy(out=xT[:], in_=pv[:, 0:1024].rearrange("p (j t) -> p j t", j=8)), [psr[7]], [xT_r])
                for half, b in enumerate((4, 5)):
                    for kc in range(8):
                        mm(ps[b][:], xT[:, kc, :], wpg[:, kc, half * 512:(half + 1) * 512], kc == 0, kc == 7, [xT_r, wpg_r], [psr[b]],
                           inc=(kc == 7))
                    aop(lambda: A.activation(out=sg[:, half * 512:(half + 1) * 512], in_=ps[b][:], func=AF.Sigmoid), [psr[b]], [sg_r])
                vop(lambda: V.tensor_copy(out=pb16[:], in_=pt[:]), [pr], [pb_r])
                pv6 = ps[6][:].bitcast(BF16)
                for j in range(2):
                    tp(pv6[:, j * 128:(j + 1) * 128], pb16[:, j * 128:(j + 1) * 128], [pb_r], [psr[6]], inc=(j == 1))
                vop(lambda: V.tensor_copy(out=pT[:], in_=pv6[:, 0:256].rearrange("p (j t) -> p j t", j=2)), [psr[6]], [pT_r])
                for half in range(2):
                    for kc in range(2):
                        mm(ps[6][:], pT[:, kc, :], wpp[:, kc, half * 512:(half + 1) * 512], kc == 0, kc == 1, [pT_r, wpp_r], [psr[6]],
                           inc=(kc == 1))
                    sl = slice(half * 512, (half + 1) * 512)
                    vop(lambda: V.tensor_tensor(out=tmp[:, sl], in0=ps[6][:], in1=sg[:, sl], op=ALU.mult), [psr[6], sg_r], [tmp_r])
                    gop(lambda: G.tensor_tensor(out=xt[:, sl], in0=tmp[:, sl], in1=xt[:, sl], op=ALU.add), [tmp_r, xr], [xr])
                kb.dma(out=dst_d[tt * 128:(tt + 1) * 128, :], in_=xt[:], reads=[xr], writes=[dst_r], own=xr)

            stage1(0)
            for tt in range(NT):
                if tt + 1 < NT:
                    stage1(tt + 1)
                stage2(tt)

    def front(l):
        with HTScope():
            phase0(l, None if l == 0 else xs_d)
            phase1(l)

    def mid(l):
        phase24(l)
        phase3(l)

    def back(l):
        with HTScope():
            phase5(l)
            phase6a(l)
        phase6b(l)

    def run_all():
        for l in range(DEPTH):
            front(l)
            mid(l)
            back(l)

    phases = phases or ["all"]
    kb.fn = dict(front=front, mid=mid, back=back, phase2=phase2, phase3=phase3, phase4=phase4, phase24=phase24)
    if phases == ["all"]:
        run_all()
    else:
        for ph in phases:
            name, l = ph
            kb.fn[name](l)
    barrier()
    return nc, kb


def prep_inputs(inp, S):
    f = lambda a: np.ascontiguousarray(np.asarray(a, dtype=np.float32))
    w_uk = f(inp["w_uk"])
    wukT = np.ascontiguousarray(w_uk.reshape(DEPTH, R, 4, 2, 64).transpose(0, 3, 4, 2, 1).reshape(DEPTH, 128, 4, R))
    w_uv = f(inp["w_uv"])
    wuv = np.ascontiguousarray(w_uv.reshape(DEPTH, 2, 128, NH * 64).transpose(0, 2, 1, 3))
    rel = f(inp["rel_bias"])
    i = np.arange(128)[:, None]
    c = np.arange(GW)[None, :]
    bucket = t5_bucket_np((i - c + 384).astype(np.int32))
    gtab = np.ascontiguousarray(rel[bucket].transpose(0, 2, 1))
    vecD = np.ascontiguousarray(np.stack([f(inp["pre_mix_norm"]), f(inp["post_mix_norm"]), f(inp["pre_ffn_norm"]),
                                          f(inp["post_ffn_norm"]), f(inp["ssm_norm"])], axis=1))
    scw = np.ascontiguousarray(f(inp["short_conv_w"]).reshape(DEPTH, 3, 4, 128).transpose(0, 3, 2, 1))
    sscw = np.ascontiguousarray(f(inp["ssm_conv_w"]).reshape(DEPTH, 4, 12, 128).transpose(0, 3, 2, 1))
    sscb = np.ascontiguousarray(f(inp["ssm_conv_b"]).reshape(DEPTH, 12, 128).transpose(0, 2, 1))
    v16 = np.ascontiguousarray(np.stack([f(inp["ssm_dt_bias"]), f(inp["ssm_a_log"]), f(inp["ssm_d"])], axis=1))
    shared = dict(w_in=f(inp["w_in"]), w_out=f(inp["w_out"]), w_ffn_gate=f(inp["w_ffn_gate"]), w_ffn_up=f(inp["w_ffn_up"]),
                  w_ffn_down=f(inp["w_ffn_down"]), w_ple_proj=f(inp["w_ple_proj"]), w_ple_gate=f(inp["w_ple_gate"]),
                  wukT=wukT, wuv=wuv, gtab=gtab, vecD=vecD, kv_norm=f(inp["kv_norm"]), idx_k_norm_g=f(inp["idx_k_norm_g"]),
                  idx_k_norm_b=f(inp["idx_k_norm_b"]), scw=scw, sscw=sscw, sscb=sscb, v16=v16)
    x = f(inp["x"])
    p = f(inp["p"])
    B = x.shape[0]
    maps = []
    for b in range(B):
        m = dict(shared)
        m["x"] = np.ascontiguousarray(x[b, :S])
        m["p"] = np.ascontiguousarray(p[:, b, :S])
        maps.append(m)
    return maps


_CACHE = {}


def kernel(**inputs):
    S = 4096
    maps = prep_inputs(inputs, S)
    if "nc" not in _CACHE:
        _CACHE["nc"] = build_program(S)[0]
    res = run_bass_kernel_spmd(_CACHE["nc"], maps, core_ids=list(range(8)))
    return np.stack([np.asarray(r["y"], dtype=np.float32) for r in res.results], axis=0)
```

```python
import math
from contextlib import ExitStack
import numpy as np
import concourse.bass as bass
import concourse.mybir as mybir
from concourse.bass_utils import run_bass_kernel_spmd

F32 = mybir.dt.float32
BF16 = mybir.dt.bfloat16
AF = mybir.ActivationFunctionType
ALU = mybir.AluOpType
AX = mybir.AxisListType

D = 1024
DEPTH = 2
NH = 8
R = 256
IN_W = 5204
DFF = 2816
MIXW = 2048
EPS = 1e-6
NEG = -30000.0
O_Q, O_CKV, O_IQ, O_IK, O_IW, O_BG, O_CG, O_HB, O_Z, O_XBC, O_DT = 0, 512, 768, 1024, 1088, 1092, 1604, 2116, 2628, 3652, 5188
GW = 1582
GC = 1070
SEM_LIMIT = 30000


class Sem:
    def __init__(self, h):
        self.h = h
        self.val = 0


class Res:
    __slots__ = ("name", "w", "r", "dsem")

    def __init__(self, name):
        self.name = name
        self.w = {}
        self.r = {}
        self.dsem = None


class EngW:
    def __init__(self, name, eng, is_pe=False):
        self.name = name
        self.eng = eng
        self.is_pe = is_pe
        self.sem = None
        self.seen = {}


class KB:
    def __init__(self, nc):
        self.nc = nc
        self.nsem = 0
        self.pe = EngW("pe", nc.tensor, True)
        self.act = EngW("act", nc.scalar)
        self.dve = EngW("dve", nc.vector)
        self.pool = EngW("pool", nc.gpsimd)
        self.sp = EngW("sp", nc.sync)
        for e in (self.pe, self.act, self.dve, self.pool):
            e.sem = self.new_sem(e.name)
        self.stack = ExitStack()
        self.n_ops = 0
        self.free_dsems = []
        self.phase_dsems = [[]]

    def new_sem(self, name="s"):
        self.nsem += 1
        return Sem(self.nc.alloc_semaphore(name=f"{name}_{self.nsem}"))

    def res(self, name):
        return Res(name)

    def sb(self, name, shape, dt):
        self.n_sb = getattr(self, "n_sb", 0) + 1
        return self.stack.enter_context(self.nc.sbuf_tensor(f"{name}_{self.n_sb}", list(shape), dt))

    def _wait(self, e, reads, writes):
        raw = {}
        oth = {}
        for r in reads:
            for s, v in r.w.items():
                if raw.get(s, 0) < v:
                    raw[s] = v
        for w in writes:
            for s, v in w.w.items():
                if oth.get(s, 0) < v:
                    oth[s] = v
            for s, v in w.r.items():
                if oth.get(s, 0) < v:
                    oth[s] = v
        for s, v in oth.items():
            if s is e.sem:
                continue
            if raw.get(s, 0) < v:
                raw[s] = v
        for s, v in raw.items():
            if s is e.sem and e.is_pe:
                continue
            if e.seen.get(s, 0) >= v:
                continue
            e.eng.wait_ge(s.h, v)
            e.seen[s] = v

    def op(self, e, fn, reads=(), writes=(), inc=True):
        self._wait(e, reads, writes)
        ins = fn()
        self.n_ops += 1
        if inc:
            if e.sem.val >= SEM_LIMIT and not getattr(e, "pending", False):
                e.sem = self.new_sem(e.name)
            e.sem.val += 1
            ins.then_inc(e.sem.h, 1)
            tv = e.sem.val
            e.pending = False
        else:
            tv = e.sem.val + 1
            e.pending = True
        for w in writes:
            w.w[e.sem] = tv
        for r in reads:
            r.r[e.sem] = tv
        return ins

    def dma(self, out, in_, reads=(), writes=(), q=None, own=None):
        q = q or self.sp
        self._wait(q, reads, writes)
        ins = q.eng.dma_start(out=out, in_=in_)
        self.n_ops += 1
        if own.dsem is None or own.dsem.val >= SEM_LIMIT:
            if getattr(self, "free_dsems", None):
                own.dsem = self.free_dsems.pop()
                if own.dsem.val >= SEM_LIMIT:
                    own.dsem = self.new_sem("d" + own.name)
            else:
                own.dsem = self.new_sem("d" + own.name)
            self.phase_dsems[-1].append(own.dsem)
        s = own.dsem
        s.val += 16
        ins.then_inc(s.h, 16)
        for w in writes:
            w.w[s] = s.val
        for r in reads:
            r.r[s] = s.val
        return ins

    def drain(self, e, ress):
        self._wait(e, ress, ())


class Slots:
    def __init__(self, kb, name, shape, dt, n):
        self.t = [kb.sb(f"{name}{i}", shape, dt) for i in range(n)]
        self.r = [kb.res(f"{name}{i}") for i in range(n)]
        self.i = 0
        self.n = n

    def next(self):
        k = self.i % self.n
        self.i += 1
        return self.t[k], self.r[k]


def t5_bucket_np(rel):
    half, max_exact = 16, 8
    ret = np.where(rel > 0, half, 0)
    n = np.abs(rel)
    nf = np.maximum(n, 1).astype(np.float32)
    large = max_exact + (np.log(nf / max_exact) / math.log(1024 / max_exact) * (half - max_exact)).astype(np.int32)
    large = np.minimum(large, half - 1)
    return ret + np.where(n < max_exact, n, large)


def build_program(S, dbg=False, phases=None):
    NT = S // 128
    NB = S // 512
    nc = bass.Bass("TRN2", target_bir_lowering=False)
    kb = KB(nc)
    pe, act, dve, pool, sp = kb.pe, kb.act, kb.dve, kb.pool, kb.sp
    T, V, A, G = nc.tensor, nc.vector, nc.scalar, nc.gpsimd

    def din(name, shape, dt=F32):
        return nc.dram_tensor(name, list(shape), dt, kind="ExternalInput").ap()

    def dscr(name, shape, dt):
        return nc.dram_tensor(name, list(shape), dt, kind="ExternalOutput" if dbg else "Internal").ap()

    x_in = din("x", [S, D])
    p_in = din("p", [DEPTH, S, 256])
    w_in = din("w_in", [DEPTH, D, IN_W])
    w_out = din("w_out", [DEPTH, MIXW, D])
    w_g = din("w_ffn_gate", [DEPTH, D, DFF])
    w_u = din("w_ffn_up", [DEPTH, D, DFF])
    w_d = din("w_ffn_down", [DEPTH, DFF, D])
    w_pp = din("w_ple_proj", [DEPTH, 256, D])
    w_pg = din("w_ple_gate", [DEPTH, D, D])
    wukT = din("wukT", [DEPTH, 128, 4, R])
    wuv = din("wuv", [DEPTH, 128, 2, NH * 64])
    gtab = din("gtab", [128, NH, GW])
    vecD = din("vecD", [DEPTH, 5, D])
    kvn = din("kv_norm", [DEPTH, R])
    ikg = din("idx_k_norm_g", [DEPTH, 64])
    ikb = din("idx_k_norm_b", [DEPTH, 64])
    scw = din("scw", [DEPTH, 128, 4, 3])
    sscw = din("sscw", [DEPTH, 128, 12, 4])
    sscb = din("sscb", [DEPTH, 128, 12])
    v16 = din("v16", [DEPTH, 3, 16])
    y_out = nc.dram_tensor("y", [S, D], F32, kind="ExternalOutput").ap()

    xs_d = dscr("xs_d", [S, D], F32)
    xm_d = dscr("xm_d", [S, D], F32)
    qlat_d = dscr("qlat_d", [NH, R, S], BF16)
    iq_d = dscr("iq_d", [256, S], BF16)
    ik_d = dscr("ik_d", [64, S], BF16)
    iw_d = dscr("iw_d", [S, 4], F32)
    ckv_d = dscr("ckv_d", [S, R], BF16)
    ckvT_d = dscr("ckvT_d", [R, S], BF16)
    mixT_d = dscr("mixT_d", [MIXW, S], BF16)
    zs_d = dscr("zs_d", [S, D], F32)
    xsc_d = dscr("xsc_d", [S, D], BF16)
    bm_d = dscr("bm_d", [S, 256], BF16)
    bmT_d = dscr("bmT_d", [256, S], BF16)
    cmT_d = dscr("cmT_d", [256, S], BF16)
    dt_d = dscr("dt_d", [S, 32], F32)
    nmT_d = dscr("nmT_d", [S, S], BF16)
    gu_d = dscr("gu_d", [DFF, S], BF16)
    R_ = {n: kb.res(n) for n in ["xs", "qlat", "iq", "ik", "iw", "ckv", "ckvT", "mixT", "zs", "xsc", "bm", "bmT",
                                 "cmT", "dt", "nmT", "gu", "y", "xm"]}

    ps = [kb.stack.enter_context(nc.psum_tensor(f"ps{i}", [128, 512], F32)) for i in range(8)]
    psr = [kb.res(f"ps{i}") for i in range(8)]

    ident_f = kb.sb("ident_f", [128, 128], F32)
    ident = kb.sb("ident", [128, 128], BF16)
    ones_f = kb.sb("ones_f", [128, 128], F32)
    utri_f = kb.sb("utri_f", [128, 128], F32)
    tri_f = kb.sb("tri_f", [128, 128], F32)
    cres = kb.res("consts")
    kb.op(pool, lambda: G.memset(ident_f[:], 0.0), writes=[cres])
    kb.op(pool, lambda: G.affine_select(out=ident_f[:], in_=ident_f[:], pattern=[[-1, 128]], compare_op=ALU.not_equal,
                                        fill=1.0, base=0, channel_multiplier=1), reads=[cres], writes=[cres])
    kb.op(pool, lambda: G.tensor_copy(out=ident[:], in_=ident_f[:]), reads=[cres], writes=[cres])
    kb.op(pool, lambda: G.memset(ones_f[:], 1.0), writes=[cres])
    kb.op(pool, lambda: G.affine_select(out=utri_f[:], in_=ones_f[:], pattern=[[1, 128]], compare_op=ALU.is_ge,
                                        fill=0.0, base=0, channel_multiplier=-1), reads=[cres], writes=[cres])
    kb.op(pool, lambda: G.tensor_copy(out=tri_f[:], in_=utri_f[:]), reads=[cres], writes=[cres])

    eps_t = kb.sb("eps_t", [128, 1], F32)
    negb_t = kb.sb("negb_t", [128, 1], F32)
    kb.op(pool, lambda: G.memset(negb_t[:], NEG), writes=[cres])
    kb.op(pool, lambda: G.memset(eps_t[:], EPS), writes=[cres])
    HT = {}

    def bcast_load(dst, src_vec, n, rs):
        kb.dma(out=dst, in_=src_vec.partition_broadcast(128), writes=[rs], own=rs)

    def rms_rstd(dst, ssq, n, eng=None):
        V.tensor_scalar(out=dst, in0=ssq, scalar1=1.0 / n, scalar2=EPS, op0=ALU.mult, op1=ALU.add)

    all_sems = []
    _orig_new_sem = kb.new_sem

    def _new_sem(name="s"):
        s = _orig_new_sem(name)
        all_sems.append(s)
        return s
    kb.new_sem = _new_sem
    for e in (pe, act, dve, pool):
        all_sems.append(e.sem)

    def barrier():
        for e in (pe, act, dve, pool, sp):
            for s in all_sems:
                if s is e.sem or s.val == 0:
                    continue
                if e.seen.get(s, 0) >= s.val:
                    continue
                e.eng.wait_ge(s.h, s.val)
                e.seen[s] = s.val

    class Phase:
        def __enter__(self):
            self.es = ExitStack()
            self.es.__enter__()
            self.old = kb.stack
            kb.stack = self.es
            kb.phase_dsems.append([])
            return self

        def __exit__(self, *a):
            barrier()
            kb.free_dsems.extend(kb.phase_dsems.pop())
            kb.stack = self.old
            self.es.__exit__(None, None, None)
            return False

    def mm(out, lhsT, rhs, start, stop, reads, writes, inc=True):
        return kb.op(pe, lambda: T.matmul(out, lhsT=lhsT, rhs=rhs, start=start, stop=stop), reads=reads, writes=writes, inc=inc)

    def tp(out, in_, reads, writes, inc=True, idn=None):
        idn = ident if idn is None else idn
        k = in_.shape[0]
        return kb.op(pe, lambda: T.transpose(out, in_, idn[0:k, 0:k]), reads=list(reads) + [cres], writes=writes, inc=inc)

    def vop(fn, reads, writes):
        return kb.op(dve, fn, reads=reads, writes=writes)

    def aop(fn, reads, writes):
        return kb.op(act, fn, reads=reads, writes=writes)

    def gop(fn, reads, writes):
        return kb.op(pool, fn, reads=reads, writes=writes)

    def rstd(st, st_r, src, dst, n):
        aop(lambda: A.activation(out=st[:, 15:16], in_=st[:, src:src + 1], func=AF.Ln, scale=1.0 / n, bias=eps_t[:, 0:1]), [st_r, cres], [st_r])
        aop(lambda: A.activation(out=st[:, dst:dst + 1], in_=st[:, 15:16], func=AF.Exp, scale=-0.5), [st_r], [st_r])

    def norm_to_hT(xt, xr, g_bc, g_r, tt, tmp):
        junk, junk_r, stl, hb, hb_r = tmp
        st, st_r = stl.next()
        aop(lambda: A.activation(out=junk[:], in_=xt, func=AF.Square, accum_out=st[:, 0:1]), [xr], [junk_r, st_r])
        rstd(st, st_r, 0, 2, D)
        vop(lambda: V.scalar_tensor_tensor(out=hb[:], in0=xt, scalar=st[:, 2:3], in1=g_bc[:], op0=ALU.mult, op1=ALU.mult),
            [xr, st_r, g_r], [hb_r])
        for half in range(2):
            pb = 6 + half
            pv = ps[pb][:].bitcast(BF16)
            for j in range(4):
                dc = half * 4 + j
                tp(pv[:, j * 128:(j + 1) * 128], hb[:, dc * 128:(dc + 1) * 128], [hb_r], [psr[pb]], inc=(j == 3))
            src = pv[:, 0:512].rearrange("p (j t) -> p j t", j=4)
            dst = HT['t'][:, half * 4:(half + 1) * 4, tt * 128:(tt + 1) * 128]
            if half == 0:
                aop(lambda src=src, dst=dst: A.copy(out=dst, in_=src), [psr[pb]], [HT['r']])
            else:
                vop(lambda src=src, dst=dst: V.tensor_copy(out=dst, in_=src), [psr[pb]], [HT['r']])

    class HTScope(Phase):
        def __enter__(self):
            Phase.__enter__(self)
            HT['t'] = kb.sb("hT", [128, 8, S], BF16)
            HT['r'] = kb.res("hT")
            return self

    def phase0(l, src=None):
        src = x_in if src is None else src
        with Phase():
            g_bc = kb.sb("p0_g", [128, D], F32)
            g_r = kb.res("p0_g")
            bcast_load(g_bc[:], vecD[l, 0, :], D, g_r)
            xsl = Slots(kb, "p0_x", [128, D], F32, 2)
            junk = kb.sb("p0_junk", [128, D], F32)
            stl = Slots(kb, "p0_st", [128, 16], F32, 2)
            hb = kb.sb("p0_hb", [128, D], BF16)
            tmp = (junk, kb.res("p0_junk"), stl, hb, kb.res("p0_hb"))
            for tt in range(NT):
                xt, xr = xsl.next()
                kb.dma(out=xt[:], in_=src[tt * 128:(tt + 1) * 128, :], reads=[R_['xs']], writes=[xr], own=xr)
                norm_to_hT(xt[:], xr, g_bc, g_r, tt, tmp)

    class WStream:
        def __init__(self, name, KC, maxc, nbuf=2, nstg=2):
            self.KC = KC
            self.stg = Slots(kb, name + "_stg", [128, KC, 256], F32, nstg)
            self.wb = Slots(kb, name + "_wb", [128, KC, maxc], BF16, nbuf)

        def load(self, wap, c0, ncols, r0=0, kcn=None):
            kcn = kcn or self.KC
            wt, wr = self.wb.next()
            for p0 in range(0, ncols, 256):
                pn = min(256, ncols - p0)
                st, sr = self.stg.next()
                src = wap[r0:r0 + kcn * 128, :].rearrange("(kc p) c -> p kc c", p=128)[:, :, c0 + p0:c0 + p0 + pn]
                kb.dma(out=st[:, 0:kcn, 0:pn], in_=src, writes=[sr], own=sr)
                gop(lambda: G.tensor_copy(out=wt[:, 0:kcn, p0:p0 + pn], in_=st[:, 0:kcn, 0:pn]), [sr], [wr])
            return wt, wr

    class RR:
        def __init__(self, items):
            self.items = items
            self.i = 0

        def next(self):
            k = self.items[self.i % len(self.items)]
            self.i += 1
            return k

    def phase1(l):
        with Phase():
            hT, hT_r = HT['t'], HT['r']
            ws = WStream("w1", 8, 512, nbuf=3)
            c_r = kb.res("p1c")
            wuk_f = kb.sb("wuk_f", [128, 4, R], F32)
            wuk_b = kb.sb("wuk_b", [128, 4, R], BF16)
            kb.dma(out=wuk_f[:], in_=wukT[l], writes=[c_r], own=c_r)
            gop(lambda: G.tensor_copy(out=wuk_b[:], in_=wuk_f[:]), [c_r], [c_r])
            scw_t = kb.sb("scw_t", [128, 4, 3], F32)
            kb.dma(out=scw_t[:], in_=scw[l], writes=[c_r], own=c_r)
            sscw_t = kb.sb("sscw_t", [128, 12, 4], F32)
            kb.dma(out=sscw_t[:], in_=sscw[l], writes=[c_r], own=c_r)
            sscb_t = kb.sb("sscb_t", [128, 12], F32)
            kb.dma(out=sscb_t[:], in_=sscb[l], writes=[c_r], own=c_r)
            kvn_bc = kb.sb("kvn_bc", [128, R], F32)
            bcast_load(kvn_bc[:], kvn[l, :], R, c_r)
            ikg_bc = kb.sb("ikg_bc", [128, 64], F32)
            bcast_load(ikg_bc[:], ikg[l, :], 64, c_r)
            ikb_bc = kb.sb("ikb_bc", [128, 64], F32)
            bcast_load(ikb_bc[:], ikb[l, :], 64, c_r)
            v16_bc = kb.sb("v16_bc", [128, 3, 16], F32)
            kb.dma(out=v16_bc[:], in_=v16[l].partition_broadcast(128), writes=[c_r], own=c_r)
            A_bc = kb.sb("A_bc", [128, 16], F32)
            aop(lambda: A.activation(out=A_bc[:], in_=v16_bc[:, 1, :], func=AF.Exp), [c_r], [c_r])
            vop(lambda: V.tensor_scalar(out=A_bc[:], in0=A_bc[:], scalar1=-1.0, scalar2=None, op0=ALU.mult), [c_r], [c_r])

            bankA = RR([0, 1])
            bankB = RR([2, 3])
            o512 = Slots(kb, "p1_o512", [128, 512], BF16, 3)
            o512b = Slots(kb, "p1_o512b", [128, 512], BF16, 3)

            def fm_block(wt, wr, c, tb, ncols=128):
                b = bankA.next()
                for kc in range(8):
                    mm(ps[b][0:ncols, :], wt[:, kc, c * 128:c * 128 + ncols], hT[:, kc, tb * 512:(tb + 1) * 512],
                       kc == 0, kc == 7, [wr, hT_r], [psr[b]], inc=(kc == 7))
                return b

            wt, wr = ws.load(w_in[l], O_Q, 512)
            for c in range(4):
                for tb in range(NB):
                    b = fm_block(wt, wr, c, tb)
                    qs, qr = o512.next()
                    aop(lambda b=b, qs=qs: A.mul(out=qs[:], in_=ps[b][:], mul=0.125), [psr[b]], [qr])
                    for hh in range(2):
                        for rc in range(2):
                            b2 = bankB.next()
                            mm(ps[b2][:], wuk_b[hh * 64:(hh + 1) * 64, c, rc * 128:(rc + 1) * 128],
                               qs[hh * 64:(hh + 1) * 64, :], True, True, [c_r, qr], [psr[b2]])
                            ql, qlr = o512b.next()
                            vop(lambda b2=b2, ql=ql: V.tensor_copy(out=ql[:], in_=ps[b2][:]), [psr[b2]], [qlr])
                            kb.dma(out=qlat_d[2 * c + hh, rc * 128:(rc + 1) * 128, tb * 512:(tb + 1) * 512], in_=ql[:],
                                   reads=[qlr], writes=[R_["qlat"]], own=qlr)
            wt, wr = ws.load(w_in[l], O_IQ, 256)
            for c in range(2):
                for tb in range(NB):
                    b = fm_block(wt, wr, c, tb)
                    qs, qr = o512.next()
                    aop(lambda b=b, qs=qs: A.mul(out=qs[:], in_=ps[b][:], mul=0.125), [psr[b]], [qr])
                    kb.dma(out=iq_d[c * 128:(c + 1) * 128, tb * 512:(tb + 1) * 512], in_=qs[:], reads=[qr],
                           writes=[R_["iq"]], own=qr)
            cg = kb.sb("p1_cg", [128, S], F32)
            cg_r = kb.res("p1_cg")
            U = kb.sb("p1_U", [128, S + 4], F32)
            U_r = kb.res("p1_U")
            vop(lambda: V.memset(U[:, 0:4], 0.0), [], [U_r])
            wbg, wbg_r = ws.load(w_in[l], O_BG, 512)
            wcg, wcg_r = ws.load(w_in[l], O_CG, 512)
            whb, whb_r = ws.load(w_in[l], O_HB, 512)
            for c in range(4):
                for tb in range(NB):
                    b = fm_block(wcg, wcg_r, c, tb)
                    aop(lambda b=b, tb=tb: A.copy(out=cg[:, tb * 512:(tb + 1) * 512], in_=ps[b][:]), [psr[b]], [cg_r])
                for tb in range(NB):
                    b = fm_block(whb, whb_r, c, tb)
                    vop(lambda b=b, tb=tb: V.tensor_tensor(out=U[:, 4 + tb * 512:4 + (tb + 1) * 512], in0=ps[b][:],
                                                           in1=cg[:, tb * 512:(tb + 1) * 512], op=ALU.mult),
                        [psr[b], cg_r], [U_r])
                vop(lambda c=c: V.tensor_scalar(out=cg[:, :], in0=U[:, 2:S + 2], scalar1=scw_t[:, c, 0:1], scalar2=None,
                                                op0=ALU.mult), [U_r, c_r], [cg_r])
                for j in (1, 2):
                    vop(lambda c=c, j=j: V.scalar_tensor_tensor(out=cg[:, :], in0=U[:, 2 + j:S + 2 + j], scalar=scw_t[:, c, j:j + 1],
                                                                in1=cg[:, :], op0=ALU.mult, op1=ALU.add), [U_r, c_r, cg_r], [cg_r])
                for tb in range(NB):
                    b = fm_block(wbg, wbg_r, c, tb)
                    ob, obr = o512.next()
                    vop(lambda b=b, tb=tb, ob=ob: V.tensor_tensor(out=ob[:], in0=ps[b][:], in1=cg[:, tb * 512:(tb + 1) * 512],
                                                                  op=ALU.mult), [psr[b], cg_r], [obr])
                    kb.dma(out=mixT_d[512 + c * 128:512 + (c + 1) * 128, tb * 512:(tb + 1) * 512], in_=ob[:], reads=[obr],
                           writes=[R_["mixT"]], own=obr)
            sx = kb.sb("p1_sx", [128, S], BF16)
            sx_r = kb.res("p1_sx")
            U2 = kb.sb("p1_U2", [128, S + 4], F32)
            U2_r = kb.res("p1_U2")
            vop(lambda: V.memset(U2[:, 0:4], 0.0), [], [U2_r])
            Ua = [(U, U_r), (U2, U2_r)]
            tstg = Slots(kb, "p1_tstg", [128, 4, 128], BF16, 2)
            wts = {}

            def d_mm(cc):
                blk, c = divmod(cc, 4)
                if c == 0:
                    wts[blk] = ws.load(w_in[l], O_XBC + blk * 512, 512)
                wt, wr = wts[blk]
                U, U_r = Ua[cc % 2]
                for tb in range(NB):
                    b = fm_block(wt, wr, c, tb)
                    aop(lambda: A.copy(out=U[:, 4 + tb * 512:4 + (tb + 1) * 512], in_=ps[b][:]), [psr[b]], [U_r])

            def d_post(cc):
                U, U_r = Ua[cc % 2]
                vop(lambda: V.tensor_scalar(out=cg[:, :], in0=U[:, 1:S + 1], scalar1=sscw_t[:, cc, 0:1],
                                            scalar2=sscb_t[:, cc:cc + 1], op0=ALU.mult, op1=ALU.add), [U_r, c_r], [cg_r])
                for j in (1, 2, 3):
                    vop(lambda: V.scalar_tensor_tensor(out=cg[:, :], in0=U[:, 1 + j:S + 1 + j], scalar=sscw_t[:, cc, j:j + 1], in1=cg[:, :],
                                                       op0=ALU.mult, op1=ALU.add), [U_r, c_r, cg_r], [cg_r])
                aop(lambda: A.activation(out=sx[:, :], in_=cg[:, :], func=AF.Silu), [cg_r], [sx_r])
                if cc >= 8:
                    g = (cc - 8) % 2
                    dst = bmT_d if cc < 10 else cmT_d
                    kb.dma(out=dst[g * 128:(g + 1) * 128, :], in_=sx[:, :], reads=[sx_r],
                           writes=[R_["bmT" if cc < 10 else "cmT"]], own=sx_r)
                if cc < 10:
                    for t4 in range(NT // 4):
                        pb = bankB.next()
                        pv = ps[pb][:].bitcast(BF16)
                        for j in range(4):
                            tt = t4 * 4 + j
                            tp(pv[:, j * 128:(j + 1) * 128], sx[:, tt * 128:(tt + 1) * 128], [sx_r], [psr[pb]], inc=(j == 3))
                        tsg, tsr = tstg.next()
                        vop(lambda: V.tensor_copy(out=tsg[:], in_=pv[:, 0:512].rearrange("p (j c) -> p j c", j=4)), [psr[pb]], [tsr])
                        if cc < 8:
                            dst = xsc_d.rearrange("(tt p) c -> p tt c", p=128)[:, t4 * 4:(t4 + 1) * 4, cc * 128:(cc + 1) * 128]
                            rn = "xsc"
                        else:
                            dst = bm_d.rearrange("(tt p) c -> p tt c", p=128)[:, t4 * 4:(t4 + 1) * 4, (cc - 8) * 128:(cc - 7) * 128]
                            rn = "bm"
                        kb.dma(out=dst, in_=tsg[:], reads=[tsr], writes=[R_[rn]], own=tsr)

            d_mm(0)
            for cc in range(12):
                if cc + 1 < 12:
                    d_mm(cc + 1)
                d_post(cc)
            zsl = Slots(kb, "p1_z", [128, 512], F32, 3)
            for zb in range(2):
                wt, wr = ws.load(w_in[l], O_Z + zb * 512, 512)
                for tt in range(NT):
                    b = bankA.next()
                    for kc in range(8):
                        mm(ps[b][:], hT[:, kc, tt * 128:(tt + 1) * 128], wt[:, kc, 0:512], kc == 0, kc == 7, [wr, hT_r], [psr[b]],
                           inc=(kc == 7))
                    zt, zr = zsl.next()
                    aop(lambda b=b, zt=zt: A.activation(out=zt[:], in_=ps[b][:], func=AF.Silu), [psr[b]], [zr])
                    kb.dma(out=zs_d[tt * 128:(tt + 1) * 128, zb * 512:(zb + 1) * 512], in_=zt[:], reads=[zr], writes=[R_["zs"]], own=zr)
            wt, wr = ws.wb.next()
            wv = w_in[l].rearrange("(kc p) c -> p kc c", p=128)
            st_, sr_ = ws.stg.next()
            kb.dma(out=st_[:, :, 0:256], in_=wv[:, :, O_CKV:O_CKV + 256], writes=[sr_], own=sr_)
            gop(lambda: G.tensor_copy(out=wt[:, :, 0:256], in_=st_[:, :, 0:256]), [sr_], [wr])
            st_, sr_ = ws.stg.next()
            kb.dma(out=st_[:, :, 0:68], in_=wv[:, :, O_IK:O_IK + 68], writes=[sr_], own=sr_)
            kb.dma(out=st_[:, :, 68:84], in_=wv[:, :, O_DT:O_DT + 16], writes=[sr_], own=sr_)
            gop(lambda: G.tensor_copy(out=wt[:, :, 256:340], in_=st_[:, :, 0:84]), [sr_], [wr])
            stl = Slots(kb, "p1_st", [128, 16], F32, 2)
            junk = kb.sb("p1_junk", [128, 256], F32)
            junk_r = kb.res("p1_junk")
            cnl = Slots(kb, "p1_cn", [128, 256], BF16, 2)
            ctl = Slots(kb, "p1_ct", [128, 2, 128], BF16, 2)
            ikl = Slots(kb, "p1_ik", [128, 64], F32, 2)
            iknl = Slots(kb, "p1_ikn", [128, 64], BF16, 2)
            iktl = Slots(kb, "p1_ikt", [64, 128], BF16, 2)
            iwl = Slots(kb, "p1_iw", [128, 4], F32, 2)
            dtl = Slots(kb, "p1_dt", [128, 32], F32, 2)
            E5 = {}

            def e_mm(tt):
                b = bankA.next()
                for kc in range(8):
                    mm(ps[b][:, 0:340], hT[:, kc, tt * 128:(tt + 1) * 128], wt[:, kc, 0:340], kc == 0, kc == 7, [wr, hT_r], [psr[b]],
                       inc=(kc == 7))
                E5[tt] = b

            e_mm(0)
            for tt in range(NT):
                if tt + 1 < NT:
                    e_mm(tt + 1)
                b = E5.pop(tt)
                P = ps[b]
                st, str_ = stl.next()
                aop(lambda P=P, st=st: A.activation(out=junk[:, 0:256], in_=P[:, 0:256], func=AF.Square, accum_out=st[:, 0:1]),
                    [psr[b]], [junk_r, str_])
                rstd(st, str_, 0, 2, R)
                cn, cnr = cnl.next()
                vop(lambda P=P, st=st, cn=cn: V.scalar_tensor_tensor(out=cn[:], in0=P[:, 0:256], scalar=st[:, 2:3], in1=kvn_bc[:],
                                                                    op0=ALU.mult, op1=ALU.mult), [psr[b], str_, c_r], [cnr])
                kb.dma(out=ckv_d[tt * 128:(tt + 1) * 128, :], in_=cn[:], reads=[cnr], writes=[R_["ckv"]], own=cnr)
                pb = bankB.next()
                pv = ps[pb][:].bitcast(BF16)
                for rc in range(2):
                    tp(pv[:, rc * 128:(rc + 1) * 128], cn[:, rc * 128:(rc + 1) * 128], [cnr], [psr[pb]], inc=(rc == 1))
                ct, ctr = ctl.next()
                aop(lambda pv=pv, ct=ct: A.copy(out=ct[:], in_=pv[:, 0:256].rearrange("p (j c) -> p j c", j=2)), [psr[pb]], [ctr])
                kb.dma(out=ckvT_d.rearrange("(rc p) s -> p rc s", p=128)[:, :, tt * 128:(tt + 1) * 128], in_=ct[:], reads=[ctr],
                       writes=[R_["ckvT"]], own=ctr)
                vop(lambda P=P, st=st: V.tensor_reduce(out=st[:, 4:5], in_=P[:, 256:320], axis=AX.X, op=ALU.add), [psr[b]], [str_])
                aop(lambda P=P, st=st: A.activation(out=junk[:, 0:64], in_=P[:, 256:320], func=AF.Square, accum_out=st[:, 5:6]),
                    [psr[b]], [junk_r, str_])
                vop(lambda st=st: V.tensor_scalar(out=st[:, 6:7], in0=st[:, 4:5], scalar1=1.0 / 64, scalar2=None, op0=ALU.mult), [str_], [str_])
                vop(lambda st=st: V.tensor_tensor(out=st[:, 7:8], in0=st[:, 6:7], in1=st[:, 6:7], op=ALU.mult), [str_], [str_])
                vop(lambda st=st: V.scalar_tensor_tensor(out=st[:, 8:9], in0=st[:, 5:6], scalar=1.0 / 64, in1=st[:, 7:8],
                                                         op0=ALU.mult, op1=ALU.subtract), [str_], [str_])
                rstd(st, str_, 8, 9, 1)
                ik, ikr = ikl.next()
                vop(lambda P=P, st=st, ik=ik: V.tensor_scalar(out=ik[:], in0=P[:, 256:320], scalar1=st[:, 6:7], scalar2=st[:, 9:10],
                                                              op0=ALU.subtract, op1=ALU.mult), [psr[b], str_], [ikr])
                vop(lambda ik=ik: V.tensor_tensor(out=ik[:], in0=ik[:], in1=ikg_bc[:], op=ALU.mult), [ikr, c_r], [ikr])
                ikn, iknr = iknl.next()
                vop(lambda ik=ik, ikn=ikn: V.tensor_tensor(out=ikn[:], in0=ik[:], in1=ikb_bc[:], op=ALU.add), [ikr, c_r], [iknr])
                pb = bankB.next()
                pv = ps[pb][:].bitcast(BF16)
                tp(pv[0:64, 0:128], ikn[:, 0:64], [iknr], [psr[pb]])
                ikt, iktr = iktl.next()
                aop(lambda pv=pv, ikt=ikt: A.copy(out=ikt[:], in_=pv[0:64, 0:128]), [psr[pb]], [iktr])
                kb.dma(out=ik_d[:, tt * 128:(tt + 1) * 128], in_=ikt[:], reads=[iktr], writes=[R_["ik"]], own=iktr)
                iwt, iwr = iwl.next()
                aop(lambda P=P, iwt=iwt: A.mul(out=iwt[:], in_=P[:, 320:324], mul=0.5), [psr[b]], [iwr])
                kb.dma(out=iw_d[tt * 128:(tt + 1) * 128, :], in_=iwt[:], reads=[iwr], writes=[R_["iw"]], own=iwr)
                dtt, dtr = dtl.next()
                vop(lambda P=P, dtt=dtt: V.tensor_tensor(out=dtt[:, 16:32], in0=P[:, 324:340], in1=v16_bc[:, 0, :], op=ALU.add),
                    [psr[b], c_r], [dtr])
                aop(lambda dtt=dtt: A.activation(out=dtt[:, 16:32], in_=dtt[:, 16:32], func=AF.Exp), [dtr], [dtr])
                aop(lambda dtt=dtt: A.activation(out=dtt[:, 0:16], in_=dtt[:, 16:32], func=AF.Ln, bias=1.0, scale=1.0), [dtr], [dtr])
                vop(lambda dtt=dtt: V.tensor_tensor(out=dtt[:, 16:32], in0=dtt[:, 0:16], in1=A_bc[:], op=ALU.mult), [dtr, c_r], [dtr])
                kb.dma(out=dt_d[tt * 128:(tt + 1) * 128, :], in_=dtt[:], reads=[dtr], writes=[R_["dt"]], own=dtr)

    NIT = 16

    def gen2(l):
        c_r = kb.res("p2c")
        iqT = kb.sb("p2_iqT", [128, 2, S], BF16)
        ikT = kb.sb("p2_ikT", [128, S], BF16)
        iwa = kb.sb("p2_iw", [128, NT, 4], F32)
        kb.dma(out=iqT[:], in_=iq_d.rearrange("(c p) s -> p c s", p=128), reads=[R_["iq"]], writes=[c_r], own=c_r)
        kb.dma(out=ikT[0:64, :], in_=ik_d, reads=[R_["ik"]], writes=[c_r], own=c_r)
        kb.dma(out=ikT[64:128, :], in_=ik_d, reads=[R_["ik"]], writes=[c_r], own=c_r)
        for t0_ in range(0, NT, 8):
            t1_ = min(NT, t0_ + 8)
            kb.dma(out=iwa[:, t0_:t1_, :], in_=iw_d.rearrange("(t p) h -> p t h", p=128)[:, t0_:t1_, :], reads=[R_["iw"]], writes=[c_r], own=c_r)
        pw2 = kb.sb("p2_pw2", [128, NIT], F32)
        for k in range(NIT):
            gop(lambda: G.memset(pw2[:, k:k + 1], 2.0 ** (-(k + 1))), [], [c_r])
        SCl = Slots(kb, "p2_SC", [128, S], F32, 2)
        mkl = Slots(kb, "p2_mk", [128, S], BF16, 3)
        Rl = Slots(kb, "p2_R", [128, 512], F32, 4)
        bl = Slots(kb, "p2_b", [128, 8 + 2 * NIT], F32, 4)
        nml = Slots(kb, "p2_nm", [128, 4, 128], BF16, 3)
        bankA = RR([0, 1])
        bankT = RR([2])
        yield

        def scores(qt, out):
            N = (qt + 1) * 128
            SC, SC_r = SCl.next()
            for sb in range((N + 511) // 512):
                cols = min(512, N - sb * 512)
                cs = slice(sb * 512, sb * 512 + cols)
                for h in range(4):
                    c, hh = divmod(h, 2)
                    pr_ = slice(hh * 64, (hh + 1) * 64)
                    b = bankA.next()
                    mm(ps[b][:, 0:cols], iqT[pr_, c, qt * 128:(qt + 1) * 128], ikT[pr_, cs], True, True, [c_r], [psr[b]])
                    Rt, Rr = Rl.next()
                    aop(lambda: A.activation(out=Rt[:, 0:cols], in_=ps[b][:, 0:cols], func=AF.Relu), [psr[b]], [Rr])
                    if h == 0:
                        vop(lambda: V.tensor_scalar(out=SC[:, cs], in0=Rt[:, 0:cols], scalar1=iwa[:, qt, 0:1], scalar2=None, op0=ALU.mult),
                            [Rr, c_r], [SC_r])
                    else:
                        vop(lambda: V.scalar_tensor_tensor(out=SC[:, cs], in0=Rt[:, 0:cols], scalar=iwa[:, qt, h:h + 1], in1=SC[:, cs],
                                                           op0=ALU.mult, op1=ALU.add), [Rr, c_r, SC_r], [SC_r])
                yield
            bt, b_r = bl.next()
            mk, mk_r = mkl.next()
            vop(lambda: V.tensor_reduce(out=bt[:, 0:1], in_=SC[:, 0:N], axis=AX.X, op=ALU.max), [SC_r], [b_r])
            vop(lambda: V.tensor_reduce(out=bt[:, 1:2], in_=SC[:, 0:N], axis=AX.X, op=ALU.min), [SC_r], [b_r])
            vop(lambda: V.memset(SC[0:64, N - 64:N], -1.0e30), [], [SC_r])
            vop(lambda: V.tensor_scalar(out=bt[:, 2:3], in0=bt[:, 1:2], scalar1=-0.01, scalar2=None, op0=ALU.add), [b_r], [b_r])
            vop(lambda: V.scalar_tensor_tensor(out=bt[:, 3:4], in0=bt[:, 0:1], scalar=0.02, in1=bt[:, 1:2], op0=ALU.add, op1=ALU.subtract),
                [b_r], [b_r])
            vop(lambda: V.tensor_scalar(out=bt[:, 8:8 + NIT], in0=pw2[:], scalar1=bt[:, 3:4], scalar2=None, op0=ALU.mult), [b_r, c_r], [b_r])
            vop(lambda: V.memset(bt[:, 8 + NIT:8 + 2 * NIT], 0.0), [], [b_r])
            out.update(dict(qt=qt, N=N, SC=SC, SC_r=SC_r, bt=bt, b_r=b_r, mk=mk, mk_r=mk_r))
            yield

        def first_mid(st, on_act):
            bt, b_r = st["bt"], st["b_r"]
            if on_act:
                vop(lambda: V.scalar_tensor_tensor(out=bt[:, 4:5], in0=bt[:, 2:3], scalar=-1.0, in1=bt[:, 8:9], op0=ALU.mult, op1=ALU.subtract),
                    [b_r], [b_r])
            else:
                vop(lambda: V.tensor_tensor(out=bt[:, 4:5], in0=bt[:, 2:3], in1=bt[:, 8:9], op=ALU.add), [b_r], [b_r])

        def iteration(st, k, on_act):
            N, SC, SC_r, bt, b_r = st["N"], st["SC"], st["SC_r"], st["bt"], st["b_r"]
            jt, jr = st["mk"], st["mk_r"]
            ck = 8 + NIT + k
            if on_act:
                aop(lambda: A.activation(out=jt[:, 0:N], in_=SC[:, 0:N], func=AF.Sign, bias=bt[:, 4:5], scale=1.0,
                                         accum_out=bt[:, ck:ck + 1]), [SC_r, b_r], [jr, b_r])
                thr = 511.0 - N
            else:
                vop(lambda: V.tensor_scalar(out=jt[:, 0:N], in0=SC[:, 0:N], scalar1=bt[:, 4:5], scalar2=0.0, op0=ALU.is_ge, op1=ALU.add,
                                            accum_out=bt[:, ck:ck + 1]), [SC_r, b_r], [jr, b_r])
                thr = 255.5
            vop(lambda: V.scalar_tensor_tensor(out=bt[:, 5:6], in0=bt[:, ck:ck + 1], scalar=thr, in1=bt[:, 8 + k:9 + k],
                                               op0=ALU.is_ge, op1=ALU.mult), [b_r], [b_r])
            vop(lambda: V.tensor_tensor(out=bt[:, 2:3], in0=bt[:, 2:3], in1=bt[:, 5:6], op=ALU.add), [b_r], [b_r])
            if k + 1 < NIT:
                if on_act:
                    vop(lambda: V.scalar_tensor_tensor(out=bt[:, 4:5], in0=bt[:, 2:3], scalar=-1.0, in1=bt[:, 9 + k:10 + k],
                                                       op0=ALU.mult, op1=ALU.subtract), [b_r], [b_r])
                else:
                    vop(lambda: V.tensor_tensor(out=bt[:, 4:5], in0=bt[:, 2:3], in1=bt[:, 9 + k:10 + k], op=ALU.add), [b_r], [b_r])

        def finalize(st):
            qt, N, SC, SC_r, bt, b_r, mk, mk_r = st["qt"], st["N"], st["SC"], st["SC_r"], st["bt"], st["b_r"], st["mk"], st["mk_r"]
            vop(lambda: V.tensor_scalar(out=mk[:, 0:N], in0=SC[:, 0:N], scalar1=bt[:, 2:3], scalar2=None, op0=ALU.is_ge), [SC_r, b_r], [mk_r])
            for k0 in range(0, qt + 1, 4):
                n = min(4, qt + 1 - k0)
                pb = bankT.next()
                pv = ps[pb][:].bitcast(BF16)
                for j in range(n):
                    tp(pv[:, j * 128:(j + 1) * 128], mk[:, (k0 + j) * 128:(k0 + j + 1) * 128], [mk_r], [psr[pb]], inc=(j == n - 1))
                nm, nm_r = nml.next()
                aop(lambda: A.copy(out=nm[:, 0:n, :], in_=pv[:, 0:n * 128].rearrange("p (j t) -> p j t", j=n)), [psr[pb]], [nm_r])
                kb.dma(out=nmT_d.rearrange("(kt p) t -> p kt t", p=128)[:, k0:k0 + n, qt * 128:(qt + 1) * 128], in_=nm[:, 0:n, :],
                       reads=[nm_r], writes=[R_["nmT"]], own=nm_r)
                yield

        for q0 in range(0, NT, 2):
            sa, sb_ = {}, {}
            yield from scores(q0, sa)
            yield from scores(q0 + 1, sb_)
            first_mid(sa, False)
            first_mid(sb_, True)
            for k in range(NIT):
                iteration(sa, k, False)
                iteration(sb_, k, True)
                yield
            yield from finalize(sa)
            yield from finalize(sb_)

    def run_interleaved(gens, weights=None):
        gens = list(gens)
        weights = list(weights or [1] * len(gens))
        acc = [0.0] * len(gens)
        alive = [True] * len(gens)
        while any(alive):
            for i, g in enumerate(gens):
                if not alive[i]:
                    continue
                acc[i] += weights[i]
                while acc[i] >= 1.0 and alive[i]:
                    acc[i] -= 1.0
                    try:
                        next(g)
                    except StopIteration:
                        alive[i] = False

    def phase2(l):
        with Phase():
            run_interleaved([gen2(l)])

    def phase24(l):
        with Phase():
            run_interleaved([gen2(l), gen4(l)], [1.0, 5.5])

    def phase3(l):
        with Phase():
            c_r = kb.res("p3c")
            ckvT = kb.sb("p3_ckvT", [128, 2, S], BF16)
            kb.dma(out=ckvT[:], in_=ckvT_d.rearrange("(rc p) s -> p rc s", p=128), reads=[R_["ckvT"]], writes=[c_r], own=c_r)
            ckva = kb.sb("p3_ckva", [128, NT, 258], BF16)
            gop(lambda: G.memset(ckva[:, :, 256:258], 1.0), [], [c_r])
            for t0_ in range(0, NT, 8):
                t1_ = min(NT, t0_ + 8)
                kb.dma(out=ckva[:, t0_:t1_, 0:256], in_=ckv_d.rearrange("(kt p) r -> p kt r", p=128)[:, t0_:t1_, :], reads=[R_["ckv"]], writes=[c_r], own=c_r)
            gt = kb.sb("p3_gt", [128, NH, GW], BF16)
            b15 = kb.sb("p3_b15", [128, NH], F32)
            gstg = Slots(kb, "p3_gstg", [128, GW], F32, 2)
            for h in range(NH):
                st, sr = gstg.next()
                kb.dma(out=st[:], in_=gtab[:, h, :], writes=[sr], own=sr)
                gop(lambda st=st, h=h: G.tensor_copy(out=gt[:, h, :], in_=st[:]), [sr], [c_r])
                gop(lambda st=st, h=h: G.tensor_copy(out=b15[:, h:h + 1], in_=st[:, GW - 1:GW]), [sr], [c_r])
            wuv_f = kb.sb("p3_wuvf", [128, 2, NH * 64], F32)
            wuv_b = kb.sb("p3_wuvb", [128, 2, NH * 64], BF16)
            kb.dma(out=wuv_f[:], in_=wuv[l], writes=[c_r], own=c_r)
            gop(lambda: G.tensor_copy(out=wuv_b[:], in_=wuv_f[:]), [c_r], [c_r])
            nml = Slots(kb, "p3_nm", [128, NT, 512], BF16, 2)
            ql = Slots(kb, "p3_q", [128, 2, 512], BF16, 3)
            El = Slots(kb, "p3_E", [128, 512], BF16, 3)
            PTl = Slots(kb, "p3_PT", [128, 512], BF16, 3)
            rsl = Slots(kb, "p3_rs", [128, 4], F32, 2)
            ol = Slots(kb, "p3_o", [128, 256], BF16, 8)
            oTl = Slots(kb, "p3_oT", [128, 2, 128], BF16, 2)
            aTl = Slots(kb, "p3_aT", [64, 512], BF16, 3)
            bankS = RR([0, 1, 2])
            nmv = nmT_d.rearrange("(kt p) t -> p kt t", p=128)
            nm_tiles = {}
            q_tiles = {}

            def load_nm(tb):
                if tb >= NB or tb in nm_tiles:
                    return
                nm, nm_r = nml.next()
                for t0_ in range(0, 4 * tb, 8):
                    t1_ = min(4 * tb, t0_ + 8)
                    kb.dma(out=nm[:, t0_:t1_, :], in_=nmv[:, t0_:t1_, tb * 512:(tb + 1) * 512], reads=[R_["nmT"]], writes=[nm_r], own=nm_r)
                for i in range(4):
                    kb.dma(out=nm[:, 4 * tb + i, i * 128:512], in_=nmv[:, 4 * tb + i, tb * 512 + i * 128:(tb + 1) * 512], reads=[R_["nmT"]],
                           writes=[nm_r], own=nm_r)
                nm_tiles[tb] = (nm, nm_r)

            def load_q(idx):
                if idx >= NB * NH or idx in q_tiles:
                    return
                tb, h = divmod(idx, NH)
                q, q_r = ql.next()
                kb.dma(out=q[:], in_=qlat_d[h].rearrange("(rc p) s -> p rc s", p=128)[:, :, tb * 512:(tb + 1) * 512], reads=[R_["qlat"]],
                       writes=[q_r], own=q_r)
                q_tiles[idx] = (q, q_r)

            groups = [(tb, h, kt) for tb in range(NB) for h in range(NH) for kt in range(4 * tb + 4)]
            sbank = {}

            def emit_qk(g):
                tb, h, kt = g
                if kt == 0:
                    load_nm(tb)
                    load_q(tb * NH + h)
                    load_q(tb * NH + h + 1)
                q, q_r = q_tiles[tb * NH + h]
                nm, nm_r = nm_tiles[tb]
                cl = max(0, kt - 4 * tb) * 128
                b = bankS.next()
                sbank[g] = b
                ks = slice(kt * 128, (kt + 1) * 128)
                c0 = tb * 512 - kt * 128 + 384
                const = c0 >= GC
                mm(ps[b][:, cl:512], ckvT[:, 0, ks], q[:, 0, cl:512], True, False, [c_r, q_r], [psr[b]], inc=False)
                mm(ps[b][:, cl:512], ckvT[:, 1, ks], q[:, 1, cl:512], False, const, [c_r, q_r], [psr[b]], inc=const)
                if not const:
                    mm(ps[b][:, cl:512], ident[:], gt[:, h, c0 + cl:c0 + 512], False, True, [cres, c_r], [psr[b]])

            def emit_rest(g):
                tb, h, kt = g
                if h == 0 and kt == 0:
                    load_nm(tb + 1)
                nm, nm_r = nm_tiles[tb]
                i = max(0, kt - 4 * tb)
                cl = i * 128
                b = sbank.pop(g)
                const = (tb * 512 - kt * 128 + 384) >= GC
                E, E_r = El.next()
                if const:
                    aop(lambda: A.activation(out=E[:, cl:512], in_=ps[b][:, cl:512], func=AF.Exp, bias=b15[:, h:h + 1], scale=1.0),
                        [psr[b], c_r], [E_r])
                else:
                    aop(lambda: A.activation(out=E[:, cl:512], in_=ps[b][:, cl:512], func=AF.Exp), [psr[b]], [E_r])
                PT, PT_r = PTl.next()
                vop(lambda: V.tensor_tensor(out=PT[:, cl:512], in0=E[:, cl:512], in1=nm[:, kt, cl:512], op=ALU.mult), [E_r, nm_r], [PT_r])
                for j in range(i, 4):
                    mm(ps[4 + j][:, 0:257], PT[:, j * 128:(j + 1) * 128], ckva[:, kt, 0:257], kt == 0, kt == 4 * tb + j, [PT_r, c_r],
                       [psr[4 + j]], inc=(j == 3 or kt == 4 * tb + j))

            def emit_post_head(tb, h):
                tiles = []
                for j in range(4):
                    rs, rs_r = rsl.next()
                    vop(lambda: V.reciprocal(out=rs[:, 0:1], in_=ps[4 + j][:, 256:257]), [psr[4 + j]], [rs_r])
                    o, o_r = ol.next()
                    aop(lambda: A.activation(out=o[:], in_=ps[4 + j][:, 0:256], func=AF.Identity, scale=rs[:, 0:1]),
                        [psr[4 + j], rs_r], [o_r])
                    tiles.append((o, o_r))
                q_tiles.pop(tb * NH + h, None)
                if h == NH - 1:
                    nm_tiles.pop(tb, None)
                return tiles

            def post_gen(tb, h, tiles):
                aT, aT_r = aTl.next()
                for j, (o, o_r) in enumerate(tiles):
                    pv = ps[3][:].bitcast(BF16)
                    for rc in range(2):
                        tp(pv[:, rc * 128:(rc + 1) * 128], o[:, rc * 128:(rc + 1) * 128], [o_r], [psr[3]], inc=(rc == 1))
                    yield
                    oT, oT_r = oTl.next()
                    vop(lambda: V.tensor_copy(out=oT[:], in_=pv[:, 0:256].rearrange("p (j t) -> p j t", j=2)), [psr[3]], [oT_r])
                    yield
                    for rc in range(2):
                        mm(ps[3][0:64, 256:384], wuv_b[:, rc, h * 64:(h + 1) * 64], oT[:, rc, :], rc == 0, rc == 1, [c_r, oT_r], [psr[3]],
                           inc=(rc == 1))
                    yield
                    vop(lambda: V.tensor_copy(out=aT[:, j * 128:(j + 1) * 128], in_=ps[3][0:64, 256:384]), [psr[3]], [aT_r])
                    yield
                kb.dma(out=mixT_d[h * 64:(h + 1) * 64, tb * 512:(tb + 1) * 512], in_=aT[:], reads=[aT_r], writes=[R_["mixT"]], own=aT_r)

            def drain(gen_):
                if gen_ is not None:
                    for _ in gen_:
                        pass

            pending = None
            emit_qk(groups[0])
            for idx, g in enumerate(groups):
                if idx + 1 < len(groups):
                    emit_qk(groups[idx + 1])
                emit_rest(g)
                tb, h, kt = g
                if pending is not None:
                    nsteps = -(-17 // (4 * tb + 4))
                    for _ in range(nsteps):
                        try:
                            next(pending)
                        except StopIteration:
                            pending = None
                            break
                if kt == 4 * tb + 3:
                    drain(pending)
                    tiles = emit_post_head(tb, h)
                    pending = post_gen(tb, h, tiles)
            drain(pending)

    def gen4(l):
        if True:
            c_r = kb.res("p4c")
            negtri4 = kb.sb("negtri4", [128, 4, 128], F32)
            gop(lambda: G.memset(negtri4[:], 0.0), [], [c_r])
            for j in range(4):
                gop(lambda j=j: G.affine_select(out=negtri4[:, j, :], in_=negtri4[:, j, :], pattern=[[1, 128]],
                                                compare_op=ALU.is_ge, fill=NEG, base=0, channel_multiplier=-1), [c_r], [c_r])
            v16_bc = kb.sb("p4_v16", [128, 3, 16], F32)
            kb.dma(out=v16_bc[:], in_=v16[l].partition_broadcast(128), writes=[c_r], own=c_r)
            gn = kb.sb("p4_gn", [128, D], F32)
            bcast_load(gn[:], vecD[l, 4, :], D, c_r)
            ST = kb.sb("p4_ST", [128, 2, 512], F32)
            ST_r = kb.res("p4_ST")
            STb = kb.sb("p4_STb", [128, 2, 512], BF16)
            STb_r = kb.res("p4_STb")
            vop(lambda: V.memset(ST[:], 0.0), [], [ST_r])
            vop(lambda: V.memset(STb[:], 0.0), [], [STb_r])
            xsl = Slots(kb, "p4_xs", [128, 16, 64], BF16, 2)
            bml = Slots(kb, "p4_bm", [128, 256], BF16, 2)
            bmTl = Slots(kb, "p4_bmT", [128, 2, 128], BF16, 2)
            cmTl = Slots(kb, "p4_cmT", [128, 2, 128], BF16, 2)
            dtl = Slots(kb, "p4_dt", [128, 32], F32, 2)
            zl = Slots(kb, "p4_z", [128, D], F32, 2)
            sml = Slots(kb, "p4_sm", [128, 6, 16], F32, 2)
            xdtl = Slots(kb, "p4_xdt", [128, 16, 64], BF16, 2)
            xdwl = Slots(kb, "p4_xdw", [128, 16, 64], BF16, 2)
            CBm = kb.sb("p4_CBm", [128, 2, 128], F32)
            CBm_r = kb.res("p4_CBm")
            rhsD = kb.sb("p4_rhsD", [128, 16, 128], F32)
            rhsD_r = kb.res("p4_rhsD")
            L = kb.sb("p4_L", [128, 16, 128], F32)
            L_r = kb.res("p4_L")
            M = kb.sb("p4_M", [128, 16, 128], BF16)
            M_r = kb.res("p4_M")
            Y = kb.sb("p4_Y", [128, 16, 64], F32)
            Y_r = kb.res("p4_Y")
            tmpY = kb.sb("p4_tmpY", [128, 16, 64], F32)
            tmpY_r = kb.res("p4_tmpY")
            Yn = kb.sb("p4_Yn", [128, D], BF16)
            Yn_r = kb.res("p4_Yn")
            stl = Slots(kb, "p4_st", [128, 16], F32, 2)
            cTl = Slots(kb, "p4_cT", [128, 8, 128], BF16, 2)
            bankD = RR([4])
            stA = {}

            def stageA(ct):
                rows = slice(ct * 128, (ct + 1) * 128)
                yb = (5, 6)
                xdt, xdt_r = xdtl.next()
                xdw, xdw_r = xdwl.next()
                xs, xs_r = xsl.next()
                kb.dma(out=xs[:].rearrange("p h d -> p (h d)"), in_=xsc_d[rows, :], reads=[R_["xsc"]], writes=[xs_r], own=xs_r)
                bm, bm_r = bml.next()
                kb.dma(out=bm[:], in_=bm_d[rows, :], reads=[R_["bm"]], writes=[bm_r], own=bm_r)
                bmT, bmT_r = bmTl.next()
                kb.dma(out=bmT[:], in_=bmT_d.rearrange("(g n) s -> n g s", g=2)[:, :, rows], reads=[R_["bmT"]], writes=[bmT_r], own=bmT_r)
                cmT, cmT_r = cmTl.next()
                kb.dma(out=cmT[:], in_=cmT_d.rearrange("(g n) s -> n g s", g=2)[:, :, rows], reads=[R_["cmT"]], writes=[cmT_r], own=cmT_r)
                dtt, dt_r = dtl.next()
                kb.dma(out=dtt[:], in_=dt_d[rows, :], reads=[R_["dt"]], writes=[dt_r], own=dt_r)
                zt, z_r = zl.next()
                kb.dma(out=zt[:], in_=zs_d[rows, :], reads=[R_["zs"]], writes=[z_r], own=z_r)
                sm, sm_r = sml.next()
                mm(ps[3][:, 0:16], utri_f[:], dtt[:, 16:32], True, True, [cres, dt_r], [psr[3]])
                yield
                mm(ps[3][:, 16:32], ones_f[:], dtt[:, 16:32], True, True, [cres, dt_r], [psr[3]])
                yield
                aop(lambda sm=sm: A.copy(out=sm[:, 0, :], in_=ps[3][:, 0:16]), [psr[3]], [sm_r])
                yield
                aop(lambda sm=sm: A.mul(out=sm[:, 1, :], in_=ps[3][:, 0:16], mul=-1.0), [psr[3]], [sm_r])
                yield
                aop(lambda sm=sm: A.activation(out=sm[:, 2, :], in_=ps[3][:, 0:16], func=AF.Exp), [psr[3]], [sm_r])
                yield
                aop(lambda sm=sm: A.activation(out=sm[:, 4, :], in_=ps[3][:, 16:32], func=AF.Exp), [psr[3]], [sm_r])
                yield
                vop(lambda sm=sm: V.tensor_tensor(out=sm[:, 5, :], in0=ps[3][:, 16:32], in1=sm[:, 0, :], op=ALU.subtract), [psr[3], sm_r], [sm_r])
                yield
                aop(lambda sm=sm: A.activation(out=sm[:, 3, :], in_=sm[:, 5, :], func=AF.Exp), [sm_r], [sm_r])
                yield
                vop(lambda xs=xs, dtt=dtt: V.tensor_tensor(out=xdt[:], in0=xs[:], in1=dtt[:, 0:16].unsqueeze(2).to_broadcast([128, 16, 64]),
                                                           op=ALU.mult), [xs_r, dt_r], [xdt_r])
                yield
                gop(lambda sm=sm: G.tensor_tensor(out=xdw[:], in0=xdt[:], in1=sm[:, 3, :].unsqueeze(2).to_broadcast([128, 16, 64]),
                                                  op=ALU.mult), [xdt_r, sm_r], [xdw_r])
                yield
                for g in range(2):
                    mm(ps[3][:, 64 + g * 128:64 + (g + 1) * 128], bmT[:, g, :], cmT[:, g, :], True, True, [bmT_r, cmT_r], [psr[3]], inc=(g == 1))
                    yield
                vop(lambda: V.tensor_tensor(out=CBm[:], in0=ps[3][:, 64:320].rearrange("p (g l) -> p g l", g=2),
                                            in1=tri_f[:].unsqueeze(1).to_broadcast([128, 2, 128]), op=ALU.mult), [psr[3], cres], [CBm_r])
                yield
                gop(lambda sm=sm: G.tensor_tensor(out=rhsD[:], in0=ident_f[:].unsqueeze(1).to_broadcast([128, 16, 128]),
                                                  in1=sm[:, 0, :].unsqueeze(2).to_broadcast([128, 16, 128]), op=ALU.mult),
                    [cres, sm_r], [rhsD_r])
                yield
                for q in range(4):
                    b = bankD.next()
                    mm(ps[b][:], ones_f[:], rhsD[:, 4 * q:4 * q + 4, :].rearrange("p h l -> p (h l)"), True, False, [cres, rhsD_r], [psr[b]],
                       inc=False)
                    yield
                    mm(ps[b][:], ident_f[:], negtri4[:].rearrange("p h l -> p (h l)"), False, True, [cres, c_r], [psr[b]])
                    yield
                    for hq in range(4):
                        h = 4 * q + hq
                        aop(lambda b=b, hq=hq, h=h, sm=sm: A.activation(out=L[:, h, :], in_=ps[b][:, hq * 128:(hq + 1) * 128], func=AF.Exp,
                                                                       bias=sm[:, 1, h:h + 1], scale=1.0), [psr[b], sm_r], [L_r])
                        yield
                for g in range(2):
                    gop(lambda g=g: G.tensor_tensor(out=M[:, g * 8:(g + 1) * 8, :], in0=L[:, g * 8:(g + 1) * 8, :],
                                                    in1=CBm[:, g, :].unsqueeze(1).to_broadcast([128, 8, 128]), op=ALU.mult),
                        [L_r, CBm_r], [M_r])
                    yield
                for h in range(16):
                    b = yb[h // 8]
                    hh = h % 8
                    mm(ps[b][:, hh * 64:(hh + 1) * 64], M[:, h, :], xdt[:, h, :], True, True, [M_r, xdt_r], [psr[b]], inc=(hh == 7))
                    yield
                stA[ct] = dict(rows=rows, yb=yb, xs=xs, xs_r=xs_r, bm=bm, bm_r=bm_r, cmT=cmT, cmT_r=cmT_r, zt=zt, z_r=z_r, sm=sm, sm_r=sm_r,
                               xdw=xdw, xdw_r=xdw_r)

            def stageB(ct):
                d = stA.pop(ct)
                rows, yb, xs, xs_r, bm, bm_r, cmT, cmT_r = d["rows"], d["yb"], d["xs"], d["xs_r"], d["bm"], d["bm_r"], d["cmT"], d["cmT_r"]
                zt, z_r, sm, sm_r, xdw, xdw_r = d["zt"], d["z_r"], d["sm"], d["sm_r"], d["xdw"], d["xdw_r"]
                yield
                for g in range(2):
                    mm(ps[7][:], cmT[:, g, :], STb[:, g, :], True, True, [cmT_r, STb_r], [psr[7]])
                    yield
                    vop(lambda g=g, sm=sm: V.tensor_tensor(out=Y[:, g * 8:(g + 1) * 8, :], in0=ps[7][:].rearrange("p (h d) -> p h d", h=8),
                                                           in1=sm[:, 2, g * 8:(g + 1) * 8].unsqueeze(2).to_broadcast([128, 8, 64]),
                                                           op=ALU.mult), [psr[7], sm_r], [Y_r])
                    yield
                    vop(lambda g=g: V.tensor_tensor(out=Y[:, g * 8:(g + 1) * 8, :], in0=Y[:, g * 8:(g + 1) * 8, :],
                                                    in1=ps[yb[g]][:].rearrange("p (h d) -> p h d", h=8), op=ALU.add),
                        [Y_r, psr[yb[g]]], [Y_r])
                    yield
                gop(lambda xs=xs: G.tensor_tensor(out=tmpY[:], in0=xs[:], in1=v16_bc[:, 2, :].unsqueeze(2).to_broadcast([128, 16, 64]),
                                                  op=ALU.mult), [xs_r, c_r], [tmpY_r])
                yield
                gop(lambda: G.tensor_tensor(out=Y[:], in0=Y[:], in1=tmpY[:], op=ALU.add), [Y_r, tmpY_r], [Y_r])
                yield
                for g in range(2):
                    mm(ps[7][:], bm[:, g * 128:(g + 1) * 128], xdw[:, g * 8:(g + 1) * 8, :].rearrange("p h d -> p (h d)"), True, True,
                       [bm_r, xdw_r], [psr[7]])
                    yield
                    vop(lambda g=g, sm=sm: V.tensor_tensor(out=ST[:, g, :].rearrange("p (h d) -> p h d", h=8),
                                                           in0=ST[:, g, :].rearrange("p (h d) -> p h d", h=8),
                                                           in1=sm[:, 4, g * 8:(g + 1) * 8].unsqueeze(2).to_broadcast([128, 8, 64]),
                                                           op=ALU.mult), [ST_r, sm_r, STb_r], [ST_r])
                    yield
                    vop(lambda g=g: V.tensor_tensor(out=ST[:, g, :], in0=ST[:, g, :], in1=ps[7][:], op=ALU.add), [ST_r, psr[7]], [ST_r])
                    yield
                aop(lambda: A.copy(out=STb[:], in_=ST[:]), [ST_r], [STb_r])
                yield
                Yf = Y[:].rearrange("p h d -> p (h d)")
                gop(lambda zt=zt: G.tensor_tensor(out=Yf, in0=Yf, in1=zt[:], op=ALU.mult), [Y_r, z_r], [Y_r])
                yield
                st, st_r = stl.next()
                for g in range(2):
                    aop(lambda g=g, st=st: A.activation(out=tmpY[:].rearrange("p h d -> p (h d)")[:, g * 512:(g + 1) * 512],
                                                        in_=Yf[:, g * 512:(g + 1) * 512], func=AF.Square, accum_out=st[:, g:g + 1]),
                        [Y_r], [tmpY_r, st_r])
                    yield
                    rstd(st, st_r, g, 4 + g, 512)
                    vop(lambda g=g, st=st: V.scalar_tensor_tensor(out=Yn[:, g * 512:(g + 1) * 512], in0=Yf[:, g * 512:(g + 1) * 512],
                                                                  scalar=st[:, 4 + g:5 + g], in1=gn[:, g * 512:(g + 1) * 512],
                                                                  op0=ALU.mult, op1=ALU.mult), [Y_r, st_r, c_r], [Yn_r])
                    yield
                cT, cT_r = cTl.next()
                pv = ps[7][:].bitcast(BF16)
                for c in range(8):
                    tp(pv[:, c * 128:(c + 1) * 128], Yn[:, c * 128:(c + 1) * 128], [Yn_r], [psr[7]], inc=(c == 7))
                    yield
                aop(lambda: A.copy(out=cT[:], in_=pv[:, 0:1024].rearrange("p (j t) -> p j t", j=8)), [psr[7]], [cT_r])
                yield
                kb.dma(out=mixT_d.rearrange("(kc p) s -> p kc s", p=128)[:, 8:16, rows], in_=cT[:], reads=[cT_r], writes=[R_["mixT"]], own=cT_r)

            yield
            for ct in range(NT):
                yield from stageA(ct)
                yield from stageB(ct)

    def phase4(l):
        with Phase():
            run_interleaved([gen4(l)])

    class WLoad:
        def __init__(self, name, kc=8, nc_=256, n=3):
            self.kc, self.nc_ = kc, nc_
            self.stg = Slots(kb, name + "_stg", [128, kc, nc_], F32, n)

        def load(self, dst, dst_r, wap, r0, kcn, c0, ncols):
            assert kcn <= self.kc and ncols <= self.nc_
            st, sr = self.stg.next()
            src = wap[r0:r0 + kcn * 128, :].rearrange("(kc p) c -> p kc c", p=128)[:, :, c0:c0 + ncols]
            kb.dma(out=st[:, 0:kcn, 0:ncols], in_=src, writes=[sr], own=sr)
            self.i = getattr(self, "i", 0) + 1
            e = ("dve", "act", "pool", "dve", "act")[self.i % 5]
            if e == "pool":
                gop(lambda: G.tensor_copy(out=dst, in_=st[:, 0:kcn, 0:ncols]), [sr], [dst_r])
            elif e == "dve":
                vop(lambda: V.tensor_copy(out=dst, in_=st[:, 0:kcn, 0:ncols]), [sr], [dst_r])
            else:
                aop(lambda: A.copy(out=dst, in_=st[:, 0:kcn, 0:ncols]), [sr], [dst_r])

        def load_full(self, dst_t, dst_r, wap, KC, C):
            for k0 in range(0, KC, self.kc):
                kn = min(self.kc, KC - k0)
                for c0 in range(0, C, self.nc_):
                    cn = min(self.nc_, C - c0)
                    self.load(dst_t[:, k0:k0 + kn, c0:c0 + cn], dst_r, wap, k0 * 128, kn, c0, cn)

    def post_norm_residual(P0, P1, r0, r1, xt, xr, g_bc, g_r, stl, junk, junk_r, tmp, tmp_r):
        st, st_r = stl.next()
        aop(lambda: A.activation(out=junk[:, 0:512], in_=P0[:], func=AF.Square, accum_out=st[:, 0:1]), [r0], [junk_r, st_r])
        aop(lambda: A.activation(out=junk[:, 512:1024], in_=P1[:], func=AF.Square, accum_out=st[:, 1:2]), [r1], [junk_r, st_r])
        vop(lambda: V.tensor_tensor(out=st[:, 3:4], in0=st[:, 0:1], in1=st[:, 1:2], op=ALU.add), [st_r], [st_r])
        rstd(st, st_r, 3, 2, D)
        for half, (P, r) in enumerate(((P0, r0), (P1, r1))):
            sl = slice(half * 512, (half + 1) * 512)
            vop(lambda P=P, sl=sl: V.scalar_tensor_tensor(out=tmp[:, sl], in0=P[:], scalar=st[:, 2:3], in1=g_bc[:, sl],
                                                         op0=ALU.mult, op1=ALU.mult), [r, st_r, g_r], [tmp_r])
            gop(lambda sl=sl: G.tensor_tensor(out=xt[:, sl], in0=tmp[:, sl], in1=xt[:, sl], op=ALU.add), [tmp_r, xr], [xr])

    def phase5(l):
        with Phase():
            src = x_in if l == 0 else xs_d
            wl = WLoad("p5w")
            wo = kb.sb("p5_wo", [128, 16, D], BF16)
            wo_r = kb.res("p5_wo")
            wl.load_full(wo, wo_r, w_out[l], 16, D)
            g1 = kb.sb("p5_g1", [128, D], F32)
            g2 = kb.sb("p5_g2", [128, D], F32)
            g_r = kb.res("p5_g")
            bcast_load(g1[:], vecD[l, 1, :], D, g_r)
            bcast_load(g2[:], vecD[l, 2, :], D, g_r)
            xsl = Slots(kb, "p5_x", [128, D], F32, 2)
            mxl = Slots(kb, "p5_mx", [128, 16, 128], BF16, 2)
            junk = kb.sb("p5_junk", [128, D], F32)
            junk_r = kb.res("p5_junk")
            tmp = kb.sb("p5_tmp", [128, D], F32)
            tmp_r = kb.res("p5_tmp")
            stl = Slots(kb, "p5_st", [128, 16], F32, 2)
            hb = kb.sb("p5_hb", [128, D], BF16)
            ntmp = (junk, junk_r, stl, hb, kb.res("p5_hb"))
            banks = RR([0, 1, 2, 3])
            S5 = {}

            def p5_stage1(tt):
                xt, xr = xsl.next()
                kb.dma(out=xt[:], in_=src[tt * 128:(tt + 1) * 128, :], reads=[R_["xs"]], writes=[xr], own=xr)
                mx, mr = mxl.next()
                kb.dma(out=mx[:], in_=mixT_d.rearrange("(kc p) s -> p kc s", p=128)[:, :, tt * 128:(tt + 1) * 128],
                       reads=[R_["mixT"]], writes=[mr], own=mr)
                b0, b1 = banks.next(), banks.next()
                for half, b in enumerate((b0, b1)):
                    for kc in range(16):
                        mm(ps[b][:], mx[:, kc, :], wo[:, kc, half * 512:(half + 1) * 512], kc == 0, kc == 15, [mr, wo_r], [psr[b]],
                           inc=(kc == 15))
                S5[tt] = (xt, xr, b0, b1)

            def p5_stage2(tt):
                xt, xr, b0, b1 = S5.pop(tt)
                post_norm_residual(ps[b0], ps[b1], psr[b0], psr[b1], xt, xr, g1, g_r, stl, junk, junk_r, tmp, tmp_r)
                kb.dma(out=xm_d[tt * 128:(tt + 1) * 128, :], in_=xt[:], reads=[xr], writes=[R_["xm"]], own=xr)
                norm_to_hT(xt[:], xr, g2, g_r, tt, ntmp)

            p5_stage1(0)
            for tt in range(NT):
                if tt + 1 < NT:
                    p5_stage1(tt + 1)
                p5_stage2(tt)

    def phase6a(l):
        with Phase():
            hT, hT_r = HT['t'], HT['r']
            wl = WLoad("p6w", 8, 512, 2)
            wgl = Slots(kb, "p6_wg", [128, 8, 512], BF16, 2)
            wul = Slots(kb, "p6_wu", [128, 8, 512], BF16, 2)
            sgl = Slots(kb, "p6_sg", [128, 512], F32, 2)
            gul = Slots(kb, "p6_gu", [128, 512], BF16, 3)
            bankA = RR([0, 1])
            bankB = RR([2, 3])
            for c0 in range(0, DFF, 512):
                cn = min(512, DFF - c0)
                wg_t, wg_r = wgl.next()
                wu_t, wu_r = wul.next()
                wl.load(wg_t[:, :, 0:cn], wg_r, w_g[l], 0, 8, c0, cn)
                wl.load(wu_t[:, :, 0:cn], wu_r, w_u[l], 0, 8, c0, cn)
                for c in range(cn // 128):
                    for tb in range(NB):
                        bg_, bu_ = bankA.next(), bankB.next()
                        for kc in range(8):
                            mm(ps[bg_][:], wg_t[:, kc, c * 128:(c + 1) * 128], hT[:, kc, tb * 512:(tb + 1) * 512], kc == 0, kc == 7,
                               [wg_r, hT_r], [psr[bg_]], inc=(kc == 7))
                        for kc in range(8):
                            mm(ps[bu_][:], wu_t[:, kc, c * 128:(c + 1) * 128], hT[:, kc, tb * 512:(tb + 1) * 512], kc == 0, kc == 7,
                               [wu_r, hT_r], [psr[bu_]], inc=(kc == 7))
                        sg, sgr = sgl.next()
                        aop(lambda sg=sg, bg_=bg_: A.activation(out=sg[:], in_=ps[bg_][:], func=AF.Silu), [psr[bg_]], [sgr])
                        gu, gur = gul.next()
                        vop(lambda sg=sg, gu=gu, bu_=bu_: V.tensor_tensor(out=gu[:], in0=ps[bu_][:], in1=sg[:], op=ALU.mult),
                            [psr[bu_], sgr], [gur])
                        f0 = c0 + c * 128
                        kb.dma(out=gu_d[f0:f0 + 128, tb * 512:(tb + 1) * 512], in_=gu[:], reads=[gur], writes=[R_["gu"]], own=gur)

    def phase6b(l):
        last = (l == DEPTH - 1)
        with Phase():
            wl = WLoad("p6bw")
            KF = DFF // 128
            wd = kb.sb("p6b_wd", [128, KF, D], BF16)
            wd_r = kb.res("p6b_wd")
            wl.load_full(wd, wd_r, w_d[l], KF, D)
            wpg = kb.sb("p6b_wpg", [128, 8, D], BF16)
            wpg_r = kb.res("p6b_wpg")
            wl.load_full(wpg, wpg_r, w_pg[l], 8, D)
            wpp = kb.sb("p6b_wpp", [128, 2, D], BF16)
            wpp_r = kb.res("p6b_wpp")
            wl.load_full(wpp, wpp_r, w_pp[l], 2, D)
            g1 = kb.sb("p6b_g1", [128, D], F32)
            g_r = kb.res("p6b_g")
            bcast_load(g1[:], vecD[l, 3, :], D, g_r)
            xsl = Slots(kb, "p6b_x", [128, D], F32, 2)
            gtl = Slots(kb, "p6b_gt", [128, KF, 128], BF16, 2)
            pl = Slots(kb, "p6b_p", [128, 256], F32, 2)
            junk = kb.sb("p6b_junk", [128, D], F32)
            junk_r = kb.res("p6b_junk")
            tmp = kb.sb("p6b_tmp", [128, D], F32)
            tmp_r = kb.res("p6b_tmp")
            stl = Slots(kb, "p6b_st", [128, 16], F32, 2)
            xb = kb.sb("p6b_xb", [128, D], BF16)
            xb_r = kb.res("p6b_xb")
            xT = kb.sb("p6b_xT", [128, 8, 128], BF16)
            xT_r = kb.res("p6b_xT")
            pb16 = kb.sb("p6b_pb", [128, 256], BF16)
            pb_r = kb.res("p6b_pb")
            pT = kb.sb("p6b_pT", [128, 2, 128], BF16)
            pT_r = kb.res("p6b_pT")
            sg = kb.sb("p6b_sg", [128, D], F32)
            sg_r = kb.res("p6b_sg")
            dst_d = y_out if last else xs_d
            dst_r = R_["y"] if last else R_["xs"]
            S1 = {}

            def stage1(tt):
                xt, xr = xsl.next()
                kb.dma(out=xt[:], in_=xm_d[tt * 128:(tt + 1) * 128, :], reads=[R_["xm"]], writes=[xr], own=xr)
                gt, gr = gtl.next()
                kb.dma(out=gt[:], in_=gu_d.rearrange("(kc p) s -> p kc s", p=128)[:, :, tt * 128:(tt + 1) * 128],
                       reads=[R_["gu"]], writes=[gr], own=gr)
                pt, pr = pl.next()
                kb.dma(out=pt[:], in_=p_in[l, tt * 128:(tt + 1) * 128, :], writes=[pr], own=pr)
                db = (0, 1) if tt % 2 == 0 else (2, 3)
                for half, b in enumerate(db):
                    for kc in range(KF):
                        mm(ps[b][:], gt[:, kc, :], wd[:, kc, half * 512:(half + 1) * 512], kc == 0, kc == KF - 1, [gr, wd_r], [psr[b]],
                           inc=(kc == KF - 1))
                S1[tt] = (xt, xr, pt, pr, db)

            def stage2(tt):
                xt, xr, pt, pr, db = S1.pop(tt)
                post_norm_residual(ps[db[0]], ps[db[1]], psr[db[0]], psr[db[1]], xt, xr, g1, g_r, stl, junk, junk_r, tmp, tmp_r)
                aop(lambda: A.copy(out=xb[:], in_=xt[:]), [xr], [xb_r])
                pv = ps[7][:].bitcast(BF16)
                for dc in range(8):
                    tp(pv[:, dc * 128:(dc + 1) * 128], xb[:, dc * 128:(dc + 1) * 128], [xb_r], [psr[7]], inc=(dc == 7))
                vop(lambda: V.tensor_copy(out=xT[:], in_=pv[:, 0:1024].rearrange("p (j t) -> p j t", j=8)), [psr[7]], [xT_r])
                for half, b in enumerate((4, 5)):
                    for kc in range(8):
                        mm(ps[b][:], xT[:, kc, :], wpg[:, kc, half * 512:(half + 1) * 512], kc == 0, kc == 7, [xT_r, wpg_r], [psr[b]],
                           inc=(kc == 7))
                    aop(lambda: A.activation(out=sg[:, half * 512:(half + 1) * 512], in_=ps[b][:], func=AF.Sigmoid), [psr[b]], [sg_r])
                vop(lambda: V.tensor_copy(out=pb16[:], in_=pt[:]), [pr], [pb_r])
                pv6 = ps[6][:].bitcast(BF16)
                for j in range(2):
                    tp(pv6[:, j * 128:(j + 1) * 128], pb16[:, j * 128:(j + 1) * 128], [pb_r], [psr[6]], inc=(j == 1))
                vop(lambda: V.tensor_copy(out=pT[:], in_=pv6[:, 0:256].rearrange("p (j t) -> p j t", j=2)), [psr[6]], [pT_r])
                for half in range(2):
                    for kc in range(2):
                        mm(ps[6][:], pT[:, kc, :], wpp[:, kc, half * 512:(half + 1) * 512], kc == 0, kc == 1, [pT_r, wpp_r], [psr[6]],
                           inc=(kc == 1))
                    sl = slice(half * 512, (half + 1) * 512)
                    vop(lambda: V.tensor_tensor(out=tmp[:, sl], in0=ps[6][:], in1=sg[:, sl], op=ALU.mult), [psr[6], sg_r], [tmp_r])
                    gop(lambda: G.tensor_tensor(out=xt[:, sl], in0=tmp[:, sl], in1=xt[:, sl], op=ALU.add), [tmp_r, xr], [xr])
                kb.dma(out=dst_d[tt * 128:(tt + 1) * 128, :], in_=xt[:], reads=[xr], writes=[dst_r], own=xr)

            stage1(0)
            for tt in range(NT):
                if tt + 1 < NT:
                    stage1(tt + 1)
                stage2(tt)

    def front(l):
        with HTScope():
            phase0(l, None if l == 0 else xs_d)
            phase1(l)

    def mid(l):
        phase24(l)
        phase3(l)

    def back(l):
        with HTScope():
            phase5(l)
            phase6a(l)
        phase6b(l)

    def run_all():
        for l in range(DEPTH):
            front(l)
            mid(l)
            back(l)

    phases = phases or ["all"]
    kb.fn = dict(front=front, mid=mid, back=back, phase2=phase2, phase3=phase3, phase4=phase4, phase24=phase24)
    if phases == ["all"]:
        run_all()
    else:
        for ph in phases:
            name, l = ph
            kb.fn[name](l)
    barrier()
    return nc, kb


def prep_inputs(inp, S):
    f = lambda a: np.ascontiguousarray(np.asarray(a, dtype=np.float32))
    w_uk = f(inp["w_uk"])
    wukT = np.ascontiguousarray(w_uk.reshape(DEPTH, R, 4, 2, 64).transpose(0, 3, 4, 2, 1).reshape(DEPTH, 128, 4, R))
    w_uv = f(inp["w_uv"])
    wuv = np.ascontiguousarray(w_uv.reshape(DEPTH, 2, 128, NH * 64).transpose(0, 2, 1, 3))
    rel = f(inp["rel_bias"])
    i = np.arange(128)[:, None]
    c = np.arange(GW)[None, :]
    bucket = t5_bucket_np((i - c + 384).astype(np.int32))
    gtab = np.ascontiguousarray(rel[bucket].transpose(0, 2, 1))
    vecD = np.ascontiguousarray(np.stack([f(inp["pre_mix_norm"]), f(inp["post_mix_norm"]), f(inp["pre_ffn_norm"]),
                                          f(inp["post_ffn_norm"]), f(inp["ssm_norm"])], axis=1))
    scw = np.ascontiguousarray(f(inp["short_conv_w"]).reshape(DEPTH, 3, 4, 128).transpose(0, 3, 2, 1))
    sscw = np.ascontiguousarray(f(inp["ssm_conv_w"]).reshape(DEPTH, 4, 12, 128).transpose(0, 3, 2, 1))
    sscb = np.ascontiguousarray(f(inp["ssm_conv_b"]).reshape(DEPTH, 12, 128).transpose(0, 2, 1))
    v16 = np.ascontiguousarray(np.stack([f(inp["ssm_dt_bias"]), f(inp["ssm_a_log"]), f(inp["ssm_d"])], axis=1))
    shared = dict(w_in=f(inp["w_in"]), w_out=f(inp["w_out"]), w_ffn_gate=f(inp["w_ffn_gate"]), w_ffn_up=f(inp["w_ffn_up"]),
                  w_ffn_down=f(inp["w_ffn_down"]), w_ple_proj=f(inp["w_ple_proj"]), w_ple_gate=f(inp["w_ple_gate"]),
                  wukT=wukT, wuv=wuv, gtab=gtab, vecD=vecD, kv_norm=f(inp["kv_norm"]), idx_k_norm_g=f(inp["idx_k_norm_g"]),
                  idx_k_norm_b=f(inp["idx_k_norm_b"]), scw=scw, sscw=sscw, sscb=sscb, v16=v16)
    x = f(inp["x"])
    p = f(inp["p"])
    B = x.shape[0]
    maps = []
    for b in range(B):
        m = dict(shared)
        m["x"] = np.ascontiguousarray(x[b, :S])
        m["p"] = np.ascontiguousarray(p[:, b, :S])
        maps.append(m)
    return maps


_CACHE = {}


def kernel(**inputs):
    S = 4096
    maps = prep_inputs(inputs, S)
    if "nc" not in _CACHE:
        _CACHE["nc"] = build_program(S)[0]
    res = run_bass_kernel_spmd(_CACHE["nc"], maps, core_ids=list(range(8)))
    return np.stack([np.asarray(r["y"], dtype=np.float32) for r in res.results], axis=0)
```

```python
import math
from contextlib import ExitStack
import numpy as np
import concourse.bass as bass
import concourse.mybir as mybir
from concourse.bass_utils import run_bass_kernel_spmd

F32 = mybir.dt.float32
BF16 = mybir.dt.bfloat16
AF = mybir.ActivationFunctionType
ALU = mybir.AluOpType
AX = mybir.AxisListType

D = 1024
DEPTH = 2
NH = 8
R = 256
IN_W = 5204
DFF = 2816
MIXW = 2048
EPS = 1e-6
NEG = -30000.0
O_Q, O_CKV, O_IQ, O_IK, O_IW, O_BG, O_CG, O_HB, O_Z, O_XBC, O_DT = 0, 512, 768, 1024, 1088, 1092, 1604, 2116, 2628, 3652, 5188
GW = 1582
GC = 1070
SEM_LIMIT = 30000


class Sem:
    def __init__(self, h):
        self.h = h
        self.val = 0


class Res:
    __slots__ = ("name", "w", "r", "dsem")

    def __init__(self, name):
        self.name = name
        self.w = {}
        self.r = {}
        self.dsem = None


class EngW:
    def __init__(self, name, eng, is_pe=False):
        self.name = name
        self.eng = eng
        self.is_pe = is_pe
        self.sem = None
        self.seen = {}


class KB:
    def __init__(self, nc):
        self.nc = nc
        self.nsem = 0
        self.pe = EngW("pe", nc.tensor, True)
        self.act = EngW("act", nc.scalar)
        self.dve = EngW("dve", nc.vector)
        self.pool = EngW("pool", nc.gpsimd)
        self.sp = EngW("sp", nc.sync)
        for e in (self.pe, self.act, self.dve, self.pool):
            e.sem = self.new_sem(e.name)
        self.stack = ExitStack()
        self.n_ops = 0
        self.free_dsems = []
        self.phase_dsems = [[]]

    def new_sem(self, name="s"):
        self.nsem += 1
        return Sem(self.nc.alloc_semaphore(name=f"{name}_{self.nsem}"))

    def res(self, name):
        return Res(name)

    def sb(self, name, shape, dt):
        self.n_sb = getattr(self, "n_sb", 0) + 1
        return self.stack.enter_context(self.nc.sbuf_tensor(f"{name}_{self.n_sb}", list(shape), dt))

    def _wait(self, e, reads, writes):
        raw = {}
        oth = {}
        for r in reads:
            for s, v in r.w.items():
                if raw.get(s, 0) < v:
                    raw[s] = v
        for w in writes:
            for s, v in w.w.items():
                if oth.get(s, 0) < v:
                    oth[s] = v
            for s, v in w.r.items():
                if oth.get(s, 0) < v:
                    oth[s] = v
        for s, v in oth.items():
            if s is e.sem and e.is_pe:
                continue
            if raw.get(s, 0) < v:
                raw[s] = v
        for s, v in raw.items():
            if s is e.sem and e.is_pe:
                continue
            if e.seen.get(s, 0) >= v:
                continue
            e.eng.wait_ge(s.h, v)
            e.seen[s] = v

    def op(self, e, fn, reads=(), writes=(), inc=True):
        self._wait(e, reads, writes)
        ins = fn()
        self.n_ops += 1
        if inc:
            if e.sem.val >= SEM_LIMIT and not getattr(e, "pending", False):
                e.sem = self.new_sem(e.name)
            e.sem.val += 1
            ins.then_inc(e.sem.h, 1)
            tv = e.sem.val
            e.pending = False
        else:
            tv = e.sem.val + 1
            e.pending = True
        for w in writes:
            w.w[e.sem] = tv
        for r in reads:
            r.r[e.sem] = tv
        return ins

    def dma(self, out, in_, reads=(), writes=(), q=None, own=None):
        q = q or self.sp
        self._wait(q, reads, writes)
        ins = q.eng.dma_start(out=out, in_=in_)
        self.n_ops += 1
        if own.dsem is None or own.dsem.val >= SEM_LIMIT:
            if getattr(self, "free_dsems", None):
                own.dsem = self.free_dsems.pop()
                if own.dsem.val >= SEM_LIMIT:
                    own.dsem = self.new_sem("d" + own.name)
            else:
                own.dsem = self.new_sem("d" + own.name)
            self.phase_dsems[-1].append(own.dsem)
        s = own.dsem
        s.val += 16
        ins.then_inc(s.h, 16)
        for w in writes:
            w.w[s] = s.val
        for r in reads:
            r.r[s] = s.val
        return ins

    def drain(self, e, ress):
        self._wait(e, ress, ())


class Slots:
    def __init__(self, kb, name, shape, dt, n):
        self.t = [kb.sb(f"{name}{i}", shape, dt) for i in range(n)]
        self.r = [kb.res(f"{name}{i}") for i in range(n)]
        self.i = 0
        self.n = n

    def next(self):
        k = self.i % self.n
        self.i += 1
        return self.t[k], self.r[k]


def t5_bucket_np(rel):
    half, max_exact = 16, 8
    ret = np.where(rel > 0, half, 0)
    n = np.abs(rel)
    nf = np.maximum(n, 1).astype(np.float32)
    large = max_exact + (np.log(nf / max_exact) / math.log(1024 / max_exact) * (half - max_exact)).astype(np.int32)
    large = np.minimum(large, half - 1)
    return ret + np.where(n < max_exact, n, large)


def build_program(S, dbg=False, phases=None):
    NT = S // 128
    NB = S // 512
    nc = bass.Bass("TRN2", target_bir_lowering=False)
    kb = KB(nc)
    pe, act, dve, pool, sp = kb.pe, kb.act, kb.dve, kb.pool, kb.sp
    T, V, A, G = nc.tensor, nc.vector, nc.scalar, nc.gpsimd

    def din(name, shape, dt=F32):
        return nc.dram_tensor(name, list(shape), dt, kind="ExternalInput").ap()

    def dscr(name, shape, dt):
        return nc.dram_tensor(name, list(shape), dt, kind="ExternalOutput" if dbg else "Internal").ap()

    x_in = din("x", [S, D])
    p_in = din("p", [DEPTH, S, 256])
    w_in = din("w_in", [DEPTH, D, IN_W])
    w_out = din("w_out", [DEPTH, MIXW, D])
    w_g = din("w_ffn_gate", [DEPTH, D, DFF])
    w_u = din("w_ffn_up", [DEPTH, D, DFF])
    w_d = din("w_ffn_down", [DEPTH, DFF, D])
    w_pp = din("w_ple_proj", [DEPTH, 256, D])
    w_pg = din("w_ple_gate", [DEPTH, D, D])
    wukT = din("wukT", [DEPTH, 128, 4, R])
    wuv = din("wuv", [DEPTH, 128, 2, NH * 64])
    gtab = din("gtab", [128, NH, GW])
    vecD = din("vecD", [DEPTH, 5, D])
    kvn = din("kv_norm", [DEPTH, R])
    ikg = din("idx_k_norm_g", [DEPTH, 64])
    ikb = din("idx_k_norm_b", [DEPTH, 64])
    scw = din("scw", [DEPTH, 128, 4, 3])
    sscw = din("sscw", [DEPTH, 128, 12, 4])
    sscb = din("sscb", [DEPTH, 128, 12])
    v16 = din("v16", [DEPTH, 3, 16])
    y_out = nc.dram_tensor("y", [S, D], F32, kind="ExternalOutput").ap()

    xs_d = dscr("xs_d", [S, D], F32)
    xm_d = dscr("xm_d", [S, D], F32)
    qlat_d = dscr("qlat_d", [NH, R, S], BF16)
    iq_d = dscr("iq_d", [256, S], BF16)
    ik_d = dscr("ik_d", [64, S], BF16)
    iw_d = dscr("iw_d", [S, 4], F32)
    ckv_d = dscr("ckv_d", [S, R], BF16)
    ckvT_d = dscr("ckvT_d", [R, S], BF16)
    mixT_d = dscr("mixT_d", [MIXW, S], BF16)
    zs_d = dscr("zs_d", [S, D], F32)
    xsc_d = dscr("xsc_d", [S, D], BF16)
    bm_d = dscr("bm_d", [S, 256], BF16)
    bmT_d = dscr("bmT_d", [256, S], BF16)
    cmT_d = dscr("cmT_d", [256, S], BF16)
    dt_d = dscr("dt_d", [S, 32], F32)
    nmT_d = dscr("nmT_d", [S, S], BF16)
    gu_d = dscr("gu_d", [DFF, S], BF16)
    R_ = {n: kb.res(n) for n in ["xs", "qlat", "iq", "ik", "iw", "ckv", "ckvT", "mixT", "zs", "xsc", "bm", "bmT",
                                 "cmT", "dt", "nmT", "gu", "y", "xm"]}

    ps = [kb.stack.enter_context(nc.psum_tensor(f"ps{i}", [128, 512], F32)) for i in range(8)]
    psr = [kb.res(f"ps{i}") for i in range(8)]

    ident_f = kb.sb("ident_f", [128, 128], F32)
    ident = kb.sb("ident", [128, 128], BF16)
    ones_f = kb.sb("ones_f", [128, 128], F32)
    utri_f = kb.sb("utri_f", [128, 128], F32)
    tri_f = kb.sb("tri_f", [128, 128], F32)
    cres = kb.res("consts")
    kb.op(pool, lambda: G.memset(ident_f[:], 0.0), writes=[cres])
    kb.op(pool, lambda: G.affine_select(out=ident_f[:], in_=ident_f[:], pattern=[[-1, 128]], compare_op=ALU.not_equal,
                                        fill=1.0, base=0, channel_multiplier=1), reads=[cres], writes=[cres])
    kb.op(pool, lambda: G.tensor_copy(out=ident[:], in_=ident_f[:]), reads=[cres], writes=[cres])
    kb.op(pool, lambda: G.memset(ones_f[:], 1.0), writes=[cres])
    kb.op(pool, lambda: G.affine_select(out=utri_f[:], in_=ones_f[:], pattern=[[1, 128]], compare_op=ALU.is_ge,
                                        fill=0.0, base=0, channel_multiplier=-1), reads=[cres], writes=[cres])
    kb.op(pool, lambda: G.tensor_copy(out=tri_f[:], in_=utri_f[:]), reads=[cres], writes=[cres])

    eps_t = kb.sb("eps_t", [128, 1], F32)
    negb_t = kb.sb("negb_t", [128, 1], F32)
    kb.op(pool, lambda: G.memset(negb_t[:], NEG), writes=[cres])
    kb.op(pool, lambda: G.memset(eps_t[:], EPS), writes=[cres])
    HT = {}

    def bcast_load(dst, src_vec, n, rs):
        kb.dma(out=dst, in_=src_vec.partition_broadcast(128), writes=[rs], own=rs)

    def rms_rstd(dst, ssq, n, eng=None):
        V.tensor_scalar(out=dst, in0=ssq, scalar1=1.0 / n, scalar2=EPS, op0=ALU.mult, op1=ALU.add)

    all_sems = []
    _orig_new_sem = kb.new_sem

    def _new_sem(name="s"):
        s = _orig_new_sem(name)
        all_sems.append(s)
        return s
    kb.new_sem = _new_sem
    for e in (pe, act, dve, pool):
        all_sems.append(e.sem)

    def barrier():
        for e in (pe, act, dve, pool, sp):
            for s in all_sems:
                if s is e.sem or s.val == 0:
                    continue
                if e.seen.get(s, 0) >= s.val:
                    continue
                e.eng.wait_ge(s.h, s.val)
                e.seen[s] = s.val

    class Phase:
        def __enter__(self):
            self.es = ExitStack()
            self.es.__enter__()
            self.old = kb.stack
            kb.stack = self.es
            kb.phase_dsems.append([])
            return self

        def __exit__(self, *a):
            barrier()
            kb.free_dsems.extend(kb.phase_dsems.pop())
            kb.stack = self.old
            self.es.__exit__(None, None, None)
            return False

    def mm(out, lhsT, rhs, start, stop, reads, writes, inc=True):
        return kb.op(pe, lambda: T.matmul(out, lhsT=lhsT, rhs=rhs, start=start, stop=stop), reads=reads, writes=writes, inc=inc)

    def tp(out, in_, reads, writes, inc=True, idn=None):
        idn = ident if idn is None else idn
        k = in_.shape[0]
        return kb.op(pe, lambda: T.transpose(out, in_, idn[0:k, 0:k]), reads=list(reads) + [cres], writes=writes, inc=inc)

    def vop(fn, reads, writes):
        return kb.op(dve, fn, reads=reads, writes=writes)

    def aop(fn, reads, writes):
        return kb.op(act, fn, reads=reads, writes=writes)

    def gop(fn, reads, writes):
        return kb.op(pool, fn, reads=reads, writes=writes)

    def rstd(st, st_r, src, dst, n):
        aop(lambda: A.activation(out=st[:, 15:16], in_=st[:, src:src + 1], func=AF.Ln, scale=1.0 / n, bias=eps_t[:, 0:1]), [st_r, cres], [st_r])
        aop(lambda: A.activation(out=st[:, dst:dst + 1], in_=st[:, 15:16], func=AF.Exp, scale=-0.5), [st_r], [st_r])

    def norm_to_hT(xt, xr, g_bc, g_r, tt, tmp):
        junk, junk_r, stl, hb, hb_r = tmp
        st, st_r = stl.next()
        aop(lambda: A.activation(out=junk[:], in_=xt, func=AF.Square, accum_out=st[:, 0:1]), [xr], [junk_r, st_r])
        rstd(st, st_r, 0, 2, D)
        vop(lambda: V.scalar_tensor_tensor(out=hb[:], in0=xt, scalar=st[:, 2:3], in1=g_bc[:], op0=ALU.mult, op1=ALU.mult),
            [xr, st_r, g_r], [hb_r])
        for half in range(2):
            pb = 6 + half
            pv = ps[pb][:].bitcast(BF16)
            for j in range(4):
                dc = half * 4 + j
                tp(pv[:, j * 128:(j + 1) * 128], hb[:, dc * 128:(dc + 1) * 128], [hb_r], [psr[pb]], inc=(j == 3))
            src = pv[:, 0:512].rearrange("p (j t) -> p j t", j=4)
            dst = HT['t'][:, half * 4:(half + 1) * 4, tt * 128:(tt + 1) * 128]
            if half == 0:
                aop(lambda src=src, dst=dst: A.copy(out=dst, in_=src), [psr[pb]], [HT['r']])
            else:
                vop(lambda src=src, dst=dst: V.tensor_copy(out=dst, in_=src), [psr[pb]], [HT['r']])

    class HTScope(Phase):
        def __enter__(self):
            Phase.__enter__(self)
            HT['t'] = kb.sb("hT", [128, 8, S], BF16)
            HT['r'] = kb.res("hT")
            return self

    def phase0(l, src=None):
        src = x_in if src is None else src
        with Phase():
            g_bc = kb.sb("p0_g", [128, D], F32)
            g_r = kb.res("p0_g")
            bcast_load(g_bc[:], vecD[l, 0, :], D, g_r)
            xsl = Slots(kb, "p0_x", [128, D], F32, 2)
            junk = kb.sb("p0_junk", [128, D], F32)
            stl = Slots(kb, "p0_st", [128, 16], F32, 2)
            hb = kb.sb("p0_hb", [128, D], BF16)
            tmp = (junk, kb.res("p0_junk"), stl, hb, kb.res("p0_hb"))
            for tt in range(NT):
                xt, xr = xsl.next()
                kb.dma(out=xt[:], in_=src[tt * 128:(tt + 1) * 128, :], reads=[R_['xs']], writes=[xr], own=xr)
                norm_to_hT(xt[:], xr, g_bc, g_r, tt, tmp)

    class WStream:
        def __init__(self, name, KC, maxc, nbuf=2, nstg=2):
            self.KC = KC
            self.stg = Slots(kb, name + "_stg", [128, KC, 256], F32, nstg)
            self.wb = Slots(kb, name + "_wb", [128, KC, maxc], BF16, nbuf)

        def load(self, wap, c0, ncols, r0=0, kcn=None):
            kcn = kcn or self.KC
            wt, wr = self.wb.next()
            for p0 in range(0, ncols, 256):
                pn = min(256, ncols - p0)
                st, sr = self.stg.next()
                src = wap[r0:r0 + kcn * 128, :].rearrange("(kc p) c -> p kc c", p=128)[:, :, c0 + p0:c0 + p0 + pn]
                kb.dma(out=st[:, 0:kcn, 0:pn], in_=src, writes=[sr], own=sr)
                gop(lambda: G.tensor_copy(out=wt[:, 0:kcn, p0:p0 + pn], in_=st[:, 0:kcn, 0:pn]), [sr], [wr])
            return wt, wr

    class RR:
        def __init__(self, items):
            self.items = items
            self.i = 0

        def next(self):
            k = self.items[self.i % len(self.items)]
            self.i += 1
            return k

    def phase1(l):
        with Phase():
            hT, hT_r = HT['t'], HT['r']
            ws = WStream("w1", 8, 512, nbuf=3)
            c_r = kb.res("p1c")
            wuk_f = kb.sb("wuk_f", [128, 4, R], F32)
            wuk_b = kb.sb("wuk_b", [128, 4, R], BF16)
            kb.dma(out=wuk_f[:], in_=wukT[l], writes=[c_r], own=c_r)
            gop(lambda: G.tensor_copy(out=wuk_b[:], in_=wuk_f[:]), [c_r], [c_r])
            scw_t = kb.sb("scw_t", [128, 4, 3], F32)
            kb.dma(out=scw_t[:], in_=scw[l], writes=[c_r], own=c_r)
            sscw_t = kb.sb("sscw_t", [128, 12, 4], F32)
            kb.dma(out=sscw_t[:], in_=sscw[l], writes=[c_r], own=c_r)
            sscb_t = kb.sb("sscb_t", [128, 12], F32)
            kb.dma(out=sscb_t[:], in_=sscb[l], writes=[c_r], own=c_r)
            kvn_bc = kb.sb("kvn_bc", [128, R], F32)
            bcast_load(kvn_bc[:], kvn[l, :], R, c_r)
            ikg_bc = kb.sb("ikg_bc", [128, 64], F32)
            bcast_load(ikg_bc[:], ikg[l, :], 64, c_r)
            ikb_bc = kb.sb("ikb_bc", [128, 64], F32)
            bcast_load(ikb_bc[:], ikb[l, :], 64, c_r)
            v16_bc = kb.sb("v16_bc", [128, 3, 16], F32)
            kb.dma(out=v16_bc[:], in_=v16[l].partition_broadcast(128), writes=[c_r], own=c_r)
            A_bc = kb.sb("A_bc", [128, 16], F32)
            aop(lambda: A.activation(out=A_bc[:], in_=v16_bc[:, 1, :], func=AF.Exp), [c_r], [c_r])
            vop(lambda: V.tensor_scalar(out=A_bc[:], in0=A_bc[:], scalar1=-1.0, scalar2=None, op0=ALU.mult), [c_r], [c_r])

            bankA = RR([0, 1])
            bankB = RR([2, 3])
            o512 = Slots(kb, "p1_o512", [128, 512], BF16, 3)
            o512b = Slots(kb, "p1_o512b", [128, 512], BF16, 3)

            def fm_block(wt, wr, c, tb, ncols=128):
                b = bankA.next()
                for kc in range(8):
                    mm(ps[b][0:ncols, :], wt[:, kc, c * 128:c * 128 + ncols], hT[:, kc, tb * 512:(tb + 1) * 512],
                       kc == 0, kc == 7, [wr, hT_r], [psr[b]], inc=(kc == 7))
                return b

            wt, wr = ws.load(w_in[l], O_Q, 512)
            for c in range(4):
                for tb in range(NB):
                    b = fm_block(wt, wr, c, tb)
                    qs, qr = o512.next()
                    aop(lambda b=b, qs=qs: A.mul(out=qs[:], in_=ps[b][:], mul=0.125), [psr[b]], [qr])
                    for hh in range(2):
                        for rc in range(2):
                            b2 = bankB.next()
                            mm(ps[b2][:], wuk_b[hh * 64:(hh + 1) * 64, c, rc * 128:(rc + 1) * 128],
                               qs[hh * 64:(hh + 1) * 64, :], True, True, [c_r, qr], [psr[b2]])
                            ql, qlr = o512b.next()
                            vop(lambda b2=b2, ql=ql: V.tensor_copy(out=ql[:], in_=ps[b2][:]), [psr[b2]], [qlr])
                            kb.dma(out=qlat_d[2 * c + hh, rc * 128:(rc + 1) * 128, tb * 512:(tb + 1) * 512], in_=ql[:],
                                   reads=[qlr], writes=[R_["qlat"]], own=qlr)
            wt, wr = ws.load(w_in[l], O_IQ, 256)
            for c in range(2):
                for tb in range(NB):
                    b = fm_block(wt, wr, c, tb)
                    qs, qr = o512.next()
                    aop(lambda b=b, qs=qs: A.mul(out=qs[:], in_=ps[b][:], mul=0.125), [psr[b]], [qr])
                    kb.dma(out=iq_d[c * 128:(c + 1) * 128, tb * 512:(tb + 1) * 512], in_=qs[:], reads=[qr],
                           writes=[R_["iq"]], own=qr)
            cg = kb.sb("p1_cg", [128, S], F32)
            cg_r = kb.res("p1_cg")
            U = kb.sb("p1_U", [128, S + 4], F32)
            U_r = kb.res("p1_U")
            vop(lambda: V.memset(U[:, 0:4], 0.0), [], [U_r])
            wbg, wbg_r = ws.load(w_in[l], O_BG, 512)
            wcg, wcg_r = ws.load(w_in[l], O_CG, 512)
            whb, whb_r = ws.load(w_in[l], O_HB, 512)
            for c in range(4):
                for tb in range(NB):
                    b = fm_block(wcg, wcg_r, c, tb)
                    aop(lambda b=b, tb=tb: A.copy(out=cg[:, tb * 512:(tb + 1) * 512], in_=ps[b][:]), [psr[b]], [cg_r])
                for tb in range(NB):
                    b = fm_block(whb, whb_r, c, tb)
                    vop(lambda b=b, tb=tb: V.tensor_tensor(out=U[:, 4 + tb * 512:4 + (tb + 1) * 512], in0=ps[b][:],
                                                           in1=cg[:, tb * 512:(tb + 1) * 512], op=ALU.mult),
                        [psr[b], cg_r], [U_r])
                vop(lambda c=c: V.tensor_scalar(out=cg[:, :], in0=U[:, 2:S + 2], scalar1=scw_t[:, c, 0:1], scalar2=None,
                                                op0=ALU.mult), [U_r, c_r], [cg_r])
                for j in (1, 2):
                    vop(lambda c=c, j=j: V.scalar_tensor_tensor(out=cg[:, :], in0=U[:, 2 + j:S + 2 + j], scalar=scw_t[:, c, j:j + 1],
                                                                in1=cg[:, :], op0=ALU.mult, op1=ALU.add), [U_r, c_r, cg_r], [cg_r])
                for tb in range(NB):
                    b = fm_block(wbg, wbg_r, c, tb)
                    ob, obr = o512.next()
                    vop(lambda b=b, tb=tb, ob=ob: V.tensor_tensor(out=ob[:], in0=ps[b][:], in1=cg[:, tb * 512:(tb + 1) * 512],
                                                                  op=ALU.mult), [psr[b], cg_r], [obr])
                    kb.dma(out=mixT_d[512 + c * 128:512 + (c + 1) * 128, tb * 512:(tb + 1) * 512], in_=ob[:], reads=[obr],
                           writes=[R_["mixT"]], own=obr)
            sx = kb.sb("p1_sx", [128, S], BF16)
            sx_r = kb.res("p1_sx")
            U2 = kb.sb("p1_U2", [128, S + 4], F32)
            U2_r = kb.res("p1_U2")
            vop(lambda: V.memset(U2[:, 0:4], 0.0), [], [U2_r])
            Ua = [(U, U_r), (U2, U2_r)]
            tstg = Slots(kb, "p1_tstg", [128, 4, 128], BF16, 2)
            wts = {}

            def d_mm(cc):
                blk, c = divmod(cc, 4)
                if c == 0:
                    wts[blk] = ws.load(w_in[l], O_XBC + blk * 512, 512)
                wt, wr = wts[blk]
                U, U_r = Ua[cc % 2]
                for tb in range(NB):
                    b = fm_block(wt, wr, c, tb)
                    aop(lambda: A.copy(out=U[:, 4 + tb * 512:4 + (tb + 1) * 512], in_=ps[b][:]), [psr[b]], [U_r])

            def d_post(cc):
                U, U_r = Ua[cc % 2]
                vop(lambda: V.tensor_scalar(out=cg[:, :], in0=U[:, 1:S + 1], scalar1=sscw_t[:, cc, 0:1],
                                            scalar2=sscb_t[:, cc:cc + 1], op0=ALU.mult, op1=ALU.add), [U_r, c_r], [cg_r])
                for j in (1, 2, 3):
                    vop(lambda: V.scalar_tensor_tensor(out=cg[:, :], in0=U[:, 1 + j:S + 1 + j], scalar=sscw_t[:, cc, j:j + 1], in1=cg[:, :],
                                                       op0=ALU.mult, op1=ALU.add), [U_r, c_r, cg_r], [cg_r])
                aop(lambda: A.activation(out=sx[:, :], in_=cg[:, :], func=AF.Silu), [cg_r], [sx_r])
                if cc >= 8:
                    g = (cc - 8) % 2
                    dst = bmT_d if cc < 10 else cmT_d
                    kb.dma(out=dst[g * 128:(g + 1) * 128, :], in_=sx[:, :], reads=[sx_r],
                           writes=[R_["bmT" if cc < 10 else "cmT"]], own=sx_r)
                if cc < 10:
                    for t4 in range(NT // 4):
                        pb = bankB.next()
                        pv = ps[pb][:].bitcast(BF16)
                        for j in range(4):
                            tt = t4 * 4 + j
                            tp(pv[:, j * 128:(j + 1) * 128], sx[:, tt * 128:(tt + 1) * 128], [sx_r], [psr[pb]], inc=(j == 3))
                        tsg, tsr = tstg.next()
                        vop(lambda: V.tensor_copy(out=tsg[:], in_=pv[:, 0:512].rearrange("p (j c) -> p j c", j=4)), [psr[pb]], [tsr])
                        if cc < 8:
                            dst = xsc_d.rearrange("(tt p) c -> p tt c", p=128)[:, t4 * 4:(t4 + 1) * 4, cc * 128:(cc + 1) * 128]
                            rn = "xsc"
                        else:
                            dst = bm_d.rearrange("(tt p) c -> p tt c", p=128)[:, t4 * 4:(t4 + 1) * 4, (cc - 8) * 128:(cc - 7) * 128]
                            rn = "bm"
                        kb.dma(out=dst, in_=tsg[:], reads=[tsr], writes=[R_[rn]], own=tsr)

            d_mm(0)
            for cc in range(12):
                if cc + 1 < 12:
                    d_mm(cc + 1)
                d_post(cc)
            zsl = Slots(kb, "p1_z", [128, 512], F32, 3)
            for zb in range(2):
                wt, wr = ws.load(w_in[l], O_Z + zb * 512, 512)
                for tt in range(NT):
                    b = bankA.next()
                    for kc in range(8):
                        mm(ps[b][:], hT[:, kc, tt * 128:(tt + 1) * 128], wt[:, kc, 0:512], kc == 0, kc == 7, [wr, hT_r], [psr[b]],
                           inc=(kc == 7))
                    zt, zr = zsl.next()
                    aop(lambda b=b, zt=zt: A.activation(out=zt[:], in_=ps[b][:], func=AF.Silu), [psr[b]], [zr])
                    kb.dma(out=zs_d[tt * 128:(tt + 1) * 128, zb * 512:(zb + 1) * 512], in_=zt[:], reads=[zr], writes=[R_["zs"]], own=zr)
            wt, wr = ws.wb.next()
            wv = w_in[l].rearrange("(kc p) c -> p kc c", p=128)
            st_, sr_ = ws.stg.next()
            kb.dma(out=st_[:, :, 0:256], in_=wv[:, :, O_CKV:O_CKV + 256], writes=[sr_], own=sr_)
            gop(lambda: G.tensor_copy(out=wt[:, :, 0:256], in_=st_[:, :, 0:256]), [sr_], [wr])
            st_, sr_ = ws.stg.next()
            kb.dma(out=st_[:, :, 0:68], in_=wv[:, :, O_IK:O_IK + 68], writes=[sr_], own=sr_)
            kb.dma(out=st_[:, :, 68:84], in_=wv[:, :, O_DT:O_DT + 16], writes=[sr_], own=sr_)
            gop(lambda: G.tensor_copy(out=wt[:, :, 256:340], in_=st_[:, :, 0:84]), [sr_], [wr])
            stl = Slots(kb, "p1_st", [128, 16], F32, 2)
            junk = kb.sb("p1_junk", [128, 256], F32)
            junk_r = kb.res("p1_junk")
            cnl = Slots(kb, "p1_cn", [128, 256], BF16, 2)
            ctl = Slots(kb, "p1_ct", [128, 2, 128], BF16, 2)
            ikl = Slots(kb, "p1_ik", [128, 64], F32, 2)
            iknl = Slots(kb, "p1_ikn", [128, 64], BF16, 2)
            iktl = Slots(kb, "p1_ikt", [64, 128], BF16, 2)
            iwl = Slots(kb, "p1_iw", [128, 4], F32, 2)
            dtl = Slots(kb, "p1_dt", [128, 32], F32, 2)
            E5 = {}

            def e_mm(tt):
                b = bankA.next()
                for kc in range(8):
                    mm(ps[b][:, 0:340], hT[:, kc, tt * 128:(tt + 1) * 128], wt[:, kc, 0:340], kc == 0, kc == 7, [wr, hT_r], [psr[b]],
                       inc=(kc == 7))
                E5[tt] = b

            e_mm(0)
            for tt in range(NT):
                if tt + 1 < NT:
                    e_mm(tt + 1)
                b = E5.pop(tt)
                P = ps[b]
                st, str_ = stl.next()
                aop(lambda P=P, st=st: A.activation(out=junk[:, 0:256], in_=P[:, 0:256], func=AF.Square, accum_out=st[:, 0:1]),
                    [psr[b]], [junk_r, str_])
                rstd(st, str_, 0, 2, R)
                cn, cnr = cnl.next()
                vop(lambda P=P, st=st, cn=cn: V.scalar_tensor_tensor(out=cn[:], in0=P[:, 0:256], scalar=st[:, 2:3], in1=kvn_bc[:],
                                                                    op0=ALU.mult, op1=ALU.mult), [psr[b], str_, c_r], [cnr])
                kb.dma(out=ckv_d[tt * 128:(tt + 1) * 128, :], in_=cn[:], reads=[cnr], writes=[R_["ckv"]], own=cnr)
                pb = bankB.next()
                pv = ps[pb][:].bitcast(BF16)
                for rc in range(2):
                    tp(pv[:, rc * 128:(rc + 1) * 128], cn[:, rc * 128:(rc + 1) * 128], [cnr], [psr[pb]], inc=(rc == 1))
                ct, ctr = ctl.next()
                aop(lambda pv=pv, ct=ct: A.copy(out=ct[:], in_=pv[:, 0:256].rearrange("p (j c) -> p j c", j=2)), [psr[pb]], [ctr])
                kb.dma(out=ckvT_d.rearrange("(rc p) s -> p rc s", p=128)[:, :, tt * 128:(tt + 1) * 128], in_=ct[:], reads=[ctr],
                       writes=[R_["ckvT"]], own=ctr)
                vop(lambda P=P, st=st: V.tensor_reduce(out=st[:, 4:5], in_=P[:, 256:320], axis=AX.X, op=ALU.add), [psr[b]], [str_])
                aop(lambda P=P, st=st: A.activation(out=junk[:, 0:64], in_=P[:, 256:320], func=AF.Square, accum_out=st[:, 5:6]),
                    [psr[b]], [junk_r, str_])
                vop(lambda st=st: V.tensor_scalar(out=st[:, 6:7], in0=st[:, 4:5], scalar1=1.0 / 64, scalar2=None, op0=ALU.mult), [str_], [str_])
                vop(lambda st=st: V.tensor_tensor(out=st[:, 7:8], in0=st[:, 6:7], in1=st[:, 6:7], op=ALU.mult), [str_], [str_])
                vop(lambda st=st: V.scalar_tensor_tensor(out=st[:, 8:9], in0=st[:, 5:6], scalar=1.0 / 64, in1=st[:, 7:8],
                                                         op0=ALU.mult, op1=ALU.subtract), [str_], [str_])
                rstd(st, str_, 8, 9, 1)
                ik, ikr = ikl.next()
                vop(lambda P=P, st=st, ik=ik: V.tensor_scalar(out=ik[:], in0=P[:, 256:320], scalar1=st[:, 6:7], scalar2=st[:, 9:10],
                                                              op0=ALU.subtract, op1=ALU.mult), [psr[b], str_], [ikr])
                vop(lambda ik=ik: V.tensor_tensor(out=ik[:], in0=ik[:], in1=ikg_bc[:], op=ALU.mult), [ikr, c_r], [ikr])
                ikn, iknr = iknl.next()
                vop(lambda ik=ik, ikn=ikn: V.tensor_tensor(out=ikn[:], in0=ik[:], in1=ikb_bc[:], op=ALU.add), [ikr, c_r], [iknr])
                pb = bankB.next()
                pv = ps[pb][:].bitcast(BF16)
                tp(pv[0:64, 0:128], ikn[:, 0:64], [iknr], [psr[pb]])
                ikt, iktr = iktl.next()
                aop(lambda pv=pv, ikt=ikt: A.copy(out=ikt[:], in_=pv[0:64, 0:128]), [psr[pb]], [iktr])
                kb.dma(out=ik_d[:, tt * 128:(tt + 1) * 128], in_=ikt[:], reads=[iktr], writes=[R_["ik"]], own=iktr)
                iwt, iwr = iwl.next()
                aop(lambda P=P, iwt=iwt: A.mul(out=iwt[:], in_=P[:, 320:324], mul=0.5), [psr[b]], [iwr])
                kb.dma(out=iw_d[tt * 128:(tt + 1) * 128, :], in_=iwt[:], reads=[iwr], writes=[R_["iw"]], own=iwr)
                dtt, dtr = dtl.next()
                vop(lambda P=P, dtt=dtt: V.tensor_tensor(out=dtt[:, 16:32], in0=P[:, 324:340], in1=v16_bc[:, 0, :], op=ALU.add),
                    [psr[b], c_r], [dtr])
                aop(lambda dtt=dtt: A.activation(out=dtt[:, 16:32], in_=dtt[:, 16:32], func=AF.Exp), [dtr], [dtr])
                aop(lambda dtt=dtt: A.activation(out=dtt[:, 0:16], in_=dtt[:, 16:32], func=AF.Ln, bias=1.0, scale=1.0), [dtr], [dtr])
                vop(lambda dtt=dtt: V.tensor_tensor(out=dtt[:, 16:32], in0=dtt[:, 0:16], in1=A_bc[:], op=ALU.mult), [dtr, c_r], [dtr])
                kb.dma(out=dt_d[tt * 128:(tt + 1) * 128, :], in_=dtt[:], reads=[dtr], writes=[R_["dt"]], own=dtr)

    NIT = 14

    def gen2(l):
        c_r = kb.res("p2c")
        iqT = kb.sb("p2_iqT", [128, 2, S], BF16)
        ikT = kb.sb("p2_ikT", [128, S], BF16)
        iwa = kb.sb("p2_iw", [128, NT, 4], F32)
        kb.dma(out=iqT[:], in_=iq_d.rearrange("(c p) s -> p c s", p=128), reads=[R_["iq"]], writes=[c_r], own=c_r)
        kb.dma(out=ikT[0:64, :], in_=ik_d, reads=[R_["ik"]], writes=[c_r], own=c_r)
        kb.dma(out=ikT[64:128, :], in_=ik_d, reads=[R_["ik"]], writes=[c_r], own=c_r)
        for t0_ in range(0, NT, 8):
            t1_ = min(NT, t0_ + 8)
            kb.dma(out=iwa[:, t0_:t1_, :], in_=iw_d.rearrange("(t p) h -> p t h", p=128)[:, t0_:t1_, :], reads=[R_["iw"]], writes=[c_r], own=c_r)
        pw2 = kb.sb("p2_pw2", [128, NIT], F32)
        for k in range(NIT):
            gop(lambda: G.memset(pw2[:, k:k + 1], 2.0 ** (-(k + 1))), [], [c_r])
        SCl = Slots(kb, "p2_SC", [128, S], F32, 2)
        mkl = Slots(kb, "p2_mk", [128, S], BF16, 3)
        Rl = Slots(kb, "p2_R", [128, 512], F32, 4)
        bl = Slots(kb, "p2_b", [128, 8 + 2 * NIT], F32, 4)
        nml = Slots(kb, "p2_nm", [128, 4, 128], BF16, 3)
        bankA = RR([0, 1])
        bankT = RR([2])
        yield

        def scores(qt, out):
            N = (qt + 1) * 128
            SC, SC_r = SCl.next()
            for sb in range((N + 511) // 512):
                cols = min(512, N - sb * 512)
                cs = slice(sb * 512, sb * 512 + cols)
                for h in range(4):
                    c, hh = divmod(h, 2)
                    pr_ = slice(hh * 64, (hh + 1) * 64)
                    b = bankA.next()
                    mm(ps[b][:, 0:cols], iqT[pr_, c, qt * 128:(qt + 1) * 128], ikT[pr_, cs], True, True, [c_r], [psr[b]])
                    Rt, Rr = Rl.next()
                    aop(lambda: A.activation(out=Rt[:, 0:cols], in_=ps[b][:, 0:cols], func=AF.Relu), [psr[b]], [Rr])
                    if h == 0:
                        vop(lambda: V.tensor_scalar(out=SC[:, cs], in0=Rt[:, 0:cols], scalar1=iwa[:, qt, 0:1], scalar2=None, op0=ALU.mult),
                            [Rr, c_r], [SC_r])
                    else:
                        vop(lambda: V.scalar_tensor_tensor(out=SC[:, cs], in0=Rt[:, 0:cols], scalar=iwa[:, qt, h:h + 1], in1=SC[:, cs],
                                                           op0=ALU.mult, op1=ALU.add), [Rr, c_r, SC_r], [SC_r])
                yield
            bt, b_r = bl.next()
            mk, mk_r = mkl.next()
            vop(lambda: V.tensor_reduce(out=bt[:, 0:1], in_=SC[:, 0:N], axis=AX.X, op=ALU.max), [SC_r], [b_r])
            vop(lambda: V.tensor_reduce(out=bt[:, 1:2], in_=SC[:, 0:N], axis=AX.X, op=ALU.min), [SC_r], [b_r])
            vop(lambda: V.memset(SC[0:64, N - 64:N], -1.0e30), [], [SC_r])
            vop(lambda: V.tensor_scalar(out=bt[:, 2:3], in0=bt[:, 1:2], scalar1=-0.01, scalar2=None, op0=ALU.add), [b_r], [b_r])
            vop(lambda: V.scalar_tensor_tensor(out=bt[:, 3:4], in0=bt[:, 0:1], scalar=0.02, in1=bt[:, 1:2], op0=ALU.add, op1=ALU.subtract),
                [b_r], [b_r])
            vop(lambda: V.tensor_scalar(out=bt[:, 8:8 + NIT], in0=pw2[:], scalar1=bt[:, 3:4], scalar2=None, op0=ALU.mult), [b_r, c_r], [b_r])
            vop(lambda: V.memset(bt[:, 8 + NIT:8 + 2 * NIT], 0.0), [], [b_r])
            out.update(dict(qt=qt, N=N, SC=SC, SC_r=SC_r, bt=bt, b_r=b_r, mk=mk, mk_r=mk_r))
            yield

        def first_mid(st, on_act):
            bt, b_r = st["bt"], st["b_r"]
            if on_act:
                vop(lambda: V.scalar_tensor_tensor(out=bt[:, 4:5], in0=bt[:, 2:3], scalar=-1.0, in1=bt[:, 8:9], op0=ALU.mult, op1=ALU.subtract),
                    [b_r], [b_r])
            else:
                vop(lambda: V.tensor_tensor(out=bt[:, 4:5], in0=bt[:, 2:3], in1=bt[:, 8:9], op=ALU.add), [b_r], [b_r])

        def iteration(st, k, on_act):
            N, SC, SC_r, bt, b_r = st["N"], st["SC"], st["SC_r"], st["bt"], st["b_r"]
            jt, jr = st["mk"], st["mk_r"]
            ck = 8 + NIT + k
            if on_act:
                aop(lambda: A.activation(out=jt[:, 0:N], in_=SC[:, 0:N], func=AF.Sign, bias=bt[:, 4:5], scale=1.0,
                                         accum_out=bt[:, ck:ck + 1]), [SC_r, b_r], [jr, b_r])
                thr = 511.0 - N
            else:
                vop(lambda: V.tensor_scalar(out=jt[:, 0:N], in0=SC[:, 0:N], scalar1=bt[:, 4:5], scalar2=0.0, op0=ALU.is_ge, op1=ALU.add,
                                            accum_out=bt[:, ck:ck + 1]), [SC_r, b_r], [jr, b_r])
                thr = 255.5
            vop(lambda: V.scalar_tensor_tensor(out=bt[:, 5:6], in0=bt[:, ck:ck + 1], scalar=thr, in1=bt[:, 8 + k:9 + k],
                                               op0=ALU.is_ge, op1=ALU.mult), [b_r], [b_r])
            vop(lambda: V.tensor_tensor(out=bt[:, 2:3], in0=bt[:, 2:3], in1=bt[:, 5:6], op=ALU.add), [b_r], [b_r])
            if k + 1 < NIT:
                if on_act:
                    vop(lambda: V.scalar_tensor_tensor(out=bt[:, 4:5], in0=bt[:, 2:3], scalar=-1.0, in1=bt[:, 9 + k:10 + k],
                                                       op0=ALU.mult, op1=ALU.subtract), [b_r], [b_r])
                else:
                    vop(lambda: V.tensor_tensor(out=bt[:, 4:5], in0=bt[:, 2:3], in1=bt[:, 9 + k:10 + k], op=ALU.add), [b_r], [b_r])

        def finalize(st):
            qt, N, SC, SC_r, bt, b_r, mk, mk_r = st["qt"], st["N"], st["SC"], st["SC_r"], st["bt"], st["b_r"], st["mk"], st["mk_r"]
            vop(lambda: V.tensor_scalar(out=mk[:, 0:N], in0=SC[:, 0:N], scalar1=bt[:, 2:3], scalar2=None, op0=ALU.is_ge), [SC_r, b_r], [mk_r])
            for k0 in range(0, qt + 1, 4):
                n = min(4, qt + 1 - k0)
                pb = bankT.next()
                pv = ps[pb][:].bitcast(BF16)
                for j in range(n):
                    tp(pv[:, j * 128:(j + 1) * 128], mk[:, (k0 + j) * 128:(k0 + j + 1) * 128], [mk_r], [psr[pb]], inc=(j == n - 1))
                nm, nm_r = nml.next()
                aop(lambda: A.copy(out=nm[:, 0:n, :], in_=pv[:, 0:n * 128].rearrange("p (j t) -> p j t", j=n)), [psr[pb]], [nm_r])
                kb.dma(out=nmT_d.rearrange("(kt p) t -> p kt t", p=128)[:, k0:k0 + n, qt * 128:(qt + 1) * 128], in_=nm[:, 0:n, :],
                       reads=[nm_r], writes=[R_["nmT"]], own=nm_r)
                yield

        for q0 in range(0, NT, 2):
            sa, sb_ = {}, {}
            yield from scores(q0, sa)
            yield from scores(q0 + 1, sb_)
            first_mid(sa, False)
            first_mid(sb_, True)
            for k in range(NIT):
                iteration(sa, k, False)
                iteration(sb_, k, True)
                yield
            yield from finalize(sa)
            yield from finalize(sb_)

    def run_interleaved(gens, weights=None):
        gens = list(gens)
        weights = list(weights or [1] * len(gens))
        acc = [0.0] * len(gens)
        alive = [True] * len(gens)
        while any(alive):
            for i, g in enumerate(gens):
                if not alive[i]:
                    continue
                acc[i] += weights[i]
                while acc[i] >= 1.0 and alive[i]:
                    acc[i] -= 1.0
                    try:
                        next(g)
                    except StopIteration:
                        alive[i] = False

    def phase2(l):
        with Phase():
            run_interleaved([gen2(l)])

    def phase24(l):
        with Phase():
            run_interleaved([gen2(l), gen4(l)], [1.0, 5.5])

    def phase3(l):
        with Phase():
            c_r = kb.res("p3c")
            ckvT = kb.sb("p3_ckvT", [128, 2, S], BF16)
            kb.dma(out=ckvT[:], in_=ckvT_d.rearrange("(rc p) s -> p rc s", p=128), reads=[R_["ckvT"]], writes=[c_r], own=c_r)
            ckva = kb.sb("p3_ckva", [128, NT, 258], BF16)
            gop(lambda: G.memset(ckva[:, :, 256:258], 1.0), [], [c_r])
            for t0_ in range(0, NT, 8):
                t1_ = min(NT, t0_ + 8)
                kb.dma(out=ckva[:, t0_:t1_, 0:256], in_=ckv_d.rearrange("(kt p) r -> p kt r", p=128)[:, t0_:t1_, :], reads=[R_["ckv"]], writes=[c_r], own=c_r)
            gt = kb.sb("p3_gt", [128, NH, GW], BF16)
            b15 = kb.sb("p3_b15", [128, NH], F32)
            gstg = Slots(kb, "p3_gstg", [128, GW], F32, 2)
            for h in range(NH):
                st, sr = gstg.next()
                kb.dma(out=st[:], in_=gtab[:, h, :], writes=[sr], own=sr)
                gop(lambda st=st, h=h: G.tensor_copy(out=gt[:, h, :], in_=st[:]), [sr], [c_r])
                gop(lambda st=st, h=h: G.tensor_copy(out=b15[:, h:h + 1], in_=st[:, GW - 1:GW]), [sr], [c_r])
            wuv_f = kb.sb("p3_wuvf", [128, 2, NH * 64], F32)
            wuv_b = kb.sb("p3_wuvb", [128, 2, NH * 64], BF16)
            kb.dma(out=wuv_f[:], in_=wuv[l], writes=[c_r], own=c_r)
            gop(lambda: G.tensor_copy(out=wuv_b[:], in_=wuv_f[:]), [c_r], [c_r])
            nml = Slots(kb, "p3_nm", [128, NT, 512], BF16, 2)
            ql = Slots(kb, "p3_q", [128, 2, 512], BF16, 3)
            El = Slots(kb, "p3_E", [128, 512], BF16, 3)
            PTl = Slots(kb, "p3_PT", [128, 512], BF16, 3)
            rsl = Slots(kb, "p3_rs", [128, 4], F32, 2)
            ol = Slots(kb, "p3_o", [128, 256], BF16, 8)
            oTl = Slots(kb, "p3_oT", [128, 2, 128], BF16, 2)
            aTl = Slots(kb, "p3_aT", [64, 512], BF16, 3)
            bankS = RR([0, 1, 2])
            nmv = nmT_d.rearrange("(kt p) t -> p kt t", p=128)
            nm_tiles = {}
            q_tiles = {}

            def load_nm(tb):
                if tb >= NB or tb in nm_tiles:
                    return
                nm, nm_r = nml.next()
                for t0_ in range(0, 4 * tb, 8):
                    t1_ = min(4 * tb, t0_ + 8)
                    kb.dma(out=nm[:, t0_:t1_, :], in_=nmv[:, t0_:t1_, tb * 512:(tb + 1) * 512], reads=[R_["nmT"]], writes=[nm_r], own=nm_r)
                for i in range(4):
                    kb.dma(out=nm[:, 4 * tb + i, i * 128:512], in_=nmv[:, 4 * tb + i, tb * 512 + i * 128:(tb + 1) * 512], reads=[R_["nmT"]],
                           writes=[nm_r], own=nm_r)
                nm_tiles[tb] = (nm, nm_r)

            def load_q(idx):
                if idx >= NB * NH or idx in q_tiles:
                    return
                tb, h = divmod(idx, NH)
                q, q_r = ql.next()
                kb.dma(out=q[:], in_=qlat_d[h].rearrange("(rc p) s -> p rc s", p=128)[:, :, tb * 512:(tb + 1) * 512], reads=[R_["qlat"]],
                       writes=[q_r], own=q_r)
                q_tiles[idx] = (q, q_r)

            groups = [(tb, h, kt) for tb in range(NB) for h in range(NH) for kt in range(4 * tb + 4)]
            sbank = {}

            def emit_qk(g):
                tb, h, kt = g
                if kt == 0:
                    load_nm(tb)
                    load_q(tb * NH + h)
                    load_q(tb * NH + h + 1)
                q, q_r = q_tiles[tb * NH + h]
                nm, nm_r = nm_tiles[tb]
                cl = max(0, kt - 4 * tb) * 128
                b = bankS.next()
                sbank[g] = b
                ks = slice(kt * 128, (kt + 1) * 128)
                c0 = tb * 512 - kt * 128 + 384
                const = c0 >= GC
                mm(ps[b][:, cl:512], ckvT[:, 0, ks], q[:, 0, cl:512], True, False, [c_r, q_r], [psr[b]], inc=False)
                mm(ps[b][:, cl:512], ckvT[:, 1, ks], q[:, 1, cl:512], False, const, [c_r, q_r], [psr[b]], inc=const)
                if not const:
                    mm(ps[b][:, cl:512], ident[:], gt[:, h, c0 + cl:c0 + 512], False, True, [cres, c_r], [psr[b]])

            def emit_rest(g):
                tb, h, kt = g
                if h == 0 and kt == 0:
                    load_nm(tb + 1)
                nm, nm_r = nm_tiles[tb]
                i = max(0, kt - 4 * tb)
                cl = i * 128
                b = sbank.pop(g)
                const = (tb * 512 - kt * 128 + 384) >= GC
                E, E_r = El.next()
                if const:
                    aop(lambda: A.activation(out=E[:, cl:512], in_=ps[b][:, cl:512], func=AF.Exp, bias=b15[:, h:h + 1], scale=1.0),
                        [psr[b], c_r], [E_r])
                else:
                    aop(lambda: A.activation(out=E[:, cl:512], in_=ps[b][:, cl:512], func=AF.Exp), [psr[b]], [E_r])
                PT, PT_r = PTl.next()
                vop(lambda: V.tensor_tensor(out=PT[:, cl:512], in0=E[:, cl:512], in1=nm[:, kt, cl:512], op=ALU.mult), [E_r, nm_r], [PT_r])
                for j in range(i, 4):
                    mm(ps[4 + j][:, 0:257], PT[:, j * 128:(j + 1) * 128], ckva[:, kt, 0:257], kt == 0, kt == 4 * tb + j, [PT_r, c_r],
                       [psr[4 + j]], inc=(j == 3 or kt == 4 * tb + j))

            def emit_post_head(tb, h):
                tiles = []
                for j in range(4):
                    rs, rs_r = rsl.next()
                    vop(lambda: V.reciprocal(out=rs[:, 0:1], in_=ps[4 + j][:, 256:257]), [psr[4 + j]], [rs_r])
                    o, o_r = ol.next()
                    aop(lambda: A.activation(out=o[:], in_=ps[4 + j][:, 0:256], func=AF.Identity, scale=rs[:, 0:1]),
                        [psr[4 + j], rs_r], [o_r])
                    tiles.append((o, o_r))
                q_tiles.pop(tb * NH + h, None)
                if h == NH - 1:
                    nm_tiles.pop(tb, None)
                return tiles

            def post_gen(tb, h, tiles):
                aT, aT_r = aTl.next()
                for j, (o, o_r) in enumerate(tiles):
                    pv = ps[3][:].bitcast(BF16)
                    for rc in range(2):
                        tp(pv[:, rc * 128:(rc + 1) * 128], o[:, rc * 128:(rc + 1) * 128], [o_r], [psr[3]], inc=(rc == 1))
                    yield
                    oT, oT_r = oTl.next()
                    vop(lambda: V.tensor_copy(out=oT[:], in_=pv[:, 0:256].rearrange("p (j t) -> p j t", j=2)), [psr[3]], [oT_r])
                    yield
                    for rc in range(2):
                        mm(ps[3][0:64, 256:384], wuv_b[:, rc, h * 64:(h + 1) * 64], oT[:, rc, :], rc == 0, rc == 1, [c_r, oT_r], [psr[3]],
                           inc=(rc == 1))
                    yield
                    vop(lambda: V.tensor_copy(out=aT[:, j * 128:(j + 1) * 128], in_=ps[3][0:64, 256:384]), [psr[3]], [aT_r])
                    yield
                kb.dma(out=mixT_d[h * 64:(h + 1) * 64, tb * 512:(tb + 1) * 512], in_=aT[:], reads=[aT_r], writes=[R_["mixT"]], own=aT_r)

            def drain(gen_):
                if gen_ is not None:
                    for _ in gen_:
                        pass

            pending = None
            emit_qk(groups[0])
            for idx, g in enumerate(groups):
                if idx + 1 < len(groups):
                    emit_qk(groups[idx + 1])
                emit_rest(g)
                tb, h, kt = g
                if pending is not None:
                    nsteps = -(-17 // (4 * tb + 4))
                    for _ in range(nsteps):
                        try:
                            next(pending)
                        except StopIteration:
                            pending = None
                            break
                if kt == 4 * tb + 3:
                    drain(pending)
                    tiles = emit_post_head(tb, h)
                    pending = post_gen(tb, h, tiles)
            drain(pending)

    def gen4(l):
        if True:
            c_r = kb.res("p4c")
            negtri4 = kb.sb("negtri4", [128, 4, 128], F32)
            gop(lambda: G.memset(negtri4[:], 0.0), [], [c_r])
            for j in range(4):
                gop(lambda j=j: G.affine_select(out=negtri4[:, j, :], in_=negtri4[:, j, :], pattern=[[1, 128]],
                                                compare_op=ALU.is_ge, fill=NEG, base=0, channel_multiplier=-1), [c_r], [c_r])
            v16_bc = kb.sb("p4_v16", [128, 3, 16], F32)
            kb.dma(out=v16_bc[:], in_=v16[l].partition_broadcast(128), writes=[c_r], own=c_r)
            gn = kb.sb("p4_gn", [128, D], F32)
            bcast_load(gn[:], vecD[l, 4, :], D, c_r)
            ST = kb.sb("p4_ST", [128, 2, 512], F32)
            ST_r = kb.res("p4_ST")
            STb = kb.sb("p4_STb", [128, 2, 512], BF16)
            STb_r = kb.res("p4_STb")
            vop(lambda: V.memset(ST[:], 0.0), [], [ST_r])
            vop(lambda: V.memset(STb[:], 0.0), [], [STb_r])
            xsl = Slots(kb, "p4_xs", [128, 16, 64], BF16, 2)
            bml = Slots(kb, "p4_bm", [128, 256], BF16, 2)
            bmTl = Slots(kb, "p4_bmT", [128, 2, 128], BF16, 2)
            cmTl = Slots(kb, "p4_cmT", [128, 2, 128], BF16, 2)
            dtl = Slots(kb, "p4_dt", [128, 32], F32, 2)
            zl = Slots(kb, "p4_z", [128, D], F32, 2)
            sml = Slots(kb, "p4_sm", [128, 6, 16], F32, 2)
            xdtl = Slots(kb, "p4_xdt", [128, 16, 64], BF16, 2)
            xdwl = Slots(kb, "p4_xdw", [128, 16, 64], BF16, 2)
            CBm = kb.sb("p4_CBm", [128, 2, 128], F32)
            CBm_r = kb.res("p4_CBm")
            rhsD = kb.sb("p4_rhsD", [128, 16, 128], F32)
            rhsD_r = kb.res("p4_rhsD")
            L = kb.sb("p4_L", [128, 16, 128], F32)
            L_r = kb.res("p4_L")
            M = kb.sb("p4_M", [128, 16, 128], BF16)
            M_r = kb.res("p4_M")
            Y = kb.sb("p4_Y", [128, 16, 64], F32)
            Y_r = kb.res("p4_Y")
            tmpY = kb.sb("p4_tmpY", [128, 16, 64], F32)
            tmpY_r = kb.res("p4_tmpY")
            Yn = kb.sb("p4_Yn", [128, D], BF16)
            Yn_r = kb.res("p4_Yn")
            stl = Slots(kb, "p4_st", [128, 16], F32, 2)
            cTl = Slots(kb, "p4_cT", [128, 8, 128], BF16, 2)
            bankD = RR([4])
            stA = {}

            def stageA(ct):
                rows = slice(ct * 128, (ct + 1) * 128)
                yb = (5, 6)
                xdt, xdt_r = xdtl.next()
                xdw, xdw_r = xdwl.next()
                xs, xs_r = xsl.next()
                kb.dma(out=xs[:].rearrange("p h d -> p (h d)"), in_=xsc_d[rows, :], reads=[R_["xsc"]], writes=[xs_r], own=xs_r)
                bm, bm_r = bml.next()
                kb.dma(out=bm[:], in_=bm_d[rows, :], reads=[R_["bm"]], writes=[bm_r], own=bm_r)
                bmT, bmT_r = bmTl.next()
                kb.dma(out=bmT[:], in_=bmT_d.rearrange("(g n) s -> n g s", g=2)[:, :, rows], reads=[R_["bmT"]], writes=[bmT_r], own=bmT_r)
                cmT, cmT_r = cmTl.next()
                kb.dma(out=cmT[:], in_=cmT_d.rearrange("(g n) s -> n g s", g=2)[:, :, rows], reads=[R_["cmT"]], writes=[cmT_r], own=cmT_r)
                dtt, dt_r = dtl.next()
                kb.dma(out=dtt[:], in_=dt_d[rows, :], reads=[R_["dt"]], writes=[dt_r], own=dt_r)
                zt, z_r = zl.next()
                kb.dma(out=zt[:], in_=zs_d[rows, :], reads=[R_["zs"]], writes=[z_r], own=z_r)
                sm, sm_r = sml.next()
                mm(ps[3][:, 0:16], utri_f[:], dtt[:, 16:32], True, True, [cres, dt_r], [psr[3]])
                yield
                mm(ps[3][:, 16:32], ones_f[:], dtt[:, 16:32], True, True, [cres, dt_r], [psr[3]])
                yield
                aop(lambda sm=sm: A.copy(out=sm[:, 0, :], in_=ps[3][:, 0:16]), [psr[3]], [sm_r])
                yield
                aop(lambda sm=sm: A.mul(out=sm[:, 1, :], in_=ps[3][:, 0:16], mul=-1.0), [psr[3]], [sm_r])
                yield
                aop(lambda sm=sm: A.activation(out=sm[:, 2, :], in_=ps[3][:, 0:16], func=AF.Exp), [psr[3]], [sm_r])
                yield
                aop(lambda sm=sm: A.activation(out=sm[:, 4, :], in_=ps[3][:, 16:32], func=AF.Exp), [psr[3]], [sm_r])
                yield
                vop(lambda sm=sm: V.tensor_tensor(out=sm[:, 5, :], in0=ps[3][:, 16:32], in1=sm[:, 0, :], op=ALU.subtract), [psr[3], sm_r], [sm_r])
                yield
                aop(lambda sm=sm: A.activation(out=sm[:, 3, :], in_=sm[:, 5, :], func=AF.Exp), [sm_r], [sm_r])
                yield
                vop(lambda xs=xs, dtt=dtt: V.tensor_tensor(out=xdt[:], in0=xs[:], in1=dtt[:, 0:16].unsqueeze(2).to_broadcast([128, 16, 64]),
                                                           op=ALU.mult), [xs_r, dt_r], [xdt_r])
                yield
                gop(lambda sm=sm: G.tensor_tensor(out=xdw[:], in0=xdt[:], in1=sm[:, 3, :].unsqueeze(2).to_broadcast([128, 16, 64]),
                                                  op=ALU.mult), [xdt_r, sm_r], [xdw_r])
                yield
                for g in range(2):
                    mm(ps[3][:, 64 + g * 128:64 + (g + 1) * 128], bmT[:, g, :], cmT[:, g, :], True, True, [bmT_r, cmT_r], [psr[3]], inc=(g == 1))
                    yield
                vop(lambda: V.tensor_tensor(out=CBm[:], in0=ps[3][:, 64:320].rearrange("p (g l) -> p g l", g=2),
                                            in1=tri_f[:].unsqueeze(1).to_broadcast([128, 2, 128]), op=ALU.mult), [psr[3], cres], [CBm_r])
                yield
                gop(lambda sm=sm: G.tensor_tensor(out=rhsD[:], in0=ident_f[:].unsqueeze(1).to_broadcast([128, 16, 128]),
                                                  in1=sm[:, 0, :].unsqueeze(2).to_broadcast([128, 16, 128]), op=ALU.mult),
                    [cres, sm_r], [rhsD_r])
                yield
                for q in range(4):
                    b = bankD.next()
                    mm(ps[b][:], ones_f[:], rhsD[:, 4 * q:4 * q + 4, :].rearrange("p h l -> p (h l)"), True, False, [cres, rhsD_r], [psr[b]],
                       inc=False)
                    yield
                    mm(ps[b][:], ident_f[:], negtri4[:].rearrange("p h l -> p (h l)"), False, True, [cres, c_r], [psr[b]])
                    yield
                    for hq in range(4):
                        h = 4 * q + hq
                        aop(lambda b=b, hq=hq, h=h, sm=sm: A.activation(out=L[:, h, :], in_=ps[b][:, hq * 128:(hq + 1) * 128], func=AF.Exp,
                                                                       bias=sm[:, 1, h:h + 1], scale=1.0), [psr[b], sm_r], [L_r])
                        yield
                for g in range(2):
                    gop(lambda g=g: G.tensor_tensor(out=M[:, g * 8:(g + 1) * 8, :], in0=L[:, g * 8:(g + 1) * 8, :],
                                                    in1=CBm[:, g, :].unsqueeze(1).to_broadcast([128, 8, 128]), op=ALU.mult),
                        [L_r, CBm_r], [M_r])
                    yield
                for h in range(16):
                    b = yb[h // 8]
                    hh = h % 8
                    mm(ps[b][:, hh * 64:(hh + 1) * 64], M[:, h, :], xdt[:, h, :], True, True, [M_r, xdt_r], [psr[b]], inc=(hh == 7))
                    yield
                stA[ct] = dict(rows=rows, yb=yb, xs=xs, xs_r=xs_r, bm=bm, bm_r=bm_r, cmT=cmT, cmT_r=cmT_r, zt=zt, z_r=z_r, sm=sm, sm_r=sm_r,
                               xdw=xdw, xdw_r=xdw_r)

            def stageB(ct):
                d = stA.pop(ct)
                rows, yb, xs, xs_r, bm, bm_r, cmT, cmT_r = d["rows"], d["yb"], d["xs"], d["xs_r"], d["bm"], d["bm_r"], d["cmT"], d["cmT_r"]
                zt, z_r, sm, sm_r, xdw, xdw_r = d["zt"], d["z_r"], d["sm"], d["sm_r"], d["xdw"], d["xdw_r"]
                yield
                for g in range(2):
                    mm(ps[7][:], cmT[:, g, :], STb[:, g, :], True, True, [cmT_r, STb_r], [psr[7]])
                    yield
                    vop(lambda g=g, sm=sm: V.tensor_tensor(out=Y[:, g * 8:(g + 1) * 8, :], in0=ps[7][:].rearrange("p (h d) -> p h d", h=8),
                                                           in1=sm[:, 2, g * 8:(g + 1) * 8].unsqueeze(2).to_broadcast([128, 8, 64]),
                                                           op=ALU.mult), [psr[7], sm_r], [Y_r])
                    yield
                    vop(lambda g=g: V.tensor_tensor(out=Y[:, g * 8:(g + 1) * 8, :], in0=Y[:, g * 8:(g + 1) * 8, :],
                                                    in1=ps[yb[g]][:].rearrange("p (h d) -> p h d", h=8), op=ALU.add),
                        [Y_r, psr[yb[g]]], [Y_r])
                    yield
                gop(lambda xs=xs: G.tensor_tensor(out=tmpY[:], in0=xs[:], in1=v16_bc[:, 2, :].unsqueeze(2).to_broadcast([128, 16, 64]),
                                                  op=ALU.mult), [xs_r, c_r], [tmpY_r])
                yield
                gop(lambda: G.tensor_tensor(out=Y[:], in0=Y[:], in1=tmpY[:], op=ALU.add), [Y_r, tmpY_r], [Y_r])
                yield
                for g in range(2):
                    mm(ps[7][:], bm[:, g * 128:(g + 1) * 128], xdw[:, g * 8:(g + 1) * 8, :].rearrange("p h d -> p (h d)"), True, True,
                       [bm_r, xdw_r], [psr[7]])
                    yield
                    vop(lambda g=g, sm=sm: V.tensor_tensor(out=ST[:, g, :].rearrange("p (h d) -> p h d", h=8),
                                                           in0=ST[:, g, :].rearrange("p (h d) -> p h d", h=8),
                                                           in1=sm[:, 4, g * 8:(g + 1) * 8].unsqueeze(2).to_broadcast([128, 8, 64]),
                                                           op=ALU.mult), [ST_r, sm_r, STb_r], [ST_r])
                    yield
                    vop(lambda g=g: V.tensor_tensor(out=ST[:, g, :], in0=ST[:, g, :], in1=ps[7][:], op=ALU.add), [ST_r, psr[7]], [ST_r])
                    yield
                aop(lambda: A.copy(out=STb[:], in_=ST[:]), [ST_r], [STb_r])
                yield
                Yf = Y[:].rearrange("p h d -> p (h d)")
                gop(lambda zt=zt: G.tensor_tensor(out=Yf, in0=Yf, in1=zt[:], op=ALU.mult), [Y_r, z_r], [Y_r])
                yield
                st, st_r = stl.next()
                for g in range(2):
                    aop(lambda g=g, st=st: A.activation(out=tmpY[:].rearrange("p h d -> p (h d)")[:, g * 512:(g + 1) * 512],
                                                        in_=Yf[:, g * 512:(g + 1) * 512], func=AF.Square, accum_out=st[:, g:g + 1]),
                        [Y_r], [tmpY_r, st_r])
                    yield
                    rstd(st, st_r, g, 4 + g, 512)
                    vop(lambda g=g, st=st: V.scalar_tensor_tensor(out=Yn[:, g * 512:(g + 1) * 512], in0=Yf[:, g * 512:(g + 1) * 512],
                                                                  scalar=st[:, 4 + g:5 + g], in1=gn[:, g * 512:(g + 1) * 512],
                                                                  op0=ALU.mult, op1=ALU.mult), [Y_r, st_r, c_r], [Yn_r])
                    yield
                cT, cT_r = cTl.next()
                pv = ps[7][:].bitcast(BF16)
                for c in range(8):
                    tp(pv[:, c * 128:(c + 1) * 128], Yn[:, c * 128:(c + 1) * 128], [Yn_r], [psr[7]], inc=(c == 7))
                    yield
                aop(lambda: A.copy(out=cT[:], in_=pv[:, 0:1024].rearrange("p (j t) -> p j t", j=8)), [psr[7]], [cT_r])
                yield
                kb.dma(out=mixT_d.rearrange("(kc p) s -> p kc s", p=128)[:, 8:16, rows], in_=cT[:], reads=[cT_r], writes=[R_["mixT"]], own=cT_r)

            yield
            for ct in range(NT):
                yield from stageA(ct)
                yield from stageB(ct)

    def phase4(l):
        with Phase():
            run_interleaved([gen4(l)])

    class WLoad:
        def __init__(self, name, kc=8, nc_=256, n=3):
            self.kc, self.nc_ = kc, nc_
            self.stg = Slots(kb, name + "_stg", [128, kc, nc_], F32, n)

        def load(self, dst, dst_r, wap, r0, kcn, c0, ncols):
            assert kcn <= self.kc and ncols <= self.nc_
            st, sr = self.stg.next()
            src = wap[r0:r0 + kcn * 128, :].rearrange("(kc p) c -> p kc c", p=128)[:, :, c0:c0 + ncols]
            kb.dma(out=st[:, 0:kcn, 0:ncols], in_=src, writes=[sr], own=sr)
            self.i = getattr(self, "i", 0) + 1
            e = ("dve", "act", "pool", "dve", "act")[self.i % 5]
            if e == "pool":
                gop(lambda: G.tensor_copy(out=dst, in_=st[:, 0:kcn, 0:ncols]), [sr], [dst_r])
            elif e == "dve":
                vop(lambda: V.tensor_copy(out=dst, in_=st[:, 0:kcn, 0:ncols]), [sr], [dst_r])
            else:
                aop(lambda: A.copy(out=dst, in_=st[:, 0:kcn, 0:ncols]), [sr], [dst_r])

        def load_full(self, dst_t, dst_r, wap, KC, C):
            for k0 in range(0, KC, self.kc):
                kn = min(self.kc, KC - k0)
                for c0 in range(0, C, self.nc_):
                    cn = min(self.nc_, C - c0)
                    self.load(dst_t[:, k0:k0 + kn, c0:c0 + cn], dst_r, wap, k0 * 128, kn, c0, cn)

    def post_norm_residual(P0, P1, r0, r1, xt, xr, g_bc, g_r, stl, junk, junk_r, tmp, tmp_r):
        st, st_r = stl.next()
        aop(lambda: A.activation(out=junk[:, 0:512], in_=P0[:], func=AF.Square, accum_out=st[:, 0:1]), [r0], [junk_r, st_r])
        aop(lambda: A.activation(out=junk[:, 512:1024], in_=P1[:], func=AF.Square, accum_out=st[:, 1:2]), [r1], [junk_r, st_r])
        vop(lambda: V.tensor_tensor(out=st[:, 3:4], in0=st[:, 0:1], in1=st[:, 1:2], op=ALU.add), [st_r], [st_r])
        rstd(st, st_r, 3, 2, D)
        for half, (P, r) in enumerate(((P0, r0), (P1, r1))):
            sl = slice(half * 512, (half + 1) * 512)
            vop(lambda P=P, sl=sl: V.scalar_tensor_tensor(out=tmp[:, sl], in0=P[:], scalar=st[:, 2:3], in1=g_bc[:, sl],
                                                         op0=ALU.mult, op1=ALU.mult), [r, st_r, g_r], [tmp_r])
            gop(lambda sl=sl: G.tensor_tensor(out=xt[:, sl], in0=tmp[:, sl], in1=xt[:, sl], op=ALU.add), [tmp_r, xr], [xr])

    def phase5(l):
        with Phase():
            src = x_in if l == 0 else xs_d
            wl = WLoad("p5w")
            wo = kb.sb("p5_wo", [128, 16, D], BF16)
            wo_r = kb.res("p5_wo")
            wl.load_full(wo, wo_r, w_out[l], 16, D)
            g1 = kb.sb("p5_g1", [128, D], F32)
            g2 = kb.sb("p5_g2", [128, D], F32)
            g_r = kb.res("p5_g")
            bcast_load(g1[:], vecD[l, 1, :], D, g_r)
            bcast_load(g2[:], vecD[l, 2, :], D, g_r)
            xsl = Slots(kb, "p5_x", [128, D], F32, 2)
            mxl = Slots(kb, "p5_mx", [128, 16, 128], BF16, 2)
            junk = kb.sb("p5_junk", [128, D], F32)
            junk_r = kb.res("p5_junk")
            tmp = kb.sb("p5_tmp", [128, D], F32)
            tmp_r = kb.res("p5_tmp")
            stl = Slots(kb, "p5_st", [128, 16], F32, 2)
            hb = kb.sb("p5_hb", [128, D], BF16)
            ntmp = (junk, junk_r, stl, hb, kb.res("p5_hb"))
            banks = RR([0, 1, 2, 3])
            S5 = {}

            def p5_stage1(tt):
                xt, xr = xsl.next()
                kb.dma(out=xt[:], in_=src[tt * 128:(tt + 1) * 128, :], reads=[R_["xs"]], writes=[xr], own=xr)
                mx, mr = mxl.next()
                kb.dma(out=mx[:], in_=mixT_d.rearrange("(kc p) s -> p kc s", p=128)[:, :, tt * 128:(tt + 1) * 128],
                       reads=[R_["mixT"]], writes=[mr], own=mr)
                b0, b1 = banks.next(), banks.next()
                for half, b in enumerate((b0, b1)):
                    for kc in range(16):
                        mm(ps[b][:], mx[:, kc, :], wo[:, kc, half * 512:(half + 1) * 512], kc == 0, kc == 15, [mr, wo_r], [psr[b]],
                           inc=(kc == 15))
                S5[tt] = (xt, xr, b0, b1)

            def p5_stage2(tt):
                xt, xr, b0, b1 = S5.pop(tt)
                post_norm_residual(ps[b0], ps[b1], psr[b0], psr[b1], xt, xr, g1, g_r, stl, junk, junk_r, tmp, tmp_r)
                kb.dma(out=xm_d[tt * 128:(tt + 1) * 128, :], in_=xt[:], reads=[xr], writes=[R_["xm"]], own=xr)
                norm_to_hT(xt[:], xr, g2, g_r, tt, ntmp)

            p5_stage1(0)
            for tt in range(NT):
                if tt + 1 < NT:
                    p5_stage1(tt + 1)
                p5_stage2(tt)

    def phase6a(l):
        with Phase():
            hT, hT_r = HT['t'], HT['r']
            wl = WLoad("p6w", 8, 512, 2)
            wgl = Slots(kb, "p6_wg", [128, 8, 512], BF16, 2)
            wul = Slots(kb, "p6_wu", [128, 8, 512], BF16, 2)
            sgl = Slots(kb, "p6_sg", [128, 512], F32, 2)
            gul = Slots(kb, "p6_gu", [128, 512], BF16, 3)
            bankA = RR([0, 1])
            bankB = RR([2, 3])
            for c0 in range(0, DFF, 512):
                cn = min(512, DFF - c0)
                wg_t, wg_r = wgl.next()
                wu_t, wu_r = wul.next()
                wl.load(wg_t[:, :, 0:cn], wg_r, w_g[l], 0, 8, c0, cn)
                wl.load(wu_t[:, :, 0:cn], wu_r, w_u[l], 0, 8, c0, cn)
                for c in range(cn // 128):
                    for tb in range(NB):
                        bg_, bu_ = bankA.next(), bankB.next()
                        for kc in range(8):
                            mm(ps[bg_][:], wg_t[:, kc, c * 128:(c + 1) * 128], hT[:, kc, tb * 512:(tb + 1) * 512], kc == 0, kc == 7,
                               [wg_r, hT_r], [psr[bg_]], inc=(kc == 7))
                        for kc in range(8):
                            mm(ps[bu_][:], wu_t[:, kc, c * 128:(c + 1) * 128], hT[:, kc, tb * 512:(tb + 1) * 512], kc == 0, kc == 7,
                               [wu_r, hT_r], [psr[bu_]], inc=(kc == 7))
                        sg, sgr = sgl.next()
                        aop(lambda sg=sg, bg_=bg_: A.activation(out=sg[:], in_=ps[bg_][:], func=AF.Silu), [psr[bg_]], [sgr])
                        gu, gur = gul.next()
                        vop(lambda sg=sg, gu=gu, bu_=bu_: V.tensor_tensor(out=gu[:], in0=ps[bu_][:], in1=sg[:], op=ALU.mult),
                            [psr[bu_], sgr], [gur])
                        f0 = c0 + c * 128
                        kb.dma(out=gu_d[f0:f0 + 128, tb * 512:(tb + 1) * 512], in_=gu[:], reads=[gur], writes=[R_["gu"]], own=gur)

    def phase6b(l):
        last = (l == DEPTH - 1)
        with Phase():
            wl = WLoad("p6bw")
            KF = DFF // 128
            wd = kb.sb("p6b_wd", [128, KF, D], BF16)
            wd_r = kb.res("p6b_wd")
            wl.load_full(wd, wd_r, w_d[l], KF, D)
            wpg = kb.sb("p6b_wpg", [128, 8, D], BF16)
            wpg_r = kb.res("p6b_wpg")
            wl.load_full(wpg, wpg_r, w_pg[l], 8, D)
            wpp = kb.sb("p6b_wpp", [128, 2, D], BF16)
            wpp_r = kb.res("p6b_wpp")
            wl.load_full(wpp, wpp_r, w_pp[l], 2, D)
            g1 = kb.sb("p6b_g1", [128, D], F32)
            g_r = kb.res("p6b_g")
            bcast_load(g1[:], vecD[l, 3, :], D, g_r)
            xsl = Slots(kb, "p6b_x", [128, D], F32, 2)
            gtl = Slots(kb, "p6b_gt", [128, KF, 128], BF16, 2)
            pl = Slots(kb, "p6b_p", [128, 256], F32, 2)
            junk = kb.sb("p6b_junk", [128, D], F32)
            junk_r = kb.res("p6b_junk")
            tmp = kb.sb("p6b_tmp", [128, D], F32)
            tmp_r = kb.res("p6b_tmp")
            stl = Slots(kb, "p6b_st", [128, 16], F32, 2)
            xb = kb.sb("p6b_xb", [128, D], BF16)
            xb_r = kb.res("p6b_xb")
            xT = kb.sb("p6b_xT", [128, 8, 128], BF16)
            xT_r = kb.res("p6b_xT")
            pb16 = kb.sb("p6b_pb", [128, 256], BF16)
            pb_r = kb.res("p6b_pb")
            pT = kb.sb("p6b_pT", [128, 2, 128], BF16)
            pT_r = kb.res("p6b_pT")
            sg = kb.sb("p6b_sg", [128, D], F32)
            sg_r = kb.res("p6b_sg")
            dst_d = y_out if last else xs_d
            dst_r = R_["y"] if last else R_["xs"]
            S1 = {}

            def stage1(tt):
                xt, xr = xsl.next()
                kb.dma(out=xt[:], in_=xm_d[tt * 128:(tt + 1) * 128, :], reads=[R_["xm"]], writes=[xr], own=xr)
                gt, gr = gtl.next()
                kb.dma(out=gt[:], in_=gu_d.rearrange("(kc p) s -> p kc s", p=128)[:, :, tt * 128:(tt + 1) * 128],
                       reads=[R_["gu"]], writes=[gr], own=gr)
                pt, pr = pl.next()
                kb.dma(out=pt[:], in_=p_in[l, tt * 128:(tt + 1) * 128, :], writes=[pr], own=pr)
                db = (0, 1) if tt % 2 == 0 else (2, 3)
                for half, b in enumerate(db):
                    for kc in range(KF):
                        mm(ps[b][:], gt[:, kc, :], wd[:, kc, half * 512:(half + 1) * 512], kc == 0, kc == KF - 1, [gr, wd_r], [psr[b]],
                           inc=(kc == KF - 1))
                S1[tt] = (xt, xr, pt, pr, db)

            def stage2(tt):
                xt, xr, pt, pr, db = S1.pop(tt)
                post_norm_residual(ps[db[0]], ps[db[1]], psr[db[0]], psr[db[1]], xt, xr, g1, g_r, stl, junk, junk_r, tmp, tmp_r)
                aop(lambda: A.copy(out=xb[:], in_=xt[:]), [xr], [xb_r])
                pv = ps[7][:].bitcast(BF16)
                for dc in range(8):
                    tp(pv[:, dc * 128:(dc + 1) * 128], xb[:, dc * 128:(dc + 1) * 128], [xb_r], [psr[7]], inc=(dc == 7))
                vop(lambda: V.tensor_copy(out=xT[:], in_=pv[:, 0:1024].rearrange("p (j t) -> p j t", j=8)), [psr[7]], [xT_r])
                for half, b in enumerate((4, 5)):
                    for kc in range(8):
                        mm(ps[b][:], xT[:, kc, :], wpg[:, kc, half * 512:(half + 1) * 512], kc == 0, kc == 7, [xT_r, wpg_r], [psr[b]],
                           inc=(kc == 7))
                    aop(lambda: A.activation(out=sg[:, half * 512:(half + 1) * 512], in_=ps[b][:], func=AF.Sigmoid), [psr[b]], [sg_r])
                vop(lambda: V.tensor_copy(out=pb16[:], in_=pt[:]), [pr], [pb_r])
                pv6 = ps[6][:].bitcast(BF16)
                for j in range(2):
                    tp(pv6[:, j * 128:(j + 1) * 128], pb16[:, j * 128:(j + 1) * 128], [pb_r], [psr[6]], inc=(j == 1))
                vop(lambda: V.tensor_copy(out=pT[:], in_=pv6[:, 0:256].rearrange("p (j t) -> p j t", j=2)), [psr[6]], [pT_r])
                for half in range(2):
                    for kc in range(2):
                        mm(ps[6][:], pT[:, kc, :], wpp[:, kc, half * 512:(half + 1) * 512], kc == 0, kc == 1, [pT_r, wpp_r], [psr[6]],
                           inc=(kc == 1))
                    sl = slice(half * 512, (half + 1) * 512)
                    vop(lambda: V.tensor_tensor(out=tmp[:, sl], in0=ps[6][:], in1=sg[:, sl], op=ALU.mult), [psr[6], sg_r], [tmp_r])
                    gop(lambda: G.tensor_tensor(out=xt[:, sl], in0=tmp[:, sl], in1=xt[:, sl], op=ALU.add), [tmp_r, xr], [xr])
                kb.dma(out=dst_d[tt * 128:(tt + 1) * 128, :], in_=xt[:], reads=[xr], writes=[dst_r], own=xr)

            stage1(0)
            for tt in range(NT):
                if tt + 1 < NT:
                    stage1(tt + 1)
                stage2(tt)

    def front(l):
        with HTScope():
            phase0(l, None if l == 0 else xs_d)
            phase1(l)

    def mid(l):
        phase24(l)
        phase3(l)

    def back(l):
        with HTScope():
            phase5(l)
            phase6a(l)
        phase6b(l)

    def run_all():
        for l in range(DEPTH):
            front(l)
            mid(l)
            back(l)

    phases = phases or ["all"]
    kb.fn = dict(front=front, mid=mid, back=back, phase2=phase2, phase3=phase3, phase4=phase4, phase24=phase24)
    if phases == ["all"]:
        run_all()
    else:
        for ph in phases:
            name, l = ph
            kb.fn[name](l)
    barrier()
    return nc, kb


def prep_inputs(inp, S):
    f = lambda a: np.ascontiguousarray(np.asarray(a, dtype=np.float32))
    w_uk = f(inp["w_uk"])
    wukT = np.ascontiguousarray(w_uk.reshape(DEPTH, R, 4, 2, 64).transpose(0, 3, 4, 2, 1).reshape(DEPTH, 128, 4, R))
    w_uv = f(inp["w_uv"])
    wuv = np.ascontiguousarray(w_uv.reshape(DEPTH, 2, 128, NH * 64).transpose(0, 2, 1, 3))
    rel = f(inp["rel_bias"])
    i = np.arange(128)[:, None]
    c = np.arange(GW)[None, :]
    bucket = t5_bucket_np((i - c + 384).astype(np.int32))
    gtab = np.ascontiguousarray(rel[bucket].transpose(0, 2, 1))
    vecD = np.ascontiguousarray(np.stack([f(inp["pre_mix_norm"]), f(inp["post_mix_norm"]), f(inp["pre_ffn_norm"]),
                                          f(inp["post_ffn_norm"]), f(inp["ssm_norm"])], axis=1))
    scw = np.ascontiguousarray(f(inp["short_conv_w"]).reshape(DEPTH, 3, 4, 128).transpose(0, 3, 2, 1))
    sscw = np.ascontiguousarray(f(inp["ssm_conv_w"]).reshape(DEPTH, 4, 12, 128).transpose(0, 3, 2, 1))
    sscb = np.ascontiguousarray(f(inp["ssm_conv_b"]).reshape(DEPTH, 12, 128).transpose(0, 2, 1))
    v16 = np.ascontiguousarray(np.stack([f(inp["ssm_dt_bias"]), f(inp["ssm_a_log"]), f(inp["ssm_d"])], axis=1))
    shared = dict(w_in=f(inp["w_in"]), w_out=f(inp["w_out"]), w_ffn_gate=f(inp["w_ffn_gate"]), w_ffn_up=f(inp["w_ffn_up"]),
                  w_ffn_down=f(inp["w_ffn_down"]), w_ple_proj=f(inp["w_ple_proj"]), w_ple_gate=f(inp["w_ple_gate"]),
                  wukT=wukT, wuv=wuv, gtab=gtab, vecD=vecD, kv_norm=f(inp["kv_norm"]), idx_k_norm_g=f(inp["idx_k_norm_g"]),
                  idx_k_norm_b=f(inp["idx_k_norm_b"]), scw=scw, sscw=sscw, sscb=sscb, v16=v16)
    x = f(inp["x"])
    p = f(inp["p"])
    B = x.shape[0]
    maps = []
    for b in range(B):
        m = dict(shared)
        m["x"] = np.ascontiguousarray(x[b, :S])
        m["p"] = np.ascontiguousarray(p[:, b, :S])
        maps.append(m)
    return maps


_CACHE = {}


def kernel(**inputs):
    S = 4096
    maps = prep_inputs(inputs, S)
    if "nc" not in _CACHE:
        _CACHE["nc"] = build_program(S)[0]
    res = run_bass_kernel_spmd(_CACHE["nc"], maps, core_ids=list(range(8)))
    return np.stack([np.asarray(r["y"], dtype=np.float32) for r in res.results], axis=0)
```

```python
import math
from contextlib import ExitStack
import numpy as np
import concourse.bass as bass
import concourse.mybir as mybir
from concourse.bass_utils import run_bass_kernel_spmd

F32 = mybir.dt.float32
BF16 = mybir.dt.bfloat16
AF = mybir.ActivationFunctionType
ALU = mybir.AluOpType
AX = mybir.AxisListType

D = 1024
DEPTH = 2
NH = 8
R = 256
IN_W = 5204
DFF = 2816
MIXW = 2048
EPS = 1e-6
NEG = -30000.0
O_Q, O_CKV, O_IQ, O_IK, O_IW, O_BG, O_CG, O_HB, O_Z, O_XBC, O_DT = 0, 512, 768, 1024, 1088, 1092, 1604, 2116, 2628, 3652, 5188
GW = 1582
GC = 1070
SEM_LIMIT = 30000


class Sem:
    def __init__(self, h):
        self.h = h
        self.val = 0


class Res:
    __slots__ = ("name", "w", "r", "dsem")

    def __init__(self, name):
        self.name = name
        self.w = {}
        self.r = {}
        self.dsem = None


class EngW:
    def __init__(self, name, eng, is_pe=False):
        self.name = name
        self.eng = eng
        self.is_pe = is_pe
        self.sem = None
        self.seen = {}


class KB:
    def __init__(self, nc):
        self.nc = nc
        self.nsem = 0
        self.pe = EngW("pe", nc.tensor, True)
        self.act = EngW("act", nc.scalar)
        self.dve = EngW("dve", nc.vector)
        self.pool = EngW("pool", nc.gpsimd)
        self.sp = EngW("sp", nc.sync)
        for e in (self.pe, self.act, self.dve, self.pool):
            e.sem = self.new_sem(e.name)
        self.stack = ExitStack()
        self.n_ops = 0
        self.free_dsems = []
        self.phase_dsems = [[]]

    def new_sem(self, name="s"):
        self.nsem += 1
        return Sem(self.nc.alloc_semaphore(name=f"{name}_{self.nsem}"))

    def res(self, name):
        return Res(name)

    def sb(self, name, shape, dt):
        self.n_sb = getattr(self, "n_sb", 0) + 1
        return self.stack.enter_context(self.nc.sbuf_tensor(f"{name}_{self.n_sb}", list(shape), dt))

    def _wait(self, e, reads, writes):
        raw = {}
        oth = {}
        for r in reads:
            for s, v in r.w.items():
                if raw.get(s, 0) < v:
                    raw[s] = v
        for w in writes:
            for s, v in w.w.items():
                if oth.get(s, 0) < v:
                    oth[s] = v
            for s, v in w.r.items():
                if oth.get(s, 0) < v:
                    oth[s] = v
        for s, v in oth.items():
            if s is e.sem and e.is_pe:
                continue
            if raw.get(s, 0) < v:
                raw[s] = v
        for s, v in raw.items():
            if s is e.sem and e.is_pe:
                continue
            if e.seen.get(s, 0) >= v:
                continue
            e.eng.wait_ge(s.h, v)
            e.seen[s] = v

    def op(self, e, fn, reads=(), writes=(), inc=True):
        self._wait(e, reads, writes)
        ins = fn()
        self.n_ops += 1
        if inc:
            if e.sem.val >= SEM_LIMIT and not getattr(e, "pending", False):
                e.sem = self.new_sem(e.name)
            e.sem.val += 1
            ins.then_inc(e.sem.h, 1)
            tv = e.sem.val
            e.pending = False
        else:
            tv = e.sem.val + 1
            e.pending = True
        for w in writes:
            w.w[e.sem] = tv
        for r in reads:
            r.r[e.sem] = tv
        return ins

    def dma(self, out, in_, reads=(), writes=(), q=None, own=None):
        q = q or self.sp
        self._wait(q, reads, writes)
        ins = q.eng.dma_start(out=out, in_=in_)
        self.n_ops += 1
        if own.dsem is None or own.dsem.val >= SEM_LIMIT:
            if getattr(self, "free_dsems", None):
                own.dsem = self.free_dsems.pop()
                if own.dsem.val >= SEM_LIMIT:
                    own.dsem = self.new_sem("d" + own.name)
            else:
                own.dsem = self.new_sem("d" + own.name)
            self.phase_dsems[-1].append(own.dsem)
        s = own.dsem
        s.val += 16
        ins.then_inc(s.h, 16)
        for w in writes:
            w.w[s] = s.val
        for r in reads:
            r.r[s] = s.val
        return ins

    def drain(self, e, ress):
        self._wait(e, ress, ())


class Slots:
    def __init__(self, kb, name, shape, dt, n):
        self.t = [kb.sb(f"{name}{i}", shape, dt) for i in range(n)]
        self.r = [kb.res(f"{name}{i}") for i in range(n)]
        self.i = 0
        self.n = n

    def next(self):
        k = self.i % self.n
        self.i += 1
        return self.t[k], self.r[k]


def t5_bucket_np(rel):
    half, max_exact = 16, 8
    ret = np.where(rel > 0, half, 0)
    n = np.abs(rel)
    nf = np.maximum(n, 1).astype(np.float32)
    large = max_exact + (np.log(nf / max_exact) / math.log(1024 / max_exact) * (half - max_exact)).astype(np.int32)
    large = np.minimum(large, half - 1)
    return ret + np.where(n < max_exact, n, large)


def build_program(S, dbg=False, phases=None):
    NT = S // 128
    NB = S // 512
    nc = bass.Bass("TRN2", target_bir_lowering=False)
    kb = KB(nc)
    pe, act, dve, pool, sp = kb.pe, kb.act, kb.dve, kb.pool, kb.sp
    T, V, A, G = nc.tensor, nc.vector, nc.scalar, nc.gpsimd

    def din(name, shape, dt=F32):
        return nc.dram_tensor(name, list(shape), dt, kind="ExternalInput").ap()

    def dscr(name, shape, dt):
        return nc.dram_tensor(name, list(shape), dt, kind="ExternalOutput" if dbg else "Internal").ap()

    x_in = din("x", [S, D])
    p_in = din("p", [DEPTH, S, 256])
    w_in = din("w_in", [DEPTH, D, IN_W])
    w_out = din("w_out", [DEPTH, MIXW, D])
    w_g = din("w_ffn_gate", [DEPTH, D, DFF])
    w_u = din("w_ffn_up", [DEPTH, D, DFF])
    w_d = din("w_ffn_down", [DEPTH, DFF, D])
    w_pp = din("w_ple_proj", [DEPTH, 256, D])
    w_pg = din("w_ple_gate", [DEPTH, D, D])
    wukT = din("wukT", [DEPTH, 128, 4, R])
    wuv = din("wuv", [DEPTH, 128, 2, NH * 64])
    gtab = din("gtab", [128, NH, GW])
    vecD = din("vecD", [DEPTH, 5, D])
    kvn = din("kv_norm", [DEPTH, R])
    ikg = din("idx_k_norm_g", [DEPTH, 64])
    ikb = din("idx_k_norm_b", [DEPTH, 64])
    scw = din("scw", [DEPTH, 128, 4, 3])
    sscw = din("sscw", [DEPTH, 128, 12, 4])
    sscb = din("sscb", [DEPTH, 128, 12])
    v16 = din("v16", [DEPTH, 3, 16])
    y_out = nc.dram_tensor("y", [S, D], F32, kind="ExternalOutput").ap()

    xs_d = dscr("xs_d", [S, D], F32)
    xm_d = dscr("xm_d", [S, D], F32)
    qlat_d = dscr("qlat_d", [NH, R, S], BF16)
    iq_d = dscr("iq_d", [256, S], BF16)
    ik_d = dscr("ik_d", [64, S], BF16)
    iw_d = dscr("iw_d", [S, 4], F32)
    ckv_d = dscr("ckv_d", [S, R], BF16)
    ckvT_d = dscr("ckvT_d", [R, S], BF16)
    mixT_d = dscr("mixT_d", [MIXW, S], BF16)
    zs_d = dscr("zs_d", [S, D], F32)
    xsc_d = dscr("xsc_d", [S, D], BF16)
    bm_d = dscr("bm_d", [S, 256], BF16)
    bmT_d = dscr("bmT_d", [256, S], BF16)
    cmT_d = dscr("cmT_d", [256, S], BF16)
    dt_d = dscr("dt_d", [S, 32], F32)
    nmT_d = dscr("nmT_d", [S, S], BF16)
    gu_d = dscr("gu_d", [DFF, S], BF16)
    R_ = {n: kb.res(n) for n in ["xs", "qlat", "iq", "ik", "iw", "ckv", "ckvT", "mixT", "zs", "xsc", "bm", "bmT",
                                 "cmT", "dt", "nmT", "gu", "y", "xm"]}

    ps = [kb.stack.enter_context(nc.psum_tensor(f"ps{i}", [128, 512], F32)) for i in range(8)]
    psr = [kb.res(f"ps{i}") for i in range(8)]

    ident_f = kb.sb("ident_f", [128, 128], F32)
    ident = kb.sb("ident", [128, 128], BF16)
    ones_f = kb.sb("ones_f", [128, 128], F32)
    utri_f = kb.sb("utri_f", [128, 128], F32)
    tri_f = kb.sb("tri_f", [128, 128], F32)
    cres = kb.res("consts")
    kb.op(pool, lambda: G.memset(ident_f[:], 0.0), writes=[cres])
    kb.op(pool, lambda: G.affine_select(out=ident_f[:], in_=ident_f[:], pattern=[[-1, 128]], compare_op=ALU.not_equal,
                                        fill=1.0, base=0, channel_multiplier=1), reads=[cres], writes=[cres])
    kb.op(pool, lambda: G.tensor_copy(out=ident[:], in_=ident_f[:]), reads=[cres], writes=[cres])
    kb.op(pool, lambda: G.memset(ones_f[:], 1.0), writes=[cres])
    kb.op(pool, lambda: G.affine_select(out=utri_f[:], in_=ones_f[:], pattern=[[1, 128]], compare_op=ALU.is_ge,
                                        fill=0.0, base=0, channel_multiplier=-1), reads=[cres], writes=[cres])
    kb.op(pool, lambda: G.tensor_copy(out=tri_f[:], in_=utri_f[:]), reads=[cres], writes=[cres])

    eps_t = kb.sb("eps_t", [128, 1], F32)
    negb_t = kb.sb("negb_t", [128, 1], F32)
    kb.op(pool, lambda: G.memset(negb_t[:], NEG), writes=[cres])
    kb.op(pool, lambda: G.memset(eps_t[:], EPS), writes=[cres])
    HT = {}

    def bcast_load(dst, src_vec, n, rs):
        kb.dma(out=dst, in_=src_vec.partition_broadcast(128), writes=[rs], own=rs)

    def rms_rstd(dst, ssq, n, eng=None):
        V.tensor_scalar(out=dst, in0=ssq, scalar1=1.0 / n, scalar2=EPS, op0=ALU.mult, op1=ALU.add)

    all_sems = []
    _orig_new_sem = kb.new_sem

    def _new_sem(name="s"):
        s = _orig_new_sem(name)
        all_sems.append(s)
        return s
    kb.new_sem = _new_sem
    for e in (pe, act, dve, pool):
        all_sems.append(e.sem)

    def barrier():
        for e in (pe, act, dve, pool, sp):
            for s in all_sems:
                if s is e.sem or s.val == 0:
                    continue
                if e.seen.get(s, 0) >= s.val:
                    continue
                e.eng.wait_ge(s.h, s.val)
                e.seen[s] = s.val

    class Phase:
        def __enter__(self):
            self.es = ExitStack()
            self.es.__enter__()
            self.old = kb.stack
            kb.stack = self.es
            kb.phase_dsems.append([])
            return self

        def __exit__(self, *a):
            barrier()
            kb.free_dsems.extend(kb.phase_dsems.pop())
            kb.stack = self.old
            self.es.__exit__(None, None, None)
            return False

    def mm(out, lhsT, rhs, start, stop, reads, writes, inc=True):
        return kb.op(pe, lambda: T.matmul(out, lhsT=lhsT, rhs=rhs, start=start, stop=stop), reads=reads, writes=writes, inc=inc)

    def tp(out, in_, reads, writes, inc=True, idn=None):
        idn = ident if idn is None else idn
        k = in_.shape[0]
        return kb.op(pe, lambda: T.transpose(out, in_, idn[0:k, 0:k]), reads=list(reads) + [cres], writes=writes, inc=inc)

    def vop(fn, reads, writes):
        return kb.op(dve, fn, reads=reads, writes=writes)

    def aop(fn, reads, writes):
        return kb.op(act, fn, reads=reads, writes=writes)

    def gop(fn, reads, writes):
        return kb.op(pool, fn, reads=reads, writes=writes)

    def rstd(st, st_r, src, dst, n):
        aop(lambda: A.activation(out=st[:, 15:16], in_=st[:, src:src + 1], func=AF.Ln, scale=1.0 / n, bias=eps_t[:, 0:1]), [st_r, cres], [st_r])
        aop(lambda: A.activation(out=st[:, dst:dst + 1], in_=st[:, 15:16], func=AF.Exp, scale=-0.5), [st_r], [st_r])

    def norm_to_hT(xt, xr, g_bc, g_r, tt, tmp):
        junk, junk_r, stl, hb, hb_r = tmp
        st, st_r = stl.next()
        aop(lambda: A.activation(out=junk[:], in_=xt, func=AF.Square, accum_out=st[:, 0:1]), [xr], [junk_r, st_r])
        rstd(st, st_r, 0, 2, D)
        vop(lambda: V.scalar_tensor_tensor(out=hb[:], in0=xt, scalar=st[:, 2:3], in1=g_bc[:], op0=ALU.mult, op1=ALU.mult),
            [xr, st_r, g_r], [hb_r])
        for half in range(2):
            pb = 6 + half
            pv = ps[pb][:].bitcast(BF16)
            for j in range(4):
                dc = half * 4 + j
                tp(pv[:, j * 128:(j + 1) * 128], hb[:, dc * 128:(dc + 1) * 128], [hb_r], [psr[pb]], inc=(j == 3))
            src = pv[:, 0:512].rearrange("p (j t) -> p j t", j=4)
            dst = HT['t'][:, half * 4:(half + 1) * 4, tt * 128:(tt + 1) * 128]
            if half == 0:
                aop(lambda src=src, dst=dst: A.copy(out=dst, in_=src), [psr[pb]], [HT['r']])
            else:
                vop(lambda src=src, dst=dst: V.tensor_copy(out=dst, in_=src), [psr[pb]], [HT['r']])

    class HTScope(Phase):
        def __enter__(self):
            Phase.__enter__(self)
            HT['t'] = kb.sb("hT", [128, 8, S], BF16)
            HT['r'] = kb.res("hT")
            return self

    def phase0(l, src=None):
        src = x_in if src is None else src
        with Phase():
            g_bc = kb.sb("p0_g", [128, D], F32)
            g_r = kb.res("p0_g")
            bcast_load(g_bc[:], vecD[l, 0, :], D, g_r)
            xsl = Slots(kb, "p0_x", [128, D], F32, 2)
            junk = kb.sb("p0_junk", [128, D], F32)
            stl = Slots(kb, "p0_st", [128, 16], F32, 2)
            hb = kb.sb("p0_hb", [128, D], BF16)
            tmp = (junk, kb.res("p0_junk"), stl, hb, kb.res("p0_hb"))
            for tt in range(NT):
                xt, xr = xsl.next()
                kb.dma(out=xt[:], in_=src[tt * 128:(tt + 1) * 128, :], reads=[R_['xs']], writes=[xr], own=xr)
                norm_to_hT(xt[:], xr, g_bc, g_r, tt, tmp)

    class WStream:
        def __init__(self, name, KC, maxc, nbuf=2, nstg=2):
            self.KC = KC
            self.stg = Slots(kb, name + "_stg", [128, KC, 256], F32, nstg)
            self.wb = Slots(kb, name + "_wb", [128, KC, maxc], BF16, nbuf)

        def load(self, wap, c0, ncols, r0=0, kcn=None):
            kcn = kcn or self.KC
            wt, wr = self.wb.next()
            for p0 in range(0, ncols, 256):
                pn = min(256, ncols - p0)
                st, sr = self.stg.next()
                src = wap[r0:r0 + kcn * 128, :].rearrange("(kc p) c -> p kc c", p=128)[:, :, c0 + p0:c0 + p0 + pn]
                kb.dma(out=st[:, 0:kcn, 0:pn], in_=src, writes=[sr], own=sr)
                gop(lambda: G.tensor_copy(out=wt[:, 0:kcn, p0:p0 + pn], in_=st[:, 0:kcn, 0:pn]), [sr], [wr])
            return wt, wr

    class RR:
        def __init__(self, items):
            self.items = items
            self.i = 0

        def next(self):
            k = self.items[self.i % len(self.items)]
            self.i += 1
            return k

    def phase1(l):
        with Phase():
            hT, hT_r = HT['t'], HT['r']
            ws = WStream("w1", 8, 512, nbuf=3)
            c_r = kb.res("p1c")
            wuk_f = kb.sb("wuk_f", [128, 4, R], F32)
            wuk_b = kb.sb("wuk_b", [128, 4, R], BF16)
            kb.dma(out=wuk_f[:], in_=wukT[l], writes=[c_r], own=c_r)
            gop(lambda: G.tensor_copy(out=wuk_b[:], in_=wuk_f[:]), [c_r], [c_r])
            scw_t = kb.sb("scw_t", [128, 4, 3], F32)
            kb.dma(out=scw_t[:], in_=scw[l], writes=[c_r], own=c_r)
            sscw_t = kb.sb("sscw_t", [128, 12, 4], F32)
            kb.dma(out=sscw_t[:], in_=sscw[l], writes=[c_r], own=c_r)
            sscb_t = kb.sb("sscb_t", [128, 12], F32)
            kb.dma(out=sscb_t[:], in_=sscb[l], writes=[c_r], own=c_r)
            kvn_bc = kb.sb("kvn_bc", [128, R], F32)
            bcast_load(kvn_bc[:], kvn[l, :], R, c_r)
            ikg_bc = kb.sb("ikg_bc", [128, 64], F32)
            bcast_load(ikg_bc[:], ikg[l, :], 64, c_r)
            ikb_bc = kb.sb("ikb_bc", [128, 64], F32)
            bcast_load(ikb_bc[:], ikb[l, :], 64, c_r)
            v16_bc = kb.sb("v16_bc", [128, 3, 16], F32)
            kb.dma(out=v16_bc[:], in_=v16[l].partition_broadcast(128), writes=[c_r], own=c_r)
            A_bc = kb.sb("A_bc", [128, 16], F32)
            aop(lambda: A.activation(out=A_bc[:], in_=v16_bc[:, 1, :], func=AF.Exp), [c_r], [c_r])
            vop(lambda: V.tensor_scalar(out=A_bc[:], in0=A_bc[:], scalar1=-1.0, scalar2=None, op0=ALU.mult), [c_r], [c_r])

            bankA = RR([0, 1])
            bankB = RR([2, 3])
            o512 = Slots(kb, "p1_o512", [128, 512], BF16, 3)
            o512b = Slots(kb, "p1_o512b", [128, 512], BF16, 3)

            def fm_block(wt, wr, c, tb, ncols=128):
                b = bankA.next()
                for kc in range(8):
                    mm(ps[b][0:ncols, :], wt[:, kc, c * 128:c * 128 + ncols], hT[:, kc, tb * 512:(tb + 1) * 512],
                       kc == 0, kc == 7, [wr, hT_r], [psr[b]], inc=(kc == 7))
                return b

            wt, wr = ws.load(w_in[l], O_Q, 512)
            for c in range(4):
                for tb in range(NB):
                    b = fm_block(wt, wr, c, tb)
                    qs, qr = o512.next()
                    aop(lambda b=b, qs=qs: A.mul(out=qs[:], in_=ps[b][:], mul=0.125), [psr[b]], [qr])
                    for hh in range(2):
                        for rc in range(2):
                            b2 = bankB.next()
                            mm(ps[b2][:], wuk_b[hh * 64:(hh + 1) * 64, c, rc * 128:(rc + 1) * 128],
                               qs[hh * 64:(hh + 1) * 64, :], True, True, [c_r, qr], [psr[b2]])
                            ql, qlr = o512b.next()
                            vop(lambda b2=b2, ql=ql: V.tensor_copy(out=ql[:], in_=ps[b2][:]), [psr[b2]], [qlr])
                            kb.dma(out=qlat_d[2 * c + hh, rc * 128:(rc + 1) * 128, tb * 512:(tb + 1) * 512], in_=ql[:],
                                   reads=[qlr], writes=[R_["qlat"]], own=qlr)
            wt, wr = ws.load(w_in[l], O_IQ, 256)
            for c in range(2):
                for tb in range(NB):
                    b = fm_block(wt, wr, c, tb)
                    qs, qr = o512.next()
                    aop(lambda b=b, qs=qs: A.mul(out=qs[:], in_=ps[b][:], mul=0.125), [psr[b]], [qr])
                    kb.dma(out=iq_d[c * 128:(c + 1) * 128, tb * 512:(tb + 1) * 512], in_=qs[:], reads=[qr],
                           writes=[R_["iq"]], own=qr)
            cg = kb.sb("p1_cg", [128, S], F32)
            cg_r = kb.res("p1_cg")
            U = kb.sb("p1_U", [128, S + 4], F32)
            U_r = kb.res("p1_U")
            vop(lambda: V.memset(U[:, 0:4], 0.0), [], [U_r])
            wbg, wbg_r = ws.load(w_in[l], O_BG, 512)
            wcg, wcg_r = ws.load(w_in[l], O_CG, 512)
            whb, whb_r = ws.load(w_in[l], O_HB, 512)
            for c in range(4):
                for tb in range(NB):
                    b = fm_block(wcg, wcg_r, c, tb)
                    aop(lambda b=b, tb=tb: A.copy(out=cg[:, tb * 512:(tb + 1) * 512], in_=ps[b][:]), [psr[b]], [cg_r])
                for tb in range(NB):
                    b = fm_block(whb, whb_r, c, tb)
                    vop(lambda b=b, tb=tb: V.tensor_tensor(out=U[:, 4 + tb * 512:4 + (tb + 1) * 512], in0=ps[b][:],
                                                           in1=cg[:, tb * 512:(tb + 1) * 512], op=ALU.mult),
                        [psr[b], cg_r], [U_r])
                vop(lambda c=c: V.tensor_scalar(out=cg[:, :], in0=U[:, 2:S + 2], scalar1=scw_t[:, c, 0:1], scalar2=None,
                                                op0=ALU.mult), [U_r, c_r], [cg_r])
                for j in (1, 2):
                    vop(lambda c=c, j=j: V.scalar_tensor_tensor(out=cg[:, :], in0=U[:, 2 + j:S + 2 + j], scalar=scw_t[:, c, j:j + 1],
                                                                in1=cg[:, :], op0=ALU.mult, op1=ALU.add), [U_r, c_r, cg_r], [cg_r])
                for tb in range(NB):
                    b = fm_block(wbg, wbg_r, c, tb)
                    ob, obr = o512.next()
                    vop(lambda b=b, tb=tb, ob=ob: V.tensor_tensor(out=ob[:], in0=ps[b][:], in1=cg[:, tb * 512:(tb + 1) * 512],
                                                                  op=ALU.mult), [psr[b], cg_r], [obr])
                    kb.dma(out=mixT_d[512 + c * 128:512 + (c + 1) * 128, tb * 512:(tb + 1) * 512], in_=ob[:], reads=[obr],
                           writes=[R_["mixT"]], own=obr)
            sx = kb.sb("p1_sx", [128, S], BF16)
            sx_r = kb.res("p1_sx")
            U2 = kb.sb("p1_U2", [128, S + 4], F32)
            U2_r = kb.res("p1_U2")
            vop(lambda: V.memset(U2[:, 0:4], 0.0), [], [U2_r])
            Ua = [(U, U_r), (U2, U2_r)]
            tstg = Slots(kb, "p1_tstg", [128, 4, 128], BF16, 2)
            wts = {}

            def d_mm(cc):
                blk, c = divmod(cc, 4)
                if c == 0:
                    wts[blk] = ws.load(w_in[l], O_XBC + blk * 512, 512)
                wt, wr = wts[blk]
                U, U_r = Ua[cc % 2]
                for tb in range(NB):
                    b = fm_block(wt, wr, c, tb)
                    aop(lambda: A.copy(out=U[:, 4 + tb * 512:4 + (tb + 1) * 512], in_=ps[b][:]), [psr[b]], [U_r])

            def d_post(cc):
                U, U_r = Ua[cc % 2]
                vop(lambda: V.tensor_scalar(out=cg[:, :], in0=U[:, 1:S + 1], scalar1=sscw_t[:, cc, 0:1],
                                            scalar2=sscb_t[:, cc:cc + 1], op0=ALU.mult, op1=ALU.add), [U_r, c_r], [cg_r])
                for j in (1, 2, 3):
                    vop(lambda: V.scalar_tensor_tensor(out=cg[:, :], in0=U[:, 1 + j:S + 1 + j], scalar=sscw_t[:, cc, j:j + 1], in1=cg[:, :],
                                                       op0=ALU.mult, op1=ALU.add), [U_r, c_r, cg_r], [cg_r])
                aop(lambda: A.activation(out=sx[:, :], in_=cg[:, :], func=AF.Silu), [cg_r], [sx_r])
                if cc >= 8:
                    g = (cc - 8) % 2
                    dst = bmT_d if cc < 10 else cmT_d
                    kb.dma(out=dst[g * 128:(g + 1) * 128, :], in_=sx[:, :], reads=[sx_r],
                           writes=[R_["bmT" if cc < 10 else "cmT"]], own=sx_r)
                if cc < 10:
                    for t4 in range(NT // 4):
                        pb = bankB.next()
                        pv = ps[pb][:].bitcast(BF16)
                        for j in range(4):
                            tt = t4 * 4 + j
                            tp(pv[:, j * 128:(j + 1) * 128], sx[:, tt * 128:(tt + 1) * 128], [sx_r], [psr[pb]], inc=(j == 3))
                        tsg, tsr = tstg.next()
                        vop(lambda: V.tensor_copy(out=tsg[:], in_=pv[:, 0:512].rearrange("p (j c) -> p j c", j=4)), [psr[pb]], [tsr])
                        if cc < 8:
                            dst = xsc_d.rearrange("(tt p) c -> p tt c", p=128)[:, t4 * 4:(t4 + 1) * 4, cc * 128:(cc + 1) * 128]
                            rn = "xsc"
                        else:
                            dst = bm_d.rearrange("(tt p) c -> p tt c", p=128)[:, t4 * 4:(t4 + 1) * 4, (cc - 8) * 128:(cc - 7) * 128]
                            rn = "bm"
                        kb.dma(out=dst, in_=tsg[:], reads=[tsr], writes=[R_[rn]], own=tsr)

            d_mm(0)
            for cc in range(12):
                if cc + 1 < 12:
                    d_mm(cc + 1)
                d_post(cc)
            zsl = Slots(kb, "p1_z", [128, 512], F32, 3)
            for zb in range(2):
                wt, wr = ws.load(w_in[l], O_Z + zb * 512, 512)
                for tt in range(NT):
                    b = bankA.next()
                    for kc in range(8):
                        mm(ps[b][:], hT[:, kc, tt * 128:(tt + 1) * 128], wt[:, kc, 0:512], kc == 0, kc == 7, [wr, hT_r], [psr[b]],
                           inc=(kc == 7))
                    zt, zr = zsl.next()
                    aop(lambda b=b, zt=zt: A.activation(out=zt[:], in_=ps[b][:], func=AF.Silu), [psr[b]], [zr])
                    kb.dma(out=zs_d[tt * 128:(tt + 1) * 128, zb * 512:(zb + 1) * 512], in_=zt[:], reads=[zr], writes=[R_["zs"]], own=zr)
            wt, wr = ws.wb.next()
            wv = w_in[l].rearrange("(kc p) c -> p kc c", p=128)
            st_, sr_ = ws.stg.next()
            kb.dma(out=st_[:, :, 0:256], in_=wv[:, :, O_CKV:O_CKV + 256], writes=[sr_], own=sr_)
            gop(lambda: G.tensor_copy(out=wt[:, :, 0:256], in_=st_[:, :, 0:256]), [sr_], [wr])
            st_, sr_ = ws.stg.next()
            kb.dma(out=st_[:, :, 0:68], in_=wv[:, :, O_IK:O_IK + 68], writes=[sr_], own=sr_)
            kb.dma(out=st_[:, :, 68:84], in_=wv[:, :, O_DT:O_DT + 16], writes=[sr_], own=sr_)
            gop(lambda: G.tensor_copy(out=wt[:, :, 256:340], in_=st_[:, :, 0:84]), [sr_], [wr])
            stl = Slots(kb, "p1_st", [128, 16], F32, 2)
            junk = kb.sb("p1_junk", [128, 256], F32)
            junk_r = kb.res("p1_junk")
            cnl = Slots(kb, "p1_cn", [128, 256], BF16, 2)
            ctl = Slots(kb, "p1_ct", [128, 2, 128], BF16, 2)
            ikl = Slots(kb, "p1_ik", [128, 64], F32, 2)
            iknl = Slots(kb, "p1_ikn", [128, 64], BF16, 2)
            iktl = Slots(kb, "p1_ikt", [64, 128], BF16, 2)
            iwl = Slots(kb, "p1_iw", [128, 4], F32, 2)
            dtl = Slots(kb, "p1_dt", [128, 32], F32, 2)
            E5 = {}

            def e_mm(tt):
                b = bankA.next()
                for kc in range(8):
                    mm(ps[b][:, 0:340], hT[:, kc, tt * 128:(tt + 1) * 128], wt[:, kc, 0:340], kc == 0, kc == 7, [wr, hT_r], [psr[b]],
                       inc=(kc == 7))
                E5[tt] = b

            e_mm(0)
            for tt in range(NT):
                if tt + 1 < NT:
                    e_mm(tt + 1)
                b = E5.pop(tt)
                P = ps[b]
                st, str_ = stl.next()
                aop(lambda P=P, st=st: A.activation(out=junk[:, 0:256], in_=P[:, 0:256], func=AF.Square, accum_out=st[:, 0:1]),
                    [psr[b]], [junk_r, str_])
                rstd(st, str_, 0, 2, R)
                cn, cnr = cnl.next()
                vop(lambda P=P, st=st, cn=cn: V.scalar_tensor_tensor(out=cn[:], in0=P[:, 0:256], scalar=st[:, 2:3], in1=kvn_bc[:],
                                                                    op0=ALU.mult, op1=ALU.mult), [psr[b], str_, c_r], [cnr])
                kb.dma(out=ckv_d[tt * 128:(tt + 1) * 128, :], in_=cn[:], reads=[cnr], writes=[R_["ckv"]], own=cnr)
                pb = bankB.next()
                pv = ps[pb][:].bitcast(BF16)
                for rc in range(2):
                    tp(pv[:, rc * 128:(rc + 1) * 128], cn[:, rc * 128:(rc + 1) * 128], [cnr], [psr[pb]], inc=(rc == 1))
                ct, ctr = ctl.next()
                aop(lambda pv=pv, ct=ct: A.copy(out=ct[:], in_=pv[:, 0:256].rearrange("p (j c) -> p j c", j=2)), [psr[pb]], [ctr])
                kb.dma(out=ckvT_d.rearrange("(rc p) s -> p rc s", p=128)[:, :, tt * 128:(tt + 1) * 128], in_=ct[:], reads=[ctr],
                       writes=[R_["ckvT"]], own=ctr)
                vop(lambda P=P, st=st: V.tensor_reduce(out=st[:, 4:5], in_=P[:, 256:320], axis=AX.X, op=ALU.add), [psr[b]], [str_])
                aop(lambda P=P, st=st: A.activation(out=junk[:, 0:64], in_=P[:, 256:320], func=AF.Square, accum_out=st[:, 5:6]),
                    [psr[b]], [junk_r, str_])
                vop(lambda st=st: V.tensor_scalar(out=st[:, 6:7], in0=st[:, 4:5], scalar1=1.0 / 64, scalar2=None, op0=ALU.mult), [str_], [str_])
                vop(lambda st=st: V.tensor_tensor(out=st[:, 7:8], in0=st[:, 6:7], in1=st[:, 6:7], op=ALU.mult), [str_], [str_])
                vop(lambda st=st: V.scalar_tensor_tensor(out=st[:, 8:9], in0=st[:, 5:6], scalar=1.0 / 64, in1=st[:, 7:8],
                                                         op0=ALU.mult, op1=ALU.subtract), [str_], [str_])
                rstd(st, str_, 8, 9, 1)
                ik, ikr = ikl.next()
                vop(lambda P=P, st=st, ik=ik: V.tensor_scalar(out=ik[:], in0=P[:, 256:320], scalar1=st[:, 6:7], scalar2=st[:, 9:10],
                                                              op0=ALU.subtract, op1=ALU.mult), [psr[b], str_], [ikr])
                vop(lambda ik=ik: V.tensor_tensor(out=ik[:], in0=ik[:], in1=ikg_bc[:], op=ALU.mult), [ikr, c_r], [ikr])
                ikn, iknr = iknl.next()
                vop(lambda ik=ik, ikn=ikn: V.tensor_tensor(out=ikn[:], in0=ik[:], in1=ikb_bc[:], op=ALU.add), [ikr, c_r], [iknr])
                pb = bankB.next()
                pv = ps[pb][:].bitcast(BF16)
                tp(pv[0:64, 0:128], ikn[:, 0:64], [iknr], [psr[pb]])
                ikt, iktr = iktl.next()
                aop(lambda pv=pv, ikt=ikt: A.copy(out=ikt[:], in_=pv[0:64, 0:128]), [psr[pb]], [iktr])
                kb.dma(out=ik_d[:, tt * 128:(tt + 1) * 128], in_=ikt[:], reads=[iktr], writes=[R_["ik"]], own=iktr)
                iwt, iwr = iwl.next()
                aop(lambda P=P, iwt=iwt: A.mul(out=iwt[:], in_=P[:, 320:324], mul=0.5), [psr[b]], [iwr])
                kb.dma(out=iw_d[tt * 128:(tt + 1) * 128, :], in_=iwt[:], reads=[iwr], writes=[R_["iw"]], own=iwr)
                dtt, dtr = dtl.next()
                vop(lambda P=P, dtt=dtt: V.tensor_tensor(out=dtt[:, 16:32], in0=P[:, 324:340], in1=v16_bc[:, 0, :], op=ALU.add),
                    [psr[b], c_r], [dtr])
                aop(lambda dtt=dtt: A.activation(out=dtt[:, 16:32], in_=dtt[:, 16:32], func=AF.Exp), [dtr], [dtr])
                aop(lambda dtt=dtt: A.activation(out=dtt[:, 0:16], in_=dtt[:, 16:32], func=AF.Ln, bias=1.0, scale=1.0), [dtr], [dtr])
                vop(lambda dtt=dtt: V.tensor_tensor(out=dtt[:, 16:32], in0=dtt[:, 0:16], in1=A_bc[:], op=ALU.mult), [dtr, c_r], [dtr])
                kb.dma(out=dt_d[tt * 128:(tt + 1) * 128, :], in_=dtt[:], reads=[dtr], writes=[R_["dt"]], own=dtr)

    NIT = 14

    def gen2(l):
        c_r = kb.res("p2c")
        iqT = kb.sb("p2_iqT", [128, 2, S], BF16)
        ikT = kb.sb("p2_ikT", [128, S], BF16)
        iwa = kb.sb("p2_iw", [128, NT, 4], F32)
        kb.dma(out=iqT[:], in_=iq_d.rearrange("(c p) s -> p c s", p=128), reads=[R_["iq"]], writes=[c_r], own=c_r)
        kb.dma(out=ikT[0:64, :], in_=ik_d, reads=[R_["ik"]], writes=[c_r], own=c_r)
        kb.dma(out=ikT[64:128, :], in_=ik_d, reads=[R_["ik"]], writes=[c_r], own=c_r)
        for t0_ in range(0, NT, 8):
            t1_ = min(NT, t0_ + 8)
            kb.dma(out=iwa[:, t0_:t1_, :], in_=iw_d.rearrange("(t p) h -> p t h", p=128)[:, t0_:t1_, :], reads=[R_["iw"]], writes=[c_r], own=c_r)
        pw2 = kb.sb("p2_pw2", [128, NIT], F32)
        for k in range(NIT):
            gop(lambda: G.memset(pw2[:, k:k + 1], 2.0 ** (-(k + 1))), [], [c_r])
        SCl = Slots(kb, "p2_SC", [128, S], F32, 2)
        mkl = Slots(kb, "p2_mk", [128, S], BF16, 3)
        Rl = Slots(kb, "p2_R", [128, 512], F32, 4)
        bl = Slots(kb, "p2_b", [128, 8 + 2 * NIT], F32, 4)
        nml = Slots(kb, "p2_nm", [128, 4, 128], BF16, 3)
        bankA = RR([0, 1])
        bankT = RR([2])
        yield

        def scores(qt, out):
            N = (qt + 1) * 128
            SC, SC_r = SCl.next()
            for sb in range((N + 511) // 512):
                cols = min(512, N - sb * 512)
                cs = slice(sb * 512, sb * 512 + cols)
                for h in range(4):
                    c, hh = divmod(h, 2)
                    pr_ = slice(hh * 64, (hh + 1) * 64)
                    b = bankA.next()
                    mm(ps[b][:, 0:cols], iqT[pr_, c, qt * 128:(qt + 1) * 128], ikT[pr_, cs], True, True, [c_r], [psr[b]])
                    Rt, Rr = Rl.next()
                    aop(lambda: A.activation(out=Rt[:, 0:cols], in_=ps[b][:, 0:cols], func=AF.Relu), [psr[b]], [Rr])
                    if h == 0:
                        vop(lambda: V.tensor_scalar(out=SC[:, cs], in0=Rt[:, 0:cols], scalar1=iwa[:, qt, 0:1], scalar2=None, op0=ALU.mult),
                            [Rr, c_r], [SC_r])
                    else:
                        vop(lambda: V.scalar_tensor_tensor(out=SC[:, cs], in0=Rt[:, 0:cols], scalar=iwa[:, qt, h:h + 1], in1=SC[:, cs],
                                                           op0=ALU.mult, op1=ALU.add), [Rr, c_r, SC_r], [SC_r])
                yield
            bt, b_r = bl.next()
            mk, mk_r = mkl.next()
            vop(lambda: V.tensor_reduce(out=bt[:, 0:1], in_=SC[:, 0:N], axis=AX.X, op=ALU.max), [SC_r], [b_r])
            vop(lambda: V.tensor_reduce(out=bt[:, 1:2], in_=SC[:, 0:N], axis=AX.X, op=ALU.min), [SC_r], [b_r])
            vop(lambda: V.memset(SC[0:64, N - 64:N], -1.0e30), [], [SC_r])
            vop(lambda: V.tensor_scalar(out=bt[:, 2:3], in0=bt[:, 1:2], scalar1=-0.01, scalar2=None, op0=ALU.add), [b_r], [b_r])
            vop(lambda: V.scalar_tensor_tensor(out=bt[:, 3:4], in0=bt[:, 0:1], scalar=0.02, in1=bt[:, 1:2], op0=ALU.add, op1=ALU.subtract),
                [b_r], [b_r])
            vop(lambda: V.tensor_scalar(out=bt[:, 8:8 + NIT], in0=pw2[:], scalar1=bt[:, 3:4], scalar2=None, op0=ALU.mult), [b_r, c_r], [b_r])
            vop(lambda: V.memset(bt[:, 8 + NIT:8 + 2 * NIT], 0.0), [], [b_r])
            out.update(dict(qt=qt, N=N, SC=SC, SC_r=SC_r, bt=bt, b_r=b_r, mk=mk, mk_r=mk_r))
            yield

        def first_mid(st, on_act):
            bt, b_r = st["bt"], st["b_r"]
            if on_act:
                vop(lambda: V.scalar_tensor_tensor(out=bt[:, 4:5], in0=bt[:, 2:3], scalar=-1.0, in1=bt[:, 8:9], op0=ALU.mult, op1=ALU.subtract),
                    [b_r], [b_r])
            else:
                vop(lambda: V.tensor_tensor(out=bt[:, 4:5], in0=bt[:, 2:3], in1=bt[:, 8:9], op=ALU.add), [b_r], [b_r])

        def iteration(st, k, on_act):
            N, SC, SC_r, bt, b_r = st["N"], st["SC"], st["SC_r"], st["bt"], st["b_r"]
            jt, jr = st["mk"], st["mk_r"]
            ck = 8 + NIT + k
            if on_act:
                aop(lambda: A.activation(out=jt[:, 0:N], in_=SC[:, 0:N], func=AF.Sign, bias=bt[:, 4:5], scale=1.0,
                                         accum_out=bt[:, ck:ck + 1]), [SC_r, b_r], [jr, b_r])
                thr = 511.0 - N
            else:
                vop(lambda: V.tensor_scalar(out=jt[:, 0:N], in0=SC[:, 0:N], scalar1=bt[:, 4:5], scalar2=0.0, op0=ALU.is_ge, op1=ALU.add,
                                            accum_out=bt[:, ck:ck + 1]), [SC_r, b_r], [jr, b_r])
                thr = 255.5
            vop(lambda: V.scalar_tensor_tensor(out=bt[:, 5:6], in0=bt[:, ck:ck + 1], scalar=thr, in1=bt[:, 8 + k:9 + k],
                                               op0=ALU.is_ge, op1=ALU.mult), [b_r], [b_r])
            vop(lambda: V.tensor_tensor(out=bt[:, 2:3], in0=bt[:, 2:3], in1=bt[:, 5:6], op=ALU.add), [b_r], [b_r])
            if k + 1 < NIT:
                if on_act:
                    vop(lambda: V.scalar_tensor_tensor(out=bt[:, 4:5], in0=bt[:, 2:3], scalar=-1.0, in1=bt[:, 9 + k:10 + k],
                                                       op0=ALU.mult, op1=ALU.subtract), [b_r], [b_r])
                else:
                    vop(lambda: V.tensor_tensor(out=bt[:, 4:5], in0=bt[:, 2:3], in1=bt[:, 9 + k:10 + k], op=ALU.add), [b_r], [b_r])

        def finalize(st):
            qt, N, SC, SC_r, bt, b_r, mk, mk_r = st["qt"], st["N"], st["SC"], st["SC_r"], st["bt"], st["b_r"], st["mk"], st["mk_r"]
            vop(lambda: V.tensor_scalar(out=mk[:, 0:N], in0=SC[:, 0:N], scalar1=bt[:, 2:3], scalar2=None, op0=ALU.is_ge), [SC_r, b_r], [mk_r])
            for k0 in range(0, qt + 1, 4):
                n = min(4, qt + 1 - k0)
                pb = bankT.next()
                pv = ps[pb][:].bitcast(BF16)
                for j in range(n):
                    tp(pv[:, j * 128:(j + 1) * 128], mk[:, (k0 + j) * 128:(k0 + j + 1) * 128], [mk_r], [psr[pb]], inc=(j == n - 1))
                nm, nm_r = nml.next()
                aop(lambda: A.copy(out=nm[:, 0:n, :], in_=pv[:, 0:n * 128].rearrange("p (j t) -> p j t", j=n)), [psr[pb]], [nm_r])
                kb.dma(out=nmT_d.rearrange("(kt p) t -> p kt t", p=128)[:, k0:k0 + n, qt * 128:(qt + 1) * 128], in_=nm[:, 0:n, :],
                       reads=[nm_r], writes=[R_["nmT"]], own=nm_r)
                yield

        for q0 in range(0, NT, 2):
            sa, sb_ = {}, {}
            yield from scores(q0, sa)
            yield from scores(q0 + 1, sb_)
            first_mid(sa, False)
            first_mid(sb_, True)
            for k in range(NIT):
                iteration(sa, k, False)
                iteration(sb_, k, True)
                yield
            yield from finalize(sa)
            yield from finalize(sb_)

    def run_interleaved(gens, weights=None):
        gens = list(gens)
        weights = list(weights or [1] * len(gens))
        acc = [0.0] * len(gens)
        alive = [True] * len(gens)
        while any(alive):
            for i, g in enumerate(gens):
                if not alive[i]:
                    continue
                acc[i] += weights[i]
                while acc[i] >= 1.0 and alive[i]:
                    acc[i] -= 1.0
                    try:
                        next(g)
                    except StopIteration:
                        alive[i] = False

    def phase2(l):
        with Phase():
            run_interleaved([gen2(l)])

    def phase24(l):
        with Phase():
            run_interleaved([gen2(l), gen4(l)], [1.0, 5.5])

    def phase3(l):
        with Phase():
            c_r = kb.res("p3c")
            ckvT = kb.sb("p3_ckvT", [128, 2, S], BF16)
            kb.dma(out=ckvT[:], in_=ckvT_d.rearrange("(rc p) s -> p rc s", p=128), reads=[R_["ckvT"]], writes=[c_r], own=c_r)
            ckva = kb.sb("p3_ckva", [128, NT, 258], BF16)
            gop(lambda: G.memset(ckva[:, :, 256:258], 1.0), [], [c_r])
            for t0_ in range(0, NT, 8):
                t1_ = min(NT, t0_ + 8)
                kb.dma(out=ckva[:, t0_:t1_, 0:256], in_=ckv_d.rearrange("(kt p) r -> p kt r", p=128)[:, t0_:t1_, :], reads=[R_["ckv"]], writes=[c_r], own=c_r)
            gt = kb.sb("p3_gt", [128, NH, GW], BF16)
            b15 = kb.sb("p3_b15", [128, NH], F32)
            gstg = Slots(kb, "p3_gstg", [128, GW], F32, 2)
            for h in range(NH):
                st, sr = gstg.next()
                kb.dma(out=st[:], in_=gtab[:, h, :], writes=[sr], own=sr)
                gop(lambda st=st, h=h: G.tensor_copy(out=gt[:, h, :], in_=st[:]), [sr], [c_r])
                gop(lambda st=st, h=h: G.tensor_copy(out=b15[:, h:h + 1], in_=st[:, GW - 1:GW]), [sr], [c_r])
            wuv_f = kb.sb("p3_wuvf", [128, 2, NH * 64], F32)
            wuv_b = kb.sb("p3_wuvb", [128, 2, NH * 64], BF16)
            kb.dma(out=wuv_f[:], in_=wuv[l], writes=[c_r], own=c_r)
            gop(lambda: G.tensor_copy(out=wuv_b[:], in_=wuv_f[:]), [c_r], [c_r])
            nml = Slots(kb, "p3_nm", [128, NT, 512], BF16, 2)
            ql = Slots(kb, "p3_q", [128, 2, 512], BF16, 3)
            El = Slots(kb, "p3_E", [128, 512], BF16, 3)
            PTl = Slots(kb, "p3_PT", [128, 512], BF16, 3)
            rsl = Slots(kb, "p3_rs", [128, 4], F32, 2)
            ol = Slots(kb, "p3_o", [128, 256], BF16, 8)
            oTl = Slots(kb, "p3_oT", [128, 2, 128], BF16, 2)
            aTl = Slots(kb, "p3_aT", [64, 512], BF16, 3)
            bankS = RR([0, 1, 2])
            nmv = nmT_d.rearrange("(kt p) t -> p kt t", p=128)
            nm_tiles = {}
            q_tiles = {}

            def load_nm(tb):
                if tb >= NB or tb in nm_tiles:
                    return
                nm, nm_r = nml.next()
                for t0_ in range(0, 4 * tb, 8):
                    t1_ = min(4 * tb, t0_ + 8)
                    kb.dma(out=nm[:, t0_:t1_, :], in_=nmv[:, t0_:t1_, tb * 512:(tb + 1) * 512], reads=[R_["nmT"]], writes=[nm_r], own=nm_r)
                for i in range(4):
                    kb.dma(out=nm[:, 4 * tb + i, i * 128:512], in_=nmv[:, 4 * tb + i, tb * 512 + i * 128:(tb + 1) * 512], reads=[R_["nmT"]],
                           writes=[nm_r], own=nm_r)
                nm_tiles[tb] = (nm, nm_r)

            def load_q(idx):
                if idx >= NB * NH or idx in q_tiles:
                    return
                tb, h = divmod(idx, NH)
                q, q_r = ql.next()
                kb.dma(out=q[:], in_=qlat_d[h].rearrange("(rc p) s -> p rc s", p=128)[:, :, tb * 512:(tb + 1) * 512], reads=[R_["qlat"]],
                       writes=[q_r], own=q_r)
                q_tiles[idx] = (q, q_r)

            groups = [(tb, h, kt) for tb in range(NB) for h in range(NH) for kt in range(4 * tb + 4)]
            sbank = {}

            def emit_qk(g):
                tb, h, kt = g
                if kt == 0:
                    load_nm(tb)
                    load_q(tb * NH + h)
                    load_q(tb * NH + h + 1)
                q, q_r = q_tiles[tb * NH + h]
                nm, nm_r = nm_tiles[tb]
                cl = max(0, kt - 4 * tb) * 128
                b = bankS.next()
                sbank[g] = b
                ks = slice(kt * 128, (kt + 1) * 128)
                c0 = tb * 512 - kt * 128 + 384
                const = c0 >= GC
                mm(ps[b][:, cl:512], ckvT[:, 0, ks], q[:, 0, cl:512], True, False, [c_r, q_r], [psr[b]], inc=False)
                mm(ps[b][:, cl:512], ckvT[:, 1, ks], q[:, 1, cl:512], False, const, [c_r, q_r], [psr[b]], inc=const)
                if not const:
                    mm(ps[b][:, cl:512], ident[:], gt[:, h, c0 + cl:c0 + 512], False, True, [cres, c_r], [psr[b]])

            def emit_rest(g):
                tb, h, kt = g
                if h == 0 and kt == 0:
                    load_nm(tb + 1)
                nm, nm_r = nm_tiles[tb]
                i = max(0, kt - 4 * tb)
                cl = i * 128
                b = sbank.pop(g)
                const = (tb * 512 - kt * 128 + 384) >= GC
                E, E_r = El.next()
                if const:
                    aop(lambda: A.activation(out=E[:, cl:512], in_=ps[b][:, cl:512], func=AF.Exp, bias=b15[:, h:h + 1], scale=1.0),
                        [psr[b], c_r], [E_r])
                else:
                    aop(lambda: A.activation(out=E[:, cl:512], in_=ps[b][:, cl:512], func=AF.Exp), [psr[b]], [E_r])
                PT, PT_r = PTl.next()
                vop(lambda: V.tensor_tensor(out=PT[:, cl:512], in0=E[:, cl:512], in1=nm[:, kt, cl:512], op=ALU.mult), [E_r, nm_r], [PT_r])
                for j in range(i, 4):
                    mm(ps[4 + j][:, 0:257], PT[:, j * 128:(j + 1) * 128], ckva[:, kt, 0:257], kt == 0, kt == 4 * tb + j, [PT_r, c_r],
                       [psr[4 + j]], inc=(j == 3 or kt == 4 * tb + j))

            def emit_post_head(tb, h):
                tiles = []
                for j in range(4):
                    rs, rs_r = rsl.next()
                    vop(lambda: V.reciprocal(out=rs[:, 0:1], in_=ps[4 + j][:, 256:257]), [psr[4 + j]], [rs_r])
                    o, o_r = ol.next()
                    aop(lambda: A.activation(out=o[:], in_=ps[4 + j][:, 0:256], func=AF.Identity, scale=rs[:, 0:1]),
                        [psr[4 + j], rs_r], [o_r])
                    tiles.append((o, o_r))
                q_tiles.pop(tb * NH + h, None)
                if h == NH - 1:
                    nm_tiles.pop(tb, None)
                return tiles

            def post_gen(tb, h, tiles):
                aT, aT_r = aTl.next()
                for j, (o, o_r) in enumerate(tiles):
                    pv = ps[3][:].bitcast(BF16)
                    for rc in range(2):
                        tp(pv[:, rc * 128:(rc + 1) * 128], o[:, rc * 128:(rc + 1) * 128], [o_r], [psr[3]], inc=(rc == 1))
                    yield
                    oT, oT_r = oTl.next()
                    vop(lambda: V.tensor_copy(out=oT[:], in_=pv[:, 0:256].rearrange("p (j t) -> p j t", j=2)), [psr[3]], [oT_r])
                    yield
                    for rc in range(2):
                        mm(ps[3][0:64, 256:384], wuv_b[:, rc, h * 64:(h + 1) * 64], oT[:, rc, :], rc == 0, rc == 1, [c_r, oT_r], [psr[3]],
                           inc=(rc == 1))
                    yield
                    vop(lambda: V.tensor_copy(out=aT[:, j * 128:(j + 1) * 128], in_=ps[3][0:64, 256:384]), [psr[3]], [aT_r])
                    yield
                kb.dma(out=mixT_d[h * 64:(h + 1) * 64, tb * 512:(tb + 1) * 512], in_=aT[:], reads=[aT_r], writes=[R_["mixT"]], own=aT_r)

            def drain(gen_):
                if gen_ is not None:
                    for _ in gen_:
                        pass

            pending = None
            emit_qk(groups[0])
            for idx, g in enumerate(groups):
                if idx + 1 < len(groups):
                    emit_qk(groups[idx + 1])
                emit_rest(g)
                tb, h, kt = g
                if pending is not None:
                    nsteps = -(-17 // (4 * tb + 4))
                    for _ in range(nsteps):
                        try:
                            next(pending)
                        except StopIteration:
                            pending = None
                            break
                if kt == 4 * tb + 3:
                    drain(pending)
                    tiles = emit_post_head(tb, h)
                    pending = post_gen(tb, h, tiles)
            drain(pending)

    def gen4(l):
        if True:
            c_r = kb.res("p4c")
            negtri4 = kb.sb("negtri4", [128, 4, 128], F32)
            gop(lambda: G.memset(negtri4[:], 0.0), [], [c_r])
            for j in range(4):
                gop(lambda j=j: G.affine_select(out=negtri4[:, j, :], in_=negtri4[:, j, :], pattern=[[1, 128]],
                                                compare_op=ALU.is_ge, fill=NEG, base=0, channel_multiplier=-1), [c_r], [c_r])
            v16_bc = kb.sb("p4_v16", [128, 3, 16], F32)
            kb.dma(out=v16_bc[:], in_=v16[l].partition_broadcast(128), writes=[c_r], own=c_r)
            gn = kb.sb("p4_gn", [128, D], F32)
            bcast_load(gn[:], vecD[l, 4, :], D, c_r)
            ST = kb.sb("p4_ST", [128, 2, 512], F32)
            ST_r = kb.res("p4_ST")
            STb = kb.sb("p4_STb", [128, 2, 512], BF16)
            STb_r = kb.res("p4_STb")
            vop(lambda: V.memset(ST[:], 0.0), [], [ST_r])
            vop(lambda: V.memset(STb[:], 0.0), [], [STb_r])
            xsl = Slots(kb, "p4_xs", [128, 16, 64], BF16, 2)
            bml = Slots(kb, "p4_bm", [128, 256], BF16, 2)
            bmTl = Slots(kb, "p4_bmT", [128, 2, 128], BF16, 2)
            cmTl = Slots(kb, "p4_cmT", [128, 2, 128], BF16, 2)
            dtl = Slots(kb, "p4_dt", [128, 32], F32, 2)
            zl = Slots(kb, "p4_z", [128, D], F32, 2)
            sml = Slots(kb, "p4_sm", [128, 6, 16], F32, 2)
            xdtl = Slots(kb, "p4_xdt", [128, 16, 64], BF16, 2)
            xdwl = Slots(kb, "p4_xdw", [128, 16, 64], BF16, 2)
            CBm = kb.sb("p4_CBm", [128, 2, 128], F32)
            CBm_r = kb.res("p4_CBm")
            rhsD = kb.sb("p4_rhsD", [128, 16, 128], F32)
            rhsD_r = kb.res("p4_rhsD")
            L = kb.sb("p4_L", [128, 16, 128], F32)
            L_r = kb.res("p4_L")
            M = kb.sb("p4_M", [128, 16, 128], BF16)
            M_r = kb.res("p4_M")
            Y = kb.sb("p4_Y", [128, 16, 64], F32)
            Y_r = kb.res("p4_Y")
            tmpY = kb.sb("p4_tmpY", [128, 16, 64], F32)
            tmpY_r = kb.res("p4_tmpY")
            Yn = kb.sb("p4_Yn", [128, D], BF16)
            Yn_r = kb.res("p4_Yn")
            stl = Slots(kb, "p4_st", [128, 16], F32, 2)
            cTl = Slots(kb, "p4_cT", [128, 8, 128], BF16, 2)
            bankD = RR([4])
            stA = {}

            def stageA(ct):
                rows = slice(ct * 128, (ct + 1) * 128)
                yb = (5, 6)
                xdt, xdt_r = xdtl.next()
                xdw, xdw_r = xdwl.next()
                xs, xs_r = xsl.next()
                kb.dma(out=xs[:].rearrange("p h d -> p (h d)"), in_=xsc_d[rows, :], reads=[R_["xsc"]], writes=[xs_r], own=xs_r)
                bm, bm_r = bml.next()
                kb.dma(out=bm[:], in_=bm_d[rows, :], reads=[R_["bm"]], writes=[bm_r], own=bm_r)
                bmT, bmT_r = bmTl.next()
                kb.dma(out=bmT[:], in_=bmT_d.rearrange("(g n) s -> n g s", g=2)[:, :, rows], reads=[R_["bmT"]], writes=[bmT_r], own=bmT_r)
                cmT, cmT_r = cmTl.next()
                kb.dma(out=cmT[:], in_=cmT_d.rearrange("(g n) s -> n g s", g=2)[:, :, rows], reads=[R_["cmT"]], writes=[cmT_r], own=cmT_r)
                dtt, dt_r = dtl.next()
                kb.dma(out=dtt[:], in_=dt_d[rows, :], reads=[R_["dt"]], writes=[dt_r], own=dt_r)
                zt, z_r = zl.next()
                kb.dma(out=zt[:], in_=zs_d[rows, :], reads=[R_["zs"]], writes=[z_r], own=z_r)
                sm, sm_r = sml.next()
                mm(ps[3][:, 0:16], utri_f[:], dtt[:, 16:32], True, True, [cres, dt_r], [psr[3]])
                yield
                mm(ps[3][:, 16:32], ones_f[:], dtt[:, 16:32], True, True, [cres, dt_r], [psr[3]])
                yield
                aop(lambda sm=sm: A.copy(out=sm[:, 0, :], in_=ps[3][:, 0:16]), [psr[3]], [sm_r])
                yield
                aop(lambda sm=sm: A.mul(out=sm[:, 1, :], in_=ps[3][:, 0:16], mul=-1.0), [psr[3]], [sm_r])
                yield
                aop(lambda sm=sm: A.activation(out=sm[:, 2, :], in_=ps[3][:, 0:16], func=AF.Exp), [psr[3]], [sm_r])
                yield
                aop(lambda sm=sm: A.activation(out=sm[:, 4, :], in_=ps[3][:, 16:32], func=AF.Exp), [psr[3]], [sm_r])
                yield
                vop(lambda sm=sm: V.tensor_tensor(out=sm[:, 5, :], in0=ps[3][:, 16:32], in1=sm[:, 0, :], op=ALU.subtract), [psr[3], sm_r], [sm_r])
                yield
                aop(lambda sm=sm: A.activation(out=sm[:, 3, :], in_=sm[:, 5, :], func=AF.Exp), [sm_r], [sm_r])
                yield
                vop(lambda xs=xs, dtt=dtt: V.tensor_tensor(out=xdt[:], in0=xs[:], in1=dtt[:, 0:16].unsqueeze(2).to_broadcast([128, 16, 64]),
                                                           op=ALU.mult), [xs_r, dt_r], [xdt_r])
                yield
                gop(lambda sm=sm: G.tensor_tensor(out=xdw[:], in0=xdt[:], in1=sm[:, 3, :].unsqueeze(2).to_broadcast([128, 16, 64]),
                                                  op=ALU.mult), [xdt_r, sm_r], [xdw_r])
                yield
                for g in range(2):
                    mm(ps[3][:, 64 + g * 128:64 + (g + 1) * 128], bmT[:, g, :], cmT[:, g, :], True, True, [bmT_r, cmT_r], [psr[3]], inc=(g == 1))
                    yield
                vop(lambda: V.tensor_tensor(out=CBm[:], in0=ps[3][:, 64:320].rearrange("p (g l) -> p g l", g=2),
                                            in1=tri_f[:].unsqueeze(1).to_broadcast([128, 2, 128]), op=ALU.mult), [psr[3], cres], [CBm_r])
                yield
                gop(lambda sm=sm: G.tensor_tensor(out=rhsD[:], in0=ident_f[:].unsqueeze(1).to_broadcast([128, 16, 128]),
                                                  in1=sm[:, 0, :].unsqueeze(2).to_broadcast([128, 16, 128]), op=ALU.mult),
                    [cres, sm_r], [rhsD_r])
                yield
                for q in range(4):
                    b = bankD.next()
                    mm(ps[b][:], ones_f[:], rhsD[:, 4 * q:4 * q + 4, :].rearrange("p h l -> p (h l)"), True, False, [cres, rhsD_r], [psr[b]],
                       inc=False)
                    yield
                    mm(ps[b][:], ident_f[:], negtri4[:].rearrange("p h l -> p (h l)"), False, True, [cres, c_r], [psr[b]])
                    yield
                    for hq in range(4):
                        h = 4 * q + hq
                        aop(lambda b=b, hq=hq, h=h, sm=sm: A.activation(out=L[:, h, :], in_=ps[b][:, hq * 128:(hq + 1) * 128], func=AF.Exp,
                                                                       bias=sm[:, 1, h:h + 1], scale=1.0), [psr[b], sm_r], [L_r])
                        yield
                for g in range(2):
                    gop(lambda g=g: G.tensor_tensor(out=M[:, g * 8:(g + 1) * 8, :], in0=L[:, g * 8:(g + 1) * 8, :],
                                                    in1=CBm[:, g, :].unsqueeze(1).to_broadcast([128, 8, 128]), op=ALU.mult),
                        [L_r, CBm_r], [M_r])
                    yield
                for h in range(16):
                    b = yb[h // 8]
                    hh = h % 8
                    mm(ps[b][:, hh * 64:(hh + 1) * 64], M[:, h, :], xdt[:, h, :], True, True, [M_r, xdt_r], [psr[b]], inc=(hh == 7))
                    yield
                stA[ct] = dict(rows=rows, yb=yb, xs=xs, xs_r=xs_r, bm=bm, bm_r=bm_r, cmT=cmT, cmT_r=cmT_r, zt=zt, z_r=z_r, sm=sm, sm_r=sm_r,
                               xdw=xdw, xdw_r=xdw_r)

            def stageB(ct):
                d = stA.pop(ct)
                rows, yb, xs, xs_r, bm, bm_r, cmT, cmT_r = d["rows"], d["yb"], d["xs"], d["xs_r"], d["bm"], d["bm_r"], d["cmT"], d["cmT_r"]
                zt, z_r, sm, sm_r, xdw, xdw_r = d["zt"], d["z_r"], d["sm"], d["sm_r"], d["xdw"], d["xdw_r"]
                yield
                for g in range(2):
                    mm(ps[7][:], cmT[:, g, :], STb[:, g, :], True, True, [cmT_r, STb_r], [psr[7]])
                    yield
                    vop(lambda g=g, sm=sm: V.tensor_tensor(out=Y[:, g * 8:(g + 1) * 8, :], in0=ps[7][:].rearrange("p (h d) -> p h d", h=8),
                                                           in1=sm[:, 2, g * 8:(g + 1) * 8].unsqueeze(2).to_broadcast([128, 8, 64]),
                                                           op=ALU.mult), [psr[7], sm_r], [Y_r])
                    yield
                    vop(lambda g=g: V.tensor_tensor(out=Y[:, g * 8:(g + 1) * 8, :], in0=Y[:, g * 8:(g + 1) * 8, :],
                                                    in1=ps[yb[g]][:].rearrange("p (h d) -> p h d", h=8), op=ALU.add),
                        [Y_r, psr[yb[g]]], [Y_r])
                    yield
                gop(lambda xs=xs: G.tensor_tensor(out=tmpY[:], in0=xs[:], in1=v16_bc[:, 2, :].unsqueeze(2).to_broadcast([128, 16, 64]),
                                                  op=ALU.mult), [xs_r, c_r], [tmpY_r])
                yield
                gop(lambda: G.tensor_tensor(out=Y[:], in0=Y[:], in1=tmpY[:], op=ALU.add), [Y_r, tmpY_r], [Y_r])
                yield
                for g in range(2):
                    mm(ps[7][:], bm[:, g * 128:(g + 1) * 128], xdw[:, g * 8:(g + 1) * 8, :].rearrange("p h d -> p (h d)"), True, True,
                       [bm_r, xdw_r], [psr[7]])
                    yield
                    vop(lambda g=g, sm=sm: V.tensor_tensor(out=ST[:, g, :].rearrange("p (h d) -> p h d", h=8),
                                                           in0=ST[:, g, :].rearrange("p (h d) -> p h d", h=8),
                                                           in1=sm[:, 4, g * 8:(g + 1) * 8].unsqueeze(2).to_broadcast([128, 8, 64]),
                                                           op=ALU.mult), [ST_r, sm_r, STb_r], [ST_r])
                    yield
                    vop(lambda g=g: V.tensor_tensor(out=ST[:, g, :], in0=ST[:, g, :], in1=ps[7][:], op=ALU.add), [ST_r, psr[7]], [ST_r])
                    yield
                aop(lambda: A.copy(out=STb[:], in_=ST[:]), [ST_r], [STb_r])
                yield
                Yf = Y[:].rearrange("p h d -> p (h d)")
                gop(lambda zt=zt: G.tensor_tensor(out=Yf, in0=Yf, in1=zt[:], op=ALU.mult), [Y_r, z_r], [Y_r])
                yield
                st, st_r = stl.next()
                for g in range(2):
                    aop(lambda g=g, st=st: A.activation(out=tmpY[:].rearrange("p h d -> p (h d)")[:, g * 512:(g + 1) * 512],
                                                        in_=Yf[:, g * 512:(g + 1) * 512], func=AF.Square, accum_out=st[:, g:g + 1]),
                        [Y_r], [tmpY_r, st_r])
                    yield
                    rstd(st, st_r, g, 4 + g, 512)
                    vop(lambda g=g, st=st: V.scalar_tensor_tensor(out=Yn[:, g * 512:(g + 1) * 512], in0=Yf[:, g * 512:(g + 1) * 512],
                                                                  scalar=st[:, 4 + g:5 + g], in1=gn[:, g * 512:(g + 1) * 512],
                                                                  op0=ALU.mult, op1=ALU.mult), [Y_r, st_r, c_r], [Yn_r])
                    yield
                cT, cT_r = cTl.next()
                pv = ps[7][:].bitcast(BF16)
                for c in range(8):
                    tp(pv[:, c * 128:(c + 1) * 128], Yn[:, c * 128:(c + 1) * 128], [Yn_r], [psr[7]], inc=(c == 7))
                    yield
                aop(lambda: A.copy(out=cT[:], in_=pv[:, 0:1024].rearrange("p (j t) -> p j t", j=8)), [psr[7]], [cT_r])
                yield
                kb.dma(out=mixT_d.rearrange("(kc p) s -> p kc s", p=128)[:, 8:16, rows], in_=cT[:], reads=[cT_r], writes=[R_["mixT"]], own=cT_r)

            yield
            for ct in range(NT):
                yield from stageA(ct)
                yield from stageB(ct)

    def phase4(l):
        with Phase():
            run_interleaved([gen4(l)])

    class WLoad:
        def __init__(self, name, kc=8, nc_=256, n=3):
            self.kc, self.nc_ = kc, nc_
            self.stg = Slots(kb, name + "_stg", [128, kc, nc_], F32, n)

        def load(self, dst, dst_r, wap, r0, kcn, c0, ncols):
            assert kcn <= self.kc and ncols <= self.nc_
            st, sr = self.stg.next()
            src = wap[r0:r0 + kcn * 128, :].rearrange("(kc p) c -> p kc c", p=128)[:, :, c0:c0 + ncols]
            kb.dma(out=st[:, 0:kcn, 0:ncols], in_=src, writes=[sr], own=sr)
            self.i = getattr(self, "i", 0) + 1
            e = ("dve", "act", "pool", "dve", "act")[self.i % 5]
            if e == "pool":
                gop(lambda: G.tensor_copy(out=dst, in_=st[:, 0:kcn, 0:ncols]), [sr], [dst_r])
            elif e == "dve":
                vop(lambda: V.tensor_copy(out=dst, in_=st[:, 0:kcn, 0:ncols]), [sr], [dst_r])
            else:
                aop(lambda: A.copy(out=dst, in_=st[:, 0:kcn, 0:ncols]), [sr], [dst_r])

        def load_full(self, dst_t, dst_r, wap, KC, C):
            for k0 in range(0, KC, self.kc):
                kn = min(self.kc, KC - k0)
                for c0 in range(0, C, self.nc_):
                    cn = min(self.nc_, C - c0)
                    self.load(dst_t[:, k0:k0 + kn, c0:c0 + cn], dst_r, wap, k0 * 128, kn, c0, cn)

    def post_norm_residual(P0, P1, r0, r1, xt, xr, g_bc, g_r, stl, junk, junk_r, tmp, tmp_r):
        st, st_r = stl.next()
        aop(lambda: A.activation(out=junk[:, 0:512], in_=P0[:], func=AF.Square, accum_out=st[:, 0:1]), [r0], [junk_r, st_r])
        aop(lambda: A.activation(out=junk[:, 512:1024], in_=P1[:], func=AF.Square, accum_out=st[:, 1:2]), [r1], [junk_r, st_r])
        vop(lambda: V.tensor_tensor(out=st[:, 3:4], in0=st[:, 0:1], in1=st[:, 1:2], op=ALU.add), [st_r], [st_r])
        rstd(st, st_r, 3, 2, D)
        for half, (P, r) in enumerate(((P0, r0), (P1, r1))):
            sl = slice(half * 512, (half + 1) * 512)
            vop(lambda P=P, sl=sl: V.scalar_tensor_tensor(out=tmp[:, sl], in0=P[:], scalar=st[:, 2:3], in1=g_bc[:, sl],
                                                         op0=ALU.mult, op1=ALU.mult), [r, st_r, g_r], [tmp_r])
            gop(lambda sl=sl: G.tensor_tensor(out=xt[:, sl], in0=tmp[:, sl], in1=xt[:, sl], op=ALU.add), [tmp_r, xr], [xr])

    def phase5(l):
        with Phase():
            src = x_in if l == 0 else xs_d
            wl = WLoad("p5w")
            wo = kb.sb("p5_wo", [128, 16, D], BF16)
            wo_r = kb.res("p5_wo")
            wl.load_full(wo, wo_r, w_out[l], 16, D)
            g1 = kb.sb("p5_g1", [128, D], F32)
            g2 = kb.sb("p5_g2", [128, D], F32)
            g_r = kb.res("p5_g")
            bcast_load(g1[:], vecD[l, 1, :], D, g_r)
            bcast_load(g2[:], vecD[l, 2, :], D, g_r)
            xsl = Slots(kb, "p5_x", [128, D], F32, 2)
            mxl = Slots(kb, "p5_mx", [128, 16, 128], BF16, 2)
            junk = kb.sb("p5_junk", [128, D], F32)
            junk_r = kb.res("p5_junk")
            tmp = kb.sb("p5_tmp", [128, D], F32)
            tmp_r = kb.res("p5_tmp")
            stl = Slots(kb, "p5_st", [128, 16], F32, 2)
            hb = kb.sb("p5_hb", [128, D], BF16)
            ntmp = (junk, junk_r, stl, hb, kb.res("p5_hb"))
            banks = RR([0, 1, 2, 3])
            S5 = {}

            def p5_stage1(tt):
                xt, xr = xsl.next()
                kb.dma(out=xt[:], in_=src[tt * 128:(tt + 1) * 128, :], reads=[R_["xs"]], writes=[xr], own=xr)
                mx, mr = mxl.next()
                kb.dma(out=mx[:], in_=mixT_d.rearrange("(kc p) s -> p kc s", p=128)[:, :, tt * 128:(tt + 1) * 128],
                       reads=[R_["mixT"]], writes=[mr], own=mr)
                b0, b1 = banks.next(), banks.next()
                for half, b in enumerate((b0, b1)):
                    for kc in range(16):
                        mm(ps[b][:], mx[:, kc, :], wo[:, kc, half * 512:(half + 1) * 512], kc == 0, kc == 15, [mr, wo_r], [psr[b]],
                           inc=(kc == 15))
                S5[tt] = (xt, xr, b0, b1)

            def p5_stage2(tt):
                xt, xr, b0, b1 = S5.pop(tt)
                post_norm_residual(ps[b0], ps[b1], psr[b0], psr[b1], xt, xr, g1, g_r, stl, junk, junk_r, tmp, tmp_r)
                kb.dma(out=xm_d[tt * 128:(tt + 1) * 128, :], in_=xt[:], reads=[xr], writes=[R_["xm"]], own=xr)
                norm_to_hT(xt[:], xr, g2, g_r, tt, ntmp)

            p5_stage1(0)
            for tt in range(NT):
                if tt + 1 < NT:
                    p5_stage1(tt + 1)
                p5_stage2(tt)

    def phase6a(l):
        with Phase():
            hT, hT_r = HT['t'], HT['r']
            wl = WLoad("p6w", 8, 512, 2)
            wgl = Slots(kb, "p6_wg", [128, 8, 512], BF16, 2)
            wul = Slots(kb, "p6_wu", [128, 8, 512], BF16, 2)
            sgl = Slots(kb, "p6_sg", [128, 512], F32, 2)
            gul = Slots(kb, "p6_gu", [128, 512], BF16, 3)
            bankA = RR([0, 1])
            bankB = RR([2, 3])
            blocks6 = list(range(0, DFF, 512))

            def p6a_load(c0):
                cn = min(512, DFF - c0)
                wg_t, wg_r = wgl.next()
                wu_t, wu_r = wul.next()
                wl.load(wg_t[:, :, 0:cn], wg_r, w_g[l], 0, 8, c0, cn)
                wl.load(wu_t[:, :, 0:cn], wu_r, w_u[l], 0, 8, c0, cn)
                return wg_t, wg_r, wu_t, wu_r, cn

            nxt6 = p6a_load(blocks6[0])
            for bi6, c0 in enumerate(blocks6):
                wg_t, wg_r, wu_t, wu_r, cn = nxt6
                if bi6 + 1 < len(blocks6):
                    nxt6 = p6a_load(blocks6[bi6 + 1])
                for c in range(cn // 128):
                    for tb in range(NB):
                        bg_, bu_ = bankA.next(), bankB.next()
                        for kc in range(8):
                            mm(ps[bg_][:], wg_t[:, kc, c * 128:(c + 1) * 128], hT[:, kc, tb * 512:(tb + 1) * 512], kc == 0, kc == 7,
                               [wg_r, hT_r], [psr[bg_]], inc=(kc == 7))
                        for kc in range(8):
                            mm(ps[bu_][:], wu_t[:, kc, c * 128:(c + 1) * 128], hT[:, kc, tb * 512:(tb + 1) * 512], kc == 0, kc == 7,
                               [wu_r, hT_r], [psr[bu_]], inc=(kc == 7))
                        sg, sgr = sgl.next()
                        aop(lambda sg=sg, bg_=bg_: A.activation(out=sg[:], in_=ps[bg_][:], func=AF.Silu), [psr[bg_]], [sgr])
                        gu, gur = gul.next()
                        vop(lambda sg=sg, gu=gu, bu_=bu_: V.tensor_tensor(out=gu[:], in0=ps[bu_][:], in1=sg[:], op=ALU.mult),
                            [psr[bu_], sgr], [gur])
                        f0 = c0 + c * 128
                        kb.dma(out=gu_d[f0:f0 + 128, tb * 512:(tb + 1) * 512], in_=gu[:], reads=[gur], writes=[R_["gu"]], own=gur)

    def phase6b(l):
        last = (l == DEPTH - 1)
        with Phase():
            wl = WLoad("p6bw")
            KF = DFF // 128
            wd = kb.sb("p6b_wd", [128, KF, D], BF16)
            wd_r = kb.res("p6b_wd")
            wl.load_full(wd, wd_r, w_d[l], KF, D)
            wpg = kb.sb("p6b_wpg", [128, 8, D], BF16)
            wpg_r = kb.res("p6b_wpg")
            wl.load_full(wpg, wpg_r, w_pg[l], 8, D)
            wpp = kb.sb("p6b_wpp", [128, 2, D], BF16)
            wpp_r = kb.res("p6b_wpp")
            wl.load_full(wpp, wpp_r, w_pp[l], 2, D)
            g1 = kb.sb("p6b_g1", [128, D], F32)
            g_r = kb.res("p6b_g")
            bcast_load(g1[:], vecD[l, 3, :], D, g_r)
            xsl = Slots(kb, "p6b_x", [128, D], F32, 2)
            gtl = Slots(kb, "p6b_gt", [128, KF, 128], BF16, 2)
            pl = Slots(kb, "p6b_p", [128, 256], F32, 2)
            junk = kb.sb("p6b_junk", [128, D], F32)
            junk_r = kb.res("p6b_junk")
            tmp = kb.sb("p6b_tmp", [128, D], F32)
            tmp_r = kb.res("p6b_tmp")
            stl = Slots(kb, "p6b_st", [128, 16], F32, 2)
            xb = kb.sb("p6b_xb", [128, D], BF16)
            xb_r = kb.res("p6b_xb")
            xT = kb.sb("p6b_xT", [128, 8, 128], BF16)
            xT_r = kb.res("p6b_xT")
            pb16 = kb.sb("p6b_pb", [128, 256], BF16)
            pb_r = kb.res("p6b_pb")
            pT = kb.sb("p6b_pT", [128, 2, 128], BF16)
            pT_r = kb.res("p6b_pT")
            sg = kb.sb("p6b_sg", [128, D], F32)
            sg_r = kb.res("p6b_sg")
            dst_d = y_out if last else xs_d
            dst_r = R_["y"] if last else R_["xs"]
            S1 = {}

            def stage1(tt):
                xt, xr = xsl.next()
                kb.dma(out=xt[:], in_=xm_d[tt * 128:(tt + 1) * 128, :], reads=[R_["xm"]], writes=[xr], own=xr)
                gt, gr = gtl.next()
                kb.dma(out=gt[:], in_=gu_d.rearrange("(kc p) s -> p kc s", p=128)[:, :, tt * 128:(tt + 1) * 128],
                       reads=[R_["gu"]], writes=[gr], own=gr)
                pt, pr = pl.next()
                kb.dma(out=pt[:], in_=p_in[l, tt * 128:(tt + 1) * 128, :], writes=[pr], own=pr)
                db = (0, 1) if tt % 2 == 0 else (2, 3)
                for half, b in enumerate(db):
                    for kc in range(KF):
                        mm(ps[b][:], gt[:, kc, :], wd[:, kc, half * 512:(half + 1) * 512], kc == 0, kc == KF - 1, [gr, wd_r], [psr[b]],
                           inc=(kc == KF - 1))
                S1[tt] = (xt, xr, pt, pr, db)

            def stage2(tt):
                xt, xr, pt, pr, db = S1.pop(tt)
                post_norm_residual(ps[db[0]], ps[db[1]], psr[db[0]], psr[db[1]], xt, xr, g1, g_r, stl, junk, junk_r, tmp, tmp_r)
                aop(lambda: A.copy(out=xb[:], in_=xt[:]), [xr], [xb_r])
                pv = ps[7][:].bitcast(BF16)
                for dc in range(8):
                    tp(pv[:, dc * 128:(dc + 1) * 128], xb[:, dc * 128:(dc + 1) * 128], [xb_r], [psr[7]], inc=(dc == 7))
                vop(lambda: V.tensor_copy(out=xT[:], in_=pv[:, 0:1024].rearrange("p (j t) -> p j t", j=8)), [psr[7]], [xT_r])
                for half, b in enumerate((4, 5)):
                    for kc in range(8):
                        mm(ps[b][:], xT[:, kc, :], wpg[:, kc, half * 512:(half + 1) * 512], kc == 0, kc == 7, [xT_r, wpg_r], [psr[b]],
                           inc=(kc == 7))
                    aop(lambda: A.activation(out=sg[:, half * 512:(half + 1) * 512], in_=ps[b][:], func=AF.Sigmoid), [psr[b]], [sg_r])
                vop(lambda: V.tensor_copy(out=pb16[:], in_=pt[:]), [pr], [pb_r])
                pv6 = ps[6][:].bitcast(BF16)
                for j in range(2):
                    tp(pv6[:, j * 128:(j + 1) * 128], pb16[:, j * 128:(j + 1) * 128], [pb_r], [psr[6]], inc=(j == 1))
                vop(lambda: V.tensor_copy(out=pT[:], in_=pv6[:, 0:256].rearrange("p (j t) -> p j t", j=2)), [psr[6]], [pT_r])
                for half in range(2):
                    for kc in range(2):
                        mm(ps[6][:], pT[:, kc, :], wpp[:, kc, half * 512:(half + 1) * 512], kc == 0, kc == 1, [pT_r, wpp_r], [psr[6]],
                           inc=(kc == 1))
                    sl = slice(half * 512, (half + 1) * 512)
                    vop(lambda: V.tensor_tensor(out=tmp[:, sl], in0=ps[6][:], in1=sg[:, sl], op=ALU.mult), [psr[6], sg_r], [tmp_r])
                    gop(lambda: G.tensor_tensor(out=xt[:, sl], in0=tmp[:, sl], in1=xt[:, sl], op=ALU.add), [tmp_r, xr], [xr])
                kb.dma(out=dst_d[tt * 128:(tt + 1) * 128, :], in_=xt[:], reads=[xr], writes=[dst_r], own=xr)

            stage1(0)
            for tt in range(NT):
                if tt + 1 < NT:
                    stage1(tt + 1)
                stage2(tt)

    def front(l):
        with HTScope():
            phase0(l, None if l == 0 else xs_d)
            phase1(l)

    def mid(l):
        phase24(l)
        phase3(l)

    def back(l):
        with HTScope():
            phase5(l)
            phase6a(l)
        phase6b(l)

    def run_all():
        for l in range(DEPTH):
            front(l)
            mid(l)
            back(l)

    phases = phases or ["all"]
    kb.fn = dict(front=front, mid=mid, back=back, phase2=phase2, phase3=phase3, phase4=phase4, phase24=phase24)
    if phases == ["all"]:
        run_all()
    else:
        for ph in phases:
            name, l = ph
            kb.fn[name](l)
    barrier()
    return nc, kb


def prep_inputs(inp, S):
    f = lambda a: np.ascontiguousarray(np.asarray(a, dtype=np.float32))
    w_uk = f(inp["w_uk"])
    wukT = np.ascontiguousarray(w_uk.reshape(DEPTH, R, 4, 2, 64).transpose(0, 3, 4, 2, 1).reshape(DEPTH, 128, 4, R))
    w_uv = f(inp["w_uv"])
    wuv = np.ascontiguousarray(w_uv.reshape(DEPTH, 2, 128, NH * 64).transpose(0, 2, 1, 3))
    rel = f(inp["rel_bias"])
    i = np.arange(128)[:, None]
    c = np.arange(GW)[None, :]
    bucket = t5_bucket_np((i - c + 384).astype(np.int32))
    gtab = np.ascontiguousarray(rel[bucket].transpose(0, 2, 1))
    vecD = np.ascontiguousarray(np.stack([f(inp["pre_mix_norm"]), f(inp["post_mix_norm"]), f(inp["pre_ffn_norm"]),
                                          f(inp["post_ffn_norm"]), f(inp["ssm_norm"])], axis=1))
    scw = np.ascontiguousarray(f(inp["short_conv_w"]).reshape(DEPTH, 3, 4, 128).transpose(0, 3, 2, 1))
    sscw = np.ascontiguousarray(f(inp["ssm_conv_w"]).reshape(DEPTH, 4, 12, 128).transpose(0, 3, 2, 1))
    sscb = np.ascontiguousarray(f(inp["ssm_conv_b"]).reshape(DEPTH, 12, 128).transpose(0, 2, 1))
    v16 = np.ascontiguousarray(np.stack([f(inp["ssm_dt_bias"]), f(inp["ssm_a_log"]), f(inp["ssm_d"])], axis=1))
    shared = dict(w_in=f(inp["w_in"]), w_out=f(inp["w_out"]), w_ffn_gate=f(inp["w_ffn_gate"]), w_ffn_up=f(inp["w_ffn_up"]),
                  w_ffn_down=f(inp["w_ffn_down"]), w_ple_proj=f(inp["w_ple_proj"]), w_ple_gate=f(inp["w_ple_gate"]),
                  wukT=wukT, wuv=wuv, gtab=gtab, vecD=vecD, kv_norm=f(inp["kv_norm"]), idx_k_norm_g=f(inp["idx_k_norm_g"]),
                  idx_k_norm_b=f(inp["idx_k_norm_b"]), scw=scw, sscw=sscw, sscb=sscb, v16=v16)
    x = f(inp["x"])
    p = f(inp["p"])
    B = x.shape[0]
    maps = []
    for b in range(B):
        m = dict(shared)
        m["x"] = np.ascontiguousarray(x[b, :S])
        m["p"] = np.ascontiguousarray(p[:, b, :S])
        maps.append(m)
    return maps


_CACHE = {}


def kernel(**inputs):
    S = 4096
    maps = prep_inputs(inputs, S)
    if "nc" not in _CACHE:
        _CACHE["nc"] = build_program(S)[0]
    res = run_bass_kernel_spmd(_CACHE["nc"], maps, core_ids=list(range(8)))
    return np.stack([np.asarray(r["y"], dtype=np.float32) for r in res.results], axis=0)
```
